# Optimizing a Trainium2 kernel written in Bass

```python
import math
import jax
import jax.numpy as jnp
from jax import lax
import numpy as np

D_MODEL = 1024
BATCH = 8
SEQ = 4096
DEPTH = 4

N_MEM = 256
N_MIXERS = 3
N_SUBLAYERS = 4
CHUNK = 64
CONV_W = 4
RMS_EPS = 1e-6
MACARON_W = 0.5
D_FF = 2816

DN_QK_HEADS = 8
DN_V_HEADS = 16
DN_DK = 128
DN_DV = 128
DN_Q = DN_QK_HEADS * DN_DK
DN_VAL = DN_V_HEADS * DN_DV
DN_CONV_DIM = 2 * DN_Q + DN_VAL
DN_IN = DN_CONV_DIM + DN_VAL + 2 * DN_V_HEADS

SSD_INNER = 2 * D_MODEL
SSD_HEADDIM = 64
SSD_HEADS = SSD_INNER // SSD_HEADDIM
SSD_GROUPS = 8
SSD_HPG = SSD_HEADS // SSD_GROUPS
SSD_STATE = 128
SSD_BC = SSD_GROUPS * SSD_STATE
SSD_CONV_DIM = SSD_INNER + 2 * SSD_BC
SSD_IN = SSD_INNER + SSD_CONV_DIM + SSD_HEADS

RW_HEAD = 64
RW_HEADS = D_MODEL // RW_HEAD
RW_DECAY_LORA = 64
RW_A_LORA = 64
RW_GATE_LORA = 160
RW_GN_EPS = 64e-5
N_LERP = 6

XA_HEADS = 4
XA_DH = D_MODEL // XA_HEADS

N_DN = (DEPTH + 2) // N_MIXERS
N_SSD = (DEPTH + 1) // N_MIXERS
N_RW = DEPTH // N_MIXERS

kernel_name = 'hybrid_deltanet_ssd_rwkv7_macaron_trunk'

F32 = jnp.float32


def rms_norm(x, g, eps=RMS_EPS):
    xf = x.astype(F32)
    y = xf * lax.rsqrt(jnp.mean(xf * xf, axis=-1, keepdims=True) + eps)
    return (y * g.astype(F32)).astype(x.dtype)


def l2_normalize(x, eps=1e-6):
    xf = x.astype(F32)
    return xf * lax.rsqrt(jnp.sum(xf * xf, axis=-1, keepdims=True) + eps)


def swiglu(x, w_in, w_out):
    gate, up = jnp.split(x @ w_in, 2, axis=-1)
    return (jax.nn.silu(gate) * up) @ w_out


def causal_depthwise_conv(x, w):
    c = x.shape[-1]
    return lax.conv_general_dilated(
        x, w[:, None, :], window_strides=(1,), padding=[(w.shape[0] - 1, 0)],
        dimension_numbers=('NWC', 'WIO', 'NWC'), feature_group_count=c)


def to_chunks(t):
    b, s = t.shape[:2]
    return jnp.swapaxes(t.astype(F32).reshape(b, s // CHUNK, CHUNK, *t.shape[2:]), 0, 1)


def from_chunks(t):
    t = jnp.swapaxes(t, 0, 1)
    return t.reshape(t.shape[0], t.shape[1] * t.shape[2], *t.shape[3:])


def gated_delta_rule_chunked(q, k, v, beta, g):
    b, _, h, dk = k.shape
    dv = v.shape[-1]
    incl = jnp.tril(jnp.ones((CHUNK, CHUNK), dtype=bool))
    strict = jnp.tril(jnp.ones((CHUNK, CHUNK), dtype=bool), k=-1)
    eye = jnp.eye(CHUNK, dtype=F32)

    def step(state, inp):
        qc, kc, vc, bc, gc = inp
        G = jnp.cumsum(gc, axis=1)
        Gh = jnp.swapaxes(G, 1, 2)
        decay = jnp.exp(jnp.where(incl, Gh[..., :, None] - Gh[..., None, :], -jnp.inf))
        kb = kc * bc[..., None]
        a_mat = jnp.where(strict, jnp.einsum('bihk,bjhk->bhij', kb, kc) * decay, 0.0)
        rhs = jnp.concatenate([vc * bc[..., None], kb * jnp.exp(G)[..., None]], axis=-1)
        rhs = jnp.swapaxes(rhs, 1, 2)
        sol = lax.linalg.triangular_solve(eye + a_mat, rhs, left_side=True, lower=True,
                                          unit_diagonal=True)
        u, wk = sol[..., :dv], sol[..., dv:]
        v_new = u - jnp.einsum('bhik,bhkv->bhiv', wk, state)
        qk = jnp.einsum('bihk,bjhk->bhij', qc, kc) * decay
        o = (jnp.einsum('bihk,bhkv->bihv', qc * jnp.exp(G)[..., None], state)
             + jnp.einsum('bhij,bhjv->bihv', qk, v_new))
        k_end = kc * jnp.exp(G[:, -1:, :] - G)[..., None]
        state = (state * jnp.exp(Gh[..., -1])[..., None, None]
                 + jnp.einsum('bihk,bhiv->bhkv', k_end, v_new))
        return state, o

    state0 = jnp.zeros((b, h, dk, dv), F32)
    _, o = lax.scan(step, state0, (to_chunks(q), to_chunks(k), to_chunks(v),
                                   to_chunks(beta), to_chunks(g)))
    return from_chunks(o)


def gated_deltanet_mixer(x, w_in, conv_w, a_log, dt_bias, norm_g, w_out):
    b, s, _ = x.shape
    proj = x @ w_in
    qkv = jax.nn.silu(causal_depthwise_conv(proj[..., :DN_CONV_DIM], conv_w))
    z = proj[..., DN_CONV_DIM:DN_CONV_DIM + DN_VAL]
    beta_raw = proj[..., DN_CONV_DIM + DN_VAL:DN_CONV_DIM + DN_VAL + DN_V_HEADS]
    a_raw = proj[..., DN_CONV_DIM + DN_VAL + DN_V_HEADS:]
    rep = DN_V_HEADS // DN_QK_HEADS
    q = l2_normalize(qkv[..., :DN_Q].reshape(b, s, DN_QK_HEADS, DN_DK)) * DN_DK ** -0.5
    k = l2_normalize(qkv[..., DN_Q:2 * DN_Q].reshape(b, s, DN_QK_HEADS, DN_DK))
    q = jnp.repeat(q, rep, axis=2)
    k = jnp.repeat(k, rep, axis=2)
    v = qkv[..., 2 * DN_Q:].reshape(b, s, DN_V_HEADS, DN_DV)
    beta = jax.nn.sigmoid(beta_raw.astype(F32))
    g = -jnp.exp(a_log.astype(F32)) * jax.nn.softplus(a_raw.astype(F32) + dt_bias.astype(F32))
    o = gated_delta_rule_chunked(q, k, v, beta, g)
    o = rms_norm(o, norm_g) * jax.nn.silu(z.astype(F32).reshape(b, s, DN_V_HEADS, DN_DV))
    return o.reshape(b, s, DN_VAL).astype(x.dtype) @ w_out


def ssd_chunked(xdt, a_dt, bm, cm):
    b, _, gr, e, p = xdt.shape
    n = bm.shape[-1]
    incl = jnp.tril(jnp.ones((CHUNK, CHUNK), dtype=bool))[:, :, None, None]

    def step(state, inp):
        xc, ac, bc, cc = inp
        acum = jnp.cumsum(ac, axis=1)
        seg = jnp.exp(jnp.where(incl, acum[:, :, None] - acum[:, None, :], -jnp.inf))
        cb = jnp.einsum('blgn,bsgn->blsg', cc, bc)
        y = (jnp.einsum('blsg,blsge,bsgep->blgep', cb, seg, xc)
             + jnp.einsum('blgn,bgepn,blge->blgep', cc, state, jnp.exp(acum)))
        to_end = jnp.exp(acum[:, -1:] - acum)
        state = (state * jnp.exp(acum[:, -1])[..., None, None]
                 + jnp.einsum('bsgn,bsge,bsgep->bgepn', bc, to_end, xc))
        return state, y

    state0 = jnp.zeros((b, gr, e, p, n), F32)
    _, y = lax.scan(step, state0, (to_chunks(xdt), to_chunks(a_dt), to_chunks(bm), to_chunks(cm)))
    return from_chunks(y)


def mamba2_mixer(x, w_in, conv_w, conv_b, a_log, dt_bias, d_skip, norm_g, w_out):
    b, s, _ = x.shape
    proj = x @ w_in
    z = proj[..., :SSD_INNER]
    xbc = jax.nn.silu(causal_depthwise_conv(proj[..., SSD_INNER:SSD_INNER + SSD_CONV_DIM], conv_w)
                      + conv_b)
    dt_raw = proj[..., SSD_INNER + SSD_CONV_DIM:]
    xs = xbc[..., :SSD_INNER].astype(F32).reshape(b, s, SSD_GROUPS, SSD_HPG, SSD_HEADDIM)
    bm = xbc[..., SSD_INNER:SSD_INNER + SSD_BC].reshape(b, s, SSD_GROUPS, SSD_STATE)
    cm = xbc[..., SSD_INNER + SSD_BC:].reshape(b, s, SSD_GROUPS, SSD_STATE)
    dt = jax.nn.softplus(dt_raw.astype(F32) + dt_bias.astype(F32)).reshape(b, s, SSD_GROUPS, SSD_HPG)
    a = -jnp.exp(a_log.astype(F32)).reshape(SSD_GROUPS, SSD_HPG)
    y = ssd_chunked(xs * dt[..., None], dt * a, bm, cm)
    y = y + xs * d_skip.astype(F32).reshape(SSD_GROUPS, SSD_HPG, 1)
    gsz = SSD_INNER // SSD_GROUPS
    yz = y.reshape(b, s, SSD_GROUPS, gsz) * jax.nn.silu(z.astype(F32)).reshape(b, s, SSD_GROUPS, gsz)
    yz = rms_norm(yz, norm_g.reshape(SSD_GROUPS, gsz))
    return yz.reshape(b, s, SSD_INNER).astype(x.dtype) @ w_out


def wkv7_scan(r, w, k, v, a, bb):
    b, _, h, n = r.shape

    def step(state, inp):
        rt, wt, kt, vt, at, bt = inp
        sa = jnp.einsum('bhvk,bhk->bhv', state, at)
        state = (state * wt[:, :, None, :] + sa[..., None] * bt[:, :, None, :]
                 + vt[..., None] * kt[:, :, None, :])
        return state, jnp.einsum('bhvk,bhk->bhv', state, rt)

    tm = lambda t: jnp.swapaxes(t, 0, 1)
    _, y = lax.scan(step, jnp.zeros((b, h, n, n), F32),
                    (tm(r), tm(w), tm(k), tm(v), tm(a), tm(bb)))
    return tm(y)


def rwkv7_mixer(x, mu, w_rkv, w0, w1, w2, a0, a1, a2, g1, g2, k_k, k_a, r_k, ln_g, ln_b, w_out):
    b, s, d = x.shape
    xx = jnp.pad(x, ((0, 0), (1, 0), (0, 0)))[:, :-1] - x
    xr, xw, xk, xv, xa, xg = [x + xx * mu[i] for i in range(N_LERP)]
    rkv = jnp.einsum('ibsd,ide->ibse', jnp.stack([xr, xk, xv]), w_rkv)
    r, k, v = rkv[0], rkv[1], rkv[2]
    w = -jax.nn.softplus(-(w0 + jnp.tanh(xw @ w1) @ w2)) - 0.5
    a = jax.nn.sigmoid(a0 + (xa @ a1) @ a2)
    gate = jax.nn.sigmoid(xg @ g1) @ g2
    heads = lambda t: t.astype(F32).reshape(b, s, RW_HEADS, RW_HEAD)
    kk = l2_normalize(heads(k * k_k))
    k = k * (1.0 + (a - 1.0) * k_a)
    rh, kh, vh, ah = heads(r), heads(k), heads(v), heads(a)
    decay = jnp.exp(-jnp.exp(heads(w)))
    y = wkv7_scan(rh, decay, kh, vh, -kk, kk * ah)
    mean = jnp.mean(y, axis=-1, keepdims=True)
    var = jnp.mean(jnp.square(y - mean), axis=-1, keepdims=True)
    y = ((y - mean) * lax.rsqrt(var + RW_GN_EPS)).reshape(b, s, d) * ln_g.astype(F32) + ln_b.astype(F32)
    y = y + (jnp.sum(rh * kh * r_k.astype(F32), axis=-1, keepdims=True) * vh).reshape(b, s, d)
    return (y * gate.astype(F32)).astype(x.dtype) @ w_out


def memory_cross_attention(x, mem_n, w_q, w_kv, w_o):
    b, s, d = x.shape
    m = mem_n.shape[1]
    q = (x @ w_q).reshape(b, s, XA_HEADS, XA_DH)
    k, v = jnp.split(mem_n @ w_kv, 2, axis=-1)
    k = k.reshape(b, m, XA_HEADS, XA_DH)
    v = v.reshape(b, m, XA_HEADS, XA_DH)
    scores = jnp.einsum('bshd,bmhd->bhsm', q, k).astype(F32) * XA_DH ** -0.5
    p = jax.nn.softmax(scores, axis=-1).astype(v.dtype)
    o = jnp.einsum('bhsm,bmhd->bshd', p, v).reshape(b, s, d)
    return o @ w_o


def setup_inputs(seed: int = 0) -> dict:
    key = jax.random.key(seed)
    ks = iter(jax.random.split(key, 64))

    def normal(shape, scale):
        return scale * jax.random.normal(next(ks), shape, F32)

    def unif(shape, lo, hi):
        return jax.random.uniform(next(ks), shape, F32, lo, hi)

    def gain(shape):
        return 1.0 + normal(shape, 0.02)

    def dt_bias(shape):
        dt = jnp.exp(unif(shape, math.log(1e-3), math.log(1e-1)))
        return dt + jnp.log(-jnp.expm1(-dt))

    D = D_MODEL
    return {
        'x': normal((BATCH, SEQ, D), 1.0),
        'mem': normal((BATCH, N_MEM, D), 1.0),
        'sandwich_g': gain((DEPTH, N_SUBLAYERS, 2, D)),
        'ffn_w_in': normal((DEPTH, 2, D, 2 * D_FF), D ** -0.5),
        'ffn_w_out': normal((DEPTH, 2, D_FF, D), D_FF ** -0.5),
        'mem_norm_g': gain((DEPTH, D)),
        'xa_w_q': normal((DEPTH, D, D), D ** -0.5),
        'xa_w_kv': normal((DEPTH, D, 2 * D), D ** -0.5),
        'xa_w_o': normal((DEPTH, D, D), D ** -0.5),
        'dn_w_in': normal((N_DN, D, DN_IN), D ** -0.5),
        'dn_conv_w': normal((N_DN, CONV_W, DN_CONV_DIM), CONV_W ** -0.5),
        'dn_a_log': jnp.log(unif((N_DN, DN_V_HEADS), 1.0, 16.0)),
        'dn_dt_bias': dt_bias((N_DN, DN_V_HEADS)),
        'dn_norm_g': gain((N_DN, DN_DV)),
        'dn_w_out': normal((N_DN, DN_VAL, D), DN_VAL ** -0.5),
        'ssd_w_in': normal((N_SSD, D, SSD_IN), D ** -0.5),
        'ssd_conv_w': normal((N_SSD, CONV_W, SSD_CONV_DIM), CONV_W ** -0.5),
        'ssd_conv_b': normal((N_SSD, SSD_CONV_DIM), 0.02),
        'ssd_a_log': jnp.log(unif((N_SSD, SSD_HEADS), 1.0, 16.0)),
        'ssd_dt_bias': dt_bias((N_SSD, SSD_HEADS)),
        'ssd_d': gain((N_SSD, SSD_HEADS)),
        'ssd_norm_g': gain((N_SSD, SSD_INNER)),
        'ssd_w_out': normal((N_SSD, SSD_INNER, D), SSD_INNER ** -0.5),
        'rw_mu': unif((N_RW, N_LERP, D), 0.0, 1.0),
        'rw_w_rkv': normal((N_RW, 3, D, D), D ** -0.5),
        'rw_w0': unif((N_RW, D), -6.0, -1.0),
        'rw_w1': normal((N_RW, D, RW_DECAY_LORA), D ** -0.5),
        'rw_w2': normal((N_RW, RW_DECAY_LORA, D), 0.1 * RW_DECAY_LORA ** -0.5),
        'rw_a0': normal((N_RW, D), 0.1),
        'rw_a1': normal((N_RW, D, RW_A_LORA), D ** -0.5),
        'rw_a2': normal((N_RW, RW_A_LORA, D), RW_A_LORA ** -0.5),
        'rw_g1': normal((N_RW, D, RW_GATE_LORA), D ** -0.5),
        'rw_g2': normal((N_RW, RW_GATE_LORA, D), RW_GATE_LORA ** -0.5),
        'rw_k_k': 0.85 + normal((N_RW, D), 0.02),
        'rw_k_a': gain((N_RW, D)),
        'rw_r_k': normal((N_RW, RW_HEADS, RW_HEAD), 0.1),
        'rw_ln_g': gain((N_RW, D)),
        'rw_ln_b': normal((N_RW, D), 0.02),
        'rw_w_out': normal((N_RW, D, D), D ** -0.5),
    }


def reference(x, mem, sandwich_g, ffn_w_in, ffn_w_out, mem_norm_g, xa_w_q, xa_w_kv, xa_w_o,
              dn_w_in, dn_conv_w, dn_a_log, dn_dt_bias, dn_norm_g, dn_w_out,
              ssd_w_in, ssd_conv_w, ssd_conv_b, ssd_a_log, ssd_dt_bias, ssd_d, ssd_norm_g, ssd_w_out,
              rw_mu, rw_w_rkv, rw_w0, rw_w1, rw_w2, rw_a0, rw_a1, rw_a2, rw_g1, rw_g2,
              rw_k_k, rw_k_a, rw_r_k, rw_ln_g, rw_ln_b, rw_w_out):
    h = x
    for l in range(DEPTH):
        g = sandwich_g[l]
        kind = l % N_MIXERS
        j = l // N_MIXERS
        u = swiglu(rms_norm(h, g[0, 0]), ffn_w_in[l, 0], ffn_w_out[l, 0])
        h = h + MACARON_W * rms_norm(u, g[0, 1])
        u = rms_norm(h, g[1, 0])
        if kind == 0:
            u = gated_deltanet_mixer(u, dn_w_in[j], dn_conv_w[j], dn_a_log[j], dn_dt_bias[j],
                                     dn_norm_g[j], dn_w_out[j])
        elif kind == 1:
            u = mamba2_mixer(u, ssd_w_in[j], ssd_conv_w[j], ssd_conv_b[j], ssd_a_log[j],
                             ssd_dt_bias[j], ssd_d[j], ssd_norm_g[j], ssd_w_out[j])
        else:
            u = rwkv7_mixer(u, rw_mu[j], rw_w_rkv[j], rw_w0[j], rw_w1[j], rw_w2[j], rw_a0[j],
                            rw_a1[j], rw_a2[j], rw_g1[j], rw_g2[j], rw_k_k[j], rw_k_a[j],
                            rw_r_k[j], rw_ln_g[j], rw_ln_b[j], rw_w_out[j])
        h = h + rms_norm(u, g[1, 1])
        u = memory_cross_attention(rms_norm(h, g[2, 0]), rms_norm(mem, mem_norm_g[l]),
                                   xa_w_q[l], xa_w_kv[l], xa_w_o[l])
        h = h + rms_norm(u, g[2, 1])
        u = swiglu(rms_norm(h, g[3, 0]), ffn_w_in[l, 1], ffn_w_out[l, 1])
        h = h + MACARON_W * rms_norm(u, g[3, 1])
    return h
```

```python
import numpy as np
from contextlib import ExitStack
import concourse.bass as bass
import concourse.mybir as mybir
from concourse.bass_utils import run_bass_kernel_spmd

F32 = mybir.dt.float32
BF16 = mybir.dt.bfloat16
AF = mybir.ActivationFunctionType
ALU = mybir.AluOpType
AX = mybir.AxisListType

D = 1024
DFF = 2816
NMEM = 256
EPS = 1e-6


class Tok:
    __slots__ = ("w", "r", "name")

    def __init__(self, name=""):
        self.w = None
        self.r = {}
        self.name = name


class Prog:
    def __init__(self, nc):
        self.nc = nc
        self.eng = dict(pe=nc.tensor, act=nc.scalar, dve=nc.vector, pool=nc.gpsimd, sp=nc.sync)
        self.sems = {}
        self.cnt = {}
        self.seen = {e: {} for e in self.eng}
        self.nsem = 0
        self.nins = {e: 0 for e in self.eng}
        self.dmap = {}
        self.dfree = []
        self.epoch = 0
        import os as _os
        for i_ in range(int(_os.environ.get("SEMSHIFT", "0"))):
            self.nc.alloc_semaphore(name=f"dummy{i_}")
        for e in ("pe", "act", "dve", "pool"):
            self._sem(e)

    def _sem(self, key):
        if key not in self.sems:
            self.sems[key] = self.nc.alloc_semaphore(name=f"s{self.nsem}")
            self.cnt[key] = 0
            self.nsem += 1
        return self.sems[key]

    def _wait(self, e, deps):
        eng = self.eng[e]
        for key, val in deps.items():
            if e == "pe" and key == "pe":
                continue
            if self.seen[e].get(key, 0) < val:
                eng.wait_ge(self.sems[key], val)
                self.seen[e][key] = val
                self.nins[e] += 1

    def _add(self, deps, tick):
        if tick is None or tick[2] < self.epoch:
            return
        k, v = tick[0], tick[1]
        if deps.get(k, 0) < v:
            deps[k] = v

    def op(self, e, fn, reads=(), writes=(), sig=True, dma=None):
        deps = {}
        for t in reads:
            self._add(deps, t.w)
        for t in writes:
            self._add(deps, t.w)
            for k, (v, ep) in t.r.items():
                self._add(deps, (k, v, ep))
        if dma is not None:
            key = self._dkey(dma)
            if self.cnt[key] > 0:
                self._add(deps, (key, self.cnt[key], self.epoch))
            inc = 16
        else:
            key = e
            inc = 1
        self._wait(e, deps)
        ins = fn(self.eng[e])
        self.nins[e] += 1
        if sig or dma is not None:
            ins.then_inc(self.sems[key], inc)
            self.cnt[key] += inc
            tick = (key, self.cnt[key], self.epoch)
        else:
            tick = (key, self.cnt[key] + 1, self.epoch)
        for t in reads:
            old = t.r.get(tick[0])
            if old is None or old[1] < self.epoch or old[0] < tick[1]:
                t.r[tick[0]] = (tick[1], self.epoch)
        for t in writes:
            t.w = tick
            t.r = {}
        return ins

    def _dkey(self, tok):
        k = self.dmap.get(id(tok))
        if k is None:
            if self.dfree:
                k = self.dfree.pop()
            else:
                k = ("d", self.nsem)
                self._sem(k)
            self.dmap[id(tok)] = k
        return k

    def release(self, toks):
        keys = []
        for t in toks:
            k = self.dmap.pop(id(t), None)
            if k is not None:
                keys.append(k)
        if not keys:
            return
        marks = {}
        for e in ("pe", "act", "dve", "sp"):
            key = e if e != "sp" else "spn"
            self._sem(key)
            self.eng[e].nop().then_inc(self.sems[key], 1)
            self.cnt[key] += 1
            marks[key] = self.cnt[key]
        self._wait_all("pool", marks)
        for k in keys:
            self.eng["pool"].sem_clear(self.sems[k])
            self.cnt[k] = 0
            for e in self.eng:
                self.seen[e].pop(k, None)
            self.dfree.append(k)
        self.eng["pool"].nop().then_inc(self.sems["pool"], 1)
        self.cnt["pool"] += 1
        for e in ("pe", "act", "dve", "sp"):
            self._wait_all(e, {"pool": self.cnt["pool"]})

    def barrier(self):
        deps = {k: v for k, v in self.cnt.items() if v > 0}
        for e in self.eng:
            d = dict(deps)
            self._wait_all(e, d)
        self.epoch += 1

    def _wait_all(self, e, deps):
        eng = self.eng[e]
        for key, val in deps.items():
            if self.seen[e].get(key, 0) < val:
                eng.wait_ge(self.sems[key], val)
                self.seen[e][key] = val
                self.nins[e] += 1

    def finish(self, toks):
        deps = {}
        for t in toks:
            self._add(deps, t.w)
        self._wait("sp", deps)


class T:
    def __init__(self, h, name, tok=None):
        self.h = h
        self.tok = tok or Tok(name)

    def view(self, key, name=None):
        return T(self.h[key], name, tok=self.tok)

    def __getitem__(self, k):
        return self.h[k]


class Ctx:
    def __init__(self, nc):
        self.nc = nc
        self.p = Prog(nc)
        self.n = 0
        self.es = None
        self.ptoks = []

    def phase(self):
        return _Phase(self)

    def sb(self, shape, dt, name=None):
        self.n += 1
        nm = f"{name or 'sb'}_{self.n}"
        if self.es is None:
            h = self.nc.alloc_sbuf_tensor(nm, list(shape), dt)
        else:
            h = self.es.enter_context(self.nc.sbuf_tensor(nm, list(shape), dt))
        t = T(h, name)
        if self.es is not None:
            self.ptoks.append(t.tok)
        return t

    def ps(self, shape, dt, name=None):
        self.n += 1
        nm = f"{name or 'ps'}_{self.n}"
        if self.es is None:
            h = self.nc.alloc_psum_tensor(nm, list(shape), dt)
        else:
            h = self.es.enter_context(self.nc.psum_tensor(nm, list(shape), dt))
        t = T(h, name)
        if self.es is not None:
            self.ptoks.append(t.tok)
        return t


class _Phase:
    def __init__(self, c):
        self.c = c

    def __enter__(self):
        self.saved = (self.c.es, self.c.ptoks, getattr(self.c, "stg", None))
        self.c.stg = None
        self.c.es = ExitStack()
        self.c.es.__enter__()
        self.c.ptoks = []
        return self

    def __exit__(self, *a):
        self.c.p.barrier()
        self.c.p.release(self.c.ptoks)
        self.c.es.__exit__(None, None, None)
        self.c.es, self.c.ptoks, self.c.stg = self.saved
        return False


def _toks(xs):
    return [x.tok if isinstance(x, T) else x for x in xs]


PARAM_SHAPES = {
    "sandwich_g": [4, 4, 2, 1024], "ffn_w_in": [4, 2, 1024, 5632], "ffn_w_out": [4, 2, 2816, 1024],
    "mem_norm_g": [4, 1024], "xa_w_q": [4, 1024, 1024], "xa_w_kv": [4, 1024, 2048], "xa_w_o": [4, 1024, 1024],
    "dn_w_in": [2, 1024, 6176], "dn_conv_w": [2, 4, 4096], "dn_a_log": [2, 16], "dn_dt_bias": [2, 16],
    "dn_norm_g": [2, 128], "dn_w_out": [2, 2048, 1024],
    "ssd_w_in": [1, 1024, 6176], "ssd_conv_w": [1, 4, 4096], "ssd_conv_b": [1, 4096], "ssd_a_log": [1, 32],
    "ssd_dt_bias": [1, 32], "ssd_d": [1, 32], "ssd_norm_g": [1, 2048], "ssd_w_out": [1, 2048, 1024],
    "rw_mu": [1, 6, 1024], "rw_w_rkv": [1, 3, 1024, 1024], "rw_w0": [1, 1024], "rw_w1": [1, 1024, 64],
    "rw_w2": [1, 64, 1024], "rw_a0": [1, 1024], "rw_a1": [1, 1024, 64], "rw_a2": [1, 64, 1024],
    "rw_g1": [1, 1024, 160], "rw_g2": [1, 160, 1024], "rw_k_k": [1, 1024], "rw_k_a": [1, 1024],
    "rw_r_k": [1, 16, 64], "rw_ln_g": [1, 1024], "rw_ln_b": [1, 1024], "rw_w_out": [1, 1024, 1024],
}


def make_in_map(params, x, mem):
    m = {k: np.ascontiguousarray(np.asarray(params[k], dtype=np.float32)) for k in PARAM_SHAPES}
    m["x"] = np.ascontiguousarray(x, dtype=np.float32)
    m["mem"] = np.ascontiguousarray(mem, dtype=np.float32)
    return m


class Builder:
    def __init__(self, NT, sublayers, n_layers=4, debug=False):
        self.NT = NT
        self.S = NT * 128
        self.sublayers = sublayers
        nc = bass.Bass("TRN2", target_bir_lowering=False)
        self.nc = nc
        self.c = Ctx(nc)
        self.p = self.c.p
        S = self.S
        dt = nc.dram_tensor
        self.x = dt("x", [S, D], F32, kind="ExternalInput").ap()
        self.mem = dt("mem", [NMEM, D], F32, kind="ExternalInput").ap()
        self.out = dt("out", [S, D], F32, kind="ExternalOutput").ap()
        L = n_layers
        self.sandwich_g = dt("sandwich_g", [L, 4, 2, D], F32, kind="ExternalInput").ap()
        self.ffn_w_in = dt("ffn_w_in", [L, 2, D, 2 * DFF], F32, kind="ExternalInput").ap()
        self.ffn_w_out = dt("ffn_w_out", [L, 2, DFF, D], F32, kind="ExternalInput").ap()
        def inp(name, shape):
            return dt(name, list(shape), F32, kind="ExternalInput").ap()
        self.mem_norm_g = inp("mem_norm_g", [L, D])
        self.xa_w_q = inp("xa_w_q", [L, D, D])
        self.xa_w_kv = inp("xa_w_kv", [L, D, 2 * D])
        self.xa_w_o = inp("xa_w_o", [L, D, D])
        for nm, shp in PARAM_SHAPES.items():
            if not hasattr(self, nm):
                setattr(self, nm, inp(nm, shp))
        self.htok = [Tok(f"h{i}") for i in range(NT)]
        self.scr = dt("scr", [S, 2 * D], BF16, kind="Internal").ap()
        self.ytok = [Tok(f"y{i}") for i in range(NT)]
        self.first = True
        self.debug = debug
        self.dbg_names = set()
        self.dbg_toks = []
        self._consts()

    def _consts(self):
        c, p, nc = self.c, self.p, self.nc
        self.ident = c.sb([128, 128], BF16, "ident")
        self.identf = c.sb([128, 128], F32, "identf")
        p.op("pool", lambda e: e.memset(self.identf[:, :], 0.0), writes=_toks([self.identf]))
        p.op("pool", lambda e: e.affine_select(
            out=self.identf[:, :], in_=self.identf[:, :], pattern=[[-1, 128]],
            compare_op=ALU.not_equal, fill=1.0, base=0, channel_multiplier=1),
            reads=_toks([self.identf]), writes=_toks([self.identf]))
        p.op("dve", lambda e: e.tensor_copy(out=self.ident[:, :], in_=self.identf[:, :]),
             reads=_toks([self.identf]), writes=_toks([self.ident]))
        self.epsc = c.sb([128, 1], F32, "eps")
        p.op("dve", lambda e: e.memset(self.epsc[:, :], EPS), writes=_toks([self.epsc]))
        def msk(name, pattern_step, chmul, cmp):
            t = c.sb([128, 128], F32, name)
            p.op("pool", lambda e: e.memset(t[:, :], 1.0), writes=_toks([t]))
            p.op("pool", lambda e: e.affine_select(
                out=t[:, :], in_=t[:, :], pattern=[[pattern_step, 128]], compare_op=cmp, fill=0.0, base=0,
                channel_multiplier=chmul), reads=_toks([t]), writes=_toks([t]))
            return t
        self.triLE = msk("triLE", 1, -1, ALU.is_ge)
        self.maskGT = msk("maskGT", -1, 1, ALU.is_gt)
        self.maskLT = msk("maskLT", 1, -1, ALU.is_gt)
        def bd(name, sz):
            t = c.sb([128, 128], F32, name)
            v = t[:, :].rearrange("p (a b) -> p a b", b=sz)
            p.op("pool", lambda e: e.memset(t[:, :], 1.0), writes=_toks([t]))
            p.op("pool", lambda e: e.affine_select(out=v, in_=v, pattern=[[-sz, 128 // sz], [0, sz]],
                                                   compare_op=ALU.is_ge, fill=0.0, base=0, channel_multiplier=1),
                 reads=_toks([t]), writes=_toks([t]))
            p.op("pool", lambda e: e.affine_select(out=v, in_=v, pattern=[[sz, 128 // sz], [0, sz]],
                                                   compare_op=ALU.is_ge, fill=0.0, base=sz - 1, channel_multiplier=-1),
                 reads=_toks([t]), writes=_toks([t]))
            return t
        b16, b32, b64 = bd("bd16", 16), bd("bd32", 32), bd("bd64", 64)
        self.bd64f = b64
        self.onesf = c.sb([128, 128], F32, "onesf")
        p.op("pool", lambda e: e.memset(self.onesf[:, :], 1.0), writes=_toks([self.onesf]))
        self.mlev = []
        for nm, hi, lo in (("mb16", b16, None), ("mo32", b32, b16), ("mo64", b64, b32), ("mo128", self.onesf, b64)):
            t = c.sb([128, 128], BF16, nm)
            if lo is None:
                p.op("pool", lambda e, t=t, hi=hi: e.tensor_copy(out=t[:, :], in_=hi[:, :]),
                     reads=_toks([hi]), writes=_toks([t]))
            else:
                p.op("pool", lambda e, t=t, hi=hi, lo=lo: e.tensor_tensor(out=t[:, :], in0=hi[:, :], in1=lo[:, :],
                                                                         op=ALU.subtract),
                     reads=_toks([hi, lo]), writes=_toks([t]))
            self.mlev.append(t)
        self.onesf2 = self.onesf
        p.op("pool", lambda e: e.memset(self.onesf[:, :], 1.0), writes=_toks([self.onesf]))
        self.hm = c.sb([128, 2], F32, "hm")
        p.op("pool", lambda e: e.memset(self.hm[:, :], 0.0), writes=_toks([self.hm]))
        p.op("pool", lambda e: e.memset(self.hm[0:64, 0:1], 1.0), reads=_toks([self.hm]), writes=_toks([self.hm]))
        p.op("pool", lambda e: e.memset(self.hm[64:128, 1:2], 1.0), reads=_toks([self.hm]), writes=_toks([self.hm]))
        self.onec = c.sb([128, 1], F32, "onec")
        p.op("dve", lambda e: e.memset(self.onec[:, :], 1.0), writes=_toks([self.onec]))
        self.epsc2 = c.sb([128, 1], F32, "eps2")
        p.op("dve", lambda e: e.memset(self.epsc2[:, :], 64e-5), writes=_toks([self.epsc2]))

    def dbg(self, name, t, ap, shape, dtype=F32):
        if not getattr(self, "debug", False) or name in self.dbg_names:
            return
        self.dbg_names.add(name)
        d = self.nc.dram_tensor("dbg_" + name, list(shape), dtype, kind="ExternalOutput").ap()
        tk = Tok()
        self.p.op("sp", lambda e: e.dma_start(out=d, in_=ap), reads=_toks([t]), writes=[tk], dma=tk)
        self.dbg_toks.append(tk)

    def hsrc(self, i):
        src = self.x if self.first else self.out
        return src[i * 128:(i + 1) * 128, :]

    def load_g(self, gt, layer, sub, wgt=1.0):
        p = self.p
        for j in range(gt.h.shape[1]):
            src = self.sandwich_g[layer, sub, j, :].partition_broadcast(128)
            p.op("sp", lambda e, src=src, j=j: e.dma_start(out=gt[:, j, :], in_=src),
                 writes=_toks([gt]), dma=gt.tok)
        if wgt != 1.0 and gt.h.shape[1] > 1:
            p.op("dve", lambda e: e.tensor_scalar(out=gt[:, 1, :], in0=gt[:, 1, :], scalar1=float(wgt), scalar2=None,
                                                  op0=ALU.mult),
                 reads=_toks([gt]), writes=_toks([gt]))

    def rstd(self, src_ap, src_toks, junk, ss, rs, n=D, eps=EPS):
        p = self.p
        p.op("act", lambda e: e.activation(out=junk[:, :n], in_=src_ap, func=AF.Square,
                                           scale=float(n) ** -0.5, accum_out=ss[:, 0:1]),
             reads=_toks(src_toks), writes=_toks([junk, ss]))
        self.rsqrt_small(ss[:, 0:1], rs[:, 0:1], [ss], [rs], eps)

    def rsqrt_small(self, src, dst, rt, wt, eps):
        p = self.p
        b = self.epsc[:, 0:1] if eps == EPS else self.epsc2[:, 0:1]
        p.op("act", lambda e: e.activation(out=dst, in_=src, func=AF.Ln, bias=b),
             reads=_toks(rt + [self.epsc]), writes=_toks(wt))
        p.op("act", lambda e: e.activation(out=dst, in_=dst, func=AF.Exp, scale=-0.5),
             reads=_toks(wt), writes=_toks(wt))

    def ffn(self, layer, which):
        with self.c.phase():
            self._ffn(layer, which)
        self.first = False

    def _ffn(self, layer, which):
        c, p, nc = self.c, self.p, self.nc
        NT = self.NT
        sub = 0 if which == 0 else 3
        TB = 4 if NT % 4 == 0 else 1
        NB = NT // TB
        W = TB * 128
        NH = DFF // 128
        a = {}
        a["win"] = [c.sb([128, 2 * DFF], BF16, f"win{k}") for k in range(8)]
        a["wout"] = [c.sb([128, D], BF16, f"wout{k}") for k in range(NH)]
        a["g"] = c.sb([128, 2, D], F32, "g")
        a["h"] = [c.sb([128, D], F32, f"h{i}") for i in range(2)]
        a["hr"] = [c.sb([128, D], F32, f"hr{i}") for i in range(2)]
        a["xn"] = [c.sb([128, D], BF16, f"xn{i}") for i in range(2)]
        a["xT"] = [c.sb([128, 8, W], BF16, f"xT{i}") for i in range(1)]
        a["hT"] = [c.sb([128, NH, W], BF16, f"hT{i}") for i in range(1)]
        a["sg"] = [c.sb([128, W], F32, f"sg{i}") for i in range(2)]
        a["junk"] = c.sb([128, D], BF16, "junk")
        a["ss"] = [c.sb([128, 1], F32, f"ss{i}") for i in range(4)]
        a["rs"] = [c.sb([128, 1], F32, f"rs{i}") for i in range(4)]
        a["ssb"] = [c.sb([128, 1], F32, f"ssb{i}") for i in range(2)]
        self._pk = 0
        a["t"] = [c.sb([128, D], F32, "t")] * 2
        a["pT"] = [c.ps([128, 8, 128], BF16, f"pT{i}") for i in range(1)]
        a["pg"] = [c.ps([128, W], F32, f"pg{i}") for i in range(2)]
        a["pu"] = [c.ps([128, W], F32, f"pu{i}") for i in range(2)]
        a["po"] = [c.ps([128, 512], F32, f"po{i}") for i in range(3)]
        c.stg = a["hr"] + a["h"] + [a["t"][0]]
        self._stg_i = 0
        win_src = self.ffn_w_in[layer, which]
        wout_src = self.ffn_w_out[layer, which]
        wv_ = self.wload_pieces(a["win"], win_src, order=[0, 2, 3, 1, 4, 5])
        for k in range(NH):
            self.wload(a["wout"][k], a["wout"][k][:, :], wout_src[k * 128:(k + 1) * 128, :])
        self.load_g(a["g"], layer, sub, 0.5)
        g = a["g"]
        cnt = 0
        xT = a["xT"][0]
        hT = a["hT"][0]

        def stage_a(b, j):
            nonlocal cnt
            xn = a["xn"][cnt % 2]
            cnt += 1
            self.pre(b * TB + j, a, g, xn)
            return xn

        for j in range(TB):
            xn = stage_a(0, j)
            self.to_fm(xn, xT, j, a)
        for b in range(NB):
            for cch in range(NH):
                pg, pu, sg = a["pg"][cch % 2], a["pu"][cch % 2], a["sg"][cch % 2]
                for k in range(8):
                    p.op("pe", lambda e, k=k, cch=cch, pg=pg: e.matmul(
                        pg[:, :], lhsT=a["win"][k][:, cch * 128:(cch + 1) * 128], rhs=xT[:, k, :],
                        start=(k == 0), stop=(k == 7)),
                        reads=_toks([wv_(k, cch * 128), xT]), writes=_toks([pg]), sig=(k == 7))
                for k in range(8):
                    p.op("pe", lambda e, k=k, cch=cch, pu=pu: e.matmul(
                        pu[:, :], lhsT=a["win"][k][:, DFF + cch * 128:DFF + (cch + 1) * 128], rhs=xT[:, k, :],
                        start=(k == 0), stop=(k == 7)),
                        reads=_toks([wv_(k, DFF + cch * 128), xT]), writes=_toks([pu]), sig=(k == 7))
                p.op("act", lambda e, pg=pg, sg=sg: e.activation(out=sg[:, :], in_=pg[:, :], func=AF.Exp, scale=-1.0),
                     reads=_toks([pg]), writes=_toks([sg]))
                p.op("act", lambda e, sg=sg: e.activation(out=sg[:, :], in_=sg[:, :], func=AF.Ln, bias=self.onec[:, 0:1]),
                     reads=_toks([sg, self.onec]), writes=_toks([sg]))
                p.op("act", lambda e, sg=sg: e.activation(out=sg[:, :], in_=sg[:, :], func=AF.Exp, scale=-1.0),
                     reads=_toks([sg]), writes=_toks([sg]))
                p.op("dve", lambda e, pg=pg, sg=sg: e.tensor_tensor(
                    out=sg[:, :], in0=pg[:, :], in1=sg[:, :], op=ALU.mult),
                    reads=_toks([sg, pg]), writes=_toks([sg]))
                p.op("dve", lambda e, cch=cch, pu=pu, sg=sg: e.tensor_tensor(
                    out=hT[:, cch, :], in0=sg[:, :], in1=pu[:, :], op=ALU.mult),
                    reads=_toks([sg, pu]), writes=_toks([hT]))
            for j in range(TB):
                i = b * TB + j
                pa, pb = a["po"][self._pk % 3], a["po"][(self._pk + 1) % 3]
                self._pk += 2
                xn = stage_a(b + 1, j) if b + 1 < NB else None
                for n, pp in ((0, pa), (1, pb)):
                    for cch in range(NH):
                        p.op("pe", lambda e, cch=cch, n=n, j=j, pp=pp: e.matmul(
                            pp[:, :], lhsT=hT[:, cch, j * 128:(j + 1) * 128],
                            rhs=a["wout"][cch][:, n * 512:(n + 1) * 512],
                            start=(cch == 0), stop=(cch == NH - 1)),
                            reads=_toks([hT, a["wout"][cch]]), writes=_toks([pp]),
                            sig=(cch == NH - 1))
                if xn is not None:
                    self.to_fm(xn, xT, j, a)
                self.post2(pa, pb, g, i, a)

    def post2(self, pa, pb, g, i, a):
        p = self.p
        self._pc = getattr(self, "_pc", 0) + 1
        ss, rs = a["ss"][2 + self._pc % 2], a["rs"][2 + self._pc % 2]
        ssb = a["ssb"][self._pc % 2]
        t = a["t"][self._pc % 2]
        ht = a["hr"][self._pc % 2]
        src = self.hsrc(i)
        p.op("sp", lambda e: e.dma_start(out=ht[:, :], in_=src),
             reads=[self.htok[i]], writes=_toks([ht]), dma=ht.tok)
        p.op("act", lambda e: e.activation(out=a["junk"][:, 0:512], in_=pa[:, :], func=AF.Square,
                                           scale=float(D) ** -0.5, accum_out=ss[:, 0:1]),
             reads=_toks([pa]), writes=_toks([a["junk"], ss]))
        p.op("act", lambda e: e.activation(out=a["junk"][:, 512:1024], in_=pb[:, :], func=AF.Square,
                                           scale=float(D) ** -0.5, accum_out=ssb[:, 0:1]),
             reads=_toks([pb]), writes=_toks([a["junk"], ssb]))
        p.op("dve", lambda e: e.tensor_tensor(out=ss[:, 0:1], in0=ss[:, 0:1], in1=ssb[:, 0:1], op=ALU.add),
             reads=_toks([ss, ssb]), writes=_toks([ss]))
        self.rsqrt_small(ss[:, 0:1], rs[:, 0:1], [ss], [rs], EPS)
        p.op("dve", lambda e: e.scalar_tensor_tensor(
            out=t[:, 0:512], in0=pa[:, :], scalar=rs[:, 0:1], in1=g[:, 1, 0:512], op0=ALU.mult, op1=ALU.mult),
            reads=_toks([pa, rs, g]), writes=_toks([t]))
        p.op("dve", lambda e: e.scalar_tensor_tensor(
            out=t[:, 512:1024], in0=pb[:, :], scalar=rs[:, 0:1], in1=g[:, 1, 512:1024], op0=ALU.mult, op1=ALU.mult),
            reads=_toks([pb, rs, g]), writes=_toks([t]))
        p.op("pool", lambda e: e.tensor_tensor(out=ht[:, :], in0=t[:, :], in1=ht[:, :], op=ALU.add),
             reads=_toks([t, ht]), writes=_toks([ht]))
        p.op("sp", lambda e: e.dma_start(out=self.out[i * 128:(i + 1) * 128, :], in_=ht[:, :]),
             reads=_toks([ht]), writes=[self.htok[i]], dma=ht.tok)

    def wload(self, t, dst, src):
        c, p = self.c, self.p
        if getattr(c, "stg", None) is None:
            c.stg = [c.sb([128, 1024], F32, f"stg{i}") for i in range(getattr(self, "nstg", 2))]
            self._stg_i = 0
        stg = c.stg
        rows, cols = src.shape
        for c0 in range(0, cols, 1024):
            c1 = min(cols, c0 + 1024)
            st = stg[self._stg_i % len(stg)]
            eng = ("act", "dve", "pool", "act", "dve")[self._stg_i % 5]
            self._stg_i += 1
            p.op("sp", lambda e, st=st, c0=c0, c1=c1: e.dma_start(out=st[0:rows, 0:c1 - c0], in_=src[:, c0:c1]),
                 writes=_toks([st]), dma=st.tok)
            if eng == "act":
                p.op("act", lambda e, st=st, c0=c0, c1=c1: e.copy(out=dst[:, c0:c1], in_=st[0:rows, 0:c1 - c0]),
                     reads=_toks([st]), writes=_toks([t]))
            else:
                p.op(eng, lambda e, st=st, c0=c0, c1=c1: e.tensor_copy(out=dst[:, c0:c1], in_=st[0:rows, 0:c1 - c0]),
                     reads=_toks([st]), writes=_toks([t]))

    def wload_pieces(self, tiles, src, order=None, pw_=1024):
        ncols = src.shape[1]
        npc = (ncols + pw_ - 1) // pw_
        order = list(order) if order is not None else list(range(npc))
        order += [x for x in range(npc) if x not in order]
        views = {}
        for pc in order:
            c0, c1 = pc * pw_, min(ncols, (pc + 1) * pw_)
            for k, t in enumerate(tiles):
                v = T(t.h[:, c0:c1], f"wp{k}_{pc}")
                self.c.ptoks.append(v.tok)
                views[(k, pc)] = v
                self.wload(v, t[:, c0:c1], src[k * 128:(k + 1) * 128, c0:c1])
        return lambda k, col: views[(k, col // pw_)]

    def pre(self, i, a, g, xn):
        p = self.p
        self._prc = getattr(self, "_prc", 0) + 1
        ht = a["h"][self._prc % 2]
        ss, rs = a["ss"][self._prc % 2], a["rs"][self._prc % 2]
        src = self.hsrc(i)
        p.op("sp", lambda e: e.dma_start(out=ht[:, :], in_=src),
             reads=[self.htok[i]], writes=_toks([ht]), dma=ht.tok)
        self.rstd(ht[:, :], [ht], a["junk"], ss, rs)
        p.op("dve", lambda e: e.scalar_tensor_tensor(
            out=xn[:, :], in0=ht[:, :], scalar=rs[:, 0:1], in1=g[:, 0, :],
            op0=ALU.mult, op1=ALU.mult),
            reads=_toks([ht, rs, g]), writes=_toks([xn]))

    def to_fm(self, xn, xT, j, a, nk=8, off=0):
        p = self.p
        pT = a["pT"][0]
        for k in range(nk):
            p.op("pe", lambda e, k=k: e.transpose(out=pT[:, k, :], in_=xn[:, k * 128:(k + 1) * 128],
                                                   identity=self.ident[:, :]),
                 reads=_toks([xn, self.ident]), writes=_toks([pT]), sig=(k == nk - 1))
        p.op("act", lambda e: e.copy(out=xT[:, 0:nk, off + j * 128:off + (j + 1) * 128], in_=pT[:, 0:nk, :]),
             reads=_toks([pT]), writes=_toks([xT]))

    def post(self, po, g, wgt, i, a):
        p = self.p
        self._pc = getattr(self, "_pc", 0) + 1
        ss, rs = a["ss"][2 + self._pc % 2], a["rs"][2 + self._pc % 2]
        t = a["t"][self._pc % 2]
        ht = a["hr"][self._pc % 2]
        src = self.hsrc(i)
        p.op("sp", lambda e: e.dma_start(out=ht[:, :], in_=src),
             reads=[self.htok[i]], writes=_toks([ht]), dma=ht.tok)
        self.rstd(po[:, :], [po], a["junk"], ss, rs)
        p.op("dve", lambda e: e.scalar_tensor_tensor(
            out=t[:, :], in0=po[:, :], scalar=rs[:, 0:1], in1=g[:, 1, :], op0=ALU.mult, op1=ALU.mult),
            reads=_toks([po, rs, g]), writes=_toks([t]))
        p.op("pool", lambda e: e.tensor_tensor(out=ht[:, :], in0=t[:, :], in1=ht[:, :], op=ALU.add),
             reads=_toks([t, ht]), writes=_toks([ht]))
        p.op("sp", lambda e: e.dma_start(out=self.out[i * 128:(i + 1) * 128, :], in_=ht[:, :]),
             reads=_toks([ht]), writes=[self.htok[i]], dma=ht.tok)

    def xattn(self, layer):
        with self.c.phase():
            self._xattn(layer)
        self.first = False

    def bcast_row(self, t, dst, src_row):
        self.p.op("sp", lambda e: e.dma_start(out=dst, in_=src_row.partition_broadcast(128)),
                  writes=_toks([t]), dma=t.tok)

    def _xattn(self, layer):
        c, p, nc = self.c, self.p, self.nc
        NT = self.NT
        TB = 4 if NT % 4 == 0 else 1
        NB = NT // TB
        W = TB * 128
        a = {}
        a["wq"] = [c.sb([128, D], BF16, f"wq{k}") for k in range(8)]
        a["wkv"] = [c.sb([128, 2 * D], BF16, f"wkv{k}") for k in range(8)]
        a["wo"] = [c.sb([128, D], BF16, f"wo{k}") for k in range(8)]
        a["g"] = c.sb([128, 2, D], F32, "g")
        a["gm"] = c.sb([128, D], F32, "gm")
        a["h"] = [c.sb([128, D], F32, f"h{i}") for i in range(2)]
        a["hr"] = [c.sb([128, D], F32, f"hr{i}") for i in range(2)]
        a["xn"] = [c.sb([128, D], BF16, f"xn{i}") for i in range(2)]
        a["xT"] = [c.sb([128, 8, W], BF16, "xT")]
        a["junk"] = c.sb([128, D], BF16, "junk")
        a["ss"] = [c.sb([128, 1], F32, f"ss{i}") for i in range(4)]
        a["rs"] = [c.sb([128, 1], F32, f"rs{i}") for i in range(4)]
        a["t"] = [c.sb([128, D], F32, f"t{i}") for i in range(2)]
        a["pT"] = [c.ps([128, 8, 128], BF16, "pT")]
        a["po"] = [c.ps([128, D], F32, "po")]
        memT = c.sb([128, 8, NMEM], BF16, "memT")
        KT = c.sb([128, 8, NMEM], BF16, "KT")
        V = c.sb([128, 2, D], BF16, "V")
        qT = c.sb([128, 8, W], BF16, "qT")
        oT = c.sb([128, 8, W], BF16, "oT")
        PT = c.sb([128, 8, W], BF16, "PT")
        pss2 = [c.ps([128, 2, NMEM], F32, f"pss{i}") for i in range(2)]
        Pf2 = [[c.sb([128, 2, NMEM], F32, f"Pf{h}{i}") for i in range(2)] for h in range(2)]
        Pn2 = [[c.sb([128, 2, NMEM], BF16, f"Pn{h}{i}") for i in range(2)] for h in range(2)]
        mx2 = [[c.sb([128, 2], F32, f"mx{h}{i}") for i in range(2)] for h in range(2)]
        rs2 = [[c.sb([128, 2], F32, f"rs2{h}{i}") for i in range(2)] for h in range(2)]
        pgen = [c.ps([128, 512], F32, f"pgen{i}") for i in range(2)]
        self.nstg = 4
        for k in range(8):
            self.wload(a["wkv"][k], a["wkv"][k][:, :], self.xa_w_kv[layer, k * 128:(k + 1) * 128, :])
        for k in range(8):
            self.wload(a["wq"][k], a["wq"][k][:, :], self.xa_w_q[layer, k * 128:(k + 1) * 128, :])
        for k in range(8):
            self.wload(a["wo"][k], a["wo"][k][:, :], self.xa_w_o[layer, k * 128:(k + 1) * 128, :])
        self.nstg = 2
        self.load_g(a["g"], layer, 2)
        g = a["g"]
        self.bcast_row(a["gm"], a["gm"][:, :], self.mem_norm_g[layer, :])
        for mt in range(2):
            ht = a["h"][mt]
            ss, rs = a["ss"][mt], a["rs"][mt]
            xn = a["xn"][mt]
            p.op("sp", lambda e, ht=ht, mt=mt: e.dma_start(out=ht[:, :], in_=self.mem[mt * 128:(mt + 1) * 128, :]),
                 writes=_toks([ht]), dma=ht.tok)
            self.rstd(ht[:, :], [ht], a["junk"], ss, rs)
            p.op("dve", lambda e, ht=ht, rs=rs, xn=xn: e.scalar_tensor_tensor(
                out=xn[:, :], in0=ht[:, :], scalar=rs[:, 0:1], in1=a["gm"][:, :], op0=ALU.mult, op1=ALU.mult),
                reads=_toks([ht, rs, a["gm"]]), writes=_toks([xn]))
            self.to_fm(xn, memT, mt, a)
        gi = 0
        for fc in range(8):
            pg = pgen[gi % 2]
            gi += 1
            for k in range(8):
                p.op("pe", lambda e, k=k, fc=fc, pg=pg: e.matmul(
                    pg[:, 0:NMEM], lhsT=a["wkv"][k][:, fc * 128:(fc + 1) * 128], rhs=memT[:, k, :],
                    start=(k == 0), stop=(k == 7)),
                    reads=_toks([a["wkv"][k], memT]), writes=_toks([pg]), sig=(k == 7))
            p.op("act", lambda e, fc=fc, pg=pg: e.copy(out=KT[:, fc, :], in_=pg[:, 0:NMEM]),
                 reads=_toks([pg]), writes=_toks([KT]))
        for mt in range(2):
            for n in range(2):
                pg = pgen[gi % 2]
                gi += 1
                for k in range(8):
                    p.op("pe", lambda e, k=k, mt=mt, n=n, pg=pg: e.matmul(
                        pg[:, :], lhsT=memT[:, k, mt * 128:(mt + 1) * 128],
                        rhs=a["wkv"][k][:, D + n * 512:D + (n + 1) * 512], start=(k == 0), stop=(k == 7)),
                        reads=_toks([a["wkv"][k], memT]), writes=_toks([pg]), sig=(k == 7))
                p.op("dve", lambda e, mt=mt, n=n, pg=pg: e.tensor_copy(out=V[:, mt, n * 512:(n + 1) * 512], in_=pg[:, :]),
                     reads=_toks([pg]), writes=_toks([V]))
        sc = 256.0 ** -0.5
        cnt = 0
        xT = a["xT"][0]

        def stage_a(b, j):
            nonlocal cnt
            xn = a["xn"][cnt % 2]
            cnt += 1
            self.pre(b * TB + j, a, g, xn)
            return xn

        for j in range(TB):
            xn = stage_a(0, j)
            self.to_fm(xn, xT, j, a)
        for b in range(NB):
            for fc in range(8):
                pg = pgen[gi % 2]
                gi += 1
                for k in range(8):
                    p.op("pe", lambda e, k=k, fc=fc, pg=pg: e.matmul(
                        pg[:, 0:W], lhsT=a["wq"][k][:, fc * 128:(fc + 1) * 128], rhs=xT[:, k, :],
                        start=(k == 0), stop=(k == 7)),
                        reads=_toks([a["wq"][k], xT]), writes=_toks([pg]), sig=(k == 7))
                eng = "act" if fc % 2 == 0 else "dve"
                if eng == "act":
                    p.op("act", lambda e, fc=fc, pg=pg: e.copy(out=qT[:, fc, :], in_=pg[:, 0:W]),
                         reads=_toks([pg]), writes=_toks([qT]))
                else:
                    p.op("dve", lambda e, fc=fc, pg=pg: e.tensor_copy(out=qT[:, fc, :], in_=pg[:, 0:W]),
                         reads=_toks([pg]), writes=_toks([qT]))
            def pair_gen(j, hp):
                r = j % 2
                pf, pn, m_, rsm, ps_ = Pf2[hp][r], Pn2[hp][r], mx2[hp][r], rs2[hp][r], pss2[hp]
                for h2 in range(2):
                    hd = 2 * hp + h2
                    for dc in range(2):
                        p.op("pe", lambda e, hd=hd, h2=h2, dc=dc: e.matmul(
                            ps_[:, h2, :], lhsT=qT[:, 2 * hd + dc, j * 128:(j + 1) * 128], rhs=KT[:, 2 * hd + dc, :],
                            start=(dc == 0), stop=(dc == 1)),
                            reads=_toks([qT, KT]), writes=_toks([ps_]), sig=(h2 == 1 and dc == 1))
                yield
                p.op("dve", lambda e: e.tensor_reduce(out=m_[:, :], in_=ps_[:, :, :], axis=AX.X, op=ALU.max),
                     reads=_toks([ps_]), writes=_toks([m_]))
                yield
                p.op("dve", lambda e: e.tensor_scalar(out=m_[:, :], in0=m_[:, :], scalar1=-sc, scalar2=None, op0=ALU.mult),
                     reads=_toks([m_]), writes=_toks([m_]))
                yield
                for h2 in range(2):
                    p.op("act", lambda e, h2=h2: e.activation(
                        out=pf[:, h2, :], in_=ps_[:, h2, :], func=AF.Exp, scale=sc, bias=m_[:, h2:h2 + 1],
                        accum_out=rsm[:, h2:h2 + 1]),
                        reads=_toks([ps_, m_]), writes=_toks([pf, rsm]))
                yield
                p.op("dve", lambda e: e.reciprocal(out=rsm[:, :], in_=rsm[:, :]), reads=_toks([rsm]), writes=_toks([rsm]))
                yield
                p.op("dve", lambda e: e.tensor_tensor(
                    out=pn[:, :, :], in0=pf[:, :, :], in1=rsm[:, :].unsqueeze(2).to_broadcast([128, 2, NMEM]), op=ALU.mult),
                    reads=_toks([pf, rsm]), writes=_toks([pn]))
                yield
                pT = a["pT"][0]
                for q in range(4):
                    h2, mc = q // 2, q % 2
                    p.op("pe", lambda e, q=q, h2=h2, mc=mc: e.transpose(
                        out=pT[:, q, :], in_=pn[:, h2, mc * 128:(mc + 1) * 128], identity=self.ident[:, :]),
                        reads=_toks([pn, self.ident]), writes=_toks([pT]), sig=(q == 3))
                p.op("act", lambda e: e.copy(out=PT[:, 4 * hp:4 * hp + 4, j * 128:(j + 1) * 128], in_=pT[:, 0:4, :]),
                     reads=_toks([pT]), writes=_toks([PT]))
                yield

            for j in range(TB):
                self.run_gens([pair_gen(j, 0), pair_gen(j, 1)])
            for fc in range(8):
                hd = fc // 2
                pg = pgen[gi % 2]
                gi += 1
                for mc in range(2):
                    p.op("pe", lambda e, fc=fc, hd=hd, mc=mc, pg=pg: e.matmul(
                        pg[:, 0:W], lhsT=V[:, mc, fc * 128:(fc + 1) * 128], rhs=PT[:, 2 * hd + mc, :],
                        start=(mc == 0), stop=(mc == 1)),
                        reads=_toks([V, PT]), writes=_toks([pg]), sig=(mc == 1))
                if fc % 2 == 0:
                    p.op("act", lambda e, fc=fc, pg=pg: e.copy(out=oT[:, fc, :], in_=pg[:, 0:W]),
                         reads=_toks([pg]), writes=_toks([oT]))
                else:
                    p.op("dve", lambda e, fc=fc, pg=pg: e.tensor_copy(out=oT[:, fc, :], in_=pg[:, 0:W]),
                         reads=_toks([pg]), writes=_toks([oT]))
            for j in range(TB):
                po = a["po"][0]
                xn = stage_a(b + 1, j) if b + 1 < NB else None
                for n in range(2):
                    for fc in range(8):
                        p.op("pe", lambda e, fc=fc, n=n, j=j: e.matmul(
                            po[:, n * 512:(n + 1) * 512], lhsT=oT[:, fc, j * 128:(j + 1) * 128],
                            rhs=a["wo"][fc][:, n * 512:(n + 1) * 512], start=(fc == 0), stop=(fc == 7)),
                            reads=_toks([oT, a["wo"][fc]]), writes=_toks([po]), sig=(fc == 7 and n == 1))
                if xn is not None:
                    self.to_fm(xn, xT, j, a)
                self.post(po, g, 1.0, b * TB + j, a)

    def load_cols(self, rows, nchunk, ps, name):
        c, p = self.c, self.p
        nr = len(rows)
        out = c.sb([128, nchunk, nr], F32, name)
        with c.phase():
            rt = c.sb([nr, nchunk * 128], F32, name + "_r")
            for r, row in enumerate(rows):
                p.op("sp", lambda e, r=r, row=row: e.dma_start(out=rt[r:r + 1, :], in_=row[None, :]),
                     writes=_toks([rt]), dma=rt.tok)
            for cc in range(nchunk):
                p.op("pe", lambda e, cc=cc: e.transpose(out=ps[:, cc * nr:(cc + 1) * nr],
                                                        in_=rt[0:nr, cc * 128:(cc + 1) * 128],
                                                        identity=self.identf[0:nr, 0:nr]),
                     reads=_toks([rt, self.identf]), writes=_toks([ps]), sig=(cc == nchunk - 1))
            p.op("act", lambda e: e.copy(out=out[:, :, :].rearrange("p c r -> p (c r)"), in_=ps[:, 0:nchunk * nr]),
                 reads=_toks([ps]), writes=_toks([out]))
        return out

    def sigmoid_inplace(self, ap, t, neg_bias=None, extra_reads=()):
        p = self.p
        if neg_bias is None:
            p.op("act", lambda e: e.activation(out=ap, in_=ap, func=AF.Exp, scale=-1.0),
                 reads=_toks([t]), writes=_toks([t]))
        else:
            p.op("act", lambda e: e.activation(out=ap, in_=ap, func=AF.Exp, scale=-1.0, bias=neg_bias),
                 reads=_toks([t] + list(extra_reads)), writes=_toks([t]))
        p.op("act", lambda e: e.activation(out=ap, in_=ap, func=AF.Ln, bias=self.onec[:, 0:1]),
             reads=_toks([t, self.onec]), writes=_toks([t]))
        p.op("act", lambda e: e.activation(out=ap, in_=ap, func=AF.Exp, scale=-1.0),
             reads=_toks([t]), writes=_toks([t]))

    def conv_chunk(self, ps, cb, halo, cw, ncw, cc, W, dst_ap, dst_t, sgb, has_bias, first_block):
        p = self.p
        p.op("act", lambda e: e.copy(out=cb[:, 3:3 + W], in_=ps[:, 0:W]), reads=_toks([ps]), writes=_toks([cb]))
        p.op("dve", lambda e: e.tensor_copy(out=cb[:, 0:3], in_=halo[:, cc, :]), reads=_toks([halo]), writes=_toks([cb]))
        yield
        p.op("act", lambda e: e.copy(out=halo[:, cc, :], in_=cb[:, W:W + 3]), reads=_toks([cb]), writes=_toks([halo]))
        acc = sgb["acc"]
        sg = sgb["sg"]
        p.op("dve", lambda e: e.tensor_scalar(out=acc[:, 0:W], in0=cb[:, 3:3 + W], scalar1=cw[:, cc, 3:4], scalar2=None,
                                              op0=ALU.mult), reads=_toks([cb, cw]), writes=_toks([acc]))
        yield
        for j in (2, 1, 0):
            p.op("dve", lambda e, j=j: e.scalar_tensor_tensor(
                out=acc[:, 0:W], in0=cb[:, j:j + W], scalar=cw[:, cc, j:j + 1], in1=acc[:, 0:W],
                op0=ALU.mult, op1=ALU.add), reads=_toks([cb, cw, acc]), writes=_toks([acc]))
            yield
        if has_bias:
            p.op("act", lambda e: e.activation(out=sg[:, 0:W], in_=acc[:, 0:W], func=AF.Exp, scale=-1.0,
                                               bias=ncw[:, cc:cc + 1]),
                 reads=_toks([acc, ncw]), writes=_toks([sg]))
        else:
            p.op("act", lambda e: e.activation(out=sg[:, 0:W], in_=acc[:, 0:W], func=AF.Exp, scale=-1.0),
                 reads=_toks([acc]), writes=_toks([sg]))
        yield
        p.op("act", lambda e: e.activation(out=sg[:, 0:W], in_=sg[:, 0:W], func=AF.Ln, bias=self.onec[:, 0:1]),
             reads=_toks([sg, self.onec]), writes=_toks([sg]))
        yield
        p.op("act", lambda e: e.activation(out=sg[:, 0:W], in_=sg[:, 0:W], func=AF.Exp, scale=-1.0),
             reads=_toks([sg]), writes=_toks([sg]))
        yield
        if has_bias:
            p.op("dve", lambda e: e.scalar_tensor_tensor(
                out=dst_ap, in0=acc[:, 0:W], scalar=cw[:, cc, 4:5], in1=sg[:, 0:W], op0=ALU.add, op1=ALU.mult),
                reads=_toks([acc, cw, sg]), writes=_toks([dst_t]))
        else:
            p.op("dve", lambda e: e.tensor_tensor(out=dst_ap, in0=acc[:, 0:W], in1=sg[:, 0:W], op=ALU.mult),
                 reads=_toks([acc, sg]), writes=_toks([dst_t]))
        yield

    def fm_to_tm(self, src, src_sl, dsts, a, nch=8):
        p = self.p
        pT = a["pT"][0]
        for j, (dap, dt_) in enumerate(dsts):
            for q in range(nch):
                p.op("pe", lambda e, q=q, j=j: e.transpose(out=pT[:, q, :], in_=src[:, q, j * 128:(j + 1) * 128],
                                                           identity=self.ident[:, :]),
                     reads=_toks([src, self.ident]), writes=_toks([pT]), sig=(q == nch - 1))
            p.op("act", lambda e, dap=dap: e.copy(out=dap, in_=pT[:, 0:nch, :].rearrange("p c r -> p (c r)")),
                 reads=_toks([pT]), writes=_toks([dt_]))

    def ssd(self, layer):
        with self.c.phase():
            self._ssd(layer)

    def _ssd(self, layer):
        c, p, nc = self.c, self.p, self.nc
        jx = layer // 3
        NT = self.NT
        TB = 2 if NT % 2 == 0 else 1
        NB = NT // TB
        W = TB * 128
        CW = 6176
        a = {}
        a["win"] = [c.sb([128, CW], BF16, f"win{k}") for k in range(8)]
        a["g"] = c.sb([128, 1, D], F32, "g")
        a["h"] = [c.sb([128, D], F32, "h")] * 2
        a["xn"] = [c.sb([128, D], BF16, "xn")] * 2
        a["xT"] = [c.sb([128, 8, W], BF16, "xT")]
        a["junk"] = c.sb([128, D], BF16, "junk")
        a["ss"] = [c.sb([128, 1], F32, f"ss{i}") for i in range(4)]
        a["rs"] = [c.sb([128, 1], F32, f"rs{i}") for i in range(4)]
        a["pT"] = [c.ps([128, 8, 128], BF16, "pT")]
        pgen = [c.ps([128, 512], F32, f"pgen{i}") for i in range(2)]
        pcb = c.ps([128, 128], F32, "pcb")
        pz = c.ps([128, 256], F32, "pz")
        pgate = c.ps([128, 128], F32, "pgate")
        pdt = pgate.view((slice(None), slice(0, 32)), "pdt")
        pac = pgate.view((slice(None), slice(32, 96)), "pac")
        pD = c.ps([128, 4, 128], F32, "pD")
        pyy = c.ps([128, 512], F32, "pyy")
        py = pyy.view((slice(None), slice(0, 256)), "py")
        pyi = pyy.view((slice(None), slice(256, 512)), "pyi")
        for k in range(8):
            self.wload(a["win"][k], a["win"][k][:, :], self.ssd_w_in[jx, k * 128:(k + 1) * 128, :])
        self.load_g(a["g"], layer, 1)
        g = a["g"]
        cw = self.load_cols([self.ssd_conv_w[jx, t_, :] for t_ in range(4)] + [self.ssd_conv_b[jx, :]], 32, pgen[0], "cw")
        ncw = c.sb([128, 32], F32, "ncw")
        p.op("dve", lambda e: e.tensor_scalar(out=ncw[:, :], in0=cw[:, :, 4], scalar1=-1.0, scalar2=None, op0=ALU.mult),
             reads=_toks([cw]), writes=_toks([ncw]))
        dtb = c.sb([128, 32], F32, "dtb")
        aneg = c.sb([128, 32], F32, "aneg")
        dsk = c.sb([128, 32], F32, "dsk")
        ng = c.sb([128, 2 * D], F32, "ng")
        self.bcast_row(dtb, dtb[:, :], self.ssd_dt_bias[jx, :])
        self.bcast_row(aneg, aneg[:, :], self.ssd_a_log[jx, :])
        self.bcast_row(dsk, dsk[:, :], self.ssd_d[jx, :])
        self.bcast_row(ng, ng[:, :], self.ssd_norm_g[jx, :])
        p.op("act", lambda e: e.activation(out=aneg[:, :], in_=aneg[:, :], func=AF.Exp), reads=_toks([aneg]), writes=_toks([aneg]))
        p.op("dve", lambda e: e.tensor_scalar(out=aneg[:, :], in0=aneg[:, :], scalar1=-1.0, scalar2=None, op0=ALU.mult),
             reads=_toks([aneg]), writes=_toks([aneg]))
        halo = c.sb([128, 32, 3], F32, "halo")
        p.op("pool", lambda e: e.memset(halo[:, :, :], 0.0), writes=_toks([halo]))
        Z = [c.sb([128, 256], F32, f"Z{g_}") for g_ in range(8)]
        Zb = [c.sb([128, 256], BF16, f"Zb{g_}") for g_ in range(8)]
        for g_ in range(8):
            p.op("pool", lambda e, g_=g_: e.memset(Z[g_][:, :], 0.0), writes=_toks([Z[g_]]))
            p.op("pool", lambda e, g_=g_: e.memset(Zb[g_][:, :], 0.0), writes=_toks([Zb[g_]]))
        cb = [c.sb([128, W + 3], F32, f"cb{i}") for i in range(2)]
        sgb = [dict(acc=c.sb([128, W], F32, f"acc{i}"), sg=c.sb([128, W], F32, f"sgc{i}")) for i in range(2)]
        xfm = c.sb([128, 8, W], BF16, "xfm")
        BT = c.sb([128, 8, W], BF16, "BT")
        CT = c.sb([128, 8, W], BF16, "CT")
        xtm = [c.sb([128, 2 * D], BF16, f"xtm{j}") for j in range(TB)]
        Btm = [c.sb([128, D], BF16, f"Btm{j}") for j in range(TB)]
        dtl = c.sb([128, 32], F32, "dtl")
        dt_ = c.sb([128, 32], F32, "dt")
        adt = c.sb([128, 32], F32, "adt")
        acum = c.sb([128, 32], F32, "acum")
        eacum = c.sb([128, 32], F32, "eacum")
        elast = c.sb([128, 32], F32, "elast")
        toend = c.sb([128, 32], F32, "toend")
        xdt = c.sb([128, 32, 64], BF16, "xdt")
        xde = c.sb([128, 32, 64], BF16, "xde")
        cbm = c.sb([128, 128], F32, "cbm")
        LH = c.sb([128, 4, 128], F32, "LH")
        Ed = c.sb([128, 4, 128], F32, "Ed")
        Mt = c.sb([128, 4, 128], BF16, "Mt")
        ytmp = c.sb([128, 256], F32, "ytmp")
        y = c.sb([128, 2 * D], F32, "y")
        zs = c.sb([128, 512], F32, "zs")
        ss8 = c.sb([128, 8], F32, "ss8")
        yb = c.sb([128, 2 * D], BF16, "yb")
        gi = 0
        for b in range(NB):
            xT = a["xT"][0]
            for j in range(TB):
                self.pre(b * TB + j, a, g, a["xn"][0])
                self.to_fm(a["xn"][0], xT, j, a)
            def ssd_chunk(cc):
                pg = pgen[cc % 2]
                for k in range(8):
                    p.op("pe", lambda e, k=k: e.matmul(
                        pg[:, 0:W], lhsT=a["win"][k][:, 2048 + cc * 128:2048 + (cc + 1) * 128], rhs=xT[:, k, :],
                        start=(k == 0), stop=(k == 7)),
                        reads=_toks([a["win"][k], xT]), writes=_toks([pg]), sig=(k == 7))
                yield
                if cc < 16:
                    dst_t, dst_ap = xfm, xfm[:, cc % 8, :]
                elif cc < 24:
                    dst_t, dst_ap = BT, BT[:, cc - 16, :]
                else:
                    dst_t, dst_ap = CT, CT[:, cc - 24, :]
                yield from self.conv_chunk(pg, cb[cc % 2], halo, cw, ncw, cc, W, dst_ap, dst_t, sgb[cc % 2], True, b == 0)

            for cc in range(0, 32, 2):
                self.run_gens([ssd_chunk(cc), ssd_chunk(cc + 1)])
                if cc + 1 in (7, 15):
                    grp = (cc + 1) // 8
                    self.fm_to_tm(xfm, None, [(xtm[j][:, grp * D:(grp + 1) * D], xtm[j]) for j in range(TB)], a)
                if cc + 1 == 23:
                    self.fm_to_tm(BT, None, [(Btm[j][:, :], Btm[j]) for j in range(TB)], a)
            for j in range(TB):
                i = b * TB + j
                tsl = slice(j * 128, (j + 1) * 128)
                x3 = xtm[j][:, :].rearrange("p (h d) -> p h d", d=64)
                if i == 1:
                    self.dbg("xtm", xtm[j], xtm[j][:, :], [128, 2 * D], BF16)
                    self.dbg("Btm", Btm[j], Btm[j][:, :], [128, D], BF16)
                    self.dbg("CT", CT, CT[:, :, tsl], [128, 8, 128], BF16)
                for k in range(8):
                    p.op("pe", lambda e, k=k: e.matmul(pdt[:, :], lhsT=xT[:, k, tsl], rhs=a["win"][k][:, 6144:6176],
                                                       start=(k == 0), stop=(k == 7)),
                         reads=_toks([a["win"][k], xT]), writes=_toks([pdt]), sig=(k == 7))
                p.op("dve", lambda e: e.tensor_tensor(out=dtl[:, :], in0=pdt[:, :], in1=dtb[:, :], op=ALU.add),
                     reads=_toks([pdt, dtb]), writes=_toks([dtl]))
                p.op("act", lambda e: e.activation(out=dtl[:, :], in_=dtl[:, :], func=AF.Exp),
                     reads=_toks([dtl]), writes=_toks([dtl]))
                p.op("act", lambda e: e.activation(out=dt_[:, :], in_=dtl[:, :], func=AF.Ln, bias=self.onec[:, 0:1]),
                     reads=_toks([dtl, self.onec]), writes=_toks([dt_]))
                p.op("dve", lambda e: e.tensor_tensor(out=adt[:, :], in0=dt_[:, :], in1=aneg[:, :], op=ALU.mult),
                     reads=_toks([dt_, aneg]), writes=_toks([adt]))
                p.op("pe", lambda e: e.matmul(pac[:, 0:32], lhsT=self.triLE[:, :], rhs=adt[:, :], start=True, stop=True),
                     reads=_toks([self.triLE, adt]), writes=_toks([pac]))
                p.op("pe", lambda e: e.matmul(pac[:, 32:64], lhsT=self.onesf[:, :], rhs=adt[:, :], start=True, stop=True),
                     reads=_toks([self.onesf, adt]), writes=_toks([pac]))
                p.op("act", lambda e: e.copy(out=acum[:, :], in_=pac[:, 0:32]), reads=_toks([pac]), writes=_toks([acum]))
                p.op("act", lambda e: e.activation(out=eacum[:, :], in_=acum[:, :], func=AF.Exp),
                     reads=_toks([acum]), writes=_toks([eacum]))
                p.op("act", lambda e: e.activation(out=elast[:, :], in_=pac[:, 32:64], func=AF.Exp),
                     reads=_toks([pac]), writes=_toks([elast]))
                p.op("dve", lambda e: e.tensor_tensor(out=toend[:, :], in0=pac[:, 32:64], in1=acum[:, :], op=ALU.subtract),
                     reads=_toks([pac, acum]), writes=_toks([toend]))
                p.op("act", lambda e: e.activation(out=toend[:, :], in_=toend[:, :], func=AF.Exp),
                     reads=_toks([toend]), writes=_toks([toend]))
                p.op("dve", lambda e: e.tensor_tensor(out=toend[:, :], in0=toend[:, :], in1=dt_[:, :], op=ALU.mult),
                     reads=_toks([toend, dt_]), writes=_toks([toend]))
                p.op("dve", lambda e: e.tensor_tensor(
                    out=xdt[:, :, :], in0=x3, in1=dt_[:, :].unsqueeze(2).to_broadcast([128, 32, 64]), op=ALU.mult),
                    reads=_toks([xtm[j], dt_]), writes=_toks([xdt]))
                p.op("dve", lambda e: e.tensor_tensor(
                    out=xde[:, :, :], in0=x3, in1=toend[:, :].unsqueeze(2).to_broadcast([128, 32, 64]), op=ALU.mult),
                    reads=_toks([xtm[j], toend]), writes=_toks([xde]))
                for g_ in range(8):
                    hs = slice(4 * g_, 4 * g_ + 4)
                    p.op("pe", lambda e, g_=g_: e.matmul(pcb[:, :], lhsT=BT[:, g_, tsl], rhs=CT[:, g_, tsl],
                                                         start=True, stop=True),
                         reads=_toks([BT, CT]), writes=_toks([pcb]))
                    p.op("dve", lambda e: e.tensor_tensor(out=cbm[:, :], in0=pcb[:, :], in1=self.triLE[:, :], op=ALU.mult),
                         reads=_toks([pcb, self.triLE]), writes=_toks([cbm]))
                    p.op("dve", lambda e, hs=hs: e.tensor_tensor(
                        out=LH[:, :, :], in0=self.maskGT[:, :].unsqueeze(1).to_broadcast([128, 4, 128]),
                        in1=adt[:, hs].unsqueeze(2).to_broadcast([128, 4, 128]), op=ALU.mult),
                        reads=_toks([self.maskGT, adt]), writes=_toks([LH]))
                    for e_ in range(4):
                        p.op("pe", lambda e, e_=e_: e.matmul(pD[:, e_, :], lhsT=LH[:, e_, :], rhs=self.triLE[:, :],
                                                             start=True, stop=True),
                             reads=_toks([LH, self.triLE]), writes=_toks([pD]), sig=(e_ == 3))
                    p.op("act", lambda e: e.activation(out=Ed[:, :, :], in_=pD[:, :, :], func=AF.Exp),
                         reads=_toks([pD]), writes=_toks([Ed]))
                    p.op("dve", lambda e: e.tensor_tensor(
                        out=Mt[:, :, :], in0=Ed[:, :, :], in1=cbm[:, :].unsqueeze(1).to_broadcast([128, 4, 128]),
                        op=ALU.mult), reads=_toks([Ed, cbm]), writes=_toks([Mt]))
                    for e_ in range(4):
                        p.op("pe", lambda e, e_=e_, g_=g_: e.matmul(
                            py[:, e_ * 64:(e_ + 1) * 64], lhsT=Mt[:, e_, :], rhs=xdt[:, 4 * g_ + e_, :],
                            start=True, stop=True),
                            reads=_toks([Mt, xdt]), writes=_toks([py]), sig=(e_ == 3))
                    p.op("pe", lambda e, g_=g_: e.matmul(pyi[:, :], lhsT=CT[:, g_, tsl], rhs=Zb[g_][:, :],
                                                         start=True, stop=True),
                         reads=_toks([CT, Zb[g_]]), writes=_toks([pyi]))
                    p.op("dve", lambda e, hs=hs: e.tensor_tensor(
                        out=ytmp[:, :].rearrange("p (h d) -> p h d", d=64),
                        in0=pyi[:, :].rearrange("p (h d) -> p h d", d=64),
                        in1=eacum[:, hs].unsqueeze(2).to_broadcast([128, 4, 64]), op=ALU.mult),
                        reads=_toks([pyi, eacum]), writes=_toks([ytmp]))
                    p.op("dve", lambda e, g_=g_: e.tensor_tensor(out=y[:, g_ * 256:(g_ + 1) * 256], in0=ytmp[:, :],
                                                                 in1=py[:, :], op=ALU.add),
                         reads=_toks([ytmp, py]), writes=_toks([y]))
                    p.op("pe", lambda e, g_=g_: e.matmul(
                        pz[:, :], lhsT=Btm[j][:, g_ * 128:(g_ + 1) * 128],
                        rhs=xde[:, 4 * g_:4 * g_ + 4, :].rearrange("p h d -> p (h d)"), start=True, stop=True),
                        reads=_toks([Btm[j], xde]), writes=_toks([pz]))
                    p.op("pool", lambda e, g_=g_, hs=hs: e.tensor_tensor(
                        out=Z[g_][:, :].rearrange("p (h d) -> p h d", d=64),
                        in0=Z[g_][:, :].rearrange("p (h d) -> p h d", d=64),
                        in1=elast[:, hs].unsqueeze(2).to_broadcast([128, 4, 64]), op=ALU.mult),
                        reads=_toks([Z[g_], elast]), writes=_toks([Z[g_]]))
                    p.op("dve", lambda e, g_=g_: e.tensor_tensor(out=Z[g_][:, :], in0=Z[g_][:, :], in1=pz[:, :], op=ALU.add),
                         reads=_toks([Z[g_], pz]), writes=_toks([Z[g_]]))
                    p.op("act", lambda e, g_=g_: e.copy(out=Zb[g_][:, :], in_=Z[g_][:, :]),
                         reads=_toks([Z[g_]]), writes=_toks([Zb[g_]]))
                if i == 1:
                    self.dbg("dt", dt_, dt_[:, :], [128, 32])
                    self.dbg("acum", acum, acum[:, :], [128, 32])
                    self.dbg("toend", toend, toend[:, :], [128, 32])
                    self.dbg("y0", y, y[:, :], [128, 2 * D])
                yt3 = y[:, :].rearrange("p (h d) -> p h d", d=64)
                p.op("pool", lambda e: e.tensor_tensor(
                    out=xdt[:, :, :], in0=x3, in1=dsk[:, :].unsqueeze(2).to_broadcast([128, 32, 64]), op=ALU.mult),
                    reads=_toks([xtm[j], dsk]), writes=_toks([xdt]))
                p.op("pool", lambda e: e.tensor_tensor(out=yt3, in0=yt3, in1=xdt[:, :, :], op=ALU.add),
                     reads=_toks([y, xdt]), writes=_toks([y]))
                for n in range(4):
                    pg = pgen[gi % 2]
                    gi += 1
                    for k in range(8):
                        p.op("pe", lambda e, k=k, n=n, pg=pg: e.matmul(
                            pg[:, :], lhsT=xT[:, k, tsl], rhs=a["win"][k][:, n * 512:(n + 1) * 512],
                            start=(k == 0), stop=(k == 7)),
                            reads=_toks([a["win"][k], xT]), writes=_toks([pg]), sig=(k == 7))
                    p.op("act", lambda e, pg=pg: e.copy(out=zs[:, :], in_=pg[:, :]), reads=_toks([pg]), writes=_toks([zs]))
                    self.sigmoid_inplace(zs[:, :], zs)
                    p.op("dve", lambda e, pg=pg: e.tensor_tensor(out=zs[:, :], in0=zs[:, :], in1=pg[:, :], op=ALU.mult),
                         reads=_toks([zs, pg]), writes=_toks([zs]))
                    p.op("dve", lambda e, n=n: e.tensor_tensor(out=y[:, n * 512:(n + 1) * 512], in0=y[:, n * 512:(n + 1) * 512],
                                                                in1=zs[:, :], op=ALU.mult),
                         reads=_toks([zs, y]), writes=_toks([y]))
                for g_ in range(8):
                    p.op("act", lambda e, g_=g_: e.activation(
                        out=a["junk"][:, 0:256], in_=y[:, g_ * 256:(g_ + 1) * 256], func=AF.Square, scale=1.0 / 16.0,
                        accum_out=ss8[:, g_:g_ + 1]), reads=_toks([y]), writes=_toks([a["junk"], ss8]))
                self.rsqrt_small(ss8[:, :], ss8[:, :], [ss8], [ss8], EPS)
                p.op("dve", lambda e: e.tensor_tensor(
                    out=y[:, :].rearrange("p (g d) -> p g d", d=256), in0=y[:, :].rearrange("p (g d) -> p g d", d=256),
                    in1=ss8[:, :].unsqueeze(2).to_broadcast([128, 8, 256]), op=ALU.mult),
                    reads=_toks([y, ss8]), writes=_toks([y]))
                p.op("pool", lambda e: e.tensor_tensor(out=yb[:, :], in0=y[:, :], in1=ng[:, :], op=ALU.mult),
                     reads=_toks([y, ng]), writes=_toks([yb]))
                if i == 1:
                    self.dbg("yb", yb, yb[:, :], [128, 2 * D], BF16)
                p.op("sp", lambda e, i=i: e.dma_start(out=self.scr[i * 128:(i + 1) * 128, :], in_=yb[:, :]),
                     reads=_toks([yb]), writes=[self.ytok[i]], dma=yb.tok)

    def mix_out(self, layer, w_dram, kdim, zgate=None):
        with self.c.phase():
            self._mix_out(layer, w_dram, kdim, zgate)
        self.first = False

    def _mix_out(self, layer, w_dram, kdim, zgate):
        c, p = self.c, self.p
        nk = kdim // 128
        c.stg = [c.sb([128, 1024], F32, f"stg{i}") for i in range(4)]
        self._stg_i = 0
        a = {}
        if zgate is not None:
            a["h"] = [c.sb([128, D], F32, f"h{i}") for i in range(2)]
            a["xn"] = [c.sb([128, D], BF16, f"xn{i}") for i in range(2)]
            xT1 = c.sb([128, 8, 128], BF16, "xT1")
            wz = [c.sb([128, kdim], BF16, f"wz{k}") for k in range(8)]
            zs = [c.sb([128, 512], F32, f"zs{i}") for i in range(2)]
            pgz = [c.ps([128, 512], F32, f"pgz{i}") for i in range(2)]
            for k in range(8):
                self.wload(wz[k], wz[k][:, :], zgate[0][k * 128:(k + 1) * 128, zgate[1]:zgate[1] + kdim])
        a["wout"] = [c.sb([128, D], BF16, f"wout{k}") for k in range(nk)]
        a["g"] = c.sb([128, 2, D], F32, "g")
        a["hr"] = [c.sb([128, D], F32, f"hr{i}") for i in range(2)]
        a["junk"] = c.sb([128, D], BF16, "junk")
        a["ss"] = [c.sb([128, 1], F32, f"ss{i}") for i in range(4)]
        a["rs"] = [c.sb([128, 1], F32, f"rs{i}") for i in range(4)]
        a["t"] = [c.sb([128, D], F32, f"t{i}") for i in range(2)]
        a["pT"] = [c.ps([128, 8, 128], BF16, "pT")]
        a["po"] = [c.ps([128, D], F32, f"po{i}") for i in range(2)]
        yb = [c.sb([128, kdim], BF16, f"yb{i}") for i in range(2)]
        yT = [c.sb([128, nk, 128], BF16, f"yT{i}") for i in range(2)]
        for k in range(nk):
            self.wload(a["wout"][k], a["wout"][k][:, :], w_dram[k * 128:(k + 1) * 128, :])
        self.load_g(a["g"], layer, 1)
        g = a["g"]
        if zgate is not None:
            xT1s = [xT1, c.sb([128, 8, 128], BF16, "xT1b")]

        def tile_gen(i):
            r = i % 2
            y_, yT_ = yb[r], yT[r]
            p.op("sp", lambda e: e.dma_start(out=y_[:, :], in_=self.scr[i * 128:(i + 1) * 128, 0:kdim]),
                 reads=[self.ytok[i]], writes=_toks([y_]), dma=y_.tok)
            if zgate is not None:
                xn = a["xn"][r]
                self.pre(i, a, g, xn)
                yield
                self.to_fm(xn, xT1s[r], 0, a)
                yield
                pg, z_ = pgz[r], zs[r]
                for n in range(kdim // 512):
                    for k in range(8):
                        p.op("pe", lambda e, k=k, n=n: e.matmul(
                            pg[:, :], lhsT=xT1s[r][:, k, :], rhs=wz[k][:, n * 512:(n + 1) * 512], start=(k == 0), stop=(k == 7)),
                            reads=_toks([wz[k], xT1s[r]]), writes=_toks([pg]), sig=(k == 7))
                    yield
                    p.op("act", lambda e: e.activation(out=z_[:, :], in_=pg[:, :], func=AF.Exp, scale=-1.0),
                         reads=_toks([pg]), writes=_toks([z_]))
                    yield
                    p.op("act", lambda e: e.activation(out=z_[:, :], in_=z_[:, :], func=AF.Ln, bias=self.onec[:, 0:1]),
                         reads=_toks([z_, self.onec]), writes=_toks([z_]))
                    yield
                    p.op("act", lambda e: e.activation(out=z_[:, :], in_=z_[:, :], func=AF.Exp, scale=-1.0),
                         reads=_toks([z_]), writes=_toks([z_]))
                    yield
                    p.op("dve", lambda e: e.tensor_tensor(out=z_[:, :], in0=z_[:, :], in1=pg[:, :], op=ALU.mult),
                         reads=_toks([z_, pg]), writes=_toks([z_]))
                    yield
                    p.op("dve", lambda e, n=n: e.tensor_tensor(
                        out=y_[:, n * 512:(n + 1) * 512], in0=y_[:, n * 512:(n + 1) * 512], in1=z_[:, :], op=ALU.mult),
                        reads=_toks([z_, y_]), writes=_toks([y_]))
                    yield
            for hh in range(nk // 8):
                self.to_fm_part(y_, hh * 8, yT_, hh * 8, a)
                yield
            po = a["po"][r]
            for n in range(2):
                for fc in range(nk):
                    p.op("pe", lambda e, fc=fc, n=n: e.matmul(
                        po[:, n * 512:(n + 1) * 512], lhsT=yT_[:, fc, :], rhs=a["wout"][fc][:, n * 512:(n + 1) * 512],
                        start=(fc == 0), stop=(fc == nk - 1)),
                        reads=_toks([yT_, a["wout"][fc]]), writes=_toks([po]), sig=(fc == nk - 1 and n == 1))
                yield
            self.post(po, g, 1.0, i, a)
            yield

        for i in range(0, self.NT, 2):
            gens = [tile_gen(i)]
            if i + 1 < self.NT:
                gens.append(tile_gen(i + 1))
            self.run_gens(gens)

    def to_fm_part(self, src, c0, dst, d0, a, nk=8):
        p = self.p
        pT = a["pT"][0]
        for k in range(nk):
            p.op("pe", lambda e, k=k: e.transpose(out=pT[:, k, :], in_=src[:, (c0 + k) * 128:(c0 + k + 1) * 128],
                                                   identity=self.ident[:, :]),
                 reads=_toks([src, self.ident]), writes=_toks([pT]), sig=(k == nk - 1))
        p.op("act", lambda e: e.copy(out=dst[:, d0:d0 + nk, :], in_=pT[:, 0:nk, :]),
             reads=_toks([pT]), writes=_toks([dst]))

    def dn(self, layer):
        with self.c.phase():
            self._dn(layer)

    def tri_inv(self, N, NT, ib, pw, out):
        p = self.p
        idb = self.ident[:, :].unsqueeze(1).to_broadcast([128, 4, 128])

        def msk(dst, src, m, eng="dve"):
            p.op(eng, lambda e: e.tensor_tensor(out=dst[:, :, :], in0=src[:, :, :],
                                                in1=m[:, :].unsqueeze(1).to_broadcast([128, 4, 128]), op=ALU.mult),
                 reads=_toks([src, m]), writes=_toks([dst]))

        def mm4(ps, L, R):
            for e_ in range(4):
                p.op("pe", lambda e, e_=e_: e.matmul(ps[:, e_ * 128:(e_ + 1) * 128], lhsT=L[:, e_, :], rhs=R[:, e_, :],
                                                     start=True, stop=True),
                     reads=_toks([L, R]), writes=_toks([ps]), sig=(e_ == 3))

        def ev_copy(dst, ps, eng):
            if eng == "act":
                p.op("act", lambda e: e.copy(out=dst[:, :, :].rearrange("p a b -> p (a b)"), in_=ps[:, :]),
                     reads=_toks([ps]), writes=_toks([dst]))
            else:
                p.op("dve", lambda e: e.tensor_copy(out=dst[:, :, :].rearrange("p a b -> p (a b)"), in_=ps[:, :]),
                     reads=_toks([ps]), writes=_toks([dst]))

        def ev_comb(dst, base, ps, op):
            p.op("dve", lambda e: e.tensor_tensor(out=dst[:, :, :].rearrange("p a b -> p (a b)"),
                                                  in0=base[:, :, :].rearrange("p a b -> p (a b)"), in1=ps[:, :], op=op),
                 reads=_toks([base, ps]), writes=_toks([dst]))

        A, AT = ib["A"], ib["AT"]
        msk(A[0], N, self.mlev[0])
        msk(AT[0], NT, self.mlev[0], "pool")
        yield
        for lv in range(3):
            ps = pw()
            mm4(ps, AT[lv], A[lv])
            ps2 = pw()
            mm4(ps2, A[lv], AT[lv])
            yield
            ev_copy(A[lv + 1], ps, "act")
            ev_copy(AT[lv + 1], ps2, "act")
            yield
        X, XT = ib["X"], ib["XT"]
        cur = 0
        p.op("dve", lambda e: e.tensor_tensor(out=X[0][:, :, :], in0=idb, in1=A[0][:, :, :], op=ALU.subtract),
             reads=_toks([self.ident, A[0]]), writes=_toks([X[0]]))
        p.op("pool", lambda e: e.tensor_tensor(out=XT[0][:, :, :], in0=idb, in1=AT[0][:, :, :], op=ALU.subtract),
             reads=_toks([self.ident, AT[0]]), writes=_toks([XT[0]]))
        yield
        for lv in range(1, 4):
            nx = 1 - cur
            ps = pw()
            mm4(ps, XT[cur], A[lv])
            ps2 = pw()
            mm4(ps2, A[lv], XT[cur])
            yield
            ev_comb(X[nx], X[cur], ps, ALU.add)
            ev_comb(XT[nx], XT[cur], ps2, ALU.add)
            yield
            cur = nx
        O, OT, Y, W_ = ib["O"], ib["OT"], ib["Y"], ib["W"]
        for li in range(1, 4):
            last = (li == 3)
            msk(O, N, self.mlev[li])
            nx = 1 - cur
            if not last:
                msk(OT, NT, self.mlev[li], "pool")
                yield
                ps = pw()
                mm4(ps, OT, X[cur])
                ps2 = pw()
                mm4(ps2, O, XT[cur])
                yield
                ev_copy(Y, ps, "act")
                ev_copy(W_, ps2, "act")
                yield
                ps = pw()
                mm4(ps, XT[cur], Y)
                ps2 = pw()
                mm4(ps2, X[cur], W_)
                yield
                ev_comb(X[nx], X[cur], ps, ALU.subtract)
                ev_comb(XT[nx], XT[cur], ps2, ALU.subtract)
                yield
            else:
                yield
                ps2 = pw()
                mm4(ps2, O, XT[cur])
                yield
                ev_copy(W_, ps2, "act")
                yield
                ps2 = pw()
                mm4(ps2, X[cur], W_)
                yield
                ev_comb(XT[nx], XT[cur], ps2, ALU.subtract)
                yield
            cur = nx
        out[0] = XT[cur]

    @staticmethod
    def run_gens(gens):
        act = list(gens)
        while act:
            for g_ in list(act):
                try:
                    next(g_)
                except StopIteration:
                    act.remove(g_)

    def _dn(self, layer):
        c, p, nc = self.c, self.p, self.nc
        jx = layer // 3
        NT = self.NT
        TB = 2 if NT % 2 == 0 else 1
        NB = NT // TB
        W = TB * 128
        CWN = 4096 + 32
        a = {}
        a["win"] = [c.sb([128, CWN], BF16, f"win{k}") for k in range(8)]
        a["g"] = c.sb([128, 1, D], F32, "g")
        a["h"] = [c.sb([128, D], F32, "h")] * 2
        a["xn"] = [c.sb([128, D], BF16, "xn")] * 2
        a["xT"] = [c.sb([128, 8, W], BF16, "xT")]
        ob = c.sb([128, 2 * D], BF16, "ob")
        a["junk"] = T(ob.h[:, 0:D], "junk", tok=ob.tok)
        a["ss"] = [c.sb([128, 1], F32, f"ss{i}") for i in range(4)]
        a["rs"] = [c.sb([128, 1], F32, f"rs{i}") for i in range(4)]
        a["pT"] = [c.ps([128, 8, 128], BF16, "pT")]
        pgen = [c.ps([128, 512], F32, f"pgen{i}") for i in range(2)]
        pgate = c.ps([128, 64], F32, "pgate")
        pws = [c.ps([128, 512], F32, f"pw{i}") for i in range(4)]
        pwi = [0]

        def pw():
            pwi[0] += 1
            return (pws + pgen)[pwi[0] % 6]
        o = c.sb([128, 2 * D], F32, "o")
        c.stg = [T(o.h[:, 0:1024], "stgA"), T(o.h[:, 1024:2048], "stgB")]
        self._stg_i = 0
        wv_ = self.wload_pieces([T(t_.h[:, 0:4096], "w") for t_ in a["win"]], self.dn_w_in[jx, :, 0:4096])
        for k in range(8):
            self.wload(a["win"][k], a["win"][k][:, 4096:CWN], self.dn_w_in[jx, k * 128:(k + 1) * 128, 6144:6176])
        self.load_g(a["g"], layer, 1)
        g = a["g"]
        cw = self.load_cols([self.dn_conv_w[jx, t_, :] for t_ in range(4)], 32, pgen[0], "cw")
        dtb = c.sb([128, 16], F32, "dtb")
        aneg = c.sb([128, 16], F32, "aneg")
        ngb = c.sb([128, 128], F32, "ngb")
        self.bcast_row(dtb, dtb[:, :], self.dn_dt_bias[jx, :])
        self.bcast_row(aneg, aneg[:, :], self.dn_a_log[jx, :])
        self.bcast_row(ngb, ngb[:, :], self.dn_norm_g[jx, :])
        p.op("act", lambda e: e.activation(out=aneg[:, :], in_=aneg[:, :], func=AF.Exp), reads=_toks([aneg]), writes=_toks([aneg]))
        p.op("dve", lambda e: e.tensor_scalar(out=aneg[:, :], in0=aneg[:, :], scalar1=-1.0, scalar2=None, op0=ALU.mult),
             reads=_toks([aneg]), writes=_toks([aneg]))
        halo = c.sb([128, 32, 3], F32, "halo")
        p.op("pool", lambda e: e.memset(halo[:, :, :], 0.0), writes=_toks([halo]))
        Sf = [c.sb([128, 4, 128], F32, f"S{h}") for h in range(4)]
        Sb = [c.sb([128, 4, 128], BF16, f"Sb{h}") for h in range(4)]
        for h in range(4):
            p.op("pool", lambda e, h=h: e.memset(Sf[h][:, :, :], 0.0), writes=_toks([Sf[h]]))
            p.op("pool", lambda e, h=h: e.memset(Sb[h][:, :, :], 0.0), writes=_toks([Sb[h]]))
        cb = [c.sb([128, W + 3], F32, f"cb{i}") for i in range(2)]
        sgb = [dict(acc=c.sb([128, W], F32, f"acc{i}"), sg=c.sb([128, W], F32, f"sgc{i}")) for i in range(2)]
        craw = [c.sb([128, W], F32, f"craw{i}") for i in range(2)]
        sq = [c.sb([128, W], F32, f"sq{i}") for i in range(2)]
        rn = [c.sb([128, W], F32, f"rn{i}") for i in range(2)]
        qn = c.sb([128, 8, W], BF16, "qn")
        kn = c.sb([128, 8, W], BF16, "kn")
        vfm = c.sb([128, 8, W], BF16, "vfm")
        vtm = [c.sb([128, 2 * D], BF16, f"vtm{j}") for j in range(TB)]
        ktm = [c.sb([128, D], BF16, f"ktm{j}") for j in range(TB)]
        pbs = c.sb([128, 32], F32, "pbs")
        beta = c.sb([128, 16], F32, "beta")
        gg = c.sb([128, 16], F32, "gg")
        Gc = c.sb([128, 16], F32, "Gc")
        eG = c.sb([128, 16], F32, "eG")
        bg = c.sb([128, 16], F32, "bg")
        elast = c.sb([128, 16], F32, "elast")
        kes = c.sb([128, 16], F32, "kes")
        mk = lambda nm: c.sb([128, 4, 128], BF16, nm)
        mkf = lambda nm: c.sb([128, 4, 128], F32, nm)

        def mkres(r):
            return dict(LH=mkf(f"LH{r}"), Ed=mk(f"Ed{r}"), EdT=mk(f"EdT{r}"),
                        KKm=c.sb([128, 2, 128], BF16, f"KKm{r}"), QKm=c.sb([128, 2, 128], BF16, f"QKm{r}"),
                        N=mk(f"N{r}"), NT=mk(f"NT{r}"), QKT=mk(f"QKT{r}"),
                        ib=dict(A=[mk(f"A{i}_{r}") for i in range(4)], AT=[mk(f"AT{i}_{r}") for i in range(4)],
                                X=[mk(f"X0_{r}"), mk(f"X1_{r}")], XT=[mk(f"XT0_{r}"), mk(f"XT1_{r}")],
                                O=mk(f"O{r}"), OT=mk(f"OT{r}"), Y=mk(f"Y{r}"), W=mk(f"Wt{r}")),
                        kb=mk(f"kb{r}"), ke=mk(f"ke{r}"), vb=mk(f"vb{r}"), wk=mk(f"wk{r}"), vn=mk(f"vn{r}"),
                        ot=mk(f"ot{r}"))
        RES = [mkres(0), mkres(1)]
        ss16 = c.sb([128, 16], F32, "ss16")
        osq = ob
        gi = 0
        hc = 0
        for b in range(NB):
            xT = a["xT"][0]
            for j in range(TB):
                self.pre(b * TB + j, a, g, a["xn"][0])
                self.to_fm(a["xn"][0], xT, j, a)
            def dn_chunk(cc):
                pg = pgen[cc % 2]
                craw_, sq_, rn_ = craw[cc % 2], sq[cc % 2], rn[cc % 2]
                for k in range(8):
                    p.op("pe", lambda e, k=k: e.matmul(
                        pg[:, 0:W], lhsT=a["win"][k][:, cc * 128:(cc + 1) * 128], rhs=xT[:, k, :],
                        start=(k == 0), stop=(k == 7)),
                        reads=_toks([wv_(k, cc * 128), xT]), writes=_toks([pg]), sig=(k == 7))
                yield
                if cc < 16:
                    yield from self.conv_chunk(pg, cb[cc % 2], halo, cw, None, cc, W, craw_[:, :], craw_, sgb[cc % 2], False, b == 0)
                    p.op("pool", lambda e: e.tensor_tensor(out=sq_[:, :], in0=craw_[:, :], in1=craw_[:, :], op=ALU.mult),
                         reads=_toks([craw_]), writes=_toks([sq_]))
                    yield
                    pn = pws[cc % 2]
                    p.op("pe", lambda e: e.matmul(pn[:, 0:W], lhsT=self.onesf[:, :], rhs=sq_[:, :], start=True, stop=True),
                         reads=_toks([self.onesf, sq_]), writes=_toks([pn]))
                    yield
                    p.op("act", lambda e: e.activation(out=rn_[:, :], in_=pn[:, 0:W], func=AF.Ln, bias=self.epsc[:, 0:1]),
                         reads=_toks([pn, self.epsc]), writes=_toks([rn_]))
                    yield
                    p.op("act", lambda e: e.activation(out=rn_[:, :], in_=rn_[:, :], func=AF.Exp, scale=-0.5),
                         reads=_toks([rn_]), writes=_toks([rn_]))
                    yield
                    dstt = qn if cc < 8 else kn
                    scl = 128.0 ** -0.5 if cc < 8 else 1.0
                    p.op("dve", lambda e: e.scalar_tensor_tensor(
                        out=dstt[:, cc % 8, :], in0=craw_[:, :], scalar=scl, in1=rn_[:, :], op0=ALU.mult, op1=ALU.mult),
                        reads=_toks([craw_, rn_]), writes=_toks([dstt]))
                    yield
                else:
                    yield from self.conv_chunk(pg, cb[cc % 2], halo, cw, None, cc, W, vfm[:, cc % 8, :], vfm, sgb[cc % 2], False, b == 0)

            for cc in range(0, 32, 2):
                self.run_gens([dn_chunk(cc), dn_chunk(cc + 1)])
                if cc + 1 == 15:
                    self.fm_to_tm(kn, None, [(ktm[j][:, :], ktm[j]) for j in range(TB)], a)
                if cc + 1 in (23, 31):
                    grp = (cc + 1 - 16) // 8
                    self.fm_to_tm(vfm, None, [(vtm[j][:, grp * D:(grp + 1) * D], vtm[j]) for j in range(TB)], a)
            for j in range(TB):
                i = b * TB + j
                tsl = slice(j * 128, (j + 1) * 128)
                for k in range(8):
                    p.op("pe", lambda e, k=k: e.matmul(pgate[:, 0:32], lhsT=xT[:, k, tsl], rhs=a["win"][k][:, 4096:4128],
                                                       start=(k == 0), stop=(k == 7)),
                         reads=_toks([a["win"][k], xT]), writes=_toks([pgate]), sig=(k == 7))
                p.op("act", lambda e: e.copy(out=beta[:, :], in_=pgate[:, 0:16]), reads=_toks([pgate]), writes=_toks([beta]))
                self.sigmoid_inplace(beta[:, :], beta)
                p.op("dve", lambda e: e.tensor_tensor(out=gg[:, :], in0=pgate[:, 16:32], in1=dtb[:, :], op=ALU.add),
                     reads=_toks([pgate, dtb]), writes=_toks([gg]))
                p.op("act", lambda e: e.activation(out=gg[:, :], in_=gg[:, :], func=AF.Exp), reads=_toks([gg]), writes=_toks([gg]))
                p.op("act", lambda e: e.activation(out=gg[:, :], in_=gg[:, :], func=AF.Ln, bias=self.onec[:, 0:1]),
                     reads=_toks([gg, self.onec]), writes=_toks([gg]))
                p.op("dve", lambda e: e.tensor_tensor(out=gg[:, :], in0=gg[:, :], in1=aneg[:, :], op=ALU.mult),
                     reads=_toks([gg, aneg]), writes=_toks([gg]))
                p.op("pe", lambda e: e.matmul(pgate[:, 32:48], lhsT=self.triLE[:, :], rhs=gg[:, :], start=True, stop=True),
                     reads=_toks([self.triLE, gg]), writes=_toks([pgate]))
                p.op("pe", lambda e: e.matmul(pgate[:, 48:64], lhsT=self.onesf[:, :], rhs=gg[:, :], start=True, stop=True),
                     reads=_toks([self.onesf, gg]), writes=_toks([pgate]))
                p.op("act", lambda e: e.copy(out=Gc[:, :], in_=pgate[:, 32:48]), reads=_toks([pgate]), writes=_toks([Gc]))
                p.op("act", lambda e: e.activation(out=eG[:, :], in_=Gc[:, :], func=AF.Exp), reads=_toks([Gc]), writes=_toks([eG]))
                p.op("act", lambda e: e.activation(out=elast[:, :], in_=pgate[:, 48:64], func=AF.Exp),
                     reads=_toks([pgate]), writes=_toks([elast]))
                p.op("dve", lambda e: e.tensor_tensor(out=kes[:, :], in0=pgate[:, 48:64], in1=Gc[:, :], op=ALU.subtract),
                     reads=_toks([pgate, Gc]), writes=_toks([kes]))
                p.op("act", lambda e: e.activation(out=kes[:, :], in_=kes[:, :], func=AF.Exp), reads=_toks([kes]), writes=_toks([kes]))
                p.op("dve", lambda e: e.tensor_tensor(out=bg[:, :], in0=beta[:, :], in1=eG[:, :], op=ALU.mult),
                     reads=_toks([beta, eG]), writes=_toks([bg]))
                def batch_gen(bt, R, slot):
                    hs = slice(4 * bt, 4 * bt + 4)
                    banks = [pws[2 * slot], pws[2 * slot + 1], pgen[slot]]
                    bi = [0]

                    def pw():
                        bi[0] += 1
                        return banks[bi[0] % 3]
                    LH, Ed, EdT, KKm, QKm, Nn, NnT, QKT = (R[k_] for k_ in ("LH", "Ed", "EdT", "KKm", "QKm", "N", "NT", "QKT"))
                    p.op("pool", lambda e: e.tensor_tensor(
                        out=LH[:, :, :], in0=self.maskGT[:, :].unsqueeze(1).to_broadcast([128, 4, 128]),
                        in1=gg[:, hs].unsqueeze(2).to_broadcast([128, 4, 128]), op=ALU.mult),
                        reads=_toks([self.maskGT, gg]), writes=_toks([LH]))
                    yield
                    pD, pDT, pK = pw(), pw(), pw()
                    for e_ in range(4):
                        p.op("pe", lambda e, e_=e_: e.matmul(pD[:, e_ * 128:(e_ + 1) * 128], lhsT=self.triLE[:, :],
                                                             rhs=LH[:, e_, :], start=True, stop=True),
                             reads=_toks([LH, self.triLE]), writes=_toks([pD]), sig=(e_ == 3))
                    for e_ in range(4):
                        p.op("pe", lambda e, e_=e_: e.matmul(pDT[:, e_ * 128:(e_ + 1) * 128], lhsT=LH[:, e_, :],
                                                             rhs=self.triLE[:, :], start=True, stop=True),
                             reads=_toks([LH, self.triLE]), writes=_toks([pDT]), sig=(e_ == 3))
                    for q_ in range(2):
                        hq = 2 * bt + q_
                        p.op("pe", lambda e, q_=q_, hq=hq: e.matmul(
                            pK[:, q_ * 128:(q_ + 1) * 128], lhsT=kn[:, hq, tsl], rhs=kn[:, hq, tsl], start=True, stop=True),
                            reads=_toks([kn]), writes=_toks([pK]), sig=False)
                        p.op("pe", lambda e, q_=q_, hq=hq: e.matmul(
                            pK[:, 256 + q_ * 128:256 + (q_ + 1) * 128], lhsT=kn[:, hq, tsl], rhs=qn[:, hq, tsl],
                            start=True, stop=True),
                            reads=_toks([kn, qn]), writes=_toks([pK]), sig=(q_ == 1))
                    yield
                    p.op("act", lambda e: e.activation(out=Ed[:, :, :].rearrange("p a b -> p (a b)"), in_=pD[:, :],
                                                       func=AF.Exp), reads=_toks([pD]), writes=_toks([Ed]))
                    p.op("act", lambda e: e.activation(out=EdT[:, :, :].rearrange("p a b -> p (a b)"), in_=pDT[:, :],
                                                       func=AF.Exp), reads=_toks([pDT]), writes=_toks([EdT]))
                    p.op("dve", lambda e: e.tensor_tensor(
                        out=KKm[:, :, :], in0=pK[:, 0:256].rearrange("p (a b) -> p a b", b=128),
                        in1=self.maskGT[:, :].unsqueeze(1).to_broadcast([128, 2, 128]), op=ALU.mult),
                        reads=_toks([pK, self.maskGT]), writes=_toks([KKm]))
                    p.op("dve", lambda e: e.tensor_tensor(
                        out=QKm[:, :, :], in0=pK[:, 256:512].rearrange("p (a b) -> p a b", b=128),
                        in1=self.triLE[:, :].unsqueeze(1).to_broadcast([128, 2, 128]), op=ALU.mult),
                        reads=_toks([pK, self.triLE]), writes=_toks([QKm]))
                    yield
                    for e_ in range(4):
                        h = 4 * bt + e_
                        p.op("dve", lambda e, e_=e_, h=h: e.scalar_tensor_tensor(
                            out=Nn[:, e_, :], in0=Ed[:, e_, :], scalar=beta[:, h:h + 1], in1=KKm[:, e_ // 2, :],
                            op0=ALU.mult, op1=ALU.mult), reads=_toks([Ed, beta, KKm]), writes=_toks([Nn]))
                    p.op("pool", lambda e: e.tensor_tensor(
                        out=QKT[:, :, :].rearrange("p (q r) b -> p q r b", r=2),
                        in0=EdT[:, :, :].rearrange("p (q r) b -> p q r b", r=2),
                        in1=QKm[:, :, :].unsqueeze(2).to_broadcast([128, 2, 2, 128]), op=ALU.mult),
                        reads=_toks([EdT, QKm]), writes=_toks([QKT]))
                    yield
                    pT = a["pT"][0]
                    for e_ in range(4):
                        p.op("pe", lambda e, e_=e_: e.transpose(out=pT[:, e_, :], in_=Nn[:, e_, :], identity=self.ident[:, :]),
                             reads=_toks([Nn, self.ident]), writes=_toks([pT]), sig=(e_ == 3))
                    p.op("act", lambda e: e.copy(out=NnT[:, :, :], in_=pT[:, 0:4, :]), reads=_toks([pT]), writes=_toks([NnT]))
                    yield
                    uo = [None]
                    yield from self.tri_inv(Nn, NnT, R["ib"], pw, uo)
                    U = uo[0]
                    kb, ke, vb4, wk, vn, ot = R["kb"], R["ke"], R["vb"], R["wk"], R["vn"], R["ot"]
                    for e_ in range(4):
                        h = 4 * bt + e_
                        ksl = ktm[j][:, (h // 2) * 128:(h // 2 + 1) * 128]
                        p.op("act", lambda e, e_=e_, ksl=ksl, h=h: e.activation(out=kb[:, e_, :], in_=ksl, func=AF.Copy,
                                                                               scale=bg[:, h:h + 1]),
                             reads=_toks([ktm[j], bg]), writes=_toks([kb]))
                        p.op("act", lambda e, e_=e_, ksl=ksl, h=h: e.activation(out=ke[:, e_, :], in_=ksl, func=AF.Copy,
                                                                               scale=kes[:, h:h + 1]),
                             reads=_toks([ktm[j], kes]), writes=_toks([ke]))
                    p.op("pool", lambda e: e.tensor_tensor(
                        out=vb4[:, :, :], in0=vtm[j][:, 4 * bt * 128:(4 * bt + 4) * 128].rearrange("p (h d) -> p h d", d=128),
                        in1=beta[:, hs].unsqueeze(2).to_broadcast([128, 4, 128]), op=ALU.mult),
                        reads=_toks([vtm[j], beta]), writes=_toks([vb4]))
                    yield
                    ps1 = pw()
                    for e_ in range(4):
                        p.op("pe", lambda e, e_=e_: e.matmul(ps1[:, e_ * 128:(e_ + 1) * 128], lhsT=kb[:, e_, :], rhs=U[:, e_, :],
                                                             start=True, stop=True),
                             reads=_toks([kb, U]), writes=_toks([ps1]), sig=(e_ == 3))
                    yield
                    p.op("act", lambda e: e.activation(out=wk[:, :, :].rearrange("p a b -> p (a b)"), in_=ps1[:, :], func=AF.Copy,
                                                       scale=-1.0), reads=_toks([ps1]), writes=_toks([wk]))
                    yield
                    ps2 = pw()
                    for e_ in range(4):
                        p.op("pe", lambda e, e_=e_: e.matmul(ps2[:, e_ * 128:(e_ + 1) * 128], lhsT=U[:, e_, :], rhs=vb4[:, e_, :],
                                                             start=True, stop=False),
                             reads=_toks([vb4, U]), writes=_toks([ps2]), sig=False)
                        p.op("pe", lambda e, e_=e_: e.matmul(ps2[:, e_ * 128:(e_ + 1) * 128], lhsT=wk[:, e_, :], rhs=Sb[bt][:, e_, :],
                                                             start=False, stop=True),
                             reads=_toks([wk, Sb[bt]]), writes=_toks([ps2]), sig=(e_ == 3))
                    ps3 = pw()
                    for e_ in range(4):
                        hq = (4 * bt + e_) // 2
                        p.op("pe", lambda e, e_=e_, hq=hq: e.matmul(ps3[:, e_ * 128:(e_ + 1) * 128], lhsT=qn[:, hq, tsl],
                                                                    rhs=Sb[bt][:, e_, :], start=True, stop=True),
                             reads=_toks([qn, Sb[bt]]), writes=_toks([ps3]), sig=(e_ == 3))
                    yield
                    p.op("dve", lambda e: e.tensor_copy(out=vn[:, :, :].rearrange("p a b -> p (a b)"), in_=ps2[:, :]),
                         reads=_toks([ps2]), writes=_toks([vn]))
                    p.op("dve", lambda e: e.tensor_tensor(
                        out=ot[:, :, :], in0=ps3[:, :].rearrange("p (a b) -> p a b", b=128),
                        in1=eG[:, hs].unsqueeze(2).to_broadcast([128, 4, 128]), op=ALU.mult),
                        reads=_toks([ps3, eG]), writes=_toks([ot]))
                    yield
                    ps4, ps5 = pw(), pw()
                    for e_ in range(4):
                        p.op("pe", lambda e, e_=e_: e.matmul(ps4[:, e_ * 128:(e_ + 1) * 128], lhsT=QKT[:, e_, :], rhs=vn[:, e_, :],
                                                             start=True, stop=True),
                             reads=_toks([QKT, vn]), writes=_toks([ps4]), sig=(e_ == 3))
                    for e_ in range(4):
                        p.op("pe", lambda e, e_=e_: e.matmul(ps5[:, e_ * 128:(e_ + 1) * 128], lhsT=ke[:, e_, :], rhs=vn[:, e_, :],
                                                             start=True, stop=True),
                             reads=_toks([ke, vn]), writes=_toks([ps5]), sig=(e_ == 3))
                    yield
                    p.op("dve", lambda e: e.tensor_tensor(
                        out=o[:, 4 * bt * 128:(4 * bt + 4) * 128], in0=ot[:, :, :].rearrange("p a b -> p (a b)"), in1=ps4[:, :],
                        op=ALU.add), reads=_toks([ot, ps4]), writes=_toks([o]))
                    p.op("pool", lambda e: e.tensor_tensor(
                        out=Sf[bt][:, :, :], in0=Sf[bt][:, :, :], in1=elast[:, hs].unsqueeze(2).to_broadcast([128, 4, 128]),
                        op=ALU.mult), reads=_toks([Sf[bt], elast]), writes=_toks([Sf[bt]]))
                    yield
                    p.op("dve", lambda e: e.tensor_tensor(
                        out=Sf[bt][:, :, :].rearrange("p a b -> p (a b)"), in0=Sf[bt][:, :, :].rearrange("p a b -> p (a b)"),
                        in1=ps5[:, :], op=ALU.add), reads=_toks([Sf[bt], ps5]), writes=_toks([Sf[bt]]))
                    yield
                    p.op("act", lambda e: e.copy(out=Sb[bt][:, :, :], in_=Sf[bt][:, :, :]),
                         reads=_toks([Sf[bt]]), writes=_toks([Sb[bt]]))
                    yield

                import os as _os
                if _os.environ.get("NOINTER"):
                    for bt_ in range(4):
                        self.run_gens([batch_gen(bt_, RES[bt_ % 2], bt_ % 2)])
                else:
                    self.run_gens([batch_gen(0, RES[0], 0), batch_gen(1, RES[1], 1)])
                    self.run_gens([batch_gen(2, RES[0], 0), batch_gen(3, RES[1], 1)])
                if i == 1:
                    self.dbg("o", o, o[:, :], [128, 2 * D])
                p.op("pool", lambda e: e.tensor_tensor(out=osq[:, :], in0=o[:, :], in1=o[:, :], op=ALU.mult),
                     reads=_toks([o]), writes=_toks([osq]))
                p.op("dve", lambda e: e.tensor_reduce(out=ss16[:, :], in_=osq[:, :].rearrange("p (h d) -> p h d", d=128),
                                                      axis=AX.X, op=ALU.add), reads=_toks([osq]), writes=_toks([ss16]))
                p.op("dve", lambda e: e.tensor_scalar(out=ss16[:, :], in0=ss16[:, :], scalar1=1.0 / 128.0, scalar2=None,
                                                      op0=ALU.mult), reads=_toks([ss16]), writes=_toks([ss16]))
                self.rsqrt_small(ss16[:, :], ss16[:, :], [ss16], [ss16], EPS)
                p.op("dve", lambda e: e.tensor_tensor(
                    out=o[:, :].rearrange("p (h d) -> p h d", d=128), in0=o[:, :].rearrange("p (h d) -> p h d", d=128),
                    in1=ss16[:, :].unsqueeze(2).to_broadcast([128, 16, 128]), op=ALU.mult),
                    reads=_toks([o, ss16]), writes=_toks([o]))
                p.op("pool", lambda e: e.tensor_tensor(
                    out=ob[:, :].rearrange("p (h d) -> p h d", d=128), in0=o[:, :].rearrange("p (h d) -> p h d", d=128),
                    in1=ngb[:, :].unsqueeze(1).to_broadcast([128, 16, 128]), op=ALU.mult),
                    reads=_toks([o, ngb]), writes=_toks([ob]))
                p.op("sp", lambda e, i=i: e.dma_start(out=self.scr[i * 128:(i + 1) * 128, :], in_=ob[:, :]),
                     reads=_toks([ob]), writes=[self.ytok[i]], dma=ob.tok)

    def rw(self, layer):
        with self.c.phase():
            self._rw(layer)

    def _rw(self, layer):
        c, p, nc = self.c, self.p, self.nc
        jx = layer // 3
        NT = self.NT
        TB = 2 if NT % 2 == 0 else 1
        NB = NT // TB
        W = TB * 128
        a = {}
        wr = [c.sb([128, D], BF16, f"wr{k}") for k in range(8)]
        wk = [c.sb([128, D], BF16, f"wk{k}") for k in range(8)]
        wv = [c.sb([128, D], BF16, f"wv{k}") for k in range(8)]
        wl1t = c.sb([128, 8, 288], BF16, "wl1")
        wl1 = [wl1t.view((slice(None), k, slice(None))) for k in range(8)]
        w2t = c.sb([64, D], BF16, "w2t")
        a2t = c.sb([64, D], BF16, "a2t")
        g2a = c.sb([128, D], BF16, "g2a")
        g2b = c.sb([32, D], BF16, "g2b")
        a["g"] = c.sb([128, 1, D], F32, "g")
        a["h"] = [c.sb([128, D], F32, "h")] * 2
        a["xn"] = [c.sb([128, D], BF16, "xn")] * 2
        a["junk"] = c.sb([128, D], BF16, "junk")
        a["ss"] = [c.sb([128, 1], F32, f"ss{i}") for i in range(4)]
        a["rs"] = [c.sb([128, 1], F32, f"rs{i}") for i in range(4)]
        a["pT"] = [c.ps([128, 8, 128], BF16, "pT")]
        pgen = [c.ps([128, 512], F32, f"pgen{i}") for i in range(2)]
        prk = c.ps([128, 16], F32, "prk")
        pws = [c.ps([128, 512], F32, f"pw{i}") for i in range(4)]
        pwi = [0]

        def pw():
            pwi[0] += 1
            return pws[pwi[0] % 4]
        for k in range(8):
            ks = slice(k * 128, (k + 1) * 128)
            self.wload(wr[k], wr[k][:, :], self.rw_w_rkv[jx, 0, ks, :])
            self.wload(wk[k], wk[k][:, :], self.rw_w_rkv[jx, 1, ks, :])
            self.wload(wv[k], wv[k][:, :], self.rw_w_rkv[jx, 2, ks, :])
        for k in range(8):
            ks = slice(k * 128, (k + 1) * 128)
            self.wload(wl1[k], wl1[k][:, 0:64], self.rw_w1[jx, ks, :])
            self.wload(wl1[k], wl1[k][:, 64:128], self.rw_a1[jx, ks, :])
            self.wload(wl1[k], wl1[k][:, 128:288], self.rw_g1[jx, ks, :])
        self.wload(w2t, w2t[:, :], self.rw_w2[jx, :, :])
        self.wload(a2t, a2t[:, :], self.rw_a2[jx, :, :])
        self.wload(g2a, g2a[:, :], self.rw_g2[jx, 0:128, :])
        self.wload(g2b, g2b[:, :], self.rw_g2[jx, 128:160, :])
        self.load_g(a["g"], layer, 1)
        g = a["g"]
        cols = self.load_cols([self.rw_mu[jx, i_, :] for i_ in range(6)] +
                              [self.rw_w0[jx, :], self.rw_a0[jx, :], self.rw_k_k[jx, :], self.rw_k_a[jx, :],
                               self.rw_r_k[jx].rearrange("h d -> (h d)")], 8, pgen[0], "cols")
        ncol = c.sb([128, 8, 2], F32, "ncol")
        p.op("dve", lambda e: e.tensor_scalar(out=ncol[:, :, :], in0=cols[:, :, 6:8], scalar1=-1.0, scalar2=None, op0=ALU.mult),
             reads=_toks([cols]), writes=_toks([ncol]))
        lng = c.sb([128, D], F32, "lng")
        lnb = c.sb([128, D], F32, "lnb")
        self.bcast_row(lng, lng[:, :], self.rw_ln_g[jx, :])
        self.bcast_row(lnb, lnb[:, :], self.rw_ln_b[jx, :])
        ind = c.sb([128, 8, 16], BF16, "ind")
        p.op("pool", lambda e: e.memset(ind[:, :, :], 0.0), writes=_toks([ind]))
        for cc in range(8):
            for hp in range(2):
                p.op("pool", lambda e, cc=cc, hp=hp: e.memset(ind[hp * 64:(hp + 1) * 64, cc, 2 * cc + hp:2 * cc + hp + 1], 1.0),
                     reads=_toks([ind]), writes=_toks([ind]))
        Zc = [c.sb([128, 64], F32, f"Zc{i}") for i in range(8)]
        Zb = [c.sb([128, 64], BF16, f"Zb{i}") for i in range(8)]
        for i_ in range(8):
            p.op("pool", lambda e, i_=i_: e.memset(Zc[i_][:, :], 0.0), writes=_toks([Zc[i_]]))
            p.op("pool", lambda e, i_=i_: e.memset(Zb[i_][:, :], 0.0), writes=_toks([Zb[i_]]))
        xTh = c.sb([128, 8, W + 1], BF16, "xTh")
        p.op("pool", lambda e: e.memset(xTh[:, :, 0:1], 0.0), writes=_toks([xTh]))
        xx = c.sb([128, 8, W], BF16, "xx")
        xm = [c.sb([128, 8, W], BF16, f"xm{i}") for i in range(6)]
        h1 = c.sb([64, W], BF16, "h1")
        h2 = c.sb([64, W], BF16, "h2")
        h3a = c.sb([128, W], BF16, "h3a")
        h3b = c.sb([32, W], BF16, "h3b")
        h3f = c.sb([128, W], F32, "h3f")
        h1f = T(h3f.h[0:64, :], "h1f", tok=h3f.tok)
        fm = lambda nm: c.sb([128, W], F32, nm)
        r_c, k_c, lw, a_c, kkr, sqk, rn, kk, kmod, cl, eW, eWi = [fm(n) for n in
            ("r_c", "k_c", "lw", "a_c", "kkr", "sqk", "rn", "kk", "kmod", "cl", "eW", "eWi")]
        t1, bt_, eWm = sqk, kkr, rn
        rt_ = c.sb([128, 8, W], BF16, "rt")
        kt_ = c.sb([128, 8, W], BF16, "kt")
        at_ = c.sb([128, 8, W], BF16, "at")
        btl = c.sb([128, 8, W], BF16, "btl")
        rkr = c.sb([128, 8, W], BF16, "rkr")
        eWl = c.sb([128, 8, TB], F32, "eWl")
        ktm = [c.sb([128, D], BF16, f"ktm{j}") for j in range(TB)]
        btm = [c.sb([128, D], BF16, f"btm{j}") for j in range(TB)]
        vtm = [c.sb([128, D], BF16, f"vtm{j}") for j in range(TB)]
        gtm = [c.sb([128, D], BF16, f"gtm{j}") for j in range(TB)]
        rks = c.sb([128, 16], F32, "rks")
        mk = lambda nm: c.sb([128, 4, 128], BF16, nm)
        Nn, NnT, LakT, MrbT, MrkT = mk("N"), mk("NT"), mk("LakT"), mk("MrbT"), mk("MrkT")
        ib = dict(A=[mk(f"A{i}") for i in range(4)], AT=[mk(f"AT{i}") for i in range(4)],
                  X=[mk("X0"), mk("X1")], XT=[mk("XT0"), mk("XT1")], O=mk("O"), OT=mk("OT"), Y=mk("Y"), W=mk("Wt"))
        inner = [c.sb([128, 64], BF16, f"inner{i}") for i in range(2)]
        Pm = [c.sb([128, 128], BF16, f"Pm{i}") for i in range(2)]
        ztmp = c.sb([128, 64], F32, "ztmp")
        y = c.sb([128, D], F32, "y")
        yc = a["h"][0]
        s16 = c.sb([128, 16], F32, "s16")
        v16 = c.sb([128, 16], F32, "v16")
        ob = a["xn"][0]
        gi = 0
        ic = 0
        import os as _os
        _stop = _os.environ.get("RW_STOP")
        if _stop == "0":
            return
        for b in range(NB):
            for j in range(TB):
                self.pre(b * TB + j, a, g, a["xn"][0])
                self.to_fm(a["xn"][0], xTh, j, a, off=1)
            xc = xTh[:, :, 1:W + 1]
            p.op("dve", lambda e: e.tensor_tensor(out=xx[:, :, :], in0=xTh[:, :, 0:W], in1=xc, op=ALU.subtract),
                 reads=_toks([xTh]), writes=_toks([xx]))
            for i_ in range(6):
                eng = "dve"
                p.op(eng, lambda e, i_=i_: e.tensor_tensor(
                    out=xm[i_][:, :, :], in0=xx[:, :, :], in1=cols[:, :, i_:i_ + 1].to_broadcast([128, 8, W]), op=ALU.mult),
                    reads=_toks([xx, cols]), writes=_toks([xm[i_]]))
                p.op(eng, lambda e, i_=i_: e.tensor_tensor(out=xm[i_][:, :, :], in0=xm[i_][:, :, :], in1=xc, op=ALU.add),
                     reads=_toks([xm[i_], xTh]), writes=_toks([xm[i_]]))
            p.op("pool", lambda e: e.tensor_copy(out=xTh[:, :, 0:1], in_=xTh[:, :, W:W + 1]),
                 reads=_toks([xTh]), writes=_toks([xTh]))
            xr, xw_, xk, xv, xa, xg = xm
            ph = pw()
            for k in range(8):
                p.op("pe", lambda e, k=k, ph=ph: e.matmul(ph[0:64, 0:W], lhsT=wl1[k][:, 0:64], rhs=xw_[:, k, :],
                                                          start=(k == 0), stop=(k == 7)),
                     reads=_toks([wl1[k], xw_]), writes=_toks([ph]), sig=(k == 7))
            p.op("act", lambda e, ph=ph: e.activation(out=h1f[:, :], in_=ph[0:64, 0:W], func=AF.Exp, scale=-2.0),
                 reads=_toks([ph]), writes=_toks([h1f]))
            p.op("act", lambda e: e.activation(out=h1f[:, :], in_=h1f[:, :], func=AF.Ln, bias=self.onec[0:64, 0:1]),
                 reads=_toks([h1f, self.onec]), writes=_toks([h1f]))
            p.op("act", lambda e: e.activation(out=h1f[:, :], in_=h1f[:, :], func=AF.Exp, scale=-1.0),
                 reads=_toks([h1f]), writes=_toks([h1f]))
            p.op("dve", lambda e: e.tensor_scalar(out=h1[:, :], in0=h1f[:, :], scalar1=2.0, scalar2=-1.0, op0=ALU.mult, op1=ALU.add),
                 reads=_toks([h1f]), writes=_toks([h1]))
            ph = pw()
            for k in range(8):
                p.op("pe", lambda e, k=k, ph=ph: e.matmul(ph[0:64, 0:W], lhsT=wl1[k][:, 64:128], rhs=xa[:, k, :],
                                                          start=(k == 0), stop=(k == 7)),
                     reads=_toks([wl1[k], xa]), writes=_toks([ph]), sig=(k == 7))
            p.op("act", lambda e, ph=ph: e.copy(out=h2[:, :], in_=ph[0:64, 0:W]), reads=_toks([ph]), writes=_toks([h2]))
            for part, (c0_, c1_, rows, dst) in enumerate(((128, 256, 128, h3a), (256, 288, 32, h3b))):
                ph = pw()
                for k in range(8):
                    p.op("pe", lambda e, k=k, ph=ph, c0_=c0_, c1_=c1_, rows=rows: e.matmul(
                        ph[0:rows, 0:W], lhsT=wl1[k][:, c0_:c1_], rhs=xg[:, k, :], start=(k == 0), stop=(k == 7)),
                        reads=_toks([wl1[k], xg]), writes=_toks([ph]), sig=(k == 7))
                p.op("act", lambda e, ph=ph, rows=rows: e.activation(out=h3f[0:rows, :], in_=ph[0:rows, 0:W], func=AF.Exp, scale=-1.0),
                     reads=_toks([ph]), writes=_toks([h3f]))
                p.op("act", lambda e, rows=rows: e.activation(out=h3f[0:rows, :], in_=h3f[0:rows, :], func=AF.Ln,
                                                              bias=self.onec[0:rows, 0:1]),
                     reads=_toks([h3f, self.onec]), writes=_toks([h3f]))
                p.op("act", lambda e, rows=rows, dst=dst: e.activation(out=dst[:, :], in_=h3f[0:rows, :], func=AF.Exp, scale=-1.0),
                     reads=_toks([h3f]), writes=_toks([dst]))
            if _stop == "1":
                return
            for cc in range(8):
                cs = slice(cc * 128, (cc + 1) * 128)
                pr, pk = pgen[0], pgen[1]
                for k in range(8):
                    p.op("pe", lambda e, k=k, cs=cs: e.matmul(pr[:, 0:W], lhsT=wr[k][:, cs], rhs=xr[:, k, :],
                                                              start=(k == 0), stop=(k == 7)),
                         reads=_toks([wr[k], xr]), writes=_toks([pr]), sig=(k == 7))
                for k in range(8):
                    p.op("pe", lambda e, k=k, cs=cs: e.matmul(pk[:, 0:W], lhsT=wk[k][:, cs], rhs=xk[:, k, :],
                                                              start=(k == 0), stop=(k == 7)),
                         reads=_toks([wk[k], xk]), writes=_toks([pk]), sig=(k == 7))
                p.op("act", lambda e: e.copy(out=r_c[:, :], in_=pr[:, 0:W]), reads=_toks([pr]), writes=_toks([r_c]))
                p.op("act", lambda e: e.copy(out=k_c[:, :], in_=pk[:, 0:W]), reads=_toks([pk]), writes=_toks([k_c]))
                pl = pw()
                p.op("pe", lambda e, pl=pl, cs=cs: e.matmul(pl[:, 0:W], lhsT=w2t[0:64, cs], rhs=h1[0:64, :], start=True, stop=True),
                     reads=_toks([w2t, h1]), writes=_toks([pl]))
                p.op("pe", lambda e, pl=pl, cs=cs: e.matmul(pl[:, 256:256 + W], lhsT=a2t[0:64, cs], rhs=h2[0:64, :], start=True, stop=True),
                     reads=_toks([a2t, h2]), writes=_toks([pl]))
                for (dst, off_, ci) in ((lw, 0, 0), (a_c, 256, 1)):
                    p.op("act", lambda e, dst=dst, off_=off_, ci=ci, pl=pl, cc=cc: e.activation(
                        out=dst[:, :], in_=pl[:, off_:off_ + W], func=AF.Exp, scale=-1.0, bias=ncol[:, cc, ci:ci + 1]),
                        reads=_toks([pl, ncol]), writes=_toks([dst]))
                    p.op("act", lambda e, dst=dst: e.activation(out=dst[:, :], in_=dst[:, :], func=AF.Ln, bias=self.onec[:, 0:1]),
                         reads=_toks([dst, self.onec]), writes=_toks([dst]))
                    p.op("act", lambda e, dst=dst: e.activation(out=dst[:, :], in_=dst[:, :], func=AF.Exp, scale=-1.0),
                         reads=_toks([dst]), writes=_toks([dst]))
                p.op("dve", lambda e, cc=cc: e.tensor_scalar(out=kkr[:, :], in0=k_c[:, :], scalar1=cols[:, cc, 8:9], scalar2=None,
                                                             op0=ALU.mult), reads=_toks([k_c, cols]), writes=_toks([kkr]))
                p.op("act", lambda e: e.activation(out=sqk[:, :], in_=kkr[:, :], func=AF.Square),
                     reads=_toks([kkr]), writes=_toks([sqk]))
                pn = pw()
                p.op("pe", lambda e, pn=pn: e.matmul(pn[:, 0:W], lhsT=self.bd64f[:, :], rhs=sqk[:, :], start=True, stop=True),
                     reads=_toks([self.bd64f, sqk]), writes=_toks([pn]))
                p.op("act", lambda e, pn=pn: e.activation(out=rn[:, :], in_=pn[:, 0:W], func=AF.Ln, bias=self.epsc[:, 0:1]),
                     reads=_toks([pn, self.epsc]), writes=_toks([rn]))
                p.op("act", lambda e: e.activation(out=rn[:, :], in_=rn[:, :], func=AF.Exp, scale=-0.5),
                     reads=_toks([rn]), writes=_toks([rn]))
                p.op("dve", lambda e: e.tensor_tensor(out=kk[:, :], in0=kkr[:, :], in1=rn[:, :], op=ALU.mult),
                     reads=_toks([kkr, rn]), writes=_toks([kk]))
                p.op("dve", lambda e, cc=cc: e.tensor_scalar(out=t1[:, :], in0=a_c[:, :], scalar1=-1.0, scalar2=cols[:, cc, 9:10],
                                                             op0=ALU.add, op1=ALU.mult), reads=_toks([a_c, cols]), writes=_toks([t1]))
                p.op("dve", lambda e: e.scalar_tensor_tensor(out=kmod[:, :], in0=t1[:, :], scalar=1.0, in1=k_c[:, :],
                                                             op0=ALU.add, op1=ALU.mult), reads=_toks([t1, k_c]), writes=_toks([kmod]))
                for j in range(TB):
                    sl = slice(j * 128, (j + 1) * 128)
                    p.op("dve", lambda e, sl=sl: e.tensor_tensor_scan(out=cl[:, sl], data0=self.onesf[:, 0:128], data1=lw[:, sl],
                                                                      initial=0.0, op0=ALU.mult, op1=ALU.add),
                         reads=_toks([self.onesf, lw]), writes=_toks([cl]))
                p.op("act", lambda e: e.activation(out=eW[:, :], in_=cl[:, :], func=AF.Exp, scale=-0.6065306597126334),
                     reads=_toks([cl]), writes=_toks([eW]))
                p.op("act", lambda e: e.activation(out=eWi[:, :], in_=cl[:, :], func=AF.Exp, scale=0.6065306597126334),
                     reads=_toks([cl]), writes=_toks([eWi]))
                p.op("dve", lambda e: e.tensor_tensor(out=eWm[:, :], in0=cl[:, :], in1=lw[:, :], op=ALU.subtract),
                     reads=_toks([cl, lw]), writes=_toks([eWm]))
                p.op("act", lambda e: e.activation(out=eWm[:, :], in_=eWm[:, :], func=AF.Exp, scale=-0.6065306597126334),
                     reads=_toks([eWm]), writes=_toks([eWm]))
                p.op("dve", lambda e, cc=cc: e.tensor_tensor(out=rt_[:, cc, :], in0=r_c[:, :], in1=eW[:, :], op=ALU.mult),
                     reads=_toks([r_c, eW]), writes=_toks([rt_]))
                p.op("dve", lambda e, cc=cc: e.tensor_tensor(out=kt_[:, cc, :], in0=kmod[:, :], in1=eWi[:, :], op=ALU.mult),
                     reads=_toks([kmod, eWi]), writes=_toks([kt_]))
                p.op("pool", lambda e: e.tensor_tensor(out=bt_[:, :], in0=kk[:, :], in1=a_c[:, :], op=ALU.mult),
                     reads=_toks([kk, a_c]), writes=_toks([bt_]))
                p.op("dve", lambda e, cc=cc: e.tensor_tensor(out=btl[:, cc, :], in0=bt_[:, :], in1=eWi[:, :], op=ALU.mult),
                     reads=_toks([bt_, eWi]), writes=_toks([btl]))
                p.op("dve", lambda e, cc=cc: e.scalar_tensor_tensor(out=at_[:, cc, :], in0=kk[:, :], scalar=-1.0, in1=eWm[:, :],
                                                                    op0=ALU.mult, op1=ALU.mult),
                     reads=_toks([kk, eWm]), writes=_toks([at_]))
                p.op("dve", lambda e, cc=cc: e.scalar_tensor_tensor(out=rkr[:, cc, :], in0=r_c[:, :], scalar=cols[:, cc, 10:11],
                                                                    in1=kmod[:, :], op0=ALU.mult, op1=ALU.mult),
                     reads=_toks([r_c, cols, kmod]), writes=_toks([rkr]))
                for j in range(TB):
                    p.op("act", lambda e, cc=cc, j=j: e.copy(out=eWl[:, cc, j:j + 1], in_=eW[:, j * 128 + 127:j * 128 + 128]),
                         reads=_toks([eW]), writes=_toks([eWl]))
            if _stop == "2":
                return
            self.fm_to_tm(kt_, None, [(ktm[j][:, :], ktm[j]) for j in range(TB)], a)
            self.fm_to_tm(btl, None, [(btm[j][:, :], btm[j]) for j in range(TB)], a)
            for j in range(TB):
                i = b * TB + j
                tsl = slice(j * 128, (j + 1) * 128)
                for n in range(2):
                    pg = pgen[gi % 2]
                    gi += 1
                    for k in range(8):
                        p.op("pe", lambda e, k=k, n=n, pg=pg: e.matmul(pg[:, :], lhsT=xv[:, k, tsl], rhs=wv[k][:, n * 512:(n + 1) * 512],
                                                                       start=(k == 0), stop=(k == 7)),
                             reads=_toks([wv[k], xv]), writes=_toks([pg]), sig=(k == 7))
                    p.op("act", lambda e, n=n, pg=pg: e.copy(out=vtm[j][:, n * 512:(n + 1) * 512], in_=pg[:, :]),
                         reads=_toks([pg]), writes=_toks([vtm[j]]))
                for n in range(2):
                    pg = pgen[gi % 2]
                    gi += 1
                    p.op("pe", lambda e, n=n, pg=pg: e.matmul(pg[:, :], lhsT=h3a[:, tsl], rhs=g2a[:, n * 512:(n + 1) * 512],
                                                              start=True, stop=False),
                         reads=_toks([h3a, g2a]), writes=_toks([pg]), sig=False)
                    p.op("pe", lambda e, n=n, pg=pg: e.matmul(pg[:, :], lhsT=h3b[0:32, tsl], rhs=g2b[0:32, n * 512:(n + 1) * 512],
                                                              start=False, stop=True),
                         reads=_toks([h3b, g2b]), writes=_toks([pg]))
                    p.op("dve", lambda e, n=n, pg=pg: e.tensor_copy(out=gtm[j][:, n * 512:(n + 1) * 512], in_=pg[:, :]),
                         reads=_toks([pg]), writes=_toks([gtm[j]]))
                for cc in range(8):
                    p.op("pe", lambda e, cc=cc: e.matmul(prk[:, :], lhsT=rkr[:, cc, tsl], rhs=ind[:, cc, :],
                                                         start=(cc == 0), stop=(cc == 7)),
                         reads=_toks([rkr, ind]), writes=_toks([prk]), sig=(cc == 7))
                p.op("act", lambda e: e.copy(out=rks[:, :], in_=prk[:, :]), reads=_toks([prk]), writes=_toks([rks]))
                if _stop == "3":
                    return
                for bt in range(4):
                    xxv = xx[:, :, :].rearrange("p a w -> p (a w)").rearrange("p (t e c) -> p t e c", t=4, e=4)
                    msk_t = {}
                    for ti, src_ in enumerate((at_, btl, kt_, rt_)):
                        for e_ in range(4):
                            h = 4 * bt + e_
                            p.op("act", lambda e, ti=ti, e_=e_, h=h, src_=src_: e.activation(
                                out=xxv[:, ti, e_, :], in_=src_[:, h // 2, tsl], func=AF.Copy, scale=self.hm[:, h % 2:h % 2 + 1]),
                                reads=_toks([src_, self.hm]), writes=_toks([xx]))
                        msk_t[id(src_)] = ti

                    def gram(L, R, dst, mask, neg):
                        ps = pw()
                        ti = msk_t[id(L)]
                        for e_ in range(4):
                            h = 4 * bt + e_
                            cc = h // 2
                            p.op("pe", lambda e, e_=e_, cc=cc, ps=ps, ti=ti: e.matmul(
                                ps[:, e_ * 128:(e_ + 1) * 128], lhsT=xxv[:, ti, e_, :], rhs=R[:, cc, tsl], start=True, stop=True),
                                reads=_toks([xx, R]), writes=_toks([ps]), sig=(e_ == 3))
                        mb = mask[:, :].unsqueeze(1).to_broadcast([128, 4, 128])
                        psv = ps[:, :].rearrange("p (a b) -> p a b", b=128)
                        if neg:
                            p.op("dve", lambda e: e.scalar_tensor_tensor(out=dst[:, :, :], in0=psv, scalar=-1.0, in1=mb,
                                                                         op0=ALU.mult, op1=ALU.mult),
                                 reads=_toks([ps, mask]), writes=_toks([dst]))
                        else:
                            p.op("dve", lambda e: e.tensor_tensor(out=dst[:, :, :], in0=psv, in1=mb, op=ALU.mult),
                                 reads=_toks([ps, mask]), writes=_toks([dst]))
                    gram(at_, btl, Nn, self.maskGT, True)
                    gram(btl, at_, NnT, self.maskLT, True)
                    gram(kt_, at_, LakT, self.maskLT, False)
                    gram(btl, rt_, MrbT, self.triLE, False)
                    gram(kt_, rt_, MrkT, self.triLE, False)
                    uo = [None]
                    for _ in self.tri_inv(Nn, NnT, ib, pw, uo):
                        pass
                    U = uo[0]
                    if _stop == "4":
                        return
                    for q_ in range(2):
                        cc = 2 * bt + q_
                        Pm_ = Pm[ic % 2]
                        ic += 1
                        for hp in range(2):
                            e_ = 2 * q_ + hp
                            h = 2 * cc + hp
                            rs_ = slice(hp * 64, hp * 64 + 64)
                            hsl = slice(h * 64, (h + 1) * 64)
                            in_ = inner[hp]
                            ps1 = pw()
                            p.op("pe", lambda e, ps1=ps1, cc=cc, e_=e_: e.matmul(ps1[:, 0:64], lhsT=xxv[:, 0, e_, :], rhs=Zb[cc][:, :],
                                                                                 start=True, stop=False),
                                 reads=_toks([xx, Zb[cc]]), writes=_toks([ps1]), sig=False)
                            p.op("pe", lambda e, ps1=ps1, e_=e_, hsl=hsl: e.matmul(ps1[:, 0:64], lhsT=LakT[:, e_, :], rhs=vtm[j][:, hsl],
                                                                                   start=False, stop=True),
                                 reads=_toks([LakT, vtm[j]]), writes=_toks([ps1]))
                            p.op("act", lambda e, ps1=ps1, in_=in_: e.copy(out=in_[:, :], in_=ps1[:, 0:64]),
                                 reads=_toks([ps1]), writes=_toks([in_]))
                            p.op("pe", lambda e, ps1=ps1, e_=e_, in_=in_: e.matmul(ps1[:, 64:128], lhsT=U[:, e_, :], rhs=in_[:, :],
                                                                                   start=True, stop=True),
                                 reads=_toks([U, in_]), writes=_toks([ps1]))
                            p.op("dve", lambda e, ps1=ps1, Pm_=Pm_, rs_=rs_: e.tensor_copy(out=Pm_[:, rs_], in_=ps1[:, 64:128]),
                                 reads=_toks([ps1]), writes=_toks([Pm_]))
                            ps2 = pw()
                            p.op("pe", lambda e, ps2=ps2, cc=cc, e_=e_: e.matmul(ps2[:, 0:64], lhsT=xxv[:, 3, e_, :], rhs=Zb[cc][:, :],
                                                                                 start=True, stop=False),
                                 reads=_toks([xx, Zb[cc]]), writes=_toks([ps2]), sig=False)
                            p.op("pe", lambda e, ps2=ps2, e_=e_, Pm_=Pm_, rs_=rs_: e.matmul(ps2[:, 0:64], lhsT=MrbT[:, e_, :], rhs=Pm_[:, rs_],
                                                                                            start=False, stop=False),
                                 reads=_toks([MrbT, Pm_]), writes=_toks([ps2]), sig=False)
                            p.op("pe", lambda e, ps2=ps2, e_=e_, hsl=hsl: e.matmul(ps2[:, 0:64], lhsT=MrkT[:, e_, :], rhs=vtm[j][:, hsl],
                                                                                   start=False, stop=True),
                                 reads=_toks([MrkT, vtm[j]]), writes=_toks([ps2]))
                            p.op("act", lambda e, ps2=ps2, hsl=hsl: e.copy(out=y[:, hsl], in_=ps2[:, 0:64]),
                                 reads=_toks([ps2]), writes=_toks([y]))
                        csl = slice(cc * 128, (cc + 1) * 128)
                        ps3 = pw()
                        p.op("pe", lambda e, ps3=ps3, csl=csl, Pm_=Pm_: e.matmul(ps3[:, 0:128], lhsT=btm[j][:, csl], rhs=Pm_[:, :],
                                                                                 start=True, stop=False),
                             reads=_toks([btm[j], Pm_]), writes=_toks([ps3]), sig=False)
                        p.op("pe", lambda e, ps3=ps3, csl=csl: e.matmul(ps3[:, 0:128], lhsT=ktm[j][:, csl], rhs=vtm[j][:, csl],
                                                                        start=False, stop=True),
                             reads=_toks([ktm[j], vtm[j]]), writes=_toks([ps3]))
                        for hp in range(2):
                            rs_ = slice(hp * 64, hp * 64 + 64)
                            p.op("dve", lambda e, ps3=ps3, cc=cc, rs_=rs_, hp=hp: e.tensor_tensor(
                                out=ztmp[rs_, :], in0=Zc[cc][rs_, :], in1=ps3[rs_, hp * 64:(hp + 1) * 64], op=ALU.add),
                                reads=_toks([Zc[cc], ps3]), writes=_toks([ztmp]))
                            p.op("dve", lambda e, cc=cc, rs_=rs_: e.tensor_scalar(
                                out=Zc[cc][rs_, :], in0=ztmp[rs_, :], scalar1=eWl[rs_, cc, j:j + 1], scalar2=None, op0=ALU.mult),
                                reads=_toks([ztmp, eWl]), writes=_toks([Zc[cc]]))
                        p.op("act", lambda e, cc=cc: e.copy(out=Zb[cc][:, :], in_=Zc[cc][:, :]),
                             reads=_toks([Zc[cc]]), writes=_toks([Zb[cc]]))
                if _stop == "5":
                    return
                y3 = y[:, :].rearrange("p (h d) -> p h d", d=64)
                yc3 = yc[:, :].rearrange("p (h d) -> p h d", d=64)
                p.op("dve", lambda e: e.tensor_reduce(out=s16[:, :], in_=y3, axis=AX.X, op=ALU.add), reads=_toks([y]), writes=_toks([s16]))
                p.op("dve", lambda e: e.tensor_scalar(out=s16[:, :], in0=s16[:, :], scalar1=1.0 / 64.0, scalar2=None, op0=ALU.mult),
                     reads=_toks([s16]), writes=_toks([s16]))
                p.op("dve", lambda e: e.tensor_tensor(out=y3, in0=y3, in1=s16[:, :].unsqueeze(2).to_broadcast([128, 16, 64]),
                                                      op=ALU.subtract), reads=_toks([y, s16]), writes=_toks([y]))
                p.op("pool", lambda e: e.tensor_tensor(out=yc[:, :], in0=y[:, :], in1=y[:, :], op=ALU.mult),
                     reads=_toks([y]), writes=_toks([yc]))
                p.op("dve", lambda e: e.tensor_reduce(out=v16[:, :], in_=yc3, axis=AX.X, op=ALU.add), reads=_toks([yc]), writes=_toks([v16]))
                p.op("dve", lambda e: e.tensor_scalar(out=v16[:, :], in0=v16[:, :], scalar1=1.0 / 64.0, scalar2=None, op0=ALU.mult),
                     reads=_toks([v16]), writes=_toks([v16]))
                self.rsqrt_small(v16[:, :], v16[:, :], [v16], [v16], 64e-5)
                p.op("dve", lambda e: e.tensor_tensor(out=y3, in0=y3, in1=v16[:, :].unsqueeze(2).to_broadcast([128, 16, 64]),
                                                      op=ALU.mult), reads=_toks([y, v16]), writes=_toks([y]))
                p.op("pool", lambda e: e.tensor_tensor(out=y[:, :], in0=y[:, :], in1=lng[:, :], op=ALU.mult),
                     reads=_toks([y, lng]), writes=_toks([y]))
                p.op("pool", lambda e: e.tensor_tensor(out=y[:, :], in0=y[:, :], in1=lnb[:, :], op=ALU.add),
                     reads=_toks([y, lnb]), writes=_toks([y]))
                p.op("dve", lambda e: e.tensor_tensor(out=yc3, in0=vtm[j][:, :].rearrange("p (h d) -> p h d", d=64),
                                                      in1=rks[:, :].unsqueeze(2).to_broadcast([128, 16, 64]), op=ALU.mult),
                     reads=_toks([vtm[j], rks]), writes=_toks([yc]))
                p.op("pool", lambda e: e.tensor_tensor(out=y[:, :], in0=y[:, :], in1=yc[:, :], op=ALU.add),
                     reads=_toks([y, yc]), writes=_toks([y]))
                p.op("dve", lambda e: e.tensor_tensor(out=ob[:, :], in0=y[:, :], in1=gtm[j][:, :], op=ALU.mult),
                     reads=_toks([y, gtm[j]]), writes=_toks([ob]))
                p.op("sp", lambda e, i=i: e.dma_start(out=self.scr[i * 128:(i + 1) * 128, 0:D], in_=ob[:, :]),
                     reads=_toks([ob]), writes=[self.ytok[i]], dma=ob.tok)

    def build(self):
        for (kind, layer, arg) in self.sublayers:
            if kind == "ffn":
                self.ffn(layer, arg)
            elif kind == "xa":
                self.xattn(layer)
            elif kind == "dn":
                self.dn(layer)
                self.mix_out(layer, self.dn_w_out[layer // 3], 2 * D, zgate=(self.dn_w_in[layer // 3], 4096))
            elif kind == "rw":
                self.rw(layer)
                self.mix_out(layer, self.rw_w_out[layer // 3], D)
            elif kind == "ssd":
                self.ssd(layer)
                self.mix_out(layer, self.ssd_w_out[layer // 3], 2 * D)
            else:
                raise ValueError(kind)
        self.p.finish(self.htok + self.dbg_toks)
        return self.nc


FULL = []
for _l in range(4):
    FULL.append(("ffn", _l, 0))
    FULL.append((("dn", "ssd", "rw")[_l % 3], _l, 0))
    FULL.append(("xa", _l, 0))
    FULL.append(("ffn", _l, 1))


def kernel(**inputs):
    x = np.asarray(inputs["x"], dtype=np.float32)
    mem = np.asarray(inputs["mem"], dtype=np.float32)
    B, S, _ = x.shape
    nc = Builder(S // 128, FULL).build()
    in_maps = [make_in_map(inputs, x[b], mem[b]) for b in range(B)]
    res = run_bass_kernel_spmd(nc, in_maps, core_ids=list(range(B)))
    return np.stack([np.asarray(r["out"], dtype=np.float32) for r in res.results], axis=0)
```

```python
import numpy as np
from contextlib import ExitStack
import concourse.bass as bass
import concourse.mybir as mybir
from concourse.bass_utils import run_bass_kernel_spmd

F32 = mybir.dt.float32
BF16 = mybir.dt.bfloat16
AF = mybir.ActivationFunctionType
ALU = mybir.AluOpType
AX = mybir.AxisListType

D = 1024
DFF = 2816
NMEM = 256
EPS = 1e-6


class Tok:
    __slots__ = ("w", "r", "name")

    def __init__(self, name=""):
        self.w = None
        self.r = {}
        self.name = name


class Prog:
    def __init__(self, nc):
        self.nc = nc
        self.eng = dict(pe=nc.tensor, act=nc.scalar, dve=nc.vector, pool=nc.gpsimd, sp=nc.sync)
        self.sems = {}
        self.cnt = {}
        self.seen = {e: {} for e in self.eng}
        self.nsem = 0
        self.nins = {e: 0 for e in self.eng}
        self.dmap = {}
        self.dfree = []
        self.epoch = 0
        import os as _os
        for i_ in range(int(_os.environ.get("SEMSHIFT", "0"))):
            self.nc.alloc_semaphore(name=f"dummy{i_}")
        for e in ("pe", "act", "dve", "pool"):
            self._sem(e)

    def _sem(self, key):
        if key not in self.sems:
            self.sems[key] = self.nc.alloc_semaphore(name=f"s{self.nsem}")
            self.cnt[key] = 0
            self.nsem += 1
        return self.sems[key]

    def _wait(self, e, deps):
        eng = self.eng[e]
        for key, val in deps.items():
            if e == "pe" and key == "pe":
                continue
            if self.seen[e].get(key, 0) < val:
                eng.wait_ge(self.sems[key], val)
                self.seen[e][key] = val
                self.nins[e] += 1

    def _add(self, deps, tick):
        if tick is None or tick[2] < self.epoch:
            return
        k, v = tick[0], tick[1]
        if deps.get(k, 0) < v:
            deps[k] = v

    def op(self, e, fn, reads=(), writes=(), sig=True, dma=None):
        deps = {}
        for t in reads:
            self._add(deps, t.w)
        for t in writes:
            self._add(deps, t.w)
            for k, (v, ep) in t.r.items():
                self._add(deps, (k, v, ep))
        if dma is not None:
            key = self._dkey(dma)
            if self.cnt[key] > 0:
                self._add(deps, (key, self.cnt[key], self.epoch))
            inc = 16
        else:
            key = e
            inc = 1
        self._wait(e, deps)
        ins = fn(self.eng[e])
        self.nins[e] += 1
        if sig or dma is not None:
            ins.then_inc(self.sems[key], inc)
            self.cnt[key] += inc
            tick = (key, self.cnt[key], self.epoch)
        else:
            tick = (key, self.cnt[key] + 1, self.epoch)
        for t in reads:
            old = t.r.get(tick[0])
            if old is None or old[1] < self.epoch or old[0] < tick[1]:
                t.r[tick[0]] = (tick[1], self.epoch)
        for t in writes:
            t.w = tick
            t.r = {}
        return ins

    def _dkey(self, tok):
        k = self.dmap.get(id(tok))
        if k is None:
            if self.dfree:
                k = self.dfree.pop()
            else:
                k = ("d", self.nsem)
                self._sem(k)
            self.dmap[id(tok)] = k
        return k

    def release(self, toks):
        keys = []
        for t in toks:
            k = self.dmap.pop(id(t), None)
            if k is not None:
                keys.append(k)
        if not keys:
            return
        marks = {}
        for e in ("pe", "act", "dve", "sp"):
            key = e if e != "sp" else "spn"
            self._sem(key)
            self.eng[e].nop().then_inc(self.sems[key], 1)
            self.cnt[key] += 1
            marks[key] = self.cnt[key]
        self._wait_all("pool", marks)
        for k in keys:
            self.eng["pool"].sem_clear(self.sems[k])
            self.cnt[k] = 0
            for e in self.eng:
                self.seen[e].pop(k, None)
            self.dfree.append(k)
        self.eng["pool"].nop().then_inc(self.sems["pool"], 1)
        self.cnt["pool"] += 1
        for e in ("pe", "act", "dve", "sp"):
            self._wait_all(e, {"pool": self.cnt["pool"]})

    def barrier(self):
        deps = {k: v for k, v in self.cnt.items() if v > 0}
        for e in self.eng:
            d = dict(deps)
            self._wait_all(e, d)
        self.epoch += 1

    def _wait_all(self, e, deps):
        eng = self.eng[e]
        for key, val in deps.items():
            if self.seen[e].get(key, 0) < val:
                eng.wait_ge(self.sems[key], val)
                self.seen[e][key] = val
                self.nins[e] += 1

    def finish(self, toks):
        deps = {}
        for t in toks:
            self._add(deps, t.w)
        self._wait("sp", deps)


class T:
    def __init__(self, h, name, tok=None):
        self.h = h
        self.tok = tok or Tok(name)

    def view(self, key, name=None):
        return T(self.h[key], name, tok=self.tok)

    def __getitem__(self, k):
        return self.h[k]


class Ctx:
    def __init__(self, nc):
        self.nc = nc
        self.p = Prog(nc)
        self.n = 0
        self.es = None
        self.ptoks = []

    def phase(self):
        return _Phase(self)

    def sb(self, shape, dt, name=None):
        self.n += 1
        nm = f"{name or 'sb'}_{self.n}"
        if self.es is None:
            h = self.nc.alloc_sbuf_tensor(nm, list(shape), dt)
        else:
            h = self.es.enter_context(self.nc.sbuf_tensor(nm, list(shape), dt))
        t = T(h, name)
        if self.es is not None:
            self.ptoks.append(t.tok)
        return t

    def ps(self, shape, dt, name=None):
        self.n += 1
        nm = f"{name or 'ps'}_{self.n}"
        if self.es is None:
            h = self.nc.alloc_psum_tensor(nm, list(shape), dt)
        else:
            h = self.es.enter_context(self.nc.psum_tensor(nm, list(shape), dt))
        t = T(h, name)
        if self.es is not None:
            self.ptoks.append(t.tok)
        return t


class _Phase:
    def __init__(self, c):
        self.c = c

    def __enter__(self):
        self.saved = (self.c.es, self.c.ptoks, getattr(self.c, "stg", None))
        self.c.stg = None
        self.c.es = ExitStack()
        self.c.es.__enter__()
        self.c.ptoks = []
        return self

    def __exit__(self, *a):
        self.c.p.barrier()
        self.c.p.release(self.c.ptoks)
        self.c.es.__exit__(None, None, None)
        self.c.es, self.c.ptoks, self.c.stg = self.saved
        return False


def _toks(xs):
    return [x.tok if isinstance(x, T) else x for x in xs]


PARAM_SHAPES = {
    "sandwich_g": [4, 4, 2, 1024], "ffn_w_in": [4, 2, 1024, 5632], "ffn_w_out": [4, 2, 2816, 1024],
    "mem_norm_g": [4, 1024], "xa_w_q": [4, 1024, 1024], "xa_w_kv": [4, 1024, 2048], "xa_w_o": [4, 1024, 1024],
    "dn_w_in": [2, 1024, 6176], "dn_conv_w": [2, 4, 4096], "dn_a_log": [2, 16], "dn_dt_bias": [2, 16],
    "dn_norm_g": [2, 128], "dn_w_out": [2, 2048, 1024],
    "ssd_w_in": [1, 1024, 6176], "ssd_conv_w": [1, 4, 4096], "ssd_conv_b": [1, 4096], "ssd_a_log": [1, 32],
    "ssd_dt_bias": [1, 32], "ssd_d": [1, 32], "ssd_norm_g": [1, 2048], "ssd_w_out": [1, 2048, 1024],
    "rw_mu": [1, 6, 1024], "rw_w_rkv": [1, 3, 1024, 1024], "rw_w0": [1, 1024], "rw_w1": [1, 1024, 64],
    "rw_w2": [1, 64, 1024], "rw_a0": [1, 1024], "rw_a1": [1, 1024, 64], "rw_a2": [1, 64, 1024],
    "rw_g1": [1, 1024, 160], "rw_g2": [1, 160, 1024], "rw_k_k": [1, 1024], "rw_k_a": [1, 1024],
    "rw_r_k": [1, 16, 64], "rw_ln_g": [1, 1024], "rw_ln_b": [1, 1024], "rw_w_out": [1, 1024, 1024],
}


def make_in_map(params, x, mem):
    m = {k: np.ascontiguousarray(np.asarray(params[k], dtype=np.float32)) for k in PARAM_SHAPES}
    m["x"] = np.ascontiguousarray(x, dtype=np.float32)
    m["mem"] = np.ascontiguousarray(mem, dtype=np.float32)
    return m


class Builder:
    def __init__(self, NT, sublayers, n_layers=4, debug=False):
        self.NT = NT
        self.S = NT * 128
        self.sublayers = sublayers
        nc = bass.Bass("TRN2", target_bir_lowering=False)
        self.nc = nc
        self.c = Ctx(nc)
        self.p = self.c.p
        S = self.S
        dt = nc.dram_tensor
        self.x = dt("x", [S, D], F32, kind="ExternalInput").ap()
        self.mem = dt("mem", [NMEM, D], F32, kind="ExternalInput").ap()
        self.out = dt("out", [S, D], F32, kind="ExternalOutput").ap()
        L = n_layers
        self.sandwich_g = dt("sandwich_g", [L, 4, 2, D], F32, kind="ExternalInput").ap()
        self.ffn_w_in = dt("ffn_w_in", [L, 2, D, 2 * DFF], F32, kind="ExternalInput").ap()
        self.ffn_w_out = dt("ffn_w_out", [L, 2, DFF, D], F32, kind="ExternalInput").ap()
        def inp(name, shape):
            return dt(name, list(shape), F32, kind="ExternalInput").ap()
        self.mem_norm_g = inp("mem_norm_g", [L, D])
        self.xa_w_q = inp("xa_w_q", [L, D, D])
        self.xa_w_kv = inp("xa_w_kv", [L, D, 2 * D])
        self.xa_w_o = inp("xa_w_o", [L, D, D])
        for nm, shp in PARAM_SHAPES.items():
            if not hasattr(self, nm):
                setattr(self, nm, inp(nm, shp))
        self.htok = [Tok(f"h{i}") for i in range(NT)]
        self.scr = dt("scr", [S, 2 * D], BF16, kind="Internal").ap()
        self.ytok = [Tok(f"y{i}") for i in range(NT)]
        self.first = True
        self.debug = debug
        self.dbg_names = set()
        self.dbg_toks = []
        self._consts()

    def _consts(self):
        c, p, nc = self.c, self.p, self.nc
        self.ident = c.sb([128, 128], BF16, "ident")
        self.identf = c.sb([128, 128], F32, "identf")
        p.op("pool", lambda e: e.memset(self.identf[:, :], 0.0), writes=_toks([self.identf]))
        p.op("pool", lambda e: e.affine_select(
            out=self.identf[:, :], in_=self.identf[:, :], pattern=[[-1, 128]],
            compare_op=ALU.not_equal, fill=1.0, base=0, channel_multiplier=1),
            reads=_toks([self.identf]), writes=_toks([self.identf]))
        p.op("dve", lambda e: e.tensor_copy(out=self.ident[:, :], in_=self.identf[:, :]),
             reads=_toks([self.identf]), writes=_toks([self.ident]))
        self.epsc = c.sb([128, 1], F32, "eps")
        p.op("dve", lambda e: e.memset(self.epsc[:, :], EPS), writes=_toks([self.epsc]))
        def msk(name, pattern_step, chmul, cmp):
            t = c.sb([128, 128], F32, name)
            p.op("pool", lambda e: e.memset(t[:, :], 1.0), writes=_toks([t]))
            p.op("pool", lambda e: e.affine_select(
                out=t[:, :], in_=t[:, :], pattern=[[pattern_step, 128]], compare_op=cmp, fill=0.0, base=0,
                channel_multiplier=chmul), reads=_toks([t]), writes=_toks([t]))
            return t
        self.triLE = msk("triLE", 1, -1, ALU.is_ge)
        self.maskGT = msk("maskGT", -1, 1, ALU.is_gt)
        self.maskLT = msk("maskLT", 1, -1, ALU.is_gt)
        def bd(name, sz):
            t = c.sb([128, 128], F32, name)
            v = t[:, :].rearrange("p (a b) -> p a b", b=sz)
            p.op("pool", lambda e: e.memset(t[:, :], 1.0), writes=_toks([t]))
            p.op("pool", lambda e: e.affine_select(out=v, in_=v, pattern=[[-sz, 128 // sz], [0, sz]],
                                                   compare_op=ALU.is_ge, fill=0.0, base=0, channel_multiplier=1),
                 reads=_toks([t]), writes=_toks([t]))
            p.op("pool", lambda e: e.affine_select(out=v, in_=v, pattern=[[sz, 128 // sz], [0, sz]],
                                                   compare_op=ALU.is_ge, fill=0.0, base=sz - 1, channel_multiplier=-1),
                 reads=_toks([t]), writes=_toks([t]))
            return t
        b16, b32, b64 = bd("bd16", 16), bd("bd32", 32), bd("bd64", 64)
        self.bd64f = b64
        self.onesf = c.sb([128, 128], F32, "onesf")
        p.op("pool", lambda e: e.memset(self.onesf[:, :], 1.0), writes=_toks([self.onesf]))
        self.mlev = []
        for nm, hi, lo in (("mb16", b16, None), ("mo32", b32, b16), ("mo64", b64, b32), ("mo128", self.onesf, b64)):
            t = c.sb([128, 128], BF16, nm)
            if lo is None:
                p.op("pool", lambda e, t=t, hi=hi: e.tensor_copy(out=t[:, :], in_=hi[:, :]),
                     reads=_toks([hi]), writes=_toks([t]))
            else:
                p.op("pool", lambda e, t=t, hi=hi, lo=lo: e.tensor_tensor(out=t[:, :], in0=hi[:, :], in1=lo[:, :],
                                                                         op=ALU.subtract),
                     reads=_toks([hi, lo]), writes=_toks([t]))
            self.mlev.append(t)
        self.onesf2 = self.onesf
        p.op("pool", lambda e: e.memset(self.onesf[:, :], 1.0), writes=_toks([self.onesf]))
        self.hm = c.sb([128, 2], F32, "hm")
        p.op("pool", lambda e: e.memset(self.hm[:, :], 0.0), writes=_toks([self.hm]))
        p.op("pool", lambda e: e.memset(self.hm[0:64, 0:1], 1.0), reads=_toks([self.hm]), writes=_toks([self.hm]))
        p.op("pool", lambda e: e.memset(self.hm[64:128, 1:2], 1.0), reads=_toks([self.hm]), writes=_toks([self.hm]))
        self.onec = c.sb([128, 1], F32, "onec")
        p.op("dve", lambda e: e.memset(self.onec[:, :], 1.0), writes=_toks([self.onec]))
        self.epsc2 = c.sb([128, 1], F32, "eps2")
        p.op("dve", lambda e: e.memset(self.epsc2[:, :], 64e-5), writes=_toks([self.epsc2]))

    def dbg(self, name, t, ap, shape, dtype=F32):
        if not getattr(self, "debug", False) or name in self.dbg_names:
            return
        self.dbg_names.add(name)
        d = self.nc.dram_tensor("dbg_" + name, list(shape), dtype, kind="ExternalOutput").ap()
        tk = Tok()
        self.p.op("sp", lambda e: e.dma_start(out=d, in_=ap), reads=_toks([t]), writes=[tk], dma=tk)
        self.dbg_toks.append(tk)

    def hsrc(self, i):
        src = self.x if self.first else self.out
        return src[i * 128:(i + 1) * 128, :]

    def load_g(self, gt, layer, sub, wgt=1.0):
        p = self.p
        for j in range(gt.h.shape[1]):
            src = self.sandwich_g[layer, sub, j, :].partition_broadcast(128)
            p.op("sp", lambda e, src=src, j=j: e.dma_start(out=gt[:, j, :], in_=src),
                 writes=_toks([gt]), dma=gt.tok)
        if wgt != 1.0 and gt.h.shape[1] > 1:
            p.op("dve", lambda e: e.tensor_scalar(out=gt[:, 1, :], in0=gt[:, 1, :], scalar1=float(wgt), scalar2=None,
                                                  op0=ALU.mult),
                 reads=_toks([gt]), writes=_toks([gt]))

    def rstd(self, src_ap, src_toks, junk, ss, rs, n=D, eps=EPS):
        p = self.p
        p.op("act", lambda e: e.activation(out=junk[:, :n], in_=src_ap, func=AF.Square,
                                           scale=float(n) ** -0.5, accum_out=ss[:, 0:1]),
             reads=_toks(src_toks), writes=_toks([junk, ss]))
        self.rsqrt_small(ss[:, 0:1], rs[:, 0:1], [ss], [rs], eps)

    def rsqrt_small(self, src, dst, rt, wt, eps):
        p = self.p
        b = self.epsc[:, 0:1] if eps == EPS else self.epsc2[:, 0:1]
        p.op("act", lambda e: e.activation(out=dst, in_=src, func=AF.Ln, bias=b),
             reads=_toks(rt + [self.epsc]), writes=_toks(wt))
        p.op("act", lambda e: e.activation(out=dst, in_=dst, func=AF.Exp, scale=-0.5),
             reads=_toks(wt), writes=_toks(wt))

    def ffn(self, layer, which):
        with self.c.phase():
            self._ffn(layer, which)
        self.first = False

    def _ffn(self, layer, which):
        c, p, nc = self.c, self.p, self.nc
        NT = self.NT
        sub = 0 if which == 0 else 3
        TB = 4 if NT % 4 == 0 else 1
        NB = NT // TB
        W = TB * 128
        NH = DFF // 128
        a = {}
        a["win"] = [c.sb([128, 2 * DFF], BF16, f"win{k}") for k in range(8)]
        a["wout"] = [c.sb([128, D], BF16, f"wout{k}") for k in range(NH)]
        a["g"] = c.sb([128, 2, D], F32, "g")
        a["h"] = [c.sb([128, D], F32, f"h{i}") for i in range(2)]
        a["hr"] = [c.sb([128, D], F32, f"hr{i}") for i in range(2)]
        a["xn"] = [c.sb([128, D], BF16, f"xn{i}") for i in range(2)]
        a["xT"] = [c.sb([128, 8, W], BF16, f"xT{i}") for i in range(1)]
        a["hT"] = [c.sb([128, NH, W], BF16, f"hT{i}") for i in range(1)]
        a["sg"] = [c.sb([128, W], F32, f"sg{i}") for i in range(2)]
        a["junk"] = c.sb([128, D], BF16, "junk")
        a["ss"] = [c.sb([128, 1], F32, f"ss{i}") for i in range(4)]
        a["rs"] = [c.sb([128, 1], F32, f"rs{i}") for i in range(4)]
        a["ssb"] = [c.sb([128, 1], F32, f"ssb{i}") for i in range(2)]
        self._pk = 0
        a["t"] = [c.sb([128, D], F32, "t")] * 2
        a["pT"] = [c.ps([128, 8, 128], BF16, f"pT{i}") for i in range(1)]
        a["pg"] = [c.ps([128, W], F32, f"pg{i}") for i in range(2)]
        a["pu"] = [c.ps([128, W], F32, f"pu{i}") for i in range(2)]
        a["po"] = [c.ps([128, 512], F32, f"po{i}") for i in range(3)]
        self.load_g(a["g"], layer, sub, 0.5)
        g = a["g"]
        cnt = 0
        xT = a["xT"][0]
        hT = a["hT"][0]

        def stage_a(b, j):
            nonlocal cnt
            xn = a["xn"][cnt % 2]
            cnt += 1
            self.pre(b * TB + j, a, g, xn)
            return xn

        for j in range(TB):
            xn = stage_a(0, j)
            self.to_fm(xn, xT, j, a)
        c.stg = a["hr"] + [a["t"][0]]
        self._stg_i = 0
        win_src = self.ffn_w_in[layer, which]
        wout_src = self.ffn_w_out[layer, which]
        wv_ = self.wload_pieces(a["win"], win_src, order=[0, 2, 3, 1, 4, 5])
        for k in range(NH):
            self.wload(a["wout"][k], a["wout"][k][:, :], wout_src[k * 128:(k + 1) * 128, :])
        for b in range(NB):
            for cch in range(NH):
                pg, pu, sg = a["pg"][cch % 2], a["pu"][cch % 2], a["sg"][cch % 2]
                for k in range(8):
                    p.op("pe", lambda e, k=k, cch=cch, pg=pg: e.matmul(
                        pg[:, :], lhsT=a["win"][k][:, cch * 128:(cch + 1) * 128], rhs=xT[:, k, :],
                        start=(k == 0), stop=(k == 7)),
                        reads=_toks([wv_(k, cch * 128), xT]), writes=_toks([pg]), sig=(k == 7))
                for k in range(8):
                    p.op("pe", lambda e, k=k, cch=cch, pu=pu: e.matmul(
                        pu[:, :], lhsT=a["win"][k][:, DFF + cch * 128:DFF + (cch + 1) * 128], rhs=xT[:, k, :],
                        start=(k == 0), stop=(k == 7)),
                        reads=_toks([wv_(k, DFF + cch * 128), xT]), writes=_toks([pu]), sig=(k == 7))
                p.op("act", lambda e, pg=pg, sg=sg: e.activation(out=sg[:, :], in_=pg[:, :], func=AF.Exp, scale=-1.0),
                     reads=_toks([pg]), writes=_toks([sg]))
                p.op("act", lambda e, sg=sg: e.activation(out=sg[:, :], in_=sg[:, :], func=AF.Ln, bias=self.onec[:, 0:1]),
                     reads=_toks([sg, self.onec]), writes=_toks([sg]))
                p.op("act", lambda e, sg=sg: e.activation(out=sg[:, :], in_=sg[:, :], func=AF.Exp, scale=-1.0),
                     reads=_toks([sg]), writes=_toks([sg]))
                p.op("dve", lambda e, pg=pg, sg=sg: e.tensor_tensor(
                    out=sg[:, :], in0=pg[:, :], in1=sg[:, :], op=ALU.mult),
                    reads=_toks([sg, pg]), writes=_toks([sg]))
                p.op("dve", lambda e, cch=cch, pu=pu, sg=sg: e.tensor_tensor(
                    out=hT[:, cch, :], in0=sg[:, :], in1=pu[:, :], op=ALU.mult),
                    reads=_toks([sg, pu]), writes=_toks([hT]))
            for j in range(TB):
                i = b * TB + j
                pa, pb = a["po"][self._pk % 3], a["po"][(self._pk + 1) % 3]
                self._pk += 2
                xn = stage_a(b + 1, j) if b + 1 < NB else None
                for n, pp in ((0, pa), (1, pb)):
                    for cch in range(NH):
                        p.op("pe", lambda e, cch=cch, n=n, j=j, pp=pp: e.matmul(
                            pp[:, :], lhsT=hT[:, cch, j * 128:(j + 1) * 128],
                            rhs=a["wout"][cch][:, n * 512:(n + 1) * 512],
                            start=(cch == 0), stop=(cch == NH - 1)),
                            reads=_toks([hT, a["wout"][cch]]), writes=_toks([pp]),
                            sig=(cch == NH - 1))
                if xn is not None:
                    self.to_fm(xn, xT, j, a)
                self.post2(pa, pb, g, i, a)

    def post2(self, pa, pb, g, i, a):
        p = self.p
        self._pc = getattr(self, "_pc", 0) + 1
        ss, rs = a["ss"][2 + self._pc % 2], a["rs"][2 + self._pc % 2]
        ssb = a["ssb"][self._pc % 2]
        t = a["t"][self._pc % 2]
        ht = a["hr"][self._pc % 2]
        src = self.hsrc(i)
        p.op("sp", lambda e: e.dma_start(out=ht[:, :], in_=src),
             reads=[self.htok[i]], writes=_toks([ht]), dma=ht.tok)
        p.op("act", lambda e: e.activation(out=a["junk"][:, 0:512], in_=pa[:, :], func=AF.Square,
                                           scale=float(D) ** -0.5, accum_out=ss[:, 0:1]),
             reads=_toks([pa]), writes=_toks([a["junk"], ss]))
        p.op("act", lambda e: e.activation(out=a["junk"][:, 512:1024], in_=pb[:, :], func=AF.Square,
                                           scale=float(D) ** -0.5, accum_out=ssb[:, 0:1]),
             reads=_toks([pb]), writes=_toks([a["junk"], ssb]))
        p.op("dve", lambda e: e.tensor_tensor(out=ss[:, 0:1], in0=ss[:, 0:1], in1=ssb[:, 0:1], op=ALU.add),
             reads=_toks([ss, ssb]), writes=_toks([ss]))
        self.rsqrt_small(ss[:, 0:1], rs[:, 0:1], [ss], [rs], EPS)
        p.op("dve", lambda e: e.scalar_tensor_tensor(
            out=t[:, 0:512], in0=pa[:, :], scalar=rs[:, 0:1], in1=g[:, 1, 0:512], op0=ALU.mult, op1=ALU.mult),
            reads=_toks([pa, rs, g]), writes=_toks([t]))
        p.op("dve", lambda e: e.scalar_tensor_tensor(
            out=t[:, 512:1024], in0=pb[:, :], scalar=rs[:, 0:1], in1=g[:, 1, 512:1024], op0=ALU.mult, op1=ALU.mult),
            reads=_toks([pb, rs, g]), writes=_toks([t]))
        p.op("pool", lambda e: e.tensor_tensor(out=ht[:, :], in0=t[:, :], in1=ht[:, :], op=ALU.add),
             reads=_toks([t, ht]), writes=_toks([ht]))
        p.op("sp", lambda e: e.dma_start(out=self.out[i * 128:(i + 1) * 128, :], in_=ht[:, :]),
             reads=_toks([ht]), writes=[self.htok[i]], dma=ht.tok)

    def wload(self, t, dst, src):
        c, p = self.c, self.p
        if getattr(c, "stg", None) is None:
            c.stg = [c.sb([128, 1024], F32, f"stg{i}") for i in range(getattr(self, "nstg", 2))]
            self._stg_i = 0
        stg = c.stg
        rows, cols = src.shape
        for c0 in range(0, cols, 1024):
            c1 = min(cols, c0 + 1024)
            st = stg[self._stg_i % len(stg)]
            eng = ("act", "dve", "pool", "act", "dve")[self._stg_i % 5]
            self._stg_i += 1
            p.op("sp", lambda e, st=st, c0=c0, c1=c1: e.dma_start(out=st[0:rows, 0:c1 - c0], in_=src[:, c0:c1]),
                 writes=_toks([st]), dma=st.tok)
            if eng == "act":
                p.op("act", lambda e, st=st, c0=c0, c1=c1: e.copy(out=dst[:, c0:c1], in_=st[0:rows, 0:c1 - c0]),
                     reads=_toks([st]), writes=_toks([t]))
            else:
                p.op(eng, lambda e, st=st, c0=c0, c1=c1: e.tensor_copy(out=dst[:, c0:c1], in_=st[0:rows, 0:c1 - c0]),
                     reads=_toks([st]), writes=_toks([t]))

    def wload_pieces(self, tiles, src, order=None, pw_=1024):
        ncols = src.shape[1]
        npc = (ncols + pw_ - 1) // pw_
        order = list(order) if order is not None else list(range(npc))
        order += [x for x in range(npc) if x not in order]
        views = {}
        for pc in order:
            c0, c1 = pc * pw_, min(ncols, (pc + 1) * pw_)
            for k, t in enumerate(tiles):
                v = T(t.h[:, c0:c1], f"wp{k}_{pc}")
                self.c.ptoks.append(v.tok)
                views[(k, pc)] = v
                self.wload(v, t[:, c0:c1], src[k * 128:(k + 1) * 128, c0:c1])
        return lambda k, col: views[(k, col // pw_)]

    def pre(self, i, a, g, xn):
        p = self.p
        self._prc = getattr(self, "_prc", 0) + 1
        ht = a["h"][self._prc % 2]
        ss, rs = a["ss"][self._prc % 2], a["rs"][self._prc % 2]
        src = self.hsrc(i)
        p.op("sp", lambda e: e.dma_start(out=ht[:, :], in_=src),
             reads=[self.htok[i]], writes=_toks([ht]), dma=ht.tok)
        self.rstd(ht[:, :], [ht], a["junk"], ss, rs)
        p.op("dve", lambda e: e.scalar_tensor_tensor(
            out=xn[:, :], in0=ht[:, :], scalar=rs[:, 0:1], in1=g[:, 0, :],
            op0=ALU.mult, op1=ALU.mult),
            reads=_toks([ht, rs, g]), writes=_toks([xn]))

    def to_fm(self, xn, xT, j, a, nk=8, off=0):
        p = self.p
        pT = a["pT"][0]
        for k in range(nk):
            p.op("pe", lambda e, k=k: e.transpose(out=pT[:, k, :], in_=xn[:, k * 128:(k + 1) * 128],
                                                   identity=self.ident[:, :]),
                 reads=_toks([xn, self.ident]), writes=_toks([pT]), sig=(k == nk - 1))
        p.op("act", lambda e: e.copy(out=xT[:, 0:nk, off + j * 128:off + (j + 1) * 128], in_=pT[:, 0:nk, :]),
             reads=_toks([pT]), writes=_toks([xT]))

    def post(self, po, g, wgt, i, a):
        p = self.p
        self._pc = getattr(self, "_pc", 0) + 1
        ss, rs = a["ss"][2 + self._pc % 2], a["rs"][2 + self._pc % 2]
        t = a["t"][self._pc % 2]
        ht = a["hr"][self._pc % 2]
        src = self.hsrc(i)
        p.op("sp", lambda e: e.dma_start(out=ht[:, :], in_=src),
             reads=[self.htok[i]], writes=_toks([ht]), dma=ht.tok)
        self.rstd(po[:, :], [po], a["junk"], ss, rs)
        p.op("dve", lambda e: e.scalar_tensor_tensor(
            out=t[:, :], in0=po[:, :], scalar=rs[:, 0:1], in1=g[:, 1, :], op0=ALU.mult, op1=ALU.mult),
            reads=_toks([po, rs, g]), writes=_toks([t]))
        p.op("pool", lambda e: e.tensor_tensor(out=ht[:, :], in0=t[:, :], in1=ht[:, :], op=ALU.add),
             reads=_toks([t, ht]), writes=_toks([ht]))
        p.op("sp", lambda e: e.dma_start(out=self.out[i * 128:(i + 1) * 128, :], in_=ht[:, :]),
             reads=_toks([ht]), writes=[self.htok[i]], dma=ht.tok)

    def xattn(self, layer):
        with self.c.phase():
            self._xattn(layer)
        self.first = False

    def bcast_row(self, t, dst, src_row):
        self.p.op("sp", lambda e: e.dma_start(out=dst, in_=src_row.partition_broadcast(128)),
                  writes=_toks([t]), dma=t.tok)

    def _xattn(self, layer):
        c, p, nc = self.c, self.p, self.nc
        NT = self.NT
        TB = 4 if NT % 4 == 0 else 1
        NB = NT // TB
        W = TB * 128
        a = {}
        a["wq"] = [c.sb([128, D], BF16, f"wq{k}") for k in range(8)]
        a["wkv"] = [c.sb([128, 2 * D], BF16, f"wkv{k}") for k in range(8)]
        a["wo"] = [c.sb([128, D], BF16, f"wo{k}") for k in range(8)]
        a["g"] = c.sb([128, 2, D], F32, "g")
        a["gm"] = c.sb([128, D], F32, "gm")
        a["h"] = [c.sb([128, D], F32, f"h{i}") for i in range(2)]
        a["hr"] = [c.sb([128, D], F32, f"hr{i}") for i in range(2)]
        a["xn"] = [c.sb([128, D], BF16, f"xn{i}") for i in range(2)]
        a["xT"] = [c.sb([128, 8, W], BF16, "xT")]
        a["junk"] = c.sb([128, D], BF16, "junk")
        a["ss"] = [c.sb([128, 1], F32, f"ss{i}") for i in range(4)]
        a["rs"] = [c.sb([128, 1], F32, f"rs{i}") for i in range(4)]
        a["t"] = [c.sb([128, D], F32, f"t{i}") for i in range(2)]
        a["pT"] = [c.ps([128, 8, 128], BF16, "pT")]
        a["po"] = [c.ps([128, D], F32, "po")]
        memT = c.sb([128, 8, NMEM], BF16, "memT")
        KT = c.sb([128, 8, NMEM], BF16, "KT")
        V = c.sb([128, 2, D], BF16, "V")
        qT = c.sb([128, 8, W], BF16, "qT")
        oT = c.sb([128, 8, W], BF16, "oT")
        PT = c.sb([128, 8, W], BF16, "PT")
        pss2 = [c.ps([128, 2, NMEM], F32, f"pss{i}") for i in range(2)]
        Pf2 = [[c.sb([128, 2, NMEM], F32, f"Pf{h}{i}") for i in range(2)] for h in range(2)]
        Pn2 = [[c.sb([128, 2, NMEM], BF16, f"Pn{h}{i}") for i in range(2)] for h in range(2)]
        mx2 = [[c.sb([128, 2], F32, f"mx{h}{i}") for i in range(2)] for h in range(2)]
        rs2 = [[c.sb([128, 2], F32, f"rs2{h}{i}") for i in range(2)] for h in range(2)]
        pgen = [c.ps([128, 512], F32, f"pgen{i}") for i in range(2)]
        self.nstg = 4
        for k in range(8):
            self.wload(a["wkv"][k], a["wkv"][k][:, :], self.xa_w_kv[layer, k * 128:(k + 1) * 128, :])
        for k in range(8):
            self.wload(a["wq"][k], a["wq"][k][:, :], self.xa_w_q[layer, k * 128:(k + 1) * 128, :])
        for k in range(8):
            self.wload(a["wo"][k], a["wo"][k][:, :], self.xa_w_o[layer, k * 128:(k + 1) * 128, :])
        self.nstg = 2
        self.load_g(a["g"], layer, 2)
        g = a["g"]
        self.bcast_row(a["gm"], a["gm"][:, :], self.mem_norm_g[layer, :])
        for mt in range(2):
            ht = a["h"][mt]
            ss, rs = a["ss"][mt], a["rs"][mt]
            xn = a["xn"][mt]
            p.op("sp", lambda e, ht=ht, mt=mt: e.dma_start(out=ht[:, :], in_=self.mem[mt * 128:(mt + 1) * 128, :]),
                 writes=_toks([ht]), dma=ht.tok)
            self.rstd(ht[:, :], [ht], a["junk"], ss, rs)
            p.op("dve", lambda e, ht=ht, rs=rs, xn=xn: e.scalar_tensor_tensor(
                out=xn[:, :], in0=ht[:, :], scalar=rs[:, 0:1], in1=a["gm"][:, :], op0=ALU.mult, op1=ALU.mult),
                reads=_toks([ht, rs, a["gm"]]), writes=_toks([xn]))
            self.to_fm(xn, memT, mt, a)
        gi = 0
        for fc in range(8):
            pg = pgen[gi % 2]
            gi += 1
            for k in range(8):
                p.op("pe", lambda e, k=k, fc=fc, pg=pg: e.matmul(
                    pg[:, 0:NMEM], lhsT=a["wkv"][k][:, fc * 128:(fc + 1) * 128], rhs=memT[:, k, :],
                    start=(k == 0), stop=(k == 7)),
                    reads=_toks([a["wkv"][k], memT]), writes=_toks([pg]), sig=(k == 7))
            p.op("act", lambda e, fc=fc, pg=pg: e.copy(out=KT[:, fc, :], in_=pg[:, 0:NMEM]),
                 reads=_toks([pg]), writes=_toks([KT]))
        for mt in range(2):
            for n in range(2):
                pg = pgen[gi % 2]
                gi += 1
                for k in range(8):
                    p.op("pe", lambda e, k=k, mt=mt, n=n, pg=pg: e.matmul(
                        pg[:, :], lhsT=memT[:, k, mt * 128:(mt + 1) * 128],
                        rhs=a["wkv"][k][:, D + n * 512:D + (n + 1) * 512], start=(k == 0), stop=(k == 7)),
                        reads=_toks([a["wkv"][k], memT]), writes=_toks([pg]), sig=(k == 7))
                p.op("dve", lambda e, mt=mt, n=n, pg=pg: e.tensor_copy(out=V[:, mt, n * 512:(n + 1) * 512], in_=pg[:, :]),
                     reads=_toks([pg]), writes=_toks([V]))
        sc = 256.0 ** -0.5
        cnt = 0
        xT = a["xT"][0]

        def stage_a(b, j):
            nonlocal cnt
            xn = a["xn"][cnt % 2]
            cnt += 1
            self.pre(b * TB + j, a, g, xn)
            return xn

        for j in range(TB):
            xn = stage_a(0, j)
            self.to_fm(xn, xT, j, a)
        for b in range(NB):
            for fc in range(8):
                pg = pgen[gi % 2]
                gi += 1
                for k in range(8):
                    p.op("pe", lambda e, k=k, fc=fc, pg=pg: e.matmul(
                        pg[:, 0:W], lhsT=a["wq"][k][:, fc * 128:(fc + 1) * 128], rhs=xT[:, k, :],
                        start=(k == 0), stop=(k == 7)),
                        reads=_toks([a["wq"][k], xT]), writes=_toks([pg]), sig=(k == 7))
                eng = "act" if fc % 2 == 0 else "dve"
                if eng == "act":
                    p.op("act", lambda e, fc=fc, pg=pg: e.copy(out=qT[:, fc, :], in_=pg[:, 0:W]),
                         reads=_toks([pg]), writes=_toks([qT]))
                else:
                    p.op("dve", lambda e, fc=fc, pg=pg: e.tensor_copy(out=qT[:, fc, :], in_=pg[:, 0:W]),
                         reads=_toks([pg]), writes=_toks([qT]))
            def pair_gen(j, hp):
                r = j % 2
                pf, pn, m_, rsm, ps_ = Pf2[hp][r], Pn2[hp][r], mx2[hp][r], rs2[hp][r], pss2[hp]
                for h2 in range(2):
                    hd = 2 * hp + h2
                    for dc in range(2):
                        p.op("pe", lambda e, hd=hd, h2=h2, dc=dc: e.matmul(
                            ps_[:, h2, :], lhsT=qT[:, 2 * hd + dc, j * 128:(j + 1) * 128], rhs=KT[:, 2 * hd + dc, :],
                            start=(dc == 0), stop=(dc == 1)),
                            reads=_toks([qT, KT]), writes=_toks([ps_]), sig=(h2 == 1 and dc == 1))
                yield
                p.op("dve", lambda e: e.tensor_reduce(out=m_[:, :], in_=ps_[:, :, :], axis=AX.X, op=ALU.max),
                     reads=_toks([ps_]), writes=_toks([m_]))
                yield
                p.op("dve", lambda e: e.tensor_scalar(out=m_[:, :], in0=m_[:, :], scalar1=-sc, scalar2=None, op0=ALU.mult),
                     reads=_toks([m_]), writes=_toks([m_]))
                yield
                for h2 in range(2):
                    p.op("act", lambda e, h2=h2: e.activation(
                        out=pf[:, h2, :], in_=ps_[:, h2, :], func=AF.Exp, scale=sc, bias=m_[:, h2:h2 + 1],
                        accum_out=rsm[:, h2:h2 + 1]),
                        reads=_toks([ps_, m_]), writes=_toks([pf, rsm]))
                yield
                p.op("dve", lambda e: e.reciprocal(out=rsm[:, :], in_=rsm[:, :]), reads=_toks([rsm]), writes=_toks([rsm]))
                yield
                p.op("dve", lambda e: e.tensor_tensor(
                    out=pn[:, :, :], in0=pf[:, :, :], in1=rsm[:, :].unsqueeze(2).to_broadcast([128, 2, NMEM]), op=ALU.mult),
                    reads=_toks([pf, rsm]), writes=_toks([pn]))
                yield
                pT = a["pT"][0]
                for q in range(4):
                    h2, mc = q // 2, q % 2
                    p.op("pe", lambda e, q=q, h2=h2, mc=mc: e.transpose(
                        out=pT[:, q, :], in_=pn[:, h2, mc * 128:(mc + 1) * 128], identity=self.ident[:, :]),
                        reads=_toks([pn, self.ident]), writes=_toks([pT]), sig=(q == 3))
                p.op("act", lambda e: e.copy(out=PT[:, 4 * hp:4 * hp + 4, j * 128:(j + 1) * 128], in_=pT[:, 0:4, :]),
                     reads=_toks([pT]), writes=_toks([PT]))
                yield

            for j in range(TB):
                self.run_gens([pair_gen(j, 0), pair_gen(j, 1)])
            for fc in range(8):
                hd = fc // 2
                pg = pgen[gi % 2]
                gi += 1
                for mc in range(2):
                    p.op("pe", lambda e, fc=fc, hd=hd, mc=mc, pg=pg: e.matmul(
                        pg[:, 0:W], lhsT=V[:, mc, fc * 128:(fc + 1) * 128], rhs=PT[:, 2 * hd + mc, :],
                        start=(mc == 0), stop=(mc == 1)),
                        reads=_toks([V, PT]), writes=_toks([pg]), sig=(mc == 1))
                if fc % 2 == 0:
                    p.op("act", lambda e, fc=fc, pg=pg: e.copy(out=oT[:, fc, :], in_=pg[:, 0:W]),
                         reads=_toks([pg]), writes=_toks([oT]))
                else:
                    p.op("dve", lambda e, fc=fc, pg=pg: e.tensor_copy(out=oT[:, fc, :], in_=pg[:, 0:W]),
                         reads=_toks([pg]), writes=_toks([oT]))
            for j in range(TB):
                po = a["po"][0]
                xn = stage_a(b + 1, j) if b + 1 < NB else None
                for n in range(2):
                    for fc in range(8):
                        p.op("pe", lambda e, fc=fc, n=n, j=j: e.matmul(
                            po[:, n * 512:(n + 1) * 512], lhsT=oT[:, fc, j * 128:(j + 1) * 128],
                            rhs=a["wo"][fc][:, n * 512:(n + 1) * 512], start=(fc == 0), stop=(fc == 7)),
                            reads=_toks([oT, a["wo"][fc]]), writes=_toks([po]), sig=(fc == 7 and n == 1))
                if xn is not None:
                    self.to_fm(xn, xT, j, a)
                self.post(po, g, 1.0, b * TB + j, a)

    def load_cols(self, rows, nchunk, ps, name):
        c, p = self.c, self.p
        nr = len(rows)
        out = c.sb([128, nchunk, nr], F32, name)
        with c.phase():
            rt = c.sb([nr, nchunk * 128], F32, name + "_r")
            for r, row in enumerate(rows):
                p.op("sp", lambda e, r=r, row=row: e.dma_start(out=rt[r:r + 1, :], in_=row[None, :]),
                     writes=_toks([rt]), dma=rt.tok)
            for cc in range(nchunk):
                p.op("pe", lambda e, cc=cc: e.transpose(out=ps[:, cc * nr:(cc + 1) * nr],
                                                        in_=rt[0:nr, cc * 128:(cc + 1) * 128],
                                                        identity=self.identf[0:nr, 0:nr]),
                     reads=_toks([rt, self.identf]), writes=_toks([ps]), sig=(cc == nchunk - 1))
            p.op("act", lambda e: e.copy(out=out[:, :, :].rearrange("p c r -> p (c r)"), in_=ps[:, 0:nchunk * nr]),
                 reads=_toks([ps]), writes=_toks([out]))
        return out

    def sigmoid_inplace(self, ap, t, neg_bias=None, extra_reads=()):
        p = self.p
        if neg_bias is None:
            p.op("act", lambda e: e.activation(out=ap, in_=ap, func=AF.Exp, scale=-1.0),
                 reads=_toks([t]), writes=_toks([t]))
        else:
            p.op("act", lambda e: e.activation(out=ap, in_=ap, func=AF.Exp, scale=-1.0, bias=neg_bias),
                 reads=_toks([t] + list(extra_reads)), writes=_toks([t]))
        p.op("act", lambda e: e.activation(out=ap, in_=ap, func=AF.Ln, bias=self.onec[:, 0:1]),
             reads=_toks([t, self.onec]), writes=_toks([t]))
        p.op("act", lambda e: e.activation(out=ap, in_=ap, func=AF.Exp, scale=-1.0),
             reads=_toks([t]), writes=_toks([t]))

    def conv_chunk(self, ps, cb, halo, cw, ncw, cc, W, dst_ap, dst_t, sgb, has_bias, first_block):
        p = self.p
        p.op("act", lambda e: e.copy(out=cb[:, 3:3 + W], in_=ps[:, 0:W]), reads=_toks([ps]), writes=_toks([cb]))
        p.op("dve", lambda e: e.tensor_copy(out=cb[:, 0:3], in_=halo[:, cc, :]), reads=_toks([halo]), writes=_toks([cb]))
        yield
        p.op("act", lambda e: e.copy(out=halo[:, cc, :], in_=cb[:, W:W + 3]), reads=_toks([cb]), writes=_toks([halo]))
        acc = sgb["acc"]
        sg = sgb["sg"]
        p.op("dve", lambda e: e.tensor_scalar(out=acc[:, 0:W], in0=cb[:, 3:3 + W], scalar1=cw[:, cc, 3:4], scalar2=None,
                                              op0=ALU.mult), reads=_toks([cb, cw]), writes=_toks([acc]))
        yield
        for j in (2, 1, 0):
            p.op("dve", lambda e, j=j: e.scalar_tensor_tensor(
                out=acc[:, 0:W], in0=cb[:, j:j + W], scalar=cw[:, cc, j:j + 1], in1=acc[:, 0:W],
                op0=ALU.mult, op1=ALU.add), reads=_toks([cb, cw, acc]), writes=_toks([acc]))
            yield
        if has_bias:
            p.op("act", lambda e: e.activation(out=sg[:, 0:W], in_=acc[:, 0:W], func=AF.Exp, scale=-1.0,
                                               bias=ncw[:, cc:cc + 1]),
                 reads=_toks([acc, ncw]), writes=_toks([sg]))
        else:
            p.op("act", lambda e: e.activation(out=sg[:, 0:W], in_=acc[:, 0:W], func=AF.Exp, scale=-1.0),
                 reads=_toks([acc]), writes=_toks([sg]))
        yield
        p.op("act", lambda e: e.activation(out=sg[:, 0:W], in_=sg[:, 0:W], func=AF.Ln, bias=self.onec[:, 0:1]),
             reads=_toks([sg, self.onec]), writes=_toks([sg]))
        yield
        p.op("act", lambda e: e.activation(out=sg[:, 0:W], in_=sg[:, 0:W], func=AF.Exp, scale=-1.0),
             reads=_toks([sg]), writes=_toks([sg]))
        yield
        if has_bias:
            p.op("dve", lambda e: e.scalar_tensor_tensor(
                out=dst_ap, in0=acc[:, 0:W], scalar=cw[:, cc, 4:5], in1=sg[:, 0:W], op0=ALU.add, op1=ALU.mult),
                reads=_toks([acc, cw, sg]), writes=_toks([dst_t]))
        else:
            p.op("dve", lambda e: e.tensor_tensor(out=dst_ap, in0=acc[:, 0:W], in1=sg[:, 0:W], op=ALU.mult),
                 reads=_toks([acc, sg]), writes=_toks([dst_t]))
        yield

    def fm_to_tm(self, src, src_sl, dsts, a, nch=8):
        p = self.p
        pT = a["pT"][0]
        for j, (dap, dt_) in enumerate(dsts):
            for q in range(nch):
                p.op("pe", lambda e, q=q, j=j: e.transpose(out=pT[:, q, :], in_=src[:, q, j * 128:(j + 1) * 128],
                                                           identity=self.ident[:, :]),
                     reads=_toks([src, self.ident]), writes=_toks([pT]), sig=(q == nch - 1))
            p.op("act", lambda e, dap=dap: e.copy(out=dap, in_=pT[:, 0:nch, :].rearrange("p c r -> p (c r)")),
                 reads=_toks([pT]), writes=_toks([dt_]))

    def ssd(self, layer):
        with self.c.phase():
            self._ssd(layer)

    def _ssd(self, layer):
        c, p, nc = self.c, self.p, self.nc
        jx = layer // 3
        NT = self.NT
        TB = 2 if NT % 2 == 0 else 1
        NB = NT // TB
        W = TB * 128
        CW = 6176
        a = {}
        a["win"] = [c.sb([128, CW], BF16, f"win{k}") for k in range(8)]
        a["g"] = c.sb([128, 1, D], F32, "g")
        a["h"] = [c.sb([128, D], F32, "h")] * 2
        a["xn"] = [c.sb([128, D], BF16, "xn")] * 2
        a["xT"] = [c.sb([128, 8, W], BF16, "xT")]
        a["junk"] = c.sb([128, D], BF16, "junk")
        a["ss"] = [c.sb([128, 1], F32, f"ss{i}") for i in range(4)]
        a["rs"] = [c.sb([128, 1], F32, f"rs{i}") for i in range(4)]
        a["pT"] = [c.ps([128, 8, 128], BF16, "pT")]
        pgen = [c.ps([128, 512], F32, f"pgen{i}") for i in range(2)]
        pcb = c.ps([128, 128], F32, "pcb")
        pz = c.ps([128, 256], F32, "pz")
        pgate = c.ps([128, 128], F32, "pgate")
        pdt = pgate.view((slice(None), slice(0, 32)), "pdt")
        pac = pgate.view((slice(None), slice(32, 96)), "pac")
        pD = c.ps([128, 4, 128], F32, "pD")
        pyy = c.ps([128, 512], F32, "pyy")
        py = pyy.view((slice(None), slice(0, 256)), "py")
        pyi = pyy.view((slice(None), slice(256, 512)), "pyi")
        for k in range(8):
            self.wload(a["win"][k], a["win"][k][:, :], self.ssd_w_in[jx, k * 128:(k + 1) * 128, :])
        self.load_g(a["g"], layer, 1)
        g = a["g"]
        cw = self.load_cols([self.ssd_conv_w[jx, t_, :] for t_ in range(4)] + [self.ssd_conv_b[jx, :]], 32, pgen[0], "cw")
        ncw = c.sb([128, 32], F32, "ncw")
        p.op("dve", lambda e: e.tensor_scalar(out=ncw[:, :], in0=cw[:, :, 4], scalar1=-1.0, scalar2=None, op0=ALU.mult),
             reads=_toks([cw]), writes=_toks([ncw]))
        dtb = c.sb([128, 32], F32, "dtb")
        aneg = c.sb([128, 32], F32, "aneg")
        dsk = c.sb([128, 32], F32, "dsk")
        ng = c.sb([128, 2 * D], F32, "ng")
        self.bcast_row(dtb, dtb[:, :], self.ssd_dt_bias[jx, :])
        self.bcast_row(aneg, aneg[:, :], self.ssd_a_log[jx, :])
        self.bcast_row(dsk, dsk[:, :], self.ssd_d[jx, :])
        self.bcast_row(ng, ng[:, :], self.ssd_norm_g[jx, :])
        p.op("act", lambda e: e.activation(out=aneg[:, :], in_=aneg[:, :], func=AF.Exp), reads=_toks([aneg]), writes=_toks([aneg]))
        p.op("dve", lambda e: e.tensor_scalar(out=aneg[:, :], in0=aneg[:, :], scalar1=-1.0, scalar2=None, op0=ALU.mult),
             reads=_toks([aneg]), writes=_toks([aneg]))
        halo = c.sb([128, 32, 3], F32, "halo")
        p.op("pool", lambda e: e.memset(halo[:, :, :], 0.0), writes=_toks([halo]))
        Z = [c.sb([128, 256], F32, f"Z{g_}") for g_ in range(8)]
        Zb = [c.sb([128, 256], BF16, f"Zb{g_}") for g_ in range(8)]
        for g_ in range(8):
            p.op("pool", lambda e, g_=g_: e.memset(Z[g_][:, :], 0.0), writes=_toks([Z[g_]]))
            p.op("pool", lambda e, g_=g_: e.memset(Zb[g_][:, :], 0.0), writes=_toks([Zb[g_]]))
        cb = [c.sb([128, W + 3], F32, f"cb{i}") for i in range(2)]
        sgb = [dict(acc=c.sb([128, W], F32, f"acc{i}"), sg=c.sb([128, W], F32, f"sgc{i}")) for i in range(2)]
        xfm = c.sb([128, 8, W], BF16, "xfm")
        BT = c.sb([128, 8, W], BF16, "BT")
        CT = c.sb([128, 8, W], BF16, "CT")
        xtm = [c.sb([128, 2 * D], BF16, f"xtm{j}") for j in range(TB)]
        Btm = [c.sb([128, D], BF16, f"Btm{j}") for j in range(TB)]
        dtl = c.sb([128, 32], F32, "dtl")
        dt_ = c.sb([128, 32], F32, "dt")
        adt = c.sb([128, 32], F32, "adt")
        acum = c.sb([128, 32], F32, "acum")
        eacum = c.sb([128, 32], F32, "eacum")
        elast = c.sb([128, 32], F32, "elast")
        toend = c.sb([128, 32], F32, "toend")
        xdt = c.sb([128, 32, 64], BF16, "xdt")
        xde = c.sb([128, 32, 64], BF16, "xde")
        cbm = c.sb([128, 128], F32, "cbm")
        LH = c.sb([128, 4, 128], F32, "LH")
        Ed = c.sb([128, 4, 128], F32, "Ed")
        Mt = c.sb([128, 4, 128], BF16, "Mt")
        ytmp = c.sb([128, 256], F32, "ytmp")
        y = c.sb([128, 2 * D], F32, "y")
        zs = c.sb([128, 512], F32, "zs")
        ss8 = c.sb([128, 8], F32, "ss8")
        yb = c.sb([128, 2 * D], BF16, "yb")
        gi = 0
        for b in range(NB):
            xT = a["xT"][0]
            for j in range(TB):
                self.pre(b * TB + j, a, g, a["xn"][0])
                self.to_fm(a["xn"][0], xT, j, a)
            def ssd_chunk(cc):
                pg = pgen[cc % 2]
                for k in range(8):
                    p.op("pe", lambda e, k=k: e.matmul(
                        pg[:, 0:W], lhsT=a["win"][k][:, 2048 + cc * 128:2048 + (cc + 1) * 128], rhs=xT[:, k, :],
                        start=(k == 0), stop=(k == 7)),
                        reads=_toks([a["win"][k], xT]), writes=_toks([pg]), sig=(k == 7))
                yield
                if cc < 16:
                    dst_t, dst_ap = xfm, xfm[:, cc % 8, :]
                elif cc < 24:
                    dst_t, dst_ap = BT, BT[:, cc - 16, :]
                else:
                    dst_t, dst_ap = CT, CT[:, cc - 24, :]
                yield from self.conv_chunk(pg, cb[cc % 2], halo, cw, ncw, cc, W, dst_ap, dst_t, sgb[cc % 2], True, b == 0)

            for cc in range(0, 32, 2):
                self.run_gens([ssd_chunk(cc), ssd_chunk(cc + 1)])
                if cc + 1 in (7, 15):
                    grp = (cc + 1) // 8
                    self.fm_to_tm(xfm, None, [(xtm[j][:, grp * D:(grp + 1) * D], xtm[j]) for j in range(TB)], a)
                if cc + 1 == 23:
                    self.fm_to_tm(BT, None, [(Btm[j][:, :], Btm[j]) for j in range(TB)], a)
            for j in range(TB):
                i = b * TB + j
                tsl = slice(j * 128, (j + 1) * 128)
                x3 = xtm[j][:, :].rearrange("p (h d) -> p h d", d=64)
                if i == 1:
                    self.dbg("xtm", xtm[j], xtm[j][:, :], [128, 2 * D], BF16)
                    self.dbg("Btm", Btm[j], Btm[j][:, :], [128, D], BF16)
                    self.dbg("CT", CT, CT[:, :, tsl], [128, 8, 128], BF16)
                for k in range(8):
                    p.op("pe", lambda e, k=k: e.matmul(pdt[:, :], lhsT=xT[:, k, tsl], rhs=a["win"][k][:, 6144:6176],
                                                       start=(k == 0), stop=(k == 7)),
                         reads=_toks([a["win"][k], xT]), writes=_toks([pdt]), sig=(k == 7))
                p.op("dve", lambda e: e.tensor_tensor(out=dtl[:, :], in0=pdt[:, :], in1=dtb[:, :], op=ALU.add),
                     reads=_toks([pdt, dtb]), writes=_toks([dtl]))
                p.op("act", lambda e: e.activation(out=dtl[:, :], in_=dtl[:, :], func=AF.Exp),
                     reads=_toks([dtl]), writes=_toks([dtl]))
                p.op("act", lambda e: e.activation(out=dt_[:, :], in_=dtl[:, :], func=AF.Ln, bias=self.onec[:, 0:1]),
                     reads=_toks([dtl, self.onec]), writes=_toks([dt_]))
                p.op("dve", lambda e: e.tensor_tensor(out=adt[:, :], in0=dt_[:, :], in1=aneg[:, :], op=ALU.mult),
                     reads=_toks([dt_, aneg]), writes=_toks([adt]))
                p.op("pe", lambda e: e.matmul(pac[:, 0:32], lhsT=self.triLE[:, :], rhs=adt[:, :], start=True, stop=True),
                     reads=_toks([self.triLE, adt]), writes=_toks([pac]))
                p.op("pe", lambda e: e.matmul(pac[:, 32:64], lhsT=self.onesf[:, :], rhs=adt[:, :], start=True, stop=True),
                     reads=_toks([self.onesf, adt]), writes=_toks([pac]))
                p.op("act", lambda e: e.copy(out=acum[:, :], in_=pac[:, 0:32]), reads=_toks([pac]), writes=_toks([acum]))
                p.op("act", lambda e: e.activation(out=eacum[:, :], in_=acum[:, :], func=AF.Exp),
                     reads=_toks([acum]), writes=_toks([eacum]))
                p.op("act", lambda e: e.activation(out=elast[:, :], in_=pac[:, 32:64], func=AF.Exp),
                     reads=_toks([pac]), writes=_toks([elast]))
                p.op("dve", lambda e: e.tensor_tensor(out=toend[:, :], in0=pac[:, 32:64], in1=acum[:, :], op=ALU.subtract),
                     reads=_toks([pac, acum]), writes=_toks([toend]))
                p.op("act", lambda e: e.activation(out=toend[:, :], in_=toend[:, :], func=AF.Exp),
                     reads=_toks([toend]), writes=_toks([toend]))
                p.op("dve", lambda e: e.tensor_tensor(out=toend[:, :], in0=toend[:, :], in1=dt_[:, :], op=ALU.mult),
                     reads=_toks([toend, dt_]), writes=_toks([toend]))
                p.op("dve", lambda e: e.tensor_tensor(
                    out=xdt[:, :, :], in0=x3, in1=dt_[:, :].unsqueeze(2).to_broadcast([128, 32, 64]), op=ALU.mult),
                    reads=_toks([xtm[j], dt_]), writes=_toks([xdt]))
                p.op("dve", lambda e: e.tensor_tensor(
                    out=xde[:, :, :], in0=x3, in1=toend[:, :].unsqueeze(2).to_broadcast([128, 32, 64]), op=ALU.mult),
                    reads=_toks([xtm[j], toend]), writes=_toks([xde]))
                for g_ in range(8):
                    hs = slice(4 * g_, 4 * g_ + 4)
                    p.op("pe", lambda e, g_=g_: e.matmul(pcb[:, :], lhsT=BT[:, g_, tsl], rhs=CT[:, g_, tsl],
                                                         start=True, stop=True),
                         reads=_toks([BT, CT]), writes=_toks([pcb]))
                    p.op("dve", lambda e: e.tensor_tensor(out=cbm[:, :], in0=pcb[:, :], in1=self.triLE[:, :], op=ALU.mult),
                         reads=_toks([pcb, self.triLE]), writes=_toks([cbm]))
                    p.op("dve", lambda e, hs=hs: e.tensor_tensor(
                        out=LH[:, :, :], in0=self.maskGT[:, :].unsqueeze(1).to_broadcast([128, 4, 128]),
                        in1=adt[:, hs].unsqueeze(2).to_broadcast([128, 4, 128]), op=ALU.mult),
                        reads=_toks([self.maskGT, adt]), writes=_toks([LH]))
                    for e_ in range(4):
                        p.op("pe", lambda e, e_=e_: e.matmul(pD[:, e_, :], lhsT=LH[:, e_, :], rhs=self.triLE[:, :],
                                                             start=True, stop=True),
                             reads=_toks([LH, self.triLE]), writes=_toks([pD]), sig=(e_ == 3))
                    p.op("act", lambda e: e.activation(out=Ed[:, :, :], in_=pD[:, :, :], func=AF.Exp),
                         reads=_toks([pD]), writes=_toks([Ed]))
                    p.op("dve", lambda e: e.tensor_tensor(
                        out=Mt[:, :, :], in0=Ed[:, :, :], in1=cbm[:, :].unsqueeze(1).to_broadcast([128, 4, 128]),
                        op=ALU.mult), reads=_toks([Ed, cbm]), writes=_toks([Mt]))
                    for e_ in range(4):
                        p.op("pe", lambda e, e_=e_, g_=g_: e.matmul(
                            py[:, e_ * 64:(e_ + 1) * 64], lhsT=Mt[:, e_, :], rhs=xdt[:, 4 * g_ + e_, :],
                            start=True, stop=True),
                            reads=_toks([Mt, xdt]), writes=_toks([py]), sig=(e_ == 3))
                    p.op("pe", lambda e, g_=g_: e.matmul(pyi[:, :], lhsT=CT[:, g_, tsl], rhs=Zb[g_][:, :],
                                                         start=True, stop=True),
                         reads=_toks([CT, Zb[g_]]), writes=_toks([pyi]))
                    p.op("dve", lambda e, hs=hs: e.tensor_tensor(
                        out=ytmp[:, :].rearrange("p (h d) -> p h d", d=64),
                        in0=pyi[:, :].rearrange("p (h d) -> p h d", d=64),
                        in1=eacum[:, hs].unsqueeze(2).to_broadcast([128, 4, 64]), op=ALU.mult),
                        reads=_toks([pyi, eacum]), writes=_toks([ytmp]))
                    p.op("dve", lambda e, g_=g_: e.tensor_tensor(out=y[:, g_ * 256:(g_ + 1) * 256], in0=ytmp[:, :],
                                                                 in1=py[:, :], op=ALU.add),
                         reads=_toks([ytmp, py]), writes=_toks([y]))
                    p.op("pe", lambda e, g_=g_: e.matmul(
                        pz[:, :], lhsT=Btm[j][:, g_ * 128:(g_ + 1) * 128],
                        rhs=xde[:, 4 * g_:4 * g_ + 4, :].rearrange("p h d -> p (h d)"), start=True, stop=True),
                        reads=_toks([Btm[j], xde]), writes=_toks([pz]))
                    p.op("pool", lambda e, g_=g_, hs=hs: e.tensor_tensor(
                        out=Z[g_][:, :].rearrange("p (h d) -> p h d", d=64),
                        in0=Z[g_][:, :].rearrange("p (h d) -> p h d", d=64),
                        in1=elast[:, hs].unsqueeze(2).to_broadcast([128, 4, 64]), op=ALU.mult),
                        reads=_toks([Z[g_], elast]), writes=_toks([Z[g_]]))
                    p.op("dve", lambda e, g_=g_: e.tensor_tensor(out=Z[g_][:, :], in0=Z[g_][:, :], in1=pz[:, :], op=ALU.add),
                         reads=_toks([Z[g_], pz]), writes=_toks([Z[g_]]))
                    p.op("act", lambda e, g_=g_: e.copy(out=Zb[g_][:, :], in_=Z[g_][:, :]),
                         reads=_toks([Z[g_]]), writes=_toks([Zb[g_]]))
                if i == 1:
                    self.dbg("dt", dt_, dt_[:, :], [128, 32])
                    self.dbg("acum", acum, acum[:, :], [128, 32])
                    self.dbg("toend", toend, toend[:, :], [128, 32])
                    self.dbg("y0", y, y[:, :], [128, 2 * D])
                yt3 = y[:, :].rearrange("p (h d) -> p h d", d=64)
                p.op("pool", lambda e: e.tensor_tensor(
                    out=xdt[:, :, :], in0=x3, in1=dsk[:, :].unsqueeze(2).to_broadcast([128, 32, 64]), op=ALU.mult),
                    reads=_toks([xtm[j], dsk]), writes=_toks([xdt]))
                p.op("pool", lambda e: e.tensor_tensor(out=yt3, in0=yt3, in1=xdt[:, :, :], op=ALU.add),
                     reads=_toks([y, xdt]), writes=_toks([y]))
                for n in range(4):
                    pg = pgen[gi % 2]
                    gi += 1
                    for k in range(8):
                        p.op("pe", lambda e, k=k, n=n, pg=pg: e.matmul(
                            pg[:, :], lhsT=xT[:, k, tsl], rhs=a["win"][k][:, n * 512:(n + 1) * 512],
                            start=(k == 0), stop=(k == 7)),
                            reads=_toks([a["win"][k], xT]), writes=_toks([pg]), sig=(k == 7))
                    p.op("act", lambda e, pg=pg: e.copy(out=zs[:, :], in_=pg[:, :]), reads=_toks([pg]), writes=_toks([zs]))
                    self.sigmoid_inplace(zs[:, :], zs)
                    p.op("dve", lambda e, pg=pg: e.tensor_tensor(out=zs[:, :], in0=zs[:, :], in1=pg[:, :], op=ALU.mult),
                         reads=_toks([zs, pg]), writes=_toks([zs]))
                    p.op("dve", lambda e, n=n: e.tensor_tensor(out=y[:, n * 512:(n + 1) * 512], in0=y[:, n * 512:(n + 1) * 512],
                                                                in1=zs[:, :], op=ALU.mult),
                         reads=_toks([zs, y]), writes=_toks([y]))
                for g_ in range(8):
                    p.op("act", lambda e, g_=g_: e.activation(
                        out=a["junk"][:, 0:256], in_=y[:, g_ * 256:(g_ + 1) * 256], func=AF.Square, scale=1.0 / 16.0,
                        accum_out=ss8[:, g_:g_ + 1]), reads=_toks([y]), writes=_toks([a["junk"], ss8]))
                self.rsqrt_small(ss8[:, :], ss8[:, :], [ss8], [ss8], EPS)
                p.op("dve", lambda e: e.tensor_tensor(
                    out=y[:, :].rearrange("p (g d) -> p g d", d=256), in0=y[:, :].rearrange("p (g d) -> p g d", d=256),
                    in1=ss8[:, :].unsqueeze(2).to_broadcast([128, 8, 256]), op=ALU.mult),
                    reads=_toks([y, ss8]), writes=_toks([y]))
                p.op("pool", lambda e: e.tensor_tensor(out=yb[:, :], in0=y[:, :], in1=ng[:, :], op=ALU.mult),
                     reads=_toks([y, ng]), writes=_toks([yb]))
                if i == 1:
                    self.dbg("yb", yb, yb[:, :], [128, 2 * D], BF16)
                p.op("sp", lambda e, i=i: e.dma_start(out=self.scr[i * 128:(i + 1) * 128, :], in_=yb[:, :]),
                     reads=_toks([yb]), writes=[self.ytok[i]], dma=yb.tok)

    def mix_out(self, layer, w_dram, kdim, zgate=None):
        with self.c.phase():
            self._mix_out(layer, w_dram, kdim, zgate)
        self.first = False

    def _mix_out(self, layer, w_dram, kdim, zgate):
        c, p = self.c, self.p
        nk = kdim // 128
        c.stg = [c.sb([128, 1024], F32, f"stg{i}") for i in range(4)]
        self._stg_i = 0
        a = {}
        if zgate is not None:
            a["h"] = [c.sb([128, D], F32, f"h{i}") for i in range(2)]
            a["xn"] = [c.sb([128, D], BF16, f"xn{i}") for i in range(2)]
            xT1 = c.sb([128, 8, 128], BF16, "xT1")
            wz = [c.sb([128, kdim], BF16, f"wz{k}") for k in range(8)]
            zs = [c.sb([128, 512], F32, f"zs{i}") for i in range(2)]
            pgz = [c.ps([128, 512], F32, f"pgz{i}") for i in range(2)]
            for k in range(8):
                self.wload(wz[k], wz[k][:, :], zgate[0][k * 128:(k + 1) * 128, zgate[1]:zgate[1] + kdim])
        a["wout"] = [c.sb([128, D], BF16, f"wout{k}") for k in range(nk)]
        a["g"] = c.sb([128, 2, D], F32, "g")
        a["hr"] = [c.sb([128, D], F32, f"hr{i}") for i in range(2)]
        a["junk"] = c.sb([128, D], BF16, "junk")
        a["ss"] = [c.sb([128, 1], F32, f"ss{i}") for i in range(4)]
        a["rs"] = [c.sb([128, 1], F32, f"rs{i}") for i in range(4)]
        a["t"] = [c.sb([128, D], F32, f"t{i}") for i in range(2)]
        a["pT"] = [c.ps([128, 8, 128], BF16, "pT")]
        a["po"] = [c.ps([128, D], F32, f"po{i}") for i in range(2)]
        yb = [c.sb([128, kdim], BF16, f"yb{i}") for i in range(2)]
        yT = [c.sb([128, nk, 128], BF16, f"yT{i}") for i in range(2)]
        for k in range(nk):
            self.wload(a["wout"][k], a["wout"][k][:, :], w_dram[k * 128:(k + 1) * 128, :])
        self.load_g(a["g"], layer, 1)
        g = a["g"]
        if zgate is not None:
            xT1s = [xT1, c.sb([128, 8, 128], BF16, "xT1b")]

        def tile_gen(i):
            r = i % 2
            y_, yT_ = yb[r], yT[r]
            p.op("sp", lambda e: e.dma_start(out=y_[:, :], in_=self.scr[i * 128:(i + 1) * 128, 0:kdim]),
                 reads=[self.ytok[i]], writes=_toks([y_]), dma=y_.tok)
            if zgate is not None:
                xn = a["xn"][r]
                self.pre(i, a, g, xn)
                yield
                self.to_fm(xn, xT1s[r], 0, a)
                yield
                pg, z_ = pgz[r], zs[r]
                for n in range(kdim // 512):
                    for k in range(8):
                        p.op("pe", lambda e, k=k, n=n: e.matmul(
                            pg[:, :], lhsT=xT1s[r][:, k, :], rhs=wz[k][:, n * 512:(n + 1) * 512], start=(k == 0), stop=(k == 7)),
                            reads=_toks([wz[k], xT1s[r]]), writes=_toks([pg]), sig=(k == 7))
                    yield
                    p.op("act", lambda e: e.activation(out=z_[:, :], in_=pg[:, :], func=AF.Exp, scale=-1.0),
                         reads=_toks([pg]), writes=_toks([z_]))
                    yield
                    p.op("act", lambda e: e.activation(out=z_[:, :], in_=z_[:, :], func=AF.Ln, bias=self.onec[:, 0:1]),
                         reads=_toks([z_, self.onec]), writes=_toks([z_]))
                    yield
                    p.op("act", lambda e: e.activation(out=z_[:, :], in_=z_[:, :], func=AF.Exp, scale=-1.0),
                         reads=_toks([z_]), writes=_toks([z_]))
                    yield
                    p.op("dve", lambda e: e.tensor_tensor(out=z_[:, :], in0=z_[:, :], in1=pg[:, :], op=ALU.mult),
                         reads=_toks([z_, pg]), writes=_toks([z_]))
                    yield
                    p.op("dve", lambda e, n=n: e.tensor_tensor(
                        out=y_[:, n * 512:(n + 1) * 512], in0=y_[:, n * 512:(n + 1) * 512], in1=z_[:, :], op=ALU.mult),
                        reads=_toks([z_, y_]), writes=_toks([y_]))
                    yield
            for hh in range(nk // 8):
                self.to_fm_part(y_, hh * 8, yT_, hh * 8, a)
                yield
            po = a["po"][r]
            for n in range(2):
                for fc in range(nk):
                    p.op("pe", lambda e, fc=fc, n=n: e.matmul(
                        po[:, n * 512:(n + 1) * 512], lhsT=yT_[:, fc, :], rhs=a["wout"][fc][:, n * 512:(n + 1) * 512],
                        start=(fc == 0), stop=(fc == nk - 1)),
                        reads=_toks([yT_, a["wout"][fc]]), writes=_toks([po]), sig=(fc == nk - 1 and n == 1))
                yield
            self.post(po, g, 1.0, i, a)
            yield

        for i in range(0, self.NT, 2):
            gens = [tile_gen(i)]
            if i + 1 < self.NT:
                gens.append(tile_gen(i + 1))
            self.run_gens(gens)

    def to_fm_part(self, src, c0, dst, d0, a, nk=8):
        p = self.p
        pT = a["pT"][0]
        for k in range(nk):
            p.op("pe", lambda e, k=k: e.transpose(out=pT[:, k, :], in_=src[:, (c0 + k) * 128:(c0 + k + 1) * 128],
                                                   identity=self.ident[:, :]),
                 reads=_toks([src, self.ident]), writes=_toks([pT]), sig=(k == nk - 1))
        p.op("act", lambda e: e.copy(out=dst[:, d0:d0 + nk, :], in_=pT[:, 0:nk, :]),
             reads=_toks([pT]), writes=_toks([dst]))

    def dn(self, layer):
        with self.c.phase():
            self._dn(layer)

    def tri_inv(self, N, NT, ib, pw, out):
        p = self.p
        idb = self.ident[:, :].unsqueeze(1).to_broadcast([128, 4, 128])

        def msk(dst, src, m, eng="dve"):
            p.op(eng, lambda e: e.tensor_tensor(out=dst[:, :, :], in0=src[:, :, :],
                                                in1=m[:, :].unsqueeze(1).to_broadcast([128, 4, 128]), op=ALU.mult),
                 reads=_toks([src, m]), writes=_toks([dst]))

        def mm4(ps, L, R):
            for e_ in range(4):
                p.op("pe", lambda e, e_=e_: e.matmul(ps[:, e_ * 128:(e_ + 1) * 128], lhsT=L[:, e_, :], rhs=R[:, e_, :],
                                                     start=True, stop=True),
                     reads=_toks([L, R]), writes=_toks([ps]), sig=(e_ == 3))

        def ev_copy(dst, ps, eng):
            if eng == "act":
                p.op("act", lambda e: e.copy(out=dst[:, :, :].rearrange("p a b -> p (a b)"), in_=ps[:, :]),
                     reads=_toks([ps]), writes=_toks([dst]))
            else:
                p.op("dve", lambda e: e.tensor_copy(out=dst[:, :, :].rearrange("p a b -> p (a b)"), in_=ps[:, :]),
                     reads=_toks([ps]), writes=_toks([dst]))

        def ev_comb(dst, base, ps, op):
            p.op("dve", lambda e: e.tensor_tensor(out=dst[:, :, :].rearrange("p a b -> p (a b)"),
                                                  in0=base[:, :, :].rearrange("p a b -> p (a b)"), in1=ps[:, :], op=op),
                 reads=_toks([base, ps]), writes=_toks([dst]))

        A, AT = ib["A"], ib["AT"]
        msk(A[0], N, self.mlev[0])
        msk(AT[0], NT, self.mlev[0], "pool")
        yield
        for lv in range(3):
            ps = pw()
            mm4(ps, AT[lv], A[lv])
            ps2 = pw()
            mm4(ps2, A[lv], AT[lv])
            yield
            ev_copy(A[lv + 1], ps, "act")
            ev_copy(AT[lv + 1], ps2, "act")
            yield
        X, XT = ib["X"], ib["XT"]
        cur = 0
        p.op("dve", lambda e: e.tensor_tensor(out=X[0][:, :, :], in0=idb, in1=A[0][:, :, :], op=ALU.subtract),
             reads=_toks([self.ident, A[0]]), writes=_toks([X[0]]))
        p.op("pool", lambda e: e.tensor_tensor(out=XT[0][:, :, :], in0=idb, in1=AT[0][:, :, :], op=ALU.subtract),
             reads=_toks([self.ident, AT[0]]), writes=_toks([XT[0]]))
        yield
        for lv in range(1, 4):
            nx = 1 - cur
            ps = pw()
            mm4(ps, XT[cur], A[lv])
            ps2 = pw()
            mm4(ps2, A[lv], XT[cur])
            yield
            ev_comb(X[nx], X[cur], ps, ALU.add)
            ev_comb(XT[nx], XT[cur], ps2, ALU.add)
            yield
            cur = nx
        O, OT, Y, W_ = ib["O"], ib["OT"], ib["Y"], ib["W"]
        for li in range(1, 4):
            last = (li == 3)
            msk(O, N, self.mlev[li])
            nx = 1 - cur
            if not last:
                msk(OT, NT, self.mlev[li], "pool")
                yield
                ps = pw()
                mm4(ps, OT, X[cur])
                ps2 = pw()
                mm4(ps2, O, XT[cur])
                yield
                ev_copy(Y, ps, "act")
                ev_copy(W_, ps2, "act")
                yield
                ps = pw()
                mm4(ps, XT[cur], Y)
                ps2 = pw()
                mm4(ps2, X[cur], W_)
                yield
                ev_comb(X[nx], X[cur], ps, ALU.subtract)
                ev_comb(XT[nx], XT[cur], ps2, ALU.subtract)
                yield
            else:
                yield
                ps2 = pw()
                mm4(ps2, O, XT[cur])
                yield
                ev_copy(W_, ps2, "act")
                yield
                ps2 = pw()
                mm4(ps2, X[cur], W_)
                yield
                ev_comb(XT[nx], XT[cur], ps2, ALU.subtract)
                yield
            cur = nx
        out[0] = XT[cur]

    @staticmethod
    def run_gens(gens):
        act = list(gens)
        while act:
            for g_ in list(act):
                try:
                    next(g_)
                except StopIteration:
                    act.remove(g_)

    def _dn(self, layer):
        c, p, nc = self.c, self.p, self.nc
        jx = layer // 3
        NT = self.NT
        TB = 2 if NT % 2 == 0 else 1
        NB = NT // TB
        W = TB * 128
        CWN = 4096 + 32
        a = {}
        a["win"] = [c.sb([128, CWN], BF16, f"win{k}") for k in range(8)]
        a["g"] = c.sb([128, 1, D], F32, "g")
        a["h"] = [c.sb([128, D], F32, "h")] * 2
        a["xn"] = [c.sb([128, D], BF16, "xn")] * 2
        a["xT"] = [c.sb([128, 8, W], BF16, "xT")]
        ob = c.sb([128, 2 * D], BF16, "ob")
        a["junk"] = T(ob.h[:, 0:D], "junk", tok=ob.tok)
        a["ss"] = [c.sb([128, 1], F32, f"ss{i}") for i in range(4)]
        a["rs"] = [c.sb([128, 1], F32, f"rs{i}") for i in range(4)]
        a["pT"] = [c.ps([128, 8, 128], BF16, "pT")]
        pgen = [c.ps([128, 512], F32, f"pgen{i}") for i in range(2)]
        pgate = c.ps([128, 64], F32, "pgate")
        pws = [c.ps([128, 512], F32, f"pw{i}") for i in range(4)]
        pwi = [0]

        def pw():
            pwi[0] += 1
            return (pws + pgen)[pwi[0] % 6]
        cw = self.load_cols([self.dn_conv_w[jx, t_, :] for t_ in range(4)], 32, pgen[0], "cw")
        o = c.sb([128, 2 * D], F32, "o")
        c.stg = [T(o.h[:, 0:1024], "stgA"), T(o.h[:, 1024:2048], "stgB")]
        self._stg_i = 0
        wv_ = self.wload_pieces([T(t_.h[:, 0:4096], "w") for t_ in a["win"]], self.dn_w_in[jx, :, 0:4096])
        for k in range(8):
            self.wload(a["win"][k], a["win"][k][:, 4096:CWN], self.dn_w_in[jx, k * 128:(k + 1) * 128, 6144:6176])
        self.load_g(a["g"], layer, 1)
        g = a["g"]
        dtb = c.sb([128, 16], F32, "dtb")
        aneg = c.sb([128, 16], F32, "aneg")
        ngb = c.sb([128, 128], F32, "ngb")
        self.bcast_row(dtb, dtb[:, :], self.dn_dt_bias[jx, :])
        self.bcast_row(aneg, aneg[:, :], self.dn_a_log[jx, :])
        self.bcast_row(ngb, ngb[:, :], self.dn_norm_g[jx, :])
        p.op("act", lambda e: e.activation(out=aneg[:, :], in_=aneg[:, :], func=AF.Exp), reads=_toks([aneg]), writes=_toks([aneg]))
        p.op("dve", lambda e: e.tensor_scalar(out=aneg[:, :], in0=aneg[:, :], scalar1=-1.0, scalar2=None, op0=ALU.mult),
             reads=_toks([aneg]), writes=_toks([aneg]))
        halo = c.sb([128, 32, 3], F32, "halo")
        p.op("pool", lambda e: e.memset(halo[:, :, :], 0.0), writes=_toks([halo]))
        Sf = [c.sb([128, 4, 128], F32, f"S{h}") for h in range(4)]
        Sb = [c.sb([128, 4, 128], BF16, f"Sb{h}") for h in range(4)]
        for h in range(4):
            p.op("pool", lambda e, h=h: e.memset(Sf[h][:, :, :], 0.0), writes=_toks([Sf[h]]))
            p.op("pool", lambda e, h=h: e.memset(Sb[h][:, :, :], 0.0), writes=_toks([Sb[h]]))
        cb = [c.sb([128, W + 3], F32, f"cb{i}") for i in range(2)]
        sgb = [dict(acc=c.sb([128, W], F32, f"acc{i}"), sg=c.sb([128, W], F32, f"sgc{i}")) for i in range(2)]
        craw = [c.sb([128, W], F32, f"craw{i}") for i in range(2)]
        sq = [c.sb([128, W], F32, f"sq{i}") for i in range(2)]
        rn = [c.sb([128, W], F32, f"rn{i}") for i in range(2)]
        qn = c.sb([128, 8, W], BF16, "qn")
        kn = c.sb([128, 8, W], BF16, "kn")
        vfm = c.sb([128, 8, W], BF16, "vfm")
        vtm = [c.sb([128, 2 * D], BF16, f"vtm{j}") for j in range(TB)]
        ktm = [c.sb([128, D], BF16, f"ktm{j}") for j in range(TB)]
        pbs = c.sb([128, 32], F32, "pbs")
        beta = c.sb([128, 16], F32, "beta")
        gg = c.sb([128, 16], F32, "gg")
        Gc = c.sb([128, 16], F32, "Gc")
        eG = c.sb([128, 16], F32, "eG")
        bg = c.sb([128, 16], F32, "bg")
        elast = c.sb([128, 16], F32, "elast")
        kes = c.sb([128, 16], F32, "kes")
        mk = lambda nm: c.sb([128, 4, 128], BF16, nm)
        mkf = lambda nm: c.sb([128, 4, 128], F32, nm)

        def mkres(r):
            return dict(LH=mkf(f"LH{r}"), Ed=mk(f"Ed{r}"), EdT=mk(f"EdT{r}"),
                        KKm=c.sb([128, 2, 128], BF16, f"KKm{r}"), QKm=c.sb([128, 2, 128], BF16, f"QKm{r}"),
                        N=mk(f"N{r}"), NT=mk(f"NT{r}"), QKT=mk(f"QKT{r}"),
                        ib=dict(A=[mk(f"A{i}_{r}") for i in range(4)], AT=[mk(f"AT{i}_{r}") for i in range(4)],
                                X=[mk(f"X0_{r}"), mk(f"X1_{r}")], XT=[mk(f"XT0_{r}"), mk(f"XT1_{r}")],
                                O=mk(f"O{r}"), OT=mk(f"OT{r}"), Y=mk(f"Y{r}"), W=mk(f"Wt{r}")),
                        kb=mk(f"kb{r}"), ke=mk(f"ke{r}"), vb=mk(f"vb{r}"), wk=mk(f"wk{r}"), vn=mk(f"vn{r}"),
                        ot=mk(f"ot{r}"))
        RES = [mkres(0), mkres(1)]
        ss16 = c.sb([128, 16], F32, "ss16")
        osq = ob
        gi = 0
        hc = 0
        for b in range(NB):
            xT = a["xT"][0]
            for j in range(TB):
                self.pre(b * TB + j, a, g, a["xn"][0])
                self.to_fm(a["xn"][0], xT, j, a)
            def dn_chunk(cc):
                pg = pgen[cc % 2]
                craw_, sq_, rn_ = craw[cc % 2], sq[cc % 2], rn[cc % 2]
                for k in range(8):
                    p.op("pe", lambda e, k=k: e.matmul(
                        pg[:, 0:W], lhsT=a["win"][k][:, cc * 128:(cc + 1) * 128], rhs=xT[:, k, :],
                        start=(k == 0), stop=(k == 7)),
                        reads=_toks([wv_(k, cc * 128), xT]), writes=_toks([pg]), sig=(k == 7))
                yield
                if cc < 16:
                    yield from self.conv_chunk(pg, cb[cc % 2], halo, cw, None, cc, W, craw_[:, :], craw_, sgb[cc % 2], False, b == 0)
                    p.op("pool", lambda e: e.tensor_tensor(out=sq_[:, :], in0=craw_[:, :], in1=craw_[:, :], op=ALU.mult),
                         reads=_toks([craw_]), writes=_toks([sq_]))
                    yield
                    pn = pws[cc % 2]
                    p.op("pe", lambda e: e.matmul(pn[:, 0:W], lhsT=self.onesf[:, :], rhs=sq_[:, :], start=True, stop=True),
                         reads=_toks([self.onesf, sq_]), writes=_toks([pn]))
                    yield
                    p.op("act", lambda e: e.activation(out=rn_[:, :], in_=pn[:, 0:W], func=AF.Ln, bias=self.epsc[:, 0:1]),
                         reads=_toks([pn, self.epsc]), writes=_toks([rn_]))
                    yield
                    p.op("act", lambda e: e.activation(out=rn_[:, :], in_=rn_[:, :], func=AF.Exp, scale=-0.5),
                         reads=_toks([rn_]), writes=_toks([rn_]))
                    yield
                    dstt = qn if cc < 8 else kn
                    scl = 128.0 ** -0.5 if cc < 8 else 1.0
                    p.op("dve", lambda e: e.scalar_tensor_tensor(
                        out=dstt[:, cc % 8, :], in0=craw_[:, :], scalar=scl, in1=rn_[:, :], op0=ALU.mult, op1=ALU.mult),
                        reads=_toks([craw_, rn_]), writes=_toks([dstt]))
                    yield
                else:
                    yield from self.conv_chunk(pg, cb[cc % 2], halo, cw, None, cc, W, vfm[:, cc % 8, :], vfm, sgb[cc % 2], False, b == 0)

            for cc in range(0, 32, 2):
                self.run_gens([dn_chunk(cc), dn_chunk(cc + 1)])
                if cc + 1 == 15:
                    self.fm_to_tm(kn, None, [(ktm[j][:, :], ktm[j]) for j in range(TB)], a)
                if cc + 1 in (23, 31):
                    grp = (cc + 1 - 16) // 8
                    self.fm_to_tm(vfm, None, [(vtm[j][:, grp * D:(grp + 1) * D], vtm[j]) for j in range(TB)], a)
            for j in range(TB):
                i = b * TB + j
                tsl = slice(j * 128, (j + 1) * 128)
                for k in range(8):
                    p.op("pe", lambda e, k=k: e.matmul(pgate[:, 0:32], lhsT=xT[:, k, tsl], rhs=a["win"][k][:, 4096:4128],
                                                       start=(k == 0), stop=(k == 7)),
                         reads=_toks([a["win"][k], xT]), writes=_toks([pgate]), sig=(k == 7))
                p.op("act", lambda e: e.copy(out=beta[:, :], in_=pgate[:, 0:16]), reads=_toks([pgate]), writes=_toks([beta]))
                self.sigmoid_inplace(beta[:, :], beta)
                p.op("dve", lambda e: e.tensor_tensor(out=gg[:, :], in0=pgate[:, 16:32], in1=dtb[:, :], op=ALU.add),
                     reads=_toks([pgate, dtb]), writes=_toks([gg]))
                p.op("act", lambda e: e.activation(out=gg[:, :], in_=gg[:, :], func=AF.Exp), reads=_toks([gg]), writes=_toks([gg]))
                p.op("act", lambda e: e.activation(out=gg[:, :], in_=gg[:, :], func=AF.Ln, bias=self.onec[:, 0:1]),
                     reads=_toks([gg, self.onec]), writes=_toks([gg]))
                p.op("dve", lambda e: e.tensor_tensor(out=gg[:, :], in0=gg[:, :], in1=aneg[:, :], op=ALU.mult),
                     reads=_toks([gg, aneg]), writes=_toks([gg]))
                p.op("pe", lambda e: e.matmul(pgate[:, 32:48], lhsT=self.triLE[:, :], rhs=gg[:, :], start=True, stop=True),
                     reads=_toks([self.triLE, gg]), writes=_toks([pgate]))
                p.op("pe", lambda e: e.matmul(pgate[:, 48:64], lhsT=self.onesf[:, :], rhs=gg[:, :], start=True, stop=True),
                     reads=_toks([self.onesf, gg]), writes=_toks([pgate]))
                p.op("act", lambda e: e.copy(out=Gc[:, :], in_=pgate[:, 32:48]), reads=_toks([pgate]), writes=_toks([Gc]))
                p.op("act", lambda e: e.activation(out=eG[:, :], in_=Gc[:, :], func=AF.Exp), reads=_toks([Gc]), writes=_toks([eG]))
                p.op("act", lambda e: e.activation(out=elast[:, :], in_=pgate[:, 48:64], func=AF.Exp),
                     reads=_toks([pgate]), writes=_toks([elast]))
                p.op("dve", lambda e: e.tensor_tensor(out=kes[:, :], in0=pgate[:, 48:64], in1=Gc[:, :], op=ALU.subtract),
                     reads=_toks([pgate, Gc]), writes=_toks([kes]))
                p.op("act", lambda e: e.activation(out=kes[:, :], in_=kes[:, :], func=AF.Exp), reads=_toks([kes]), writes=_toks([kes]))
                p.op("dve", lambda e: e.tensor_tensor(out=bg[:, :], in0=beta[:, :], in1=eG[:, :], op=ALU.mult),
                     reads=_toks([beta, eG]), writes=_toks([bg]))
                def batch_gen(bt, R, slot):
                    hs = slice(4 * bt, 4 * bt + 4)
                    banks = [pws[2 * slot], pws[2 * slot + 1], pgen[slot]]
                    bi = [0]

                    def pw():
                        bi[0] += 1
                        return banks[bi[0] % 3]
                    LH, Ed, EdT, KKm, QKm, Nn, NnT, QKT = (R[k_] for k_ in ("LH", "Ed", "EdT", "KKm", "QKm", "N", "NT", "QKT"))
                    p.op("pool", lambda e: e.tensor_tensor(
                        out=LH[:, :, :], in0=self.maskGT[:, :].unsqueeze(1).to_broadcast([128, 4, 128]),
                        in1=gg[:, hs].unsqueeze(2).to_broadcast([128, 4, 128]), op=ALU.mult),
                        reads=_toks([self.maskGT, gg]), writes=_toks([LH]))
                    yield
                    pD, pDT, pK = pw(), pw(), pw()
                    for e_ in range(4):
                        p.op("pe", lambda e, e_=e_: e.matmul(pD[:, e_ * 128:(e_ + 1) * 128], lhsT=self.triLE[:, :],
                                                             rhs=LH[:, e_, :], start=True, stop=True),
                             reads=_toks([LH, self.triLE]), writes=_toks([pD]), sig=(e_ == 3))
                    for e_ in range(4):
                        p.op("pe", lambda e, e_=e_: e.matmul(pDT[:, e_ * 128:(e_ + 1) * 128], lhsT=LH[:, e_, :],
                                                             rhs=self.triLE[:, :], start=True, stop=True),
                             reads=_toks([LH, self.triLE]), writes=_toks([pDT]), sig=(e_ == 3))
                    for q_ in range(2):
                        hq = 2 * bt + q_
                        p.op("pe", lambda e, q_=q_, hq=hq: e.matmul(
                            pK[:, q_ * 128:(q_ + 1) * 128], lhsT=kn[:, hq, tsl], rhs=kn[:, hq, tsl], start=True, stop=True),
                            reads=_toks([kn]), writes=_toks([pK]), sig=False)
                        p.op("pe", lambda e, q_=q_, hq=hq: e.matmul(
                            pK[:, 256 + q_ * 128:256 + (q_ + 1) * 128], lhsT=kn[:, hq, tsl], rhs=qn[:, hq, tsl],
                            start=True, stop=True),
                            reads=_toks([kn, qn]), writes=_toks([pK]), sig=(q_ == 1))
                    yield
                    p.op("act", lambda e: e.activation(out=Ed[:, :, :].rearrange("p a b -> p (a b)"), in_=pD[:, :],
                                                       func=AF.Exp), reads=_toks([pD]), writes=_toks([Ed]))
                    p.op("act", lambda e: e.activation(out=EdT[:, :, :].rearrange("p a b -> p (a b)"), in_=pDT[:, :],
                                                       func=AF.Exp), reads=_toks([pDT]), writes=_toks([EdT]))
                    p.op("dve", lambda e: e.tensor_tensor(
                        out=KKm[:, :, :], in0=pK[:, 0:256].rearrange("p (a b) -> p a b", b=128),
                        in1=self.maskGT[:, :].unsqueeze(1).to_broadcast([128, 2, 128]), op=ALU.mult),
                        reads=_toks([pK, self.maskGT]), writes=_toks([KKm]))
                    p.op("dve", lambda e: e.tensor_tensor(
                        out=QKm[:, :, :], in0=pK[:, 256:512].rearrange("p (a b) -> p a b", b=128),
                        in1=self.triLE[:, :].unsqueeze(1).to_broadcast([128, 2, 128]), op=ALU.mult),
                        reads=_toks([pK, self.triLE]), writes=_toks([QKm]))
                    yield
                    for e_ in range(4):
                        h = 4 * bt + e_
                        p.op("dve", lambda e, e_=e_, h=h: e.scalar_tensor_tensor(
                            out=Nn[:, e_, :], in0=Ed[:, e_, :], scalar=beta[:, h:h + 1], in1=KKm[:, e_ // 2, :],
                            op0=ALU.mult, op1=ALU.mult), reads=_toks([Ed, beta, KKm]), writes=_toks([Nn]))
                    p.op("pool", lambda e: e.tensor_tensor(
                        out=QKT[:, :, :].rearrange("p (q r) b -> p q r b", r=2),
                        in0=EdT[:, :, :].rearrange("p (q r) b -> p q r b", r=2),
                        in1=QKm[:, :, :].unsqueeze(2).to_broadcast([128, 2, 2, 128]), op=ALU.mult),
                        reads=_toks([EdT, QKm]), writes=_toks([QKT]))
                    yield
                    pT = a["pT"][0]
                    for e_ in range(4):
                        p.op("pe", lambda e, e_=e_: e.transpose(out=pT[:, e_, :], in_=Nn[:, e_, :], identity=self.ident[:, :]),
                             reads=_toks([Nn, self.ident]), writes=_toks([pT]), sig=(e_ == 3))
                    p.op("act", lambda e: e.copy(out=NnT[:, :, :], in_=pT[:, 0:4, :]), reads=_toks([pT]), writes=_toks([NnT]))
                    yield
                    uo = [None]
                    yield from self.tri_inv(Nn, NnT, R["ib"], pw, uo)
                    U = uo[0]
                    kb, ke, vb4, wk, vn, ot = R["kb"], R["ke"], R["vb"], R["wk"], R["vn"], R["ot"]
                    for e_ in range(4):
                        h = 4 * bt + e_
                        ksl = ktm[j][:, (h // 2) * 128:(h // 2 + 1) * 128]
                        p.op("act", lambda e, e_=e_, ksl=ksl, h=h: e.activation(out=kb[:, e_, :], in_=ksl, func=AF.Copy,
                                                                               scale=bg[:, h:h + 1]),
                             reads=_toks([ktm[j], bg]), writes=_toks([kb]))
                        p.op("act", lambda e, e_=e_, ksl=ksl, h=h: e.activation(out=ke[:, e_, :], in_=ksl, func=AF.Copy,
                                                                               scale=kes[:, h:h + 1]),
                             reads=_toks([ktm[j], kes]), writes=_toks([ke]))
                    p.op("pool", lambda e: e.tensor_tensor(
                        out=vb4[:, :, :], in0=vtm[j][:, 4 * bt * 128:(4 * bt + 4) * 128].rearrange("p (h d) -> p h d", d=128),
                        in1=beta[:, hs].unsqueeze(2).to_broadcast([128, 4, 128]), op=ALU.mult),
                        reads=_toks([vtm[j], beta]), writes=_toks([vb4]))
                    yield
                    ps1 = pw()
                    for e_ in range(4):
                        p.op("pe", lambda e, e_=e_: e.matmul(ps1[:, e_ * 128:(e_ + 1) * 128], lhsT=kb[:, e_, :], rhs=U[:, e_, :],
                                                             start=True, stop=True),
                             reads=_toks([kb, U]), writes=_toks([ps1]), sig=(e_ == 3))
                    yield
                    p.op("act", lambda e: e.activation(out=wk[:, :, :].rearrange("p a b -> p (a b)"), in_=ps1[:, :], func=AF.Copy,
                                                       scale=-1.0), reads=_toks([ps1]), writes=_toks([wk]))
                    yield
                    ps2 = pw()
                    for e_ in range(4):
                        p.op("pe", lambda e, e_=e_: e.matmul(ps2[:, e_ * 128:(e_ + 1) * 128], lhsT=U[:, e_, :], rhs=vb4[:, e_, :],
                                                             start=True, stop=False),
                             reads=_toks([vb4, U]), writes=_toks([ps2]), sig=False)
                        p.op("pe", lambda e, e_=e_: e.matmul(ps2[:, e_ * 128:(e_ + 1) * 128], lhsT=wk[:, e_, :], rhs=Sb[bt][:, e_, :],
                                                             start=False, stop=True),
                             reads=_toks([wk, Sb[bt]]), writes=_toks([ps2]), sig=(e_ == 3))
                    ps3 = pw()
                    for e_ in range(4):
                        hq = (4 * bt + e_) // 2
                        p.op("pe", lambda e, e_=e_, hq=hq: e.matmul(ps3[:, e_ * 128:(e_ + 1) * 128], lhsT=qn[:, hq, tsl],
                                                                    rhs=Sb[bt][:, e_, :], start=True, stop=True),
                             reads=_toks([qn, Sb[bt]]), writes=_toks([ps3]), sig=(e_ == 3))
                    yield
                    p.op("dve", lambda e: e.tensor_copy(out=vn[:, :, :].rearrange("p a b -> p (a b)"), in_=ps2[:, :]),
                         reads=_toks([ps2]), writes=_toks([vn]))
                    p.op("dve", lambda e: e.tensor_tensor(
                        out=ot[:, :, :], in0=ps3[:, :].rearrange("p (a b) -> p a b", b=128),
                        in1=eG[:, hs].unsqueeze(2).to_broadcast([128, 4, 128]), op=ALU.mult),
                        reads=_toks([ps3, eG]), writes=_toks([ot]))
                    yield
                    ps4, ps5 = pw(), pw()
                    for e_ in range(4):
                        p.op("pe", lambda e, e_=e_: e.matmul(ps4[:, e_ * 128:(e_ + 1) * 128], lhsT=QKT[:, e_, :], rhs=vn[:, e_, :],
                                                             start=True, stop=True),
                             reads=_toks([QKT, vn]), writes=_toks([ps4]), sig=(e_ == 3))
                    for e_ in range(4):
                        p.op("pe", lambda e, e_=e_: e.matmul(ps5[:, e_ * 128:(e_ + 1) * 128], lhsT=ke[:, e_, :], rhs=vn[:, e_, :],
                                                             start=True, stop=True),
                             reads=_toks([ke, vn]), writes=_toks([ps5]), sig=(e_ == 3))
                    yield
                    p.op("dve", lambda e: e.tensor_tensor(
                        out=o[:, 4 * bt * 128:(4 * bt + 4) * 128], in0=ot[:, :, :].rearrange("p a b -> p (a b)"), in1=ps4[:, :],
                        op=ALU.add), reads=_toks([ot, ps4]), writes=_toks([o]))
                    p.op("pool", lambda e: e.tensor_tensor(
                        out=Sf[bt][:, :, :], in0=Sf[bt][:, :, :], in1=elast[:, hs].unsqueeze(2).to_broadcast([128, 4, 128]),
                        op=ALU.mult), reads=_toks([Sf[bt], elast]), writes=_toks([Sf[bt]]))
                    yield
                    p.op("dve", lambda e: e.tensor_tensor(
                        out=Sf[bt][:, :, :].rearrange("p a b -> p (a b)"), in0=Sf[bt][:, :, :].rearrange("p a b -> p (a b)"),
                        in1=ps5[:, :], op=ALU.add), reads=_toks([Sf[bt], ps5]), writes=_toks([Sf[bt]]))
                    yield
                    p.op("act", lambda e: e.copy(out=Sb[bt][:, :, :], in_=Sf[bt][:, :, :]),
                         reads=_toks([Sf[bt]]), writes=_toks([Sb[bt]]))
                    yield

                import os as _os
                if _os.environ.get("NOINTER"):
                    for bt_ in range(4):
                        self.run_gens([batch_gen(bt_, RES[bt_ % 2], bt_ % 2)])
                else:
                    self.run_gens([batch_gen(0, RES[0], 0), batch_gen(1, RES[1], 1)])
                    self.run_gens([batch_gen(2, RES[0], 0), batch_gen(3, RES[1], 1)])
                if i == 1:
                    self.dbg("o", o, o[:, :], [128, 2 * D])
                p.op("pool", lambda e: e.tensor_tensor(out=osq[:, :], in0=o[:, :], in1=o[:, :], op=ALU.mult),
                     reads=_toks([o]), writes=_toks([osq]))
                p.op("dve", lambda e: e.tensor_reduce(out=ss16[:, :], in_=osq[:, :].rearrange("p (h d) -> p h d", d=128),
                                                      axis=AX.X, op=ALU.add), reads=_toks([osq]), writes=_toks([ss16]))
                p.op("dve", lambda e: e.tensor_scalar(out=ss16[:, :], in0=ss16[:, :], scalar1=1.0 / 128.0, scalar2=None,
                                                      op0=ALU.mult), reads=_toks([ss16]), writes=_toks([ss16]))
                self.rsqrt_small(ss16[:, :], ss16[:, :], [ss16], [ss16], EPS)
                p.op("dve", lambda e: e.tensor_tensor(
                    out=o[:, :].rearrange("p (h d) -> p h d", d=128), in0=o[:, :].rearrange("p (h d) -> p h d", d=128),
                    in1=ss16[:, :].unsqueeze(2).to_broadcast([128, 16, 128]), op=ALU.mult),
                    reads=_toks([o, ss16]), writes=_toks([o]))
                p.op("pool", lambda e: e.tensor_tensor(
                    out=ob[:, :].rearrange("p (h d) -> p h d", d=128), in0=o[:, :].rearrange("p (h d) -> p h d", d=128),
                    in1=ngb[:, :].unsqueeze(1).to_broadcast([128, 16, 128]), op=ALU.mult),
                    reads=_toks([o, ngb]), writes=_toks([ob]))
                p.op("sp", lambda e, i=i: e.dma_start(out=self.scr[i * 128:(i + 1) * 128, :], in_=ob[:, :]),
                     reads=_toks([ob]), writes=[self.ytok[i]], dma=ob.tok)

    def rw(self, layer):
        with self.c.phase():
            self._rw(layer)

    def _rw(self, layer):
        c, p, nc = self.c, self.p, self.nc
        jx = layer // 3
        NT = self.NT
        TB = 2 if NT % 2 == 0 else 1
        NB = NT // TB
        W = TB * 128
        a = {}
        wr = [c.sb([128, D], BF16, f"wr{k}") for k in range(8)]
        wk = [c.sb([128, D], BF16, f"wk{k}") for k in range(8)]
        wv = [c.sb([128, D], BF16, f"wv{k}") for k in range(8)]
        wl1t = c.sb([128, 8, 288], BF16, "wl1")
        wl1 = [wl1t.view((slice(None), k, slice(None))) for k in range(8)]
        w2t = c.sb([64, D], BF16, "w2t")
        a2t = c.sb([64, D], BF16, "a2t")
        g2a = c.sb([128, D], BF16, "g2a")
        g2b = c.sb([32, D], BF16, "g2b")
        a["g"] = c.sb([128, 1, D], F32, "g")
        a["h"] = [c.sb([128, D], F32, "h")] * 2
        a["xn"] = [c.sb([128, D], BF16, "xn")] * 2
        a["junk"] = c.sb([128, D], BF16, "junk")
        a["ss"] = [c.sb([128, 1], F32, f"ss{i}") for i in range(4)]
        a["rs"] = [c.sb([128, 1], F32, f"rs{i}") for i in range(4)]
        a["pT"] = [c.ps([128, 8, 128], BF16, "pT")]
        pgen = [c.ps([128, 512], F32, f"pgen{i}") for i in range(2)]
        prk = c.ps([128, 16], F32, "prk")
        pws = [c.ps([128, 512], F32, f"pw{i}") for i in range(4)]
        pwi = [0]

        def pw():
            pwi[0] += 1
            return pws[pwi[0] % 4]
        for k in range(8):
            ks = slice(k * 128, (k + 1) * 128)
            self.wload(wr[k], wr[k][:, :], self.rw_w_rkv[jx, 0, ks, :])
            self.wload(wk[k], wk[k][:, :], self.rw_w_rkv[jx, 1, ks, :])
            self.wload(wv[k], wv[k][:, :], self.rw_w_rkv[jx, 2, ks, :])
        for k in range(8):
            ks = slice(k * 128, (k + 1) * 128)
            self.wload(wl1[k], wl1[k][:, 0:64], self.rw_w1[jx, ks, :])
            self.wload(wl1[k], wl1[k][:, 64:128], self.rw_a1[jx, ks, :])
            self.wload(wl1[k], wl1[k][:, 128:288], self.rw_g1[jx, ks, :])
        self.wload(w2t, w2t[:, :], self.rw_w2[jx, :, :])
        self.wload(a2t, a2t[:, :], self.rw_a2[jx, :, :])
        self.wload(g2a, g2a[:, :], self.rw_g2[jx, 0:128, :])
        self.wload(g2b, g2b[:, :], self.rw_g2[jx, 128:160, :])
        self.load_g(a["g"], layer, 1)
        g = a["g"]
        cols = self.load_cols([self.rw_mu[jx, i_, :] for i_ in range(6)] +
                              [self.rw_w0[jx, :], self.rw_a0[jx, :], self.rw_k_k[jx, :], self.rw_k_a[jx, :],
                               self.rw_r_k[jx].rearrange("h d -> (h d)")], 8, pgen[0], "cols")
        ncol = c.sb([128, 8, 2], F32, "ncol")
        p.op("dve", lambda e: e.tensor_scalar(out=ncol[:, :, :], in0=cols[:, :, 6:8], scalar1=-1.0, scalar2=None, op0=ALU.mult),
             reads=_toks([cols]), writes=_toks([ncol]))
        lng = c.sb([128, D], F32, "lng")
        lnb = c.sb([128, D], F32, "lnb")
        self.bcast_row(lng, lng[:, :], self.rw_ln_g[jx, :])
        self.bcast_row(lnb, lnb[:, :], self.rw_ln_b[jx, :])
        ind = c.sb([128, 8, 16], BF16, "ind")
        p.op("pool", lambda e: e.memset(ind[:, :, :], 0.0), writes=_toks([ind]))
        for cc in range(8):
            for hp in range(2):
                p.op("pool", lambda e, cc=cc, hp=hp: e.memset(ind[hp * 64:(hp + 1) * 64, cc, 2 * cc + hp:2 * cc + hp + 1], 1.0),
                     reads=_toks([ind]), writes=_toks([ind]))
        Zc = [c.sb([128, 64], F32, f"Zc{i}") for i in range(8)]
        Zb = [c.sb([128, 64], BF16, f"Zb{i}") for i in range(8)]
        for i_ in range(8):
            p.op("pool", lambda e, i_=i_: e.memset(Zc[i_][:, :], 0.0), writes=_toks([Zc[i_]]))
            p.op("pool", lambda e, i_=i_: e.memset(Zb[i_][:, :], 0.0), writes=_toks([Zb[i_]]))
        xTh = c.sb([128, 8, W + 1], BF16, "xTh")
        p.op("pool", lambda e: e.memset(xTh[:, :, 0:1], 0.0), writes=_toks([xTh]))
        xx = c.sb([128, 8, W], BF16, "xx")
        xm = [c.sb([128, 8, W], BF16, f"xm{i}") for i in range(6)]
        h1 = c.sb([64, W], BF16, "h1")
        h2 = c.sb([64, W], BF16, "h2")
        h3a = c.sb([128, W], BF16, "h3a")
        h3b = c.sb([32, W], BF16, "h3b")
        h3f = c.sb([128, W], F32, "h3f")
        h1f = T(h3f.h[0:64, :], "h1f", tok=h3f.tok)
        fm = lambda nm: c.sb([128, W], F32, nm)
        r_c, k_c, lw, a_c, kkr, sqk, rn, kk, kmod, cl, eW, eWi = [fm(n) for n in
            ("r_c", "k_c", "lw", "a_c", "kkr", "sqk", "rn", "kk", "kmod", "cl", "eW", "eWi")]
        t1, bt_, eWm = sqk, kkr, rn
        rt_ = c.sb([128, 8, W], BF16, "rt")
        kt_ = c.sb([128, 8, W], BF16, "kt")
        at_ = c.sb([128, 8, W], BF16, "at")
        btl = c.sb([128, 8, W], BF16, "btl")
        rkr = c.sb([128, 8, W], BF16, "rkr")
        eWl = c.sb([128, 8, TB], F32, "eWl")
        ktm = [c.sb([128, D], BF16, f"ktm{j}") for j in range(TB)]
        btm = [c.sb([128, D], BF16, f"btm{j}") for j in range(TB)]
        vtm = [c.sb([128, D], BF16, f"vtm{j}") for j in range(TB)]
        gtm = [c.sb([128, D], BF16, f"gtm{j}") for j in range(TB)]
        rks = c.sb([128, 16], F32, "rks")
        mk = lambda nm: c.sb([128, 4, 128], BF16, nm)
        Nn, NnT, LakT, MrbT, MrkT = mk("N"), mk("NT"), mk("LakT"), mk("MrbT"), mk("MrkT")
        ib = dict(A=[mk(f"A{i}") for i in range(4)], AT=[mk(f"AT{i}") for i in range(4)],
                  X=[mk("X0"), mk("X1")], XT=[mk("XT0"), mk("XT1")], O=mk("O"), OT=mk("OT"), Y=mk("Y"), W=mk("Wt"))
        inner = [c.sb([128, 64], BF16, f"inner{i}") for i in range(2)]
        Pm = [c.sb([128, 128], BF16, f"Pm{i}") for i in range(2)]
        ztmp = c.sb([128, 64], F32, "ztmp")
        y = c.sb([128, D], F32, "y")
        yc = a["h"][0]
        s16 = c.sb([128, 16], F32, "s16")
        v16 = c.sb([128, 16], F32, "v16")
        ob = a["xn"][0]
        gi = 0
        ic = 0
        import os as _os
        _stop = _os.environ.get("RW_STOP")
        if _stop == "0":
            return
        for b in range(NB):
            for j in range(TB):
                self.pre(b * TB + j, a, g, a["xn"][0])
                self.to_fm(a["xn"][0], xTh, j, a, off=1)
            xc = xTh[:, :, 1:W + 1]
            p.op("dve", lambda e: e.tensor_tensor(out=xx[:, :, :], in0=xTh[:, :, 0:W], in1=xc, op=ALU.subtract),
                 reads=_toks([xTh]), writes=_toks([xx]))
            for i_ in range(6):
                eng = "dve"
                p.op(eng, lambda e, i_=i_: e.tensor_tensor(
                    out=xm[i_][:, :, :], in0=xx[:, :, :], in1=cols[:, :, i_:i_ + 1].to_broadcast([128, 8, W]), op=ALU.mult),
                    reads=_toks([xx, cols]), writes=_toks([xm[i_]]))
                p.op(eng, lambda e, i_=i_: e.tensor_tensor(out=xm[i_][:, :, :], in0=xm[i_][:, :, :], in1=xc, op=ALU.add),
                     reads=_toks([xm[i_], xTh]), writes=_toks([xm[i_]]))
            p.op("pool", lambda e: e.tensor_copy(out=xTh[:, :, 0:1], in_=xTh[:, :, W:W + 1]),
                 reads=_toks([xTh]), writes=_toks([xTh]))
            xr, xw_, xk, xv, xa, xg = xm
            ph = pw()
            for k in range(8):
                p.op("pe", lambda e, k=k, ph=ph: e.matmul(ph[0:64, 0:W], lhsT=wl1[k][:, 0:64], rhs=xw_[:, k, :],
                                                          start=(k == 0), stop=(k == 7)),
                     reads=_toks([wl1[k], xw_]), writes=_toks([ph]), sig=(k == 7))
            p.op("act", lambda e, ph=ph: e.activation(out=h1f[:, :], in_=ph[0:64, 0:W], func=AF.Exp, scale=-2.0),
                 reads=_toks([ph]), writes=_toks([h1f]))
            p.op("act", lambda e: e.activation(out=h1f[:, :], in_=h1f[:, :], func=AF.Ln, bias=self.onec[0:64, 0:1]),
                 reads=_toks([h1f, self.onec]), writes=_toks([h1f]))
            p.op("act", lambda e: e.activation(out=h1f[:, :], in_=h1f[:, :], func=AF.Exp, scale=-1.0),
                 reads=_toks([h1f]), writes=_toks([h1f]))
            p.op("dve", lambda e: e.tensor_scalar(out=h1[:, :], in0=h1f[:, :], scalar1=2.0, scalar2=-1.0, op0=ALU.mult, op1=ALU.add),
                 reads=_toks([h1f]), writes=_toks([h1]))
            ph = pw()
            for k in range(8):
                p.op("pe", lambda e, k=k, ph=ph: e.matmul(ph[0:64, 0:W], lhsT=wl1[k][:, 64:128], rhs=xa[:, k, :],
                                                          start=(k == 0), stop=(k == 7)),
                     reads=_toks([wl1[k], xa]), writes=_toks([ph]), sig=(k == 7))
            p.op("act", lambda e, ph=ph: e.copy(out=h2[:, :], in_=ph[0:64, 0:W]), reads=_toks([ph]), writes=_toks([h2]))
            for part, (c0_, c1_, rows, dst) in enumerate(((128, 256, 128, h3a), (256, 288, 32, h3b))):
                ph = pw()
                for k in range(8):
                    p.op("pe", lambda e, k=k, ph=ph, c0_=c0_, c1_=c1_, rows=rows: e.matmul(
                        ph[0:rows, 0:W], lhsT=wl1[k][:, c0_:c1_], rhs=xg[:, k, :], start=(k == 0), stop=(k == 7)),
                        reads=_toks([wl1[k], xg]), writes=_toks([ph]), sig=(k == 7))
                p.op("act", lambda e, ph=ph, rows=rows: e.activation(out=h3f[0:rows, :], in_=ph[0:rows, 0:W], func=AF.Exp, scale=-1.0),
                     reads=_toks([ph]), writes=_toks([h3f]))
                p.op("act", lambda e, rows=rows: e.activation(out=h3f[0:rows, :], in_=h3f[0:rows, :], func=AF.Ln,
                                                              bias=self.onec[0:rows, 0:1]),
                     reads=_toks([h3f, self.onec]), writes=_toks([h3f]))
                p.op("act", lambda e, rows=rows, dst=dst: e.activation(out=dst[:, :], in_=h3f[0:rows, :], func=AF.Exp, scale=-1.0),
                     reads=_toks([h3f]), writes=_toks([dst]))
            if _stop == "1":
                return
            for cc in range(8):
                cs = slice(cc * 128, (cc + 1) * 128)
                pr, pk = pgen[0], pgen[1]
                for k in range(8):
                    p.op("pe", lambda e, k=k, cs=cs: e.matmul(pr[:, 0:W], lhsT=wr[k][:, cs], rhs=xr[:, k, :],
                                                              start=(k == 0), stop=(k == 7)),
                         reads=_toks([wr[k], xr]), writes=_toks([pr]), sig=(k == 7))
                for k in range(8):
                    p.op("pe", lambda e, k=k, cs=cs: e.matmul(pk[:, 0:W], lhsT=wk[k][:, cs], rhs=xk[:, k, :],
                                                              start=(k == 0), stop=(k == 7)),
                         reads=_toks([wk[k], xk]), writes=_toks([pk]), sig=(k == 7))
                p.op("act", lambda e: e.copy(out=r_c[:, :], in_=pr[:, 0:W]), reads=_toks([pr]), writes=_toks([r_c]))
                p.op("act", lambda e: e.copy(out=k_c[:, :], in_=pk[:, 0:W]), reads=_toks([pk]), writes=_toks([k_c]))
                pl = pw()
                p.op("pe", lambda e, pl=pl, cs=cs: e.matmul(pl[:, 0:W], lhsT=w2t[0:64, cs], rhs=h1[0:64, :], start=True, stop=True),
                     reads=_toks([w2t, h1]), writes=_toks([pl]))
                p.op("pe", lambda e, pl=pl, cs=cs: e.matmul(pl[:, 256:256 + W], lhsT=a2t[0:64, cs], rhs=h2[0:64, :], start=True, stop=True),
                     reads=_toks([a2t, h2]), writes=_toks([pl]))
                for (dst, off_, ci) in ((lw, 0, 0), (a_c, 256, 1)):
                    p.op("act", lambda e, dst=dst, off_=off_, ci=ci, pl=pl, cc=cc: e.activation(
                        out=dst[:, :], in_=pl[:, off_:off_ + W], func=AF.Exp, scale=-1.0, bias=ncol[:, cc, ci:ci + 1]),
                        reads=_toks([pl, ncol]), writes=_toks([dst]))
                    p.op("act", lambda e, dst=dst: e.activation(out=dst[:, :], in_=dst[:, :], func=AF.Ln, bias=self.onec[:, 0:1]),
                         reads=_toks([dst, self.onec]), writes=_toks([dst]))
                    p.op("act", lambda e, dst=dst: e.activation(out=dst[:, :], in_=dst[:, :], func=AF.Exp, scale=-1.0),
                         reads=_toks([dst]), writes=_toks([dst]))
                p.op("dve", lambda e, cc=cc: e.tensor_scalar(out=kkr[:, :], in0=k_c[:, :], scalar1=cols[:, cc, 8:9], scalar2=None,
                                                             op0=ALU.mult), reads=_toks([k_c, cols]), writes=_toks([kkr]))
                p.op("act", lambda e: e.activation(out=sqk[:, :], in_=kkr[:, :], func=AF.Square),
                     reads=_toks([kkr]), writes=_toks([sqk]))
                pn = pw()
                p.op("pe", lambda e, pn=pn: e.matmul(pn[:, 0:W], lhsT=self.bd64f[:, :], rhs=sqk[:, :], start=True, stop=True),
                     reads=_toks([self.bd64f, sqk]), writes=_toks([pn]))
                p.op("act", lambda e, pn=pn: e.activation(out=rn[:, :], in_=pn[:, 0:W], func=AF.Ln, bias=self.epsc[:, 0:1]),
                     reads=_toks([pn, self.epsc]), writes=_toks([rn]))
                p.op("act", lambda e: e.activation(out=rn[:, :], in_=rn[:, :], func=AF.Exp, scale=-0.5),
                     reads=_toks([rn]), writes=_toks([rn]))
                p.op("dve", lambda e: e.tensor_tensor(out=kk[:, :], in0=kkr[:, :], in1=rn[:, :], op=ALU.mult),
                     reads=_toks([kkr, rn]), writes=_toks([kk]))
                p.op("dve", lambda e, cc=cc: e.tensor_scalar(out=t1[:, :], in0=a_c[:, :], scalar1=-1.0, scalar2=cols[:, cc, 9:10],
                                                             op0=ALU.add, op1=ALU.mult), reads=_toks([a_c, cols]), writes=_toks([t1]))
                p.op("dve", lambda e: e.scalar_tensor_tensor(out=kmod[:, :], in0=t1[:, :], scalar=1.0, in1=k_c[:, :],
                                                             op0=ALU.add, op1=ALU.mult), reads=_toks([t1, k_c]), writes=_toks([kmod]))
                for j in range(TB):
                    sl = slice(j * 128, (j + 1) * 128)
                    p.op("dve", lambda e, sl=sl: e.tensor_tensor_scan(out=cl[:, sl], data0=self.onesf[:, 0:128], data1=lw[:, sl],
                                                                      initial=0.0, op0=ALU.mult, op1=ALU.add),
                         reads=_toks([self.onesf, lw]), writes=_toks([cl]))
                p.op("act", lambda e: e.activation(out=eW[:, :], in_=cl[:, :], func=AF.Exp, scale=-0.6065306597126334),
                     reads=_toks([cl]), writes=_toks([eW]))
                p.op("act", lambda e: e.activation(out=eWi[:, :], in_=cl[:, :], func=AF.Exp, scale=0.6065306597126334),
                     reads=_toks([cl]), writes=_toks([eWi]))
                p.op("dve", lambda e: e.tensor_tensor(out=eWm[:, :], in0=cl[:, :], in1=lw[:, :], op=ALU.subtract),
                     reads=_toks([cl, lw]), writes=_toks([eWm]))
                p.op("act", lambda e: e.activation(out=eWm[:, :], in_=eWm[:, :], func=AF.Exp, scale=-0.6065306597126334),
                     reads=_toks([eWm]), writes=_toks([eWm]))
                p.op("dve", lambda e, cc=cc: e.tensor_tensor(out=rt_[:, cc, :], in0=r_c[:, :], in1=eW[:, :], op=ALU.mult),
                     reads=_toks([r_c, eW]), writes=_toks([rt_]))
                p.op("dve", lambda e, cc=cc: e.tensor_tensor(out=kt_[:, cc, :], in0=kmod[:, :], in1=eWi[:, :], op=ALU.mult),
                     reads=_toks([kmod, eWi]), writes=_toks([kt_]))
                p.op("pool", lambda e: e.tensor_tensor(out=bt_[:, :], in0=kk[:, :], in1=a_c[:, :], op=ALU.mult),
                     reads=_toks([kk, a_c]), writes=_toks([bt_]))
                p.op("dve", lambda e, cc=cc: e.tensor_tensor(out=btl[:, cc, :], in0=bt_[:, :], in1=eWi[:, :], op=ALU.mult),
                     reads=_toks([bt_, eWi]), writes=_toks([btl]))
                p.op("dve", lambda e, cc=cc: e.scalar_tensor_tensor(out=at_[:, cc, :], in0=kk[:, :], scalar=-1.0, in1=eWm[:, :],
                                                                    op0=ALU.mult, op1=ALU.mult),
                     reads=_toks([kk, eWm]), writes=_toks([at_]))
                p.op("dve", lambda e, cc=cc: e.scalar_tensor_tensor(out=rkr[:, cc, :], in0=r_c[:, :], scalar=cols[:, cc, 10:11],
                                                                    in1=kmod[:, :], op0=ALU.mult, op1=ALU.mult),
                     reads=_toks([r_c, cols, kmod]), writes=_toks([rkr]))
                for j in range(TB):
                    p.op("act", lambda e, cc=cc, j=j: e.copy(out=eWl[:, cc, j:j + 1], in_=eW[:, j * 128 + 127:j * 128 + 128]),
                         reads=_toks([eW]), writes=_toks([eWl]))
            if _stop == "2":
                return
            self.fm_to_tm(kt_, None, [(ktm[j][:, :], ktm[j]) for j in range(TB)], a)
            self.fm_to_tm(btl, None, [(btm[j][:, :], btm[j]) for j in range(TB)], a)
            for j in range(TB):
                i = b * TB + j
                tsl = slice(j * 128, (j + 1) * 128)
                for n in range(2):
                    pg = pgen[gi % 2]
                    gi += 1
                    for k in range(8):
                        p.op("pe", lambda e, k=k, n=n, pg=pg: e.matmul(pg[:, :], lhsT=xv[:, k, tsl], rhs=wv[k][:, n * 512:(n + 1) * 512],
                                                                       start=(k == 0), stop=(k == 7)),
                             reads=_toks([wv[k], xv]), writes=_toks([pg]), sig=(k == 7))
                    p.op("act", lambda e, n=n, pg=pg: e.copy(out=vtm[j][:, n * 512:(n + 1) * 512], in_=pg[:, :]),
                         reads=_toks([pg]), writes=_toks([vtm[j]]))
                for n in range(2):
                    pg = pgen[gi % 2]
                    gi += 1
                    p.op("pe", lambda e, n=n, pg=pg: e.matmul(pg[:, :], lhsT=h3a[:, tsl], rhs=g2a[:, n * 512:(n + 1) * 512],
                                                              start=True, stop=False),
                         reads=_toks([h3a, g2a]), writes=_toks([pg]), sig=False)
                    p.op("pe", lambda e, n=n, pg=pg: e.matmul(pg[:, :], lhsT=h3b[0:32, tsl], rhs=g2b[0:32, n * 512:(n + 1) * 512],
                                                              start=False, stop=True),
                         reads=_toks([h3b, g2b]), writes=_toks([pg]))
                    p.op("dve", lambda e, n=n, pg=pg: e.tensor_copy(out=gtm[j][:, n * 512:(n + 1) * 512], in_=pg[:, :]),
                         reads=_toks([pg]), writes=_toks([gtm[j]]))
                for cc in range(8):
                    p.op("pe", lambda e, cc=cc: e.matmul(prk[:, :], lhsT=rkr[:, cc, tsl], rhs=ind[:, cc, :],
                                                         start=(cc == 0), stop=(cc == 7)),
                         reads=_toks([rkr, ind]), writes=_toks([prk]), sig=(cc == 7))
                p.op("act", lambda e: e.copy(out=rks[:, :], in_=prk[:, :]), reads=_toks([prk]), writes=_toks([rks]))
                if _stop == "3":
                    return
                for bt in range(4):
                    xxv = xx[:, :, :].rearrange("p a w -> p (a w)").rearrange("p (t e c) -> p t e c", t=4, e=4)
                    msk_t = {}
                    for ti, src_ in enumerate((at_, btl, kt_, rt_)):
                        for e_ in range(4):
                            h = 4 * bt + e_
                            p.op("act", lambda e, ti=ti, e_=e_, h=h, src_=src_: e.activation(
                                out=xxv[:, ti, e_, :], in_=src_[:, h // 2, tsl], func=AF.Copy, scale=self.hm[:, h % 2:h % 2 + 1]),
                                reads=_toks([src_, self.hm]), writes=_toks([xx]))
                        msk_t[id(src_)] = ti

                    def gram(L, R, dst, mask, neg):
                        ps = pw()
                        ti = msk_t[id(L)]
                        for e_ in range(4):
                            h = 4 * bt + e_
                            cc = h // 2
                            p.op("pe", lambda e, e_=e_, cc=cc, ps=ps, ti=ti: e.matmul(
                                ps[:, e_ * 128:(e_ + 1) * 128], lhsT=xxv[:, ti, e_, :], rhs=R[:, cc, tsl], start=True, stop=True),
                                reads=_toks([xx, R]), writes=_toks([ps]), sig=(e_ == 3))
                        mb = mask[:, :].unsqueeze(1).to_broadcast([128, 4, 128])
                        psv = ps[:, :].rearrange("p (a b) -> p a b", b=128)
                        if neg:
                            p.op("dve", lambda e: e.scalar_tensor_tensor(out=dst[:, :, :], in0=psv, scalar=-1.0, in1=mb,
                                                                         op0=ALU.mult, op1=ALU.mult),
                                 reads=_toks([ps, mask]), writes=_toks([dst]))
                        else:
                            p.op("dve", lambda e: e.tensor_tensor(out=dst[:, :, :], in0=psv, in1=mb, op=ALU.mult),
                                 reads=_toks([ps, mask]), writes=_toks([dst]))
                    gram(at_, btl, Nn, self.maskGT, True)
                    gram(btl, at_, NnT, self.maskLT, True)
                    gram(kt_, at_, LakT, self.maskLT, False)
                    gram(btl, rt_, MrbT, self.triLE, False)
                    gram(kt_, rt_, MrkT, self.triLE, False)
                    uo = [None]
                    for _ in self.tri_inv(Nn, NnT, ib, pw, uo):
                        pass
                    U = uo[0]
                    if _stop == "4":
                        return
                    for q_ in range(2):
                        cc = 2 * bt + q_
                        Pm_ = Pm[ic % 2]
                        ic += 1
                        for hp in range(2):
                            e_ = 2 * q_ + hp
                            h = 2 * cc + hp
                            rs_ = slice(hp * 64, hp * 64 + 64)
                            hsl = slice(h * 64, (h + 1) * 64)
                            in_ = inner[hp]
                            ps1 = pw()
                            p.op("pe", lambda e, ps1=ps1, cc=cc, e_=e_: e.matmul(ps1[:, 0:64], lhsT=xxv[:, 0, e_, :], rhs=Zb[cc][:, :],
                                                                                 start=True, stop=False),
                                 reads=_toks([xx, Zb[cc]]), writes=_toks([ps1]), sig=False)
                            p.op("pe", lambda e, ps1=ps1, e_=e_, hsl=hsl: e.matmul(ps1[:, 0:64], lhsT=LakT[:, e_, :], rhs=vtm[j][:, hsl],
                                                                                   start=False, stop=True),
                                 reads=_toks([LakT, vtm[j]]), writes=_toks([ps1]))
                            p.op("act", lambda e, ps1=ps1, in_=in_: e.copy(out=in_[:, :], in_=ps1[:, 0:64]),
                                 reads=_toks([ps1]), writes=_toks([in_]))
                            p.op("pe", lambda e, ps1=ps1, e_=e_, in_=in_: e.matmul(ps1[:, 64:128], lhsT=U[:, e_, :], rhs=in_[:, :],
                                                                                   start=True, stop=True),
                                 reads=_toks([U, in_]), writes=_toks([ps1]))
                            p.op("dve", lambda e, ps1=ps1, Pm_=Pm_, rs_=rs_: e.tensor_copy(out=Pm_[:, rs_], in_=ps1[:, 64:128]),
                                 reads=_toks([ps1]), writes=_toks([Pm_]))
                            ps2 = pw()
                            p.op("pe", lambda e, ps2=ps2, cc=cc, e_=e_: e.matmul(ps2[:, 0:64], lhsT=xxv[:, 3, e_, :], rhs=Zb[cc][:, :],
                                                                                 start=True, stop=False),
                                 reads=_toks([xx, Zb[cc]]), writes=_toks([ps2]), sig=False)
                            p.op("pe", lambda e, ps2=ps2, e_=e_, Pm_=Pm_, rs_=rs_: e.matmul(ps2[:, 0:64], lhsT=MrbT[:, e_, :], rhs=Pm_[:, rs_],
                                                                                            start=False, stop=False),
                                 reads=_toks([MrbT, Pm_]), writes=_toks([ps2]), sig=False)
                            p.op("pe", lambda e, ps2=ps2, e_=e_, hsl=hsl: e.matmul(ps2[:, 0:64], lhsT=MrkT[:, e_, :], rhs=vtm[j][:, hsl],
                                                                                   start=False, stop=True),
                                 reads=_toks([MrkT, vtm[j]]), writes=_toks([ps2]))
                            p.op("act", lambda e, ps2=ps2, hsl=hsl: e.copy(out=y[:, hsl], in_=ps2[:, 0:64]),
                                 reads=_toks([ps2]), writes=_toks([y]))
                        csl = slice(cc * 128, (cc + 1) * 128)
                        ps3 = pw()
                        p.op("pe", lambda e, ps3=ps3, csl=csl, Pm_=Pm_: e.matmul(ps3[:, 0:128], lhsT=btm[j][:, csl], rhs=Pm_[:, :],
                                                                                 start=True, stop=False),
                             reads=_toks([btm[j], Pm_]), writes=_toks([ps3]), sig=False)
                        p.op("pe", lambda e, ps3=ps3, csl=csl: e.matmul(ps3[:, 0:128], lhsT=ktm[j][:, csl], rhs=vtm[j][:, csl],
                                                                        start=False, stop=True),
                             reads=_toks([ktm[j], vtm[j]]), writes=_toks([ps3]))
                        for hp in range(2):
                            rs_ = slice(hp * 64, hp * 64 + 64)
                            p.op("dve", lambda e, ps3=ps3, cc=cc, rs_=rs_, hp=hp: e.tensor_tensor(
                                out=ztmp[rs_, :], in0=Zc[cc][rs_, :], in1=ps3[rs_, hp * 64:(hp + 1) * 64], op=ALU.add),
                                reads=_toks([Zc[cc], ps3]), writes=_toks([ztmp]))
                            p.op("dve", lambda e, cc=cc, rs_=rs_: e.tensor_scalar(
                                out=Zc[cc][rs_, :], in0=ztmp[rs_, :], scalar1=eWl[rs_, cc, j:j + 1], scalar2=None, op0=ALU.mult),
                                reads=_toks([ztmp, eWl]), writes=_toks([Zc[cc]]))
                        p.op("act", lambda e, cc=cc: e.copy(out=Zb[cc][:, :], in_=Zc[cc][:, :]),
                             reads=_toks([Zc[cc]]), writes=_toks([Zb[cc]]))
                if _stop == "5":
                    return
                y3 = y[:, :].rearrange("p (h d) -> p h d", d=64)
                yc3 = yc[:, :].rearrange("p (h d) -> p h d", d=64)
                p.op("dve", lambda e: e.tensor_reduce(out=s16[:, :], in_=y3, axis=AX.X, op=ALU.add), reads=_toks([y]), writes=_toks([s16]))
                p.op("dve", lambda e: e.tensor_scalar(out=s16[:, :], in0=s16[:, :], scalar1=1.0 / 64.0, scalar2=None, op0=ALU.mult),
                     reads=_toks([s16]), writes=_toks([s16]))
                p.op("dve", lambda e: e.tensor_tensor(out=y3, in0=y3, in1=s16[:, :].unsqueeze(2).to_broadcast([128, 16, 64]),
                                                      op=ALU.subtract), reads=_toks([y, s16]), writes=_toks([y]))
                p.op("pool", lambda e: e.tensor_tensor(out=yc[:, :], in0=y[:, :], in1=y[:, :], op=ALU.mult),
                     reads=_toks([y]), writes=_toks([yc]))
                p.op("dve", lambda e: e.tensor_reduce(out=v16[:, :], in_=yc3, axis=AX.X, op=ALU.add), reads=_toks([yc]), writes=_toks([v16]))
                p.op("dve", lambda e: e.tensor_scalar(out=v16[:, :], in0=v16[:, :], scalar1=1.0 / 64.0, scalar2=None, op0=ALU.mult),
                     reads=_toks([v16]), writes=_toks([v16]))
                self.rsqrt_small(v16[:, :], v16[:, :], [v16], [v16], 64e-5)
                p.op("dve", lambda e: e.tensor_tensor(out=y3, in0=y3, in1=v16[:, :].unsqueeze(2).to_broadcast([128, 16, 64]),
                                                      op=ALU.mult), reads=_toks([y, v16]), writes=_toks([y]))
                p.op("pool", lambda e: e.tensor_tensor(out=y[:, :], in0=y[:, :], in1=lng[:, :], op=ALU.mult),
                     reads=_toks([y, lng]), writes=_toks([y]))
                p.op("pool", lambda e: e.tensor_tensor(out=y[:, :], in0=y[:, :], in1=lnb[:, :], op=ALU.add),
                     reads=_toks([y, lnb]), writes=_toks([y]))
                p.op("dve", lambda e: e.tensor_tensor(out=yc3, in0=vtm[j][:, :].rearrange("p (h d) -> p h d", d=64),
                                                      in1=rks[:, :].unsqueeze(2).to_broadcast([128, 16, 64]), op=ALU.mult),
                     reads=_toks([vtm[j], rks]), writes=_toks([yc]))
                p.op("pool", lambda e: e.tensor_tensor(out=y[:, :], in0=y[:, :], in1=yc[:, :], op=ALU.add),
                     reads=_toks([y, yc]), writes=_toks([y]))
                p.op("dve", lambda e: e.tensor_tensor(out=ob[:, :], in0=y[:, :], in1=gtm[j][:, :], op=ALU.mult),
                     reads=_toks([y, gtm[j]]), writes=_toks([ob]))
                p.op("sp", lambda e, i=i: e.dma_start(out=self.scr[i * 128:(i + 1) * 128, 0:D], in_=ob[:, :]),
                     reads=_toks([ob]), writes=[self.ytok[i]], dma=ob.tok)

    def build(self):
        for (kind, layer, arg) in self.sublayers:
            if kind == "ffn":
                self.ffn(layer, arg)
            elif kind == "xa":
                self.xattn(layer)
            elif kind == "dn":
                self.dn(layer)
                self.mix_out(layer, self.dn_w_out[layer // 3], 2 * D, zgate=(self.dn_w_in[layer // 3], 4096))
            elif kind == "rw":
                self.rw(layer)
                self.mix_out(layer, self.rw_w_out[layer // 3], D)
            elif kind == "ssd":
                self.ssd(layer)
                self.mix_out(layer, self.ssd_w_out[layer // 3], 2 * D)
            else:
                raise ValueError(kind)
        self.p.finish(self.htok + self.dbg_toks)
        return self.nc


FULL = []
for _l in range(4):
    FULL.append(("ffn", _l, 0))
    FULL.append((("dn", "ssd", "rw")[_l % 3], _l, 0))
    FULL.append(("xa", _l, 0))
    FULL.append(("ffn", _l, 1))


def kernel(**inputs):
    x = np.asarray(inputs["x"], dtype=np.float32)
    mem = np.asarray(inputs["mem"], dtype=np.float32)
    B, S, _ = x.shape
    nc = Builder(S // 128, FULL).build()
    in_maps = [make_in_map(inputs, x[b], mem[b]) for b in range(B)]
    res = run_bass_kernel_spmd(nc, in_maps, core_ids=list(range(B)))
    return np.stack([np.asarray(r["out"], dtype=np.float32) for r in res.results], axis=0)
```

```python
import numpy as np
from contextlib import ExitStack
import concourse.bass as bass
import concourse.mybir as mybir
from concourse.bass_utils import run_bass_kernel_spmd

F32 = mybir.dt.float32
BF16 = mybir.dt.bfloat16
AF = mybir.ActivationFunctionType
ALU = mybir.AluOpType
AX = mybir.AxisListType

D = 1024
DFF = 2816
NMEM = 256
EPS = 1e-6


class Tok:
    __slots__ = ("w", "r", "name")

    def __init__(self, name=""):
        self.w = None
        self.r = {}
        self.name = name


class Prog:
    def __init__(self, nc):
        self.nc = nc
        self.eng = dict(pe=nc.tensor, act=nc.scalar, dve=nc.vector, pool=nc.gpsimd, sp=nc.sync)
        self.sems = {}
        self.cnt = {}
        self.seen = {e: {} for e in self.eng}
        self.nsem = 0
        self.nins = {e: 0 for e in self.eng}
        self.dmap = {}
        self.dfree = []
        self.epoch = 0
        import os as _os
        for i_ in range(int(_os.environ.get("SEMSHIFT", "0"))):
            self.nc.alloc_semaphore(name=f"dummy{i_}")
        for e in ("pe", "act", "dve", "pool"):
            self._sem(e)

    def _sem(self, key):
        if key not in self.sems:
            self.sems[key] = self.nc.alloc_semaphore(name=f"s{self.nsem}")
            self.cnt[key] = 0
            self.nsem += 1
        return self.sems[key]

    def _wait(self, e, deps):
        eng = self.eng[e]
        for key, val in deps.items():
            if e == "pe" and key == "pe":
                continue
            if self.seen[e].get(key, 0) < val:
                eng.wait_ge(self.sems[key], val)
                self.seen[e][key] = val
                self.nins[e] += 1

    def _add(self, deps, tick):
        if tick is None or tick[2] < self.epoch:
            return
        k, v = tick[0], tick[1]
        if deps.get(k, 0) < v:
            deps[k] = v

    def op(self, e, fn, reads=(), writes=(), sig=True, dma=None):
        deps = {}
        for t in reads:
            self._add(deps, t.w)
        for t in writes:
            self._add(deps, t.w)
            for k, (v, ep) in t.r.items():
                self._add(deps, (k, v, ep))
        if dma is not None:
            key = self._dkey(dma)
            if self.cnt[key] > 0:
                self._add(deps, (key, self.cnt[key], self.epoch))
            inc = 16
        else:
            key = e
            inc = 1
        self._wait(e, deps)
        ins = fn(self.eng[e])
        self.nins[e] += 1
        if sig or dma is not None:
            ins.then_inc(self.sems[key], inc)
            self.cnt[key] += inc
            tick = (key, self.cnt[key], self.epoch)
        else:
            tick = (key, self.cnt[key] + 1, self.epoch)
        for t in reads:
            old = t.r.get(tick[0])
            if old is None or old[1] < self.epoch or old[0] < tick[1]:
                t.r[tick[0]] = (tick[1], self.epoch)
        for t in writes:
            t.w = tick
            t.r = {}
        return ins

    def _dkey(self, tok):
        k = self.dmap.get(id(tok))
        if k is None:
            if self.dfree:
                k = self.dfree.pop()
            else:
                k = ("d", self.nsem)
                self._sem(k)
            self.dmap[id(tok)] = k
        return k

    def release(self, toks):
        keys = []
        for t in toks:
            k = self.dmap.pop(id(t), None)
            if k is not None:
                keys.append(k)
        if not keys:
            return
        marks = {}
        for e in ("pe", "act", "dve", "sp"):
            key = e if e != "sp" else "spn"
            self._sem(key)
            self.eng[e].nop().then_inc(self.sems[key], 1)
            self.cnt[key] += 1
            marks[key] = self.cnt[key]
        self._wait_all("pool", marks)
        for k in keys:
            self.eng["pool"].sem_clear(self.sems[k])
            self.cnt[k] = 0
            for e in self.eng:
                self.seen[e].pop(k, None)
            self.dfree.append(k)
        self.eng["pool"].nop().then_inc(self.sems["pool"], 1)
        self.cnt["pool"] += 1
        for e in ("pe", "act", "dve", "sp"):
            self._wait_all(e, {"pool": self.cnt["pool"]})

    def barrier(self):
        deps = {k: v for k, v in self.cnt.items() if v > 0}
        for e in self.eng:
            d = dict(deps)
            self._wait_all(e, d)
        self.epoch += 1

    def _wait_all(self, e, deps):
        eng = self.eng[e]
        for key, val in deps.items():
            if self.seen[e].get(key, 0) < val:
                eng.wait_ge(self.sems[key], val)
                self.seen[e][key] = val
                self.nins[e] += 1

    def finish(self, toks):
        deps = {}
        for t in toks:
            self._add(deps, t.w)
        self._wait("sp", deps)


class T:
    def __init__(self, h, name, tok=None):
        self.h = h
        self.tok = tok or Tok(name)

    def view(self, key, name=None):
        return T(self.h[key], name, tok=self.tok)

    def __getitem__(self, k):
        return self.h[k]


class Ctx:
    def __init__(self, nc):
        self.nc = nc
        self.p = Prog(nc)
        self.n = 0
        self.es = None
        self.ptoks = []

    def phase(self):
        return _Phase(self)

    def sb(self, shape, dt, name=None):
        self.n += 1
        nm = f"{name or 'sb'}_{self.n}"
        if self.es is None:
            h = self.nc.alloc_sbuf_tensor(nm, list(shape), dt)
        else:
            h = self.es.enter_context(self.nc.sbuf_tensor(nm, list(shape), dt))
        t = T(h, name)
        if self.es is not None:
            self.ptoks.append(t.tok)
        return t

    def ps(self, shape, dt, name=None):
        self.n += 1
        nm = f"{name or 'ps'}_{self.n}"
        if self.es is None:
            h = self.nc.alloc_psum_tensor(nm, list(shape), dt)
        else:
            h = self.es.enter_context(self.nc.psum_tensor(nm, list(shape), dt))
        t = T(h, name)
        if self.es is not None:
            self.ptoks.append(t.tok)
        return t


class _Phase:
    def __init__(self, c):
        self.c = c

    def __enter__(self):
        self.saved = (self.c.es, self.c.ptoks, getattr(self.c, "stg", None))
        self.c.stg = None
        self.c.es = ExitStack()
        self.c.es.__enter__()
        self.c.ptoks = []
        return self

    def __exit__(self, *a):
        self.c.p.barrier()
        self.c.p.release(self.c.ptoks)
        self.c.es.__exit__(None, None, None)
        self.c.es, self.c.ptoks, self.c.stg = self.saved
        return False


def _toks(xs):
    return [x.tok if isinstance(x, T) else x for x in xs]


PARAM_SHAPES = {
    "sandwich_g": [4, 4, 2, 1024], "ffn_w_in": [4, 2, 1024, 5632], "ffn_w_out": [4, 2, 2816, 1024],
    "mem_norm_g": [4, 1024], "xa_w_q": [4, 1024, 1024], "xa_w_kv": [4, 1024, 2048], "xa_w_o": [4, 1024, 1024],
    "dn_w_in": [2, 1024, 6176], "dn_conv_w": [2, 4, 4096], "dn_a_log": [2, 16], "dn_dt_bias": [2, 16],
    "dn_norm_g": [2, 128], "dn_w_out": [2, 2048, 1024],
    "ssd_w_in": [1, 1024, 6176], "ssd_conv_w": [1, 4, 4096], "ssd_conv_b": [1, 4096], "ssd_a_log": [1, 32],
    "ssd_dt_bias": [1, 32], "ssd_d": [1, 32], "ssd_norm_g": [1, 2048], "ssd_w_out": [1, 2048, 1024],
    "rw_mu": [1, 6, 1024], "rw_w_rkv": [1, 3, 1024, 1024], "rw_w0": [1, 1024], "rw_w1": [1, 1024, 64],
    "rw_w2": [1, 64, 1024], "rw_a0": [1, 1024], "rw_a1": [1, 1024, 64], "rw_a2": [1, 64, 1024],
    "rw_g1": [1, 1024, 160], "rw_g2": [1, 160, 1024], "rw_k_k": [1, 1024], "rw_k_a": [1, 1024],
    "rw_r_k": [1, 16, 64], "rw_ln_g": [1, 1024], "rw_ln_b": [1, 1024], "rw_w_out": [1, 1024, 1024],
}


def make_in_map(params, x, mem):
    m = {k: np.ascontiguousarray(np.asarray(params[k], dtype=np.float32)) for k in PARAM_SHAPES}
    m["x"] = np.ascontiguousarray(x, dtype=np.float32)
    m["mem"] = np.ascontiguousarray(mem, dtype=np.float32)
    return m


class Builder:
    def __init__(self, NT, sublayers, n_layers=4, debug=False):
        self.NT = NT
        self.S = NT * 128
        self.sublayers = sublayers
        nc = bass.Bass("TRN2", target_bir_lowering=False)
        self.nc = nc
        self.c = Ctx(nc)
        self.p = self.c.p
        S = self.S
        dt = nc.dram_tensor
        self.x = dt("x", [S, D], F32, kind="ExternalInput").ap()
        self.mem = dt("mem", [NMEM, D], F32, kind="ExternalInput").ap()
        self.out = dt("out", [S, D], F32, kind="ExternalOutput").ap()
        L = n_layers
        self.sandwich_g = dt("sandwich_g", [L, 4, 2, D], F32, kind="ExternalInput").ap()
        self.ffn_w_in = dt("ffn_w_in", [L, 2, D, 2 * DFF], F32, kind="ExternalInput").ap()
        self.ffn_w_out = dt("ffn_w_out", [L, 2, DFF, D], F32, kind="ExternalInput").ap()
        def inp(name, shape):
            return dt(name, list(shape), F32, kind="ExternalInput").ap()
        self.mem_norm_g = inp("mem_norm_g", [L, D])
        self.xa_w_q = inp("xa_w_q", [L, D, D])
        self.xa_w_kv = inp("xa_w_kv", [L, D, 2 * D])
        self.xa_w_o = inp("xa_w_o", [L, D, D])
        for nm, shp in PARAM_SHAPES.items():
            if not hasattr(self, nm):
                setattr(self, nm, inp(nm, shp))
        self.htok = [Tok(f"h{i}") for i in range(NT)]
        self.scr = dt("scr", [S, 2 * D], BF16, kind="Internal").ap()
        self.ytok = [Tok(f"y{i}") for i in range(NT)]
        self.first = True
        self.debug = debug
        self.dbg_names = set()
        self.dbg_toks = []
        self._consts()

    def _consts(self):
        c, p, nc = self.c, self.p, self.nc
        self.ident = c.sb([128, 128], BF16, "ident")
        self.identf = c.sb([128, 128], F32, "identf")
        p.op("pool", lambda e: e.memset(self.identf[:, :], 0.0), writes=_toks([self.identf]))
        p.op("pool", lambda e: e.affine_select(
            out=self.identf[:, :], in_=self.identf[:, :], pattern=[[-1, 128]],
            compare_op=ALU.not_equal, fill=1.0, base=0, channel_multiplier=1),
            reads=_toks([self.identf]), writes=_toks([self.identf]))
        p.op("dve", lambda e: e.tensor_copy(out=self.ident[:, :], in_=self.identf[:, :]),
             reads=_toks([self.identf]), writes=_toks([self.ident]))
        self.epsc = c.sb([128, 1], F32, "eps")
        p.op("dve", lambda e: e.memset(self.epsc[:, :], EPS), writes=_toks([self.epsc]))
        def msk(name, pattern_step, chmul, cmp):
            t = c.sb([128, 128], F32, name)
            p.op("pool", lambda e: e.memset(t[:, :], 1.0), writes=_toks([t]))
            p.op("pool", lambda e: e.affine_select(
                out=t[:, :], in_=t[:, :], pattern=[[pattern_step, 128]], compare_op=cmp, fill=0.0, base=0,
                channel_multiplier=chmul), reads=_toks([t]), writes=_toks([t]))
            return t
        self.triLE = msk("triLE", 1, -1, ALU.is_ge)
        self.maskGT = msk("maskGT", -1, 1, ALU.is_gt)
        self.maskLT = msk("maskLT", 1, -1, ALU.is_gt)
        def bd(name, sz):
            t = c.sb([128, 128], F32, name)
            v = t[:, :].rearrange("p (a b) -> p a b", b=sz)
            p.op("pool", lambda e: e.memset(t[:, :], 1.0), writes=_toks([t]))
            p.op("pool", lambda e: e.affine_select(out=v, in_=v, pattern=[[-sz, 128 // sz], [0, sz]],
                                                   compare_op=ALU.is_ge, fill=0.0, base=0, channel_multiplier=1),
                 reads=_toks([t]), writes=_toks([t]))
            p.op("pool", lambda e: e.affine_select(out=v, in_=v, pattern=[[sz, 128 // sz], [0, sz]],
                                                   compare_op=ALU.is_ge, fill=0.0, base=sz - 1, channel_multiplier=-1),
                 reads=_toks([t]), writes=_toks([t]))
            return t
        b16, b32, b64 = bd("bd16", 16), bd("bd32", 32), bd("bd64", 64)
        self.bd64f = b64
        self.onesf = c.sb([128, 128], F32, "onesf")
        p.op("pool", lambda e: e.memset(self.onesf[:, :], 1.0), writes=_toks([self.onesf]))
        self.mlev = []
        for nm, hi, lo in (("mb16", b16, None), ("mo32", b32, b16), ("mo64", b64, b32), ("mo128", self.onesf, b64)):
            t = c.sb([128, 128], BF16, nm)
            if lo is None:
                p.op("pool", lambda e, t=t, hi=hi: e.tensor_copy(out=t[:, :], in_=hi[:, :]),
                     reads=_toks([hi]), writes=_toks([t]))
            else:
                p.op("pool", lambda e, t=t, hi=hi, lo=lo: e.tensor_tensor(out=t[:, :], in0=hi[:, :], in1=lo[:, :],
                                                                         op=ALU.subtract),
                     reads=_toks([hi, lo]), writes=_toks([t]))
            self.mlev.append(t)
        self.onesf2 = self.onesf
        p.op("pool", lambda e: e.memset(self.onesf[:, :], 1.0), writes=_toks([self.onesf]))
        self.hm = c.sb([128, 2], F32, "hm")
        p.op("pool", lambda e: e.memset(self.hm[:, :], 0.0), writes=_toks([self.hm]))
        p.op("pool", lambda e: e.memset(self.hm[0:64, 0:1], 1.0), reads=_toks([self.hm]), writes=_toks([self.hm]))
        p.op("pool", lambda e: e.memset(self.hm[64:128, 1:2], 1.0), reads=_toks([self.hm]), writes=_toks([self.hm]))
        self.onec = c.sb([128, 1], F32, "onec")
        p.op("dve", lambda e: e.memset(self.onec[:, :], 1.0), writes=_toks([self.onec]))
        self.epsc2 = c.sb([128, 1], F32, "eps2")
        p.op("dve", lambda e: e.memset(self.epsc2[:, :], 64e-5), writes=_toks([self.epsc2]))

    def dbg(self, name, t, ap, shape, dtype=F32):
        if not getattr(self, "debug", False) or name in self.dbg_names:
            return
        self.dbg_names.add(name)
        d = self.nc.dram_tensor("dbg_" + name, list(shape), dtype, kind="ExternalOutput").ap()
        tk = Tok()
        self.p.op("sp", lambda e: e.dma_start(out=d, in_=ap), reads=_toks([t]), writes=[tk], dma=tk)
        self.dbg_toks.append(tk)

    def hsrc(self, i):
        src = self.x if self.first else self.out
        return src[i * 128:(i + 1) * 128, :]

    def load_g(self, gt, layer, sub, wgt=1.0):
        p = self.p
        for j in range(gt.h.shape[1]):
            src = self.sandwich_g[layer, sub, j, :].partition_broadcast(128)
            p.op("sp", lambda e, src=src, j=j: e.dma_start(out=gt[:, j, :], in_=src),
                 writes=_toks([gt]), dma=gt.tok)
        if wgt != 1.0 and gt.h.shape[1] > 1:
            p.op("dve", lambda e: e.tensor_scalar(out=gt[:, 1, :], in0=gt[:, 1, :], scalar1=float(wgt), scalar2=None,
                                                  op0=ALU.mult),
                 reads=_toks([gt]), writes=_toks([gt]))

    def rstd(self, src_ap, src_toks, junk, ss, rs, n=D, eps=EPS):
        p = self.p
        p.op("act", lambda e: e.activation(out=junk[:, :n], in_=src_ap, func=AF.Square,
                                           scale=float(n) ** -0.5, accum_out=ss[:, 0:1]),
             reads=_toks(src_toks), writes=_toks([junk, ss]))
        self.rsqrt_small(ss[:, 0:1], rs[:, 0:1], [ss], [rs], eps)

    def rsqrt_small(self, src, dst, rt, wt, eps):
        p = self.p
        b = self.epsc[:, 0:1] if eps == EPS else self.epsc2[:, 0:1]
        p.op("act", lambda e: e.activation(out=dst, in_=src, func=AF.Ln, bias=b),
             reads=_toks(rt + [self.epsc]), writes=_toks(wt))
        p.op("act", lambda e: e.activation(out=dst, in_=dst, func=AF.Exp, scale=-0.5),
             reads=_toks(wt), writes=_toks(wt))

    def ffn(self, layer, which):
        with self.c.phase():
            self._ffn(layer, which)
        self.first = False

    def _ffn(self, layer, which):
        c, p, nc = self.c, self.p, self.nc
        NT = self.NT
        sub = 0 if which == 0 else 3
        TB = 4 if NT % 4 == 0 else 1
        NB = NT // TB
        W = TB * 128
        NH = DFF // 128
        a = {}
        a["win"] = [c.sb([128, 2 * DFF], BF16, f"win{k}") for k in range(8)]
        a["wout"] = [c.sb([128, D], BF16, f"wout{k}") for k in range(NH)]
        a["g"] = c.sb([128, 2, D], F32, "g")
        a["h"] = [c.sb([128, D], F32, f"h{i}") for i in range(2)]
        a["hr"] = [c.sb([128, D], F32, f"hr{i}") for i in range(2)]
        a["xn"] = [c.sb([128, D], BF16, f"xn{i}") for i in range(2)]
        a["xT"] = [c.sb([128, 8, W], BF16, f"xT{i}") for i in range(1)]
        a["hT"] = [c.sb([128, NH, W], BF16, f"hT{i}") for i in range(1)]
        a["sg"] = [c.sb([128, W], F32, f"sg{i}") for i in range(2)]
        a["junk"] = c.sb([128, D], BF16, "junk")
        a["ss"] = [c.sb([128, 1], F32, f"ss{i}") for i in range(4)]
        a["rs"] = [c.sb([128, 1], F32, f"rs{i}") for i in range(4)]
        a["ssb"] = [c.sb([128, 1], F32, f"ssb{i}") for i in range(2)]
        self._pk = 0
        a["t"] = [c.sb([128, D], F32, "t")] * 2
        a["pT"] = [c.ps([128, 8, 128], BF16, f"pT{i}") for i in range(1)]
        a["pg"] = [c.ps([128, W], F32, f"pg{i}") for i in range(2)]
        a["pu"] = [c.ps([128, W], F32, f"pu{i}") for i in range(2)]
        a["po"] = [c.ps([128, 512], F32, f"po{i}") for i in range(3)]
        self.load_g(a["g"], layer, sub, 0.5)
        g = a["g"]
        cnt = 0
        xT = a["xT"][0]
        hT = a["hT"][0]

        def stage_a(b, j):
            nonlocal cnt
            xn = a["xn"][cnt % 2]
            cnt += 1
            self.pre(b * TB + j, a, g, xn)
            return xn

        for j in range(TB):
            xn = stage_a(0, j)
            self.to_fm(xn, xT, j, a)
        c.stg = a["hr"] + [a["t"][0]]
        self._stg_i = 0
        win_src = self.ffn_w_in[layer, which]
        wout_src = self.ffn_w_out[layer, which]
        pend = []
        wv_ = self.wload_pieces(a["win"], win_src, order=[0, 2, 3, 1, 4, 5], pending=pend)
        for k in range(NH):
            pend.append(lambda k=k: self.wload(a["wout"][k], a["wout"][k][:, :], wout_src[k * 128:(k + 1) * 128, :]))
        for _ in range(16):
            pend.pop(0)()
        for b in range(NB):
            for cch in range(NH):
                pg, pu, sg = a["pg"][cch % 2], a["pu"][cch % 2], a["sg"][cch % 2]
                for k in range(8):
                    p.op("pe", lambda e, k=k, cch=cch, pg=pg: e.matmul(
                        pg[:, :], lhsT=a["win"][k][:, cch * 128:(cch + 1) * 128], rhs=xT[:, k, :],
                        start=(k == 0), stop=(k == 7)),
                        reads=_toks([wv_(k, cch * 128), xT]), writes=_toks([pg]), sig=(k == 7))
                for k in range(8):
                    p.op("pe", lambda e, k=k, cch=cch, pu=pu: e.matmul(
                        pu[:, :], lhsT=a["win"][k][:, DFF + cch * 128:DFF + (cch + 1) * 128], rhs=xT[:, k, :],
                        start=(k == 0), stop=(k == 7)),
                        reads=_toks([wv_(k, DFF + cch * 128), xT]), writes=_toks([pu]), sig=(k == 7))
                p.op("act", lambda e, pg=pg, sg=sg: e.activation(out=sg[:, :], in_=pg[:, :], func=AF.Exp, scale=-1.0),
                     reads=_toks([pg]), writes=_toks([sg]))
                p.op("act", lambda e, sg=sg: e.activation(out=sg[:, :], in_=sg[:, :], func=AF.Ln, bias=self.onec[:, 0:1]),
                     reads=_toks([sg, self.onec]), writes=_toks([sg]))
                p.op("act", lambda e, sg=sg: e.activation(out=sg[:, :], in_=sg[:, :], func=AF.Exp, scale=-1.0),
                     reads=_toks([sg]), writes=_toks([sg]))
                p.op("dve", lambda e, pg=pg, sg=sg: e.tensor_tensor(
                    out=sg[:, :], in0=pg[:, :], in1=sg[:, :], op=ALU.mult),
                    reads=_toks([sg, pg]), writes=_toks([sg]))
                p.op("dve", lambda e, cch=cch, pu=pu, sg=sg: e.tensor_tensor(
                    out=hT[:, cch, :], in0=sg[:, :], in1=pu[:, :], op=ALU.mult),
                    reads=_toks([sg, pu]), writes=_toks([hT]))
                for _ in range(4):
                    if pend:
                        pend.pop(0)()
            for j in range(TB):
                i = b * TB + j
                pa, pb = a["po"][self._pk % 3], a["po"][(self._pk + 1) % 3]
                self._pk += 2
                xn = stage_a(b + 1, j) if b + 1 < NB else None
                for n, pp in ((0, pa), (1, pb)):
                    for cch in range(NH):
                        p.op("pe", lambda e, cch=cch, n=n, j=j, pp=pp: e.matmul(
                            pp[:, :], lhsT=hT[:, cch, j * 128:(j + 1) * 128],
                            rhs=a["wout"][cch][:, n * 512:(n + 1) * 512],
                            start=(cch == 0), stop=(cch == NH - 1)),
                            reads=_toks([hT, a["wout"][cch]]), writes=_toks([pp]),
                            sig=(cch == NH - 1))
                if xn is not None:
                    self.to_fm(xn, xT, j, a)
                self.post2(pa, pb, g, i, a)

    def post2(self, pa, pb, g, i, a):
        p = self.p
        self._pc = getattr(self, "_pc", 0) + 1
        ss, rs = a["ss"][2 + self._pc % 2], a["rs"][2 + self._pc % 2]
        ssb = a["ssb"][self._pc % 2]
        t = a["t"][self._pc % 2]
        ht = a["hr"][self._pc % 2]
        src = self.hsrc(i)
        p.op("sp", lambda e: e.dma_start(out=ht[:, :], in_=src),
             reads=[self.htok[i]], writes=_toks([ht]), dma=ht.tok)
        p.op("act", lambda e: e.activation(out=a["junk"][:, 0:512], in_=pa[:, :], func=AF.Square,
                                           scale=float(D) ** -0.5, accum_out=ss[:, 0:1]),
             reads=_toks([pa]), writes=_toks([a["junk"], ss]))
        p.op("act", lambda e: e.activation(out=a["junk"][:, 512:1024], in_=pb[:, :], func=AF.Square,
                                           scale=float(D) ** -0.5, accum_out=ssb[:, 0:1]),
             reads=_toks([pb]), writes=_toks([a["junk"], ssb]))
        p.op("dve", lambda e: e.tensor_tensor(out=ss[:, 0:1], in0=ss[:, 0:1], in1=ssb[:, 0:1], op=ALU.add),
             reads=_toks([ss, ssb]), writes=_toks([ss]))
        self.rsqrt_small(ss[:, 0:1], rs[:, 0:1], [ss], [rs], EPS)
        p.op("dve", lambda e: e.scalar_tensor_tensor(
            out=t[:, 0:512], in0=pa[:, :], scalar=rs[:, 0:1], in1=g[:, 1, 0:512], op0=ALU.mult, op1=ALU.mult),
            reads=_toks([pa, rs, g]), writes=_toks([t]))
        p.op("dve", lambda e: e.scalar_tensor_tensor(
            out=t[:, 512:1024], in0=pb[:, :], scalar=rs[:, 0:1], in1=g[:, 1, 512:1024], op0=ALU.mult, op1=ALU.mult),
            reads=_toks([pb, rs, g]), writes=_toks([t]))
        p.op("pool", lambda e: e.tensor_tensor(out=ht[:, :], in0=t[:, :], in1=ht[:, :], op=ALU.add),
             reads=_toks([t, ht]), writes=_toks([ht]))
        p.op("sp", lambda e: e.dma_start(out=self.out[i * 128:(i + 1) * 128, :], in_=ht[:, :]),
             reads=_toks([ht]), writes=[self.htok[i]], dma=ht.tok)

    def wload(self, t, dst, src):
        c, p = self.c, self.p
        if getattr(c, "stg", None) is None:
            c.stg = [c.sb([128, 1024], F32, f"stg{i}") for i in range(getattr(self, "nstg", 2))]
            self._stg_i = 0
        stg = c.stg
        rows, cols = src.shape
        for c0 in range(0, cols, 1024):
            c1 = min(cols, c0 + 1024)
            st = stg[self._stg_i % len(stg)]
            eng = ("act", "dve", "pool", "act", "dve")[self._stg_i % 5]
            self._stg_i += 1
            p.op("sp", lambda e, st=st, c0=c0, c1=c1: e.dma_start(out=st[0:rows, 0:c1 - c0], in_=src[:, c0:c1]),
                 writes=_toks([st]), dma=st.tok)
            if eng == "act":
                p.op("act", lambda e, st=st, c0=c0, c1=c1: e.copy(out=dst[:, c0:c1], in_=st[0:rows, 0:c1 - c0]),
                     reads=_toks([st]), writes=_toks([t]))
            else:
                p.op(eng, lambda e, st=st, c0=c0, c1=c1: e.tensor_copy(out=dst[:, c0:c1], in_=st[0:rows, 0:c1 - c0]),
                     reads=_toks([st]), writes=_toks([t]))

    def wload_pieces(self, tiles, src, order=None, pw_=1024, pending=None):
        ncols = src.shape[1]
        npc = (ncols + pw_ - 1) // pw_
        order = list(order) if order is not None else list(range(npc))
        order += [x for x in range(npc) if x not in order]
        views = {}
        for pc in order:
            c0, c1 = pc * pw_, min(ncols, (pc + 1) * pw_)
            for k, t in enumerate(tiles):
                v = T(t.h[:, c0:c1], f"wp{k}_{pc}")
                self.c.ptoks.append(v.tok)
                views[(k, pc)] = v
                if pending is None:
                    self.wload(v, t[:, c0:c1], src[k * 128:(k + 1) * 128, c0:c1])
                else:
                    pending.append((lambda v=v, t=t, c0=c0, c1=c1, k=k: self.wload(v, t[:, c0:c1], src[k * 128:(k + 1) * 128, c0:c1])))
        return lambda k, col: views[(k, col // pw_)]

    def pre(self, i, a, g, xn):
        p = self.p
        self._prc = getattr(self, "_prc", 0) + 1
        ht = a["h"][self._prc % 2]
        ss, rs = a["ss"][self._prc % 2], a["rs"][self._prc % 2]
        src = self.hsrc(i)
        p.op("sp", lambda e: e.dma_start(out=ht[:, :], in_=src),
             reads=[self.htok[i]], writes=_toks([ht]), dma=ht.tok)
        self.rstd(ht[:, :], [ht], a["junk"], ss, rs)
        p.op("dve", lambda e: e.scalar_tensor_tensor(
            out=xn[:, :], in0=ht[:, :], scalar=rs[:, 0:1], in1=g[:, 0, :],
            op0=ALU.mult, op1=ALU.mult),
            reads=_toks([ht, rs, g]), writes=_toks([xn]))

    def to_fm(self, xn, xT, j, a, nk=8, off=0):
        p = self.p
        pT = a["pT"][0]
        for k in range(nk):
            p.op("pe", lambda e, k=k: e.transpose(out=pT[:, k, :], in_=xn[:, k * 128:(k + 1) * 128],
                                                   identity=self.ident[:, :]),
                 reads=_toks([xn, self.ident]), writes=_toks([pT]), sig=(k == nk - 1))
        p.op("act", lambda e: e.copy(out=xT[:, 0:nk, off + j * 128:off + (j + 1) * 128], in_=pT[:, 0:nk, :]),
             reads=_toks([pT]), writes=_toks([xT]))

    def post(self, po, g, wgt, i, a):
        p = self.p
        self._pc = getattr(self, "_pc", 0) + 1
        ss, rs = a["ss"][2 + self._pc % 2], a["rs"][2 + self._pc % 2]
        t = a["t"][self._pc % 2]
        ht = a["hr"][self._pc % 2]
        src = self.hsrc(i)
        p.op("sp", lambda e: e.dma_start(out=ht[:, :], in_=src),
             reads=[self.htok[i]], writes=_toks([ht]), dma=ht.tok)
        self.rstd(po[:, :], [po], a["junk"], ss, rs)
        p.op("dve", lambda e: e.scalar_tensor_tensor(
            out=t[:, :], in0=po[:, :], scalar=rs[:, 0:1], in1=g[:, 1, :], op0=ALU.mult, op1=ALU.mult),
            reads=_toks([po, rs, g]), writes=_toks([t]))
        p.op("pool", lambda e: e.tensor_tensor(out=ht[:, :], in0=t[:, :], in1=ht[:, :], op=ALU.add),
             reads=_toks([t, ht]), writes=_toks([ht]))
        p.op("sp", lambda e: e.dma_start(out=self.out[i * 128:(i + 1) * 128, :], in_=ht[:, :]),
             reads=_toks([ht]), writes=[self.htok[i]], dma=ht.tok)

    def xattn(self, layer):
        with self.c.phase():
            self._xattn(layer)
        self.first = False

    def bcast_row(self, t, dst, src_row):
        self.p.op("sp", lambda e: e.dma_start(out=dst, in_=src_row.partition_broadcast(128)),
                  writes=_toks([t]), dma=t.tok)

    def _xattn(self, layer):
        c, p, nc = self.c, self.p, self.nc
        NT = self.NT
        TB = 4 if NT % 4 == 0 else 1
        NB = NT // TB
        W = TB * 128
        a = {}
        a["wq"] = [c.sb([128, D], BF16, f"wq{k}") for k in range(8)]
        a["wkv"] = [c.sb([128, 2 * D], BF16, f"wkv{k}") for k in range(8)]
        a["wo"] = [c.sb([128, D], BF16, f"wo{k}") for k in range(8)]
        a["g"] = c.sb([128, 2, D], F32, "g")
        a["gm"] = c.sb([128, D], F32, "gm")
        a["h"] = [c.sb([128, D], F32, f"h{i}") for i in range(2)]
        a["hr"] = [c.sb([128, D], F32, f"hr{i}") for i in range(2)]
        a["xn"] = [c.sb([128, D], BF16, f"xn{i}") for i in range(2)]
        a["xT"] = [c.sb([128, 8, W], BF16, "xT")]
        a["junk"] = c.sb([128, D], BF16, "junk")
        a["ss"] = [c.sb([128, 1], F32, f"ss{i}") for i in range(4)]
        a["rs"] = [c.sb([128, 1], F32, f"rs{i}") for i in range(4)]
        a["t"] = [c.sb([128, D], F32, f"t{i}") for i in range(2)]
        a["pT"] = [c.ps([128, 8, 128], BF16, "pT")]
        a["po"] = [c.ps([128, D], F32, "po")]
        memT = c.sb([128, 8, NMEM], BF16, "memT")
        KT = c.sb([128, 8, NMEM], BF16, "KT")
        V = c.sb([128, 2, D], BF16, "V")
        qT = c.sb([128, 8, W], BF16, "qT")
        oT = c.sb([128, 8, W], BF16, "oT")
        PT = c.sb([128, 8, W], BF16, "PT")
        pss2 = [c.ps([128, 2, NMEM], F32, f"pss{i}") for i in range(2)]
        Pf2 = [[c.sb([128, 2, NMEM], F32, f"Pf{h}{i}") for i in range(2)] for h in range(2)]
        Pn2 = [[c.sb([128, 2, NMEM], BF16, f"Pn{h}{i}") for i in range(2)] for h in range(2)]
        mx2 = [[c.sb([128, 2], F32, f"mx{h}{i}") for i in range(2)] for h in range(2)]
        rs2 = [[c.sb([128, 2], F32, f"rs2{h}{i}") for i in range(2)] for h in range(2)]
        pgen = [c.ps([128, 512], F32, f"pgen{i}") for i in range(2)]
        self.nstg = 4
        for k in range(8):
            self.wload(a["wkv"][k], a["wkv"][k][:, :], self.xa_w_kv[layer, k * 128:(k + 1) * 128, :])
        for k in range(8):
            self.wload(a["wq"][k], a["wq"][k][:, :], self.xa_w_q[layer, k * 128:(k + 1) * 128, :])
        for k in range(8):
            self.wload(a["wo"][k], a["wo"][k][:, :], self.xa_w_o[layer, k * 128:(k + 1) * 128, :])
        self.nstg = 2
        self.load_g(a["g"], layer, 2)
        g = a["g"]
        self.bcast_row(a["gm"], a["gm"][:, :], self.mem_norm_g[layer, :])
        for mt in range(2):
            ht = a["h"][mt]
            ss, rs = a["ss"][mt], a["rs"][mt]
            xn = a["xn"][mt]
            p.op("sp", lambda e, ht=ht, mt=mt: e.dma_start(out=ht[:, :], in_=self.mem[mt * 128:(mt + 1) * 128, :]),
                 writes=_toks([ht]), dma=ht.tok)
            self.rstd(ht[:, :], [ht], a["junk"], ss, rs)
            p.op("dve", lambda e, ht=ht, rs=rs, xn=xn: e.scalar_tensor_tensor(
                out=xn[:, :], in0=ht[:, :], scalar=rs[:, 0:1], in1=a["gm"][:, :], op0=ALU.mult, op1=ALU.mult),
                reads=_toks([ht, rs, a["gm"]]), writes=_toks([xn]))
            self.to_fm(xn, memT, mt, a)
        gi = 0
        for fc in range(8):
            pg = pgen[gi % 2]
            gi += 1
            for k in range(8):
                p.op("pe", lambda e, k=k, fc=fc, pg=pg: e.matmul(
                    pg[:, 0:NMEM], lhsT=a["wkv"][k][:, fc * 128:(fc + 1) * 128], rhs=memT[:, k, :],
                    start=(k == 0), stop=(k == 7)),
                    reads=_toks([a["wkv"][k], memT]), writes=_toks([pg]), sig=(k == 7))
            p.op("act", lambda e, fc=fc, pg=pg: e.copy(out=KT[:, fc, :], in_=pg[:, 0:NMEM]),
                 reads=_toks([pg]), writes=_toks([KT]))
        for mt in range(2):
            for n in range(2):
                pg = pgen[gi % 2]
                gi += 1
                for k in range(8):
                    p.op("pe", lambda e, k=k, mt=mt, n=n, pg=pg: e.matmul(
                        pg[:, :], lhsT=memT[:, k, mt * 128:(mt + 1) * 128],
                        rhs=a["wkv"][k][:, D + n * 512:D + (n + 1) * 512], start=(k == 0), stop=(k == 7)),
                        reads=_toks([a["wkv"][k], memT]), writes=_toks([pg]), sig=(k == 7))
                p.op("dve", lambda e, mt=mt, n=n, pg=pg: e.tensor_copy(out=V[:, mt, n * 512:(n + 1) * 512], in_=pg[:, :]),
                     reads=_toks([pg]), writes=_toks([V]))
        sc = 256.0 ** -0.5
        cnt = 0
        xT = a["xT"][0]

        def stage_a(b, j):
            nonlocal cnt
            xn = a["xn"][cnt % 2]
            cnt += 1
            self.pre(b * TB + j, a, g, xn)
            return xn

        for j in range(TB):
            xn = stage_a(0, j)
            self.to_fm(xn, xT, j, a)
        for b in range(NB):
            for fc in range(8):
                pg = pgen[gi % 2]
                gi += 1
                for k in range(8):
                    p.op("pe", lambda e, k=k, fc=fc, pg=pg: e.matmul(
                        pg[:, 0:W], lhsT=a["wq"][k][:, fc * 128:(fc + 1) * 128], rhs=xT[:, k, :],
                        start=(k == 0), stop=(k == 7)),
                        reads=_toks([a["wq"][k], xT]), writes=_toks([pg]), sig=(k == 7))
                eng = "act" if fc % 2 == 0 else "dve"
                if eng == "act":
                    p.op("act", lambda e, fc=fc, pg=pg: e.copy(out=qT[:, fc, :], in_=pg[:, 0:W]),
                         reads=_toks([pg]), writes=_toks([qT]))
                else:
                    p.op("dve", lambda e, fc=fc, pg=pg: e.tensor_copy(out=qT[:, fc, :], in_=pg[:, 0:W]),
                         reads=_toks([pg]), writes=_toks([qT]))
            def pair_gen(j, hp):
                r = j % 2
                pf, pn, m_, rsm, ps_ = Pf2[hp][r], Pn2[hp][r], mx2[hp][r], rs2[hp][r], pss2[hp]
                for h2 in range(2):
                    hd = 2 * hp + h2
                    for dc in range(2):
                        p.op("pe", lambda e, hd=hd, h2=h2, dc=dc: e.matmul(
                            ps_[:, h2, :], lhsT=qT[:, 2 * hd + dc, j * 128:(j + 1) * 128], rhs=KT[:, 2 * hd + dc, :],
                            start=(dc == 0), stop=(dc == 1)),
                            reads=_toks([qT, KT]), writes=_toks([ps_]), sig=(h2 == 1 and dc == 1))
                yield
                p.op("dve", lambda e: e.tensor_reduce(out=m_[:, :], in_=ps_[:, :, :], axis=AX.X, op=ALU.max),
                     reads=_toks([ps_]), writes=_toks([m_]))
                yield
                p.op("dve", lambda e: e.tensor_scalar(out=m_[:, :], in0=m_[:, :], scalar1=-sc, scalar2=None, op0=ALU.mult),
                     reads=_toks([m_]), writes=_toks([m_]))
                yield
                for h2 in range(2):
                    p.op("act", lambda e, h2=h2: e.activation(
                        out=pf[:, h2, :], in_=ps_[:, h2, :], func=AF.Exp, scale=sc, bias=m_[:, h2:h2 + 1],
                        accum_out=rsm[:, h2:h2 + 1]),
                        reads=_toks([ps_, m_]), writes=_toks([pf, rsm]))
                yield
                p.op("dve", lambda e: e.reciprocal(out=rsm[:, :], in_=rsm[:, :]), reads=_toks([rsm]), writes=_toks([rsm]))
                yield
                p.op("dve", lambda e: e.tensor_tensor(
                    out=pn[:, :, :], in0=pf[:, :, :], in1=rsm[:, :].unsqueeze(2).to_broadcast([128, 2, NMEM]), op=ALU.mult),
                    reads=_toks([pf, rsm]), writes=_toks([pn]))
                yield
                pT = a["pT"][0]
                for q in range(4):
                    h2, mc = q // 2, q % 2
                    p.op("pe", lambda e, q=q, h2=h2, mc=mc: e.transpose(
                        out=pT[:, q, :], in_=pn[:, h2, mc * 128:(mc + 1) * 128], identity=self.ident[:, :]),
                        reads=_toks([pn, self.ident]), writes=_toks([pT]), sig=(q == 3))
                p.op("act", lambda e: e.copy(out=PT[:, 4 * hp:4 * hp + 4, j * 128:(j + 1) * 128], in_=pT[:, 0:4, :]),
                     reads=_toks([pT]), writes=_toks([PT]))
                yield

            for j in range(TB):
                self.run_gens([pair_gen(j, 0), pair_gen(j, 1)])
            for fc in range(8):
                hd = fc // 2
                pg = pgen[gi % 2]
                gi += 1
                for mc in range(2):
                    p.op("pe", lambda e, fc=fc, hd=hd, mc=mc, pg=pg: e.matmul(
                        pg[:, 0:W], lhsT=V[:, mc, fc * 128:(fc + 1) * 128], rhs=PT[:, 2 * hd + mc, :],
                        start=(mc == 0), stop=(mc == 1)),
                        reads=_toks([V, PT]), writes=_toks([pg]), sig=(mc == 1))
                if fc % 2 == 0:
                    p.op("act", lambda e, fc=fc, pg=pg: e.copy(out=oT[:, fc, :], in_=pg[:, 0:W]),
                         reads=_toks([pg]), writes=_toks([oT]))
                else:
                    p.op("dve", lambda e, fc=fc, pg=pg: e.tensor_copy(out=oT[:, fc, :], in_=pg[:, 0:W]),
                         reads=_toks([pg]), writes=_toks([oT]))
            for j in range(TB):
                po = a["po"][0]
                xn = stage_a(b + 1, j) if b + 1 < NB else None
                for n in range(2):
                    for fc in range(8):
                        p.op("pe", lambda e, fc=fc, n=n, j=j: e.matmul(
                            po[:, n * 512:(n + 1) * 512], lhsT=oT[:, fc, j * 128:(j + 1) * 128],
                            rhs=a["wo"][fc][:, n * 512:(n + 1) * 512], start=(fc == 0), stop=(fc == 7)),
                            reads=_toks([oT, a["wo"][fc]]), writes=_toks([po]), sig=(fc == 7 and n == 1))
                if xn is not None:
                    self.to_fm(xn, xT, j, a)
                self.post(po, g, 1.0, b * TB + j, a)

    def load_cols(self, rows, nchunk, ps, name):
        c, p = self.c, self.p
        nr = len(rows)
        out = c.sb([128, nchunk, nr], F32, name)
        with c.phase():
            rt = c.sb([nr, nchunk * 128], F32, name + "_r")
            for r, row in enumerate(rows):
                p.op("sp", lambda e, r=r, row=row: e.dma_start(out=rt[r:r + 1, :], in_=row[None, :]),
                     writes=_toks([rt]), dma=rt.tok)
            for cc in range(nchunk):
                p.op("pe", lambda e, cc=cc: e.transpose(out=ps[:, cc * nr:(cc + 1) * nr],
                                                        in_=rt[0:nr, cc * 128:(cc + 1) * 128],
                                                        identity=self.identf[0:nr, 0:nr]),
                     reads=_toks([rt, self.identf]), writes=_toks([ps]), sig=(cc == nchunk - 1))
            p.op("act", lambda e: e.copy(out=out[:, :, :].rearrange("p c r -> p (c r)"), in_=ps[:, 0:nchunk * nr]),
                 reads=_toks([ps]), writes=_toks([out]))
        return out

    def sigmoid_inplace(self, ap, t, neg_bias=None, extra_reads=()):
        p = self.p
        if neg_bias is None:
            p.op("act", lambda e: e.activation(out=ap, in_=ap, func=AF.Exp, scale=-1.0),
                 reads=_toks([t]), writes=_toks([t]))
        else:
            p.op("act", lambda e: e.activation(out=ap, in_=ap, func=AF.Exp, scale=-1.0, bias=neg_bias),
                 reads=_toks([t] + list(extra_reads)), writes=_toks([t]))
        p.op("act", lambda e: e.activation(out=ap, in_=ap, func=AF.Ln, bias=self.onec[:, 0:1]),
             reads=_toks([t, self.onec]), writes=_toks([t]))
        p.op("act", lambda e: e.activation(out=ap, in_=ap, func=AF.Exp, scale=-1.0),
             reads=_toks([t]), writes=_toks([t]))

    def conv_chunk(self, ps, cb, halo, cw, ncw, cc, W, dst_ap, dst_t, sgb, has_bias, first_block):
        p = self.p
        p.op("act", lambda e: e.copy(out=cb[:, 3:3 + W], in_=ps[:, 0:W]), reads=_toks([ps]), writes=_toks([cb]))
        p.op("dve", lambda e: e.tensor_copy(out=cb[:, 0:3], in_=halo[:, cc, :]), reads=_toks([halo]), writes=_toks([cb]))
        yield
        p.op("act", lambda e: e.copy(out=halo[:, cc, :], in_=cb[:, W:W + 3]), reads=_toks([cb]), writes=_toks([halo]))
        acc = sgb["acc"]
        sg = sgb["sg"]
        p.op("dve", lambda e: e.tensor_scalar(out=acc[:, 0:W], in0=cb[:, 3:3 + W], scalar1=cw[:, cc, 3:4], scalar2=None,
                                              op0=ALU.mult), reads=_toks([cb, cw]), writes=_toks([acc]))
        yield
        for j in (2, 1, 0):
            p.op("dve", lambda e, j=j: e.scalar_tensor_tensor(
                out=acc[:, 0:W], in0=cb[:, j:j + W], scalar=cw[:, cc, j:j + 1], in1=acc[:, 0:W],
                op0=ALU.mult, op1=ALU.add), reads=_toks([cb, cw, acc]), writes=_toks([acc]))
            yield
        if has_bias:
            p.op("act", lambda e: e.activation(out=sg[:, 0:W], in_=acc[:, 0:W], func=AF.Exp, scale=-1.0,
                                               bias=ncw[:, cc:cc + 1]),
                 reads=_toks([acc, ncw]), writes=_toks([sg]))
        else:
            p.op("act", lambda e: e.activation(out=sg[:, 0:W], in_=acc[:, 0:W], func=AF.Exp, scale=-1.0),
                 reads=_toks([acc]), writes=_toks([sg]))
        yield
        p.op("act", lambda e: e.activation(out=sg[:, 0:W], in_=sg[:, 0:W], func=AF.Ln, bias=self.onec[:, 0:1]),
             reads=_toks([sg, self.onec]), writes=_toks([sg]))
        yield
        p.op("act", lambda e: e.activation(out=sg[:, 0:W], in_=sg[:, 0:W], func=AF.Exp, scale=-1.0),
             reads=_toks([sg]), writes=_toks([sg]))
        yield
        if has_bias:
            p.op("dve", lambda e: e.scalar_tensor_tensor(
                out=dst_ap, in0=acc[:, 0:W], scalar=cw[:, cc, 4:5], in1=sg[:, 0:W], op0=ALU.add, op1=ALU.mult),
                reads=_toks([acc, cw, sg]), writes=_toks([dst_t]))
        else:
            p.op("dve", lambda e: e.tensor_tensor(out=dst_ap, in0=acc[:, 0:W], in1=sg[:, 0:W], op=ALU.mult),
                 reads=_toks([acc, sg]), writes=_toks([dst_t]))
        yield

    def fm_to_tm(self, src, src_sl, dsts, a, nch=8):
        p = self.p
        pT = a["pT"][0]
        for j, (dap, dt_) in enumerate(dsts):
            for q in range(nch):
                p.op("pe", lambda e, q=q, j=j: e.transpose(out=pT[:, q, :], in_=src[:, q, j * 128:(j + 1) * 128],
                                                           identity=self.ident[:, :]),
                     reads=_toks([src, self.ident]), writes=_toks([pT]), sig=(q == nch - 1))
            p.op("act", lambda e, dap=dap: e.copy(out=dap, in_=pT[:, 0:nch, :].rearrange("p c r -> p (c r)")),
                 reads=_toks([pT]), writes=_toks([dt_]))

    def ssd(self, layer):
        with self.c.phase():
            self._ssd(layer)

    def _ssd(self, layer):
        c, p, nc = self.c, self.p, self.nc
        jx = layer // 3
        NT = self.NT
        TB = 2 if NT % 2 == 0 else 1
        NB = NT // TB
        W = TB * 128
        CW = 6176
        a = {}
        a["win"] = [c.sb([128, CW], BF16, f"win{k}") for k in range(8)]
        a["g"] = c.sb([128, 1, D], F32, "g")
        a["h"] = [c.sb([128, D], F32, "h")] * 2
        a["xn"] = [c.sb([128, D], BF16, "xn")] * 2
        a["xT"] = [c.sb([128, 8, W], BF16, "xT")]
        a["junk"] = c.sb([128, D], BF16, "junk")
        a["ss"] = [c.sb([128, 1], F32, f"ss{i}") for i in range(4)]
        a["rs"] = [c.sb([128, 1], F32, f"rs{i}") for i in range(4)]
        a["pT"] = [c.ps([128, 8, 128], BF16, "pT")]
        pgen = [c.ps([128, 512], F32, f"pgen{i}") for i in range(2)]
        pcb = c.ps([128, 128], F32, "pcb")
        pz = c.ps([128, 256], F32, "pz")
        pgate = c.ps([128, 128], F32, "pgate")
        pdt = pgate.view((slice(None), slice(0, 32)), "pdt")
        pac = pgate.view((slice(None), slice(32, 96)), "pac")
        pD = c.ps([128, 4, 128], F32, "pD")
        pyy = c.ps([128, 512], F32, "pyy")
        py = pyy.view((slice(None), slice(0, 256)), "py")
        pyi = pyy.view((slice(None), slice(256, 512)), "pyi")
        for k in range(8):
            self.wload(a["win"][k], a["win"][k][:, :], self.ssd_w_in[jx, k * 128:(k + 1) * 128, :])
        self.load_g(a["g"], layer, 1)
        g = a["g"]
        cw = self.load_cols([self.ssd_conv_w[jx, t_, :] for t_ in range(4)] + [self.ssd_conv_b[jx, :]], 32, pgen[0], "cw")
        ncw = c.sb([128, 32], F32, "ncw")
        p.op("dve", lambda e: e.tensor_scalar(out=ncw[:, :], in0=cw[:, :, 4], scalar1=-1.0, scalar2=None, op0=ALU.mult),
             reads=_toks([cw]), writes=_toks([ncw]))
        dtb = c.sb([128, 32], F32, "dtb")
        aneg = c.sb([128, 32], F32, "aneg")
        dsk = c.sb([128, 32], F32, "dsk")
        ng = c.sb([128, 2 * D], F32, "ng")
        self.bcast_row(dtb, dtb[:, :], self.ssd_dt_bias[jx, :])
        self.bcast_row(aneg, aneg[:, :], self.ssd_a_log[jx, :])
        self.bcast_row(dsk, dsk[:, :], self.ssd_d[jx, :])
        self.bcast_row(ng, ng[:, :], self.ssd_norm_g[jx, :])
        p.op("act", lambda e: e.activation(out=aneg[:, :], in_=aneg[:, :], func=AF.Exp), reads=_toks([aneg]), writes=_toks([aneg]))
        p.op("dve", lambda e: e.tensor_scalar(out=aneg[:, :], in0=aneg[:, :], scalar1=-1.0, scalar2=None, op0=ALU.mult),
             reads=_toks([aneg]), writes=_toks([aneg]))
        halo = c.sb([128, 32, 3], F32, "halo")
        p.op("pool", lambda e: e.memset(halo[:, :, :], 0.0), writes=_toks([halo]))
        Z = [c.sb([128, 256], F32, f"Z{g_}") for g_ in range(8)]
        Zb = [c.sb([128, 256], BF16, f"Zb{g_}") for g_ in range(8)]
        for g_ in range(8):
            p.op("pool", lambda e, g_=g_: e.memset(Z[g_][:, :], 0.0), writes=_toks([Z[g_]]))
            p.op("pool", lambda e, g_=g_: e.memset(Zb[g_][:, :], 0.0), writes=_toks([Zb[g_]]))
        cb = [c.sb([128, W + 3], F32, f"cb{i}") for i in range(2)]
        sgb = [dict(acc=c.sb([128, W], F32, f"acc{i}"), sg=c.sb([128, W], F32, f"sgc{i}")) for i in range(2)]
        xfm = c.sb([128, 8, W], BF16, "xfm")
        BT = c.sb([128, 8, W], BF16, "BT")
        CT = c.sb([128, 8, W], BF16, "CT")
        xtm = [c.sb([128, 2 * D], BF16, f"xtm{j}") for j in range(TB)]
        Btm = [c.sb([128, D], BF16, f"Btm{j}") for j in range(TB)]
        dtl = c.sb([128, 32], F32, "dtl")
        dt_ = c.sb([128, 32], F32, "dt")
        adt = c.sb([128, 32], F32, "adt")
        acum = c.sb([128, 32], F32, "acum")
        eacum = c.sb([128, 32], F32, "eacum")
        elast = c.sb([128, 32], F32, "elast")
        toend = c.sb([128, 32], F32, "toend")
        xdt = c.sb([128, 32, 64], BF16, "xdt")
        xde = c.sb([128, 32, 64], BF16, "xde")
        cbm = c.sb([128, 128], F32, "cbm")
        LH = c.sb([128, 4, 128], F32, "LH")
        Ed = c.sb([128, 4, 128], F32, "Ed")
        Mt = c.sb([128, 4, 128], BF16, "Mt")
        ytmp = c.sb([128, 256], F32, "ytmp")
        y = c.sb([128, 2 * D], F32, "y")
        zs = c.sb([128, 512], F32, "zs")
        ss8 = c.sb([128, 8], F32, "ss8")
        yb = c.sb([128, 2 * D], BF16, "yb")
        gi = 0
        for b in range(NB):
            xT = a["xT"][0]
            for j in range(TB):
                self.pre(b * TB + j, a, g, a["xn"][0])
                self.to_fm(a["xn"][0], xT, j, a)
            def ssd_chunk(cc):
                pg = pgen[cc % 2]
                for k in range(8):
                    p.op("pe", lambda e, k=k: e.matmul(
                        pg[:, 0:W], lhsT=a["win"][k][:, 2048 + cc * 128:2048 + (cc + 1) * 128], rhs=xT[:, k, :],
                        start=(k == 0), stop=(k == 7)),
                        reads=_toks([a["win"][k], xT]), writes=_toks([pg]), sig=(k == 7))
                yield
                if cc < 16:
                    dst_t, dst_ap = xfm, xfm[:, cc % 8, :]
                elif cc < 24:
                    dst_t, dst_ap = BT, BT[:, cc - 16, :]
                else:
                    dst_t, dst_ap = CT, CT[:, cc - 24, :]
                yield from self.conv_chunk(pg, cb[cc % 2], halo, cw, ncw, cc, W, dst_ap, dst_t, sgb[cc % 2], True, b == 0)

            for cc in range(0, 32, 2):
                self.run_gens([ssd_chunk(cc), ssd_chunk(cc + 1)])
                if cc + 1 in (7, 15):
                    grp = (cc + 1) // 8
                    self.fm_to_tm(xfm, None, [(xtm[j][:, grp * D:(grp + 1) * D], xtm[j]) for j in range(TB)], a)
                if cc + 1 == 23:
                    self.fm_to_tm(BT, None, [(Btm[j][:, :], Btm[j]) for j in range(TB)], a)
            for j in range(TB):
                i = b * TB + j
                tsl = slice(j * 128, (j + 1) * 128)
                x3 = xtm[j][:, :].rearrange("p (h d) -> p h d", d=64)
                if i == 1:
                    self.dbg("xtm", xtm[j], xtm[j][:, :], [128, 2 * D], BF16)
                    self.dbg("Btm", Btm[j], Btm[j][:, :], [128, D], BF16)
                    self.dbg("CT", CT, CT[:, :, tsl], [128, 8, 128], BF16)
                for k in range(8):
                    p.op("pe", lambda e, k=k: e.matmul(pdt[:, :], lhsT=xT[:, k, tsl], rhs=a["win"][k][:, 6144:6176],
                                                       start=(k == 0), stop=(k == 7)),
                         reads=_toks([a["win"][k], xT]), writes=_toks([pdt]), sig=(k == 7))
                p.op("dve", lambda e: e.tensor_tensor(out=dtl[:, :], in0=pdt[:, :], in1=dtb[:, :], op=ALU.add),
                     reads=_toks([pdt, dtb]), writes=_toks([dtl]))
                p.op("act", lambda e: e.activation(out=dtl[:, :], in_=dtl[:, :], func=AF.Exp),
                     reads=_toks([dtl]), writes=_toks([dtl]))
                p.op("act", lambda e: e.activation(out=dt_[:, :], in_=dtl[:, :], func=AF.Ln, bias=self.onec[:, 0:1]),
                     reads=_toks([dtl, self.onec]), writes=_toks([dt_]))
                p.op("dve", lambda e: e.tensor_tensor(out=adt[:, :], in0=dt_[:, :], in1=aneg[:, :], op=ALU.mult),
                     reads=_toks([dt_, aneg]), writes=_toks([adt]))
                p.op("pe", lambda e: e.matmul(pac[:, 0:32], lhsT=self.triLE[:, :], rhs=adt[:, :], start=True, stop=True),
                     reads=_toks([self.triLE, adt]), writes=_toks([pac]))
                p.op("pe", lambda e: e.matmul(pac[:, 32:64], lhsT=self.onesf[:, :], rhs=adt[:, :], start=True, stop=True),
                     reads=_toks([self.onesf, adt]), writes=_toks([pac]))
                p.op("act", lambda e: e.copy(out=acum[:, :], in_=pac[:, 0:32]), reads=_toks([pac]), writes=_toks([acum]))
                p.op("act", lambda e: e.activation(out=eacum[:, :], in_=acum[:, :], func=AF.Exp),
                     reads=_toks([acum]), writes=_toks([eacum]))
                p.op("act", lambda e: e.activation(out=elast[:, :], in_=pac[:, 32:64], func=AF.Exp),
                     reads=_toks([pac]), writes=_toks([elast]))
                p.op("dve", lambda e: e.tensor_tensor(out=toend[:, :], in0=pac[:, 32:64], in1=acum[:, :], op=ALU.subtract),
                     reads=_toks([pac, acum]), writes=_toks([toend]))
                p.op("act", lambda e: e.activation(out=toend[:, :], in_=toend[:, :], func=AF.Exp),
                     reads=_toks([toend]), writes=_toks([toend]))
                p.op("dve", lambda e: e.tensor_tensor(out=toend[:, :], in0=toend[:, :], in1=dt_[:, :], op=ALU.mult),
                     reads=_toks([toend, dt_]), writes=_toks([toend]))
                p.op("dve", lambda e: e.tensor_tensor(
                    out=xdt[:, :, :], in0=x3, in1=dt_[:, :].unsqueeze(2).to_broadcast([128, 32, 64]), op=ALU.mult),
                    reads=_toks([xtm[j], dt_]), writes=_toks([xdt]))
                p.op("dve", lambda e: e.tensor_tensor(
                    out=xde[:, :, :], in0=x3, in1=toend[:, :].unsqueeze(2).to_broadcast([128, 32, 64]), op=ALU.mult),
                    reads=_toks([xtm[j], toend]), writes=_toks([xde]))
                for g_ in range(8):
                    hs = slice(4 * g_, 4 * g_ + 4)
                    p.op("pe", lambda e, g_=g_: e.matmul(pcb[:, :], lhsT=BT[:, g_, tsl], rhs=CT[:, g_, tsl],
                                                         start=True, stop=True),
                         reads=_toks([BT, CT]), writes=_toks([pcb]))
                    p.op("dve", lambda e: e.tensor_tensor(out=cbm[:, :], in0=pcb[:, :], in1=self.triLE[:, :], op=ALU.mult),
                         reads=_toks([pcb, self.triLE]), writes=_toks([cbm]))
                    p.op("dve", lambda e, hs=hs: e.tensor_tensor(
                        out=LH[:, :, :], in0=self.maskGT[:, :].unsqueeze(1).to_broadcast([128, 4, 128]),
                        in1=adt[:, hs].unsqueeze(2).to_broadcast([128, 4, 128]), op=ALU.mult),
                        reads=_toks([self.maskGT, adt]), writes=_toks([LH]))
                    for e_ in range(4):
                        p.op("pe", lambda e, e_=e_: e.matmul(pD[:, e_, :], lhsT=LH[:, e_, :], rhs=self.triLE[:, :],
                                                             start=True, stop=True),
                             reads=_toks([LH, self.triLE]), writes=_toks([pD]), sig=(e_ == 3))
                    p.op("act", lambda e: e.activation(out=Ed[:, :, :], in_=pD[:, :, :], func=AF.Exp),
                         reads=_toks([pD]), writes=_toks([Ed]))
                    p.op("dve", lambda e: e.tensor_tensor(
                        out=Mt[:, :, :], in0=Ed[:, :, :], in1=cbm[:, :].unsqueeze(1).to_broadcast([128, 4, 128]),
                        op=ALU.mult), reads=_toks([Ed, cbm]), writes=_toks([Mt]))
                    for e_ in range(4):
                        p.op("pe", lambda e, e_=e_, g_=g_: e.matmul(
                            py[:, e_ * 64:(e_ + 1) * 64], lhsT=Mt[:, e_, :], rhs=xdt[:, 4 * g_ + e_, :],
                            start=True, stop=True),
                            reads=_toks([Mt, xdt]), writes=_toks([py]), sig=(e_ == 3))
                    p.op("pe", lambda e, g_=g_: e.matmul(pyi[:, :], lhsT=CT[:, g_, tsl], rhs=Zb[g_][:, :],
                                                         start=True, stop=True),
                         reads=_toks([CT, Zb[g_]]), writes=_toks([pyi]))
                    p.op("dve", lambda e, hs=hs: e.tensor_tensor(
                        out=ytmp[:, :].rearrange("p (h d) -> p h d", d=64),
                        in0=pyi[:, :].rearrange("p (h d) -> p h d", d=64),
                        in1=eacum[:, hs].unsqueeze(2).to_broadcast([128, 4, 64]), op=ALU.mult),
                        reads=_toks([pyi, eacum]), writes=_toks([ytmp]))
                    p.op("dve", lambda e, g_=g_: e.tensor_tensor(out=y[:, g_ * 256:(g_ + 1) * 256], in0=ytmp[:, :],
                                                                 in1=py[:, :], op=ALU.add),
                         reads=_toks([ytmp, py]), writes=_toks([y]))
                    p.op("pe", lambda e, g_=g_: e.matmul(
                        pz[:, :], lhsT=Btm[j][:, g_ * 128:(g_ + 1) * 128],
                        rhs=xde[:, 4 * g_:4 * g_ + 4, :].rearrange("p h d -> p (h d)"), start=True, stop=True),
                        reads=_toks([Btm[j], xde]), writes=_toks([pz]))
                    p.op("pool", lambda e, g_=g_, hs=hs: e.tensor_tensor(
                        out=Z[g_][:, :].rearrange("p (h d) -> p h d", d=64),
                        in0=Z[g_][:, :].rearrange("p (h d) -> p h d", d=64),
                        in1=elast[:, hs].unsqueeze(2).to_broadcast([128, 4, 64]), op=ALU.mult),
                        reads=_toks([Z[g_], elast]), writes=_toks([Z[g_]]))
                    p.op("dve", lambda e, g_=g_: e.tensor_tensor(out=Z[g_][:, :], in0=Z[g_][:, :], in1=pz[:, :], op=ALU.add),
                         reads=_toks([Z[g_], pz]), writes=_toks([Z[g_]]))
                    p.op("act", lambda e, g_=g_: e.copy(out=Zb[g_][:, :], in_=Z[g_][:, :]),
                         reads=_toks([Z[g_]]), writes=_toks([Zb[g_]]))
                if i == 1:
                    self.dbg("dt", dt_, dt_[:, :], [128, 32])
                    self.dbg("acum", acum, acum[:, :], [128, 32])
                    self.dbg("toend", toend, toend[:, :], [128, 32])
                    self.dbg("y0", y, y[:, :], [128, 2 * D])
                yt3 = y[:, :].rearrange("p (h d) -> p h d", d=64)
                p.op("pool", lambda e: e.tensor_tensor(
                    out=xdt[:, :, :], in0=x3, in1=dsk[:, :].unsqueeze(2).to_broadcast([128, 32, 64]), op=ALU.mult),
                    reads=_toks([xtm[j], dsk]), writes=_toks([xdt]))
                p.op("pool", lambda e: e.tensor_tensor(out=yt3, in0=yt3, in1=xdt[:, :, :], op=ALU.add),
                     reads=_toks([y, xdt]), writes=_toks([y]))
                for n in range(4):
                    pg = pgen[gi % 2]
                    gi += 1
                    for k in range(8):
                        p.op("pe", lambda e, k=k, n=n, pg=pg: e.matmul(
                            pg[:, :], lhsT=xT[:, k, tsl], rhs=a["win"][k][:, n * 512:(n + 1) * 512],
                            start=(k == 0), stop=(k == 7)),
                            reads=_toks([a["win"][k], xT]), writes=_toks([pg]), sig=(k == 7))
                    p.op("act", lambda e, pg=pg: e.copy(out=zs[:, :], in_=pg[:, :]), reads=_toks([pg]), writes=_toks([zs]))
                    self.sigmoid_inplace(zs[:, :], zs)
                    p.op("dve", lambda e, pg=pg: e.tensor_tensor(out=zs[:, :], in0=zs[:, :], in1=pg[:, :], op=ALU.mult),
                         reads=_toks([zs, pg]), writes=_toks([zs]))
                    p.op("dve", lambda e, n=n: e.tensor_tensor(out=y[:, n * 512:(n + 1) * 512], in0=y[:, n * 512:(n + 1) * 512],
                                                                in1=zs[:, :], op=ALU.mult),
                         reads=_toks([zs, y]), writes=_toks([y]))
                for g_ in range(8):
                    p.op("act", lambda e, g_=g_: e.activation(
                        out=a["junk"][:, 0:256], in_=y[:, g_ * 256:(g_ + 1) * 256], func=AF.Square, scale=1.0 / 16.0,
                        accum_out=ss8[:, g_:g_ + 1]), reads=_toks([y]), writes=_toks([a["junk"], ss8]))
                self.rsqrt_small(ss8[:, :], ss8[:, :], [ss8], [ss8], EPS)
                p.op("dve", lambda e: e.tensor_tensor(
                    out=y[:, :].rearrange("p (g d) -> p g d", d=256), in0=y[:, :].rearrange("p (g d) -> p g d", d=256),
                    in1=ss8[:, :].unsqueeze(2).to_broadcast([128, 8, 256]), op=ALU.mult),
                    reads=_toks([y, ss8]), writes=_toks([y]))
                p.op("pool", lambda e: e.tensor_tensor(out=yb[:, :], in0=y[:, :], in1=ng[:, :], op=ALU.mult),
                     reads=_toks([y, ng]), writes=_toks([yb]))
                if i == 1:
                    self.dbg("yb", yb, yb[:, :], [128, 2 * D], BF16)
                p.op("sp", lambda e, i=i: e.dma_start(out=self.scr[i * 128:(i + 1) * 128, :], in_=yb[:, :]),
                     reads=_toks([yb]), writes=[self.ytok[i]], dma=yb.tok)

    def mix_out(self, layer, w_dram, kdim, zgate=None):
        with self.c.phase():
            self._mix_out(layer, w_dram, kdim, zgate)
        self.first = False

    def _mix_out(self, layer, w_dram, kdim, zgate):
        c, p = self.c, self.p
        nk = kdim // 128
        c.stg = [c.sb([128, 1024], F32, f"stg{i}") for i in range(4)]
        self._stg_i = 0
        a = {}
        if zgate is not None:
            a["h"] = [c.sb([128, D], F32, f"h{i}") for i in range(2)]
            a["xn"] = [c.sb([128, D], BF16, f"xn{i}") for i in range(2)]
            xT1 = c.sb([128, 8, 128], BF16, "xT1")
            wz = [c.sb([128, kdim], BF16, f"wz{k}") for k in range(8)]
            zs = [c.sb([128, 512], F32, f"zs{i}") for i in range(2)]
            pgz = [c.ps([128, 512], F32, f"pgz{i}") for i in range(2)]
            for k in range(8):
                self.wload(wz[k], wz[k][:, :], zgate[0][k * 128:(k + 1) * 128, zgate[1]:zgate[1] + kdim])
        a["wout"] = [c.sb([128, D], BF16, f"wout{k}") for k in range(nk)]
        a["g"] = c.sb([128, 2, D], F32, "g")
        a["hr"] = [c.sb([128, D], F32, f"hr{i}") for i in range(2)]
        a["junk"] = c.sb([128, D], BF16, "junk")
        a["ss"] = [c.sb([128, 1], F32, f"ss{i}") for i in range(4)]
        a["rs"] = [c.sb([128, 1], F32, f"rs{i}") for i in range(4)]
        a["t"] = [c.sb([128, D], F32, f"t{i}") for i in range(2)]
        a["pT"] = [c.ps([128, 8, 128], BF16, "pT")]
        a["po"] = [c.ps([128, D], F32, f"po{i}") for i in range(2)]
        yb = [c.sb([128, kdim], BF16, f"yb{i}") for i in range(2)]
        yT = [c.sb([128, nk, 128], BF16, f"yT{i}") for i in range(2)]
        for k in range(nk):
            self.wload(a["wout"][k], a["wout"][k][:, :], w_dram[k * 128:(k + 1) * 128, :])
        self.load_g(a["g"], layer, 1)
        g = a["g"]
        if zgate is not None:
            xT1s = [xT1, c.sb([128, 8, 128], BF16, "xT1b")]

        def tile_gen(i):
            r = i % 2
            y_, yT_ = yb[r], yT[r]
            p.op("sp", lambda e: e.dma_start(out=y_[:, :], in_=self.scr[i * 128:(i + 1) * 128, 0:kdim]),
                 reads=[self.ytok[i]], writes=_toks([y_]), dma=y_.tok)
            if zgate is not None:
                xn = a["xn"][r]
                self.pre(i, a, g, xn)
                yield
                self.to_fm(xn, xT1s[r], 0, a)
                yield
                pg, z_ = pgz[r], zs[r]
                for n in range(kdim // 512):
                    for k in range(8):
                        p.op("pe", lambda e, k=k, n=n: e.matmul(
                            pg[:, :], lhsT=xT1s[r][:, k, :], rhs=wz[k][:, n * 512:(n + 1) * 512], start=(k == 0), stop=(k == 7)),
                            reads=_toks([wz[k], xT1s[r]]), writes=_toks([pg]), sig=(k == 7))
                    yield
                    p.op("act", lambda e: e.activation(out=z_[:, :], in_=pg[:, :], func=AF.Exp, scale=-1.0),
                         reads=_toks([pg]), writes=_toks([z_]))
                    yield
                    p.op("act", lambda e: e.activation(out=z_[:, :], in_=z_[:, :], func=AF.Ln, bias=self.onec[:, 0:1]),
                         reads=_toks([z_, self.onec]), writes=_toks([z_]))
                    yield
                    p.op("act", lambda e: e.activation(out=z_[:, :], in_=z_[:, :], func=AF.Exp, scale=-1.0),
                         reads=_toks([z_]), writes=_toks([z_]))
                    yield
                    p.op("dve", lambda e: e.tensor_tensor(out=z_[:, :], in0=z_[:, :], in1=pg[:, :], op=ALU.mult),
                         reads=_toks([z_, pg]), writes=_toks([z_]))
                    yield
                    p.op("dve", lambda e, n=n: e.tensor_tensor(
                        out=y_[:, n * 512:(n + 1) * 512], in0=y_[:, n * 512:(n + 1) * 512], in1=z_[:, :], op=ALU.mult),
                        reads=_toks([z_, y_]), writes=_toks([y_]))
                    yield
            for hh in range(nk // 8):
                self.to_fm_part(y_, hh * 8, yT_, hh * 8, a)
                yield
            po = a["po"][r]
            for n in range(2):
                for fc in range(nk):
                    p.op("pe", lambda e, fc=fc, n=n: e.matmul(
                        po[:, n * 512:(n + 1) * 512], lhsT=yT_[:, fc, :], rhs=a["wout"][fc][:, n * 512:(n + 1) * 512],
                        start=(fc == 0), stop=(fc == nk - 1)),
                        reads=_toks([yT_, a["wout"][fc]]), writes=_toks([po]), sig=(fc == nk - 1 and n == 1))
                yield
            self.post(po, g, 1.0, i, a)
            yield

        for i in range(0, self.NT, 2):
            gens = [tile_gen(i)]
            if i + 1 < self.NT:
                gens.append(tile_gen(i + 1))
            self.run_gens(gens)

    def to_fm_part(self, src, c0, dst, d0, a, nk=8):
        p = self.p
        pT = a["pT"][0]
        for k in range(nk):
            p.op("pe", lambda e, k=k: e.transpose(out=pT[:, k, :], in_=src[:, (c0 + k) * 128:(c0 + k + 1) * 128],
                                                   identity=self.ident[:, :]),
                 reads=_toks([src, self.ident]), writes=_toks([pT]), sig=(k == nk - 1))
        p.op("act", lambda e: e.copy(out=dst[:, d0:d0 + nk, :], in_=pT[:, 0:nk, :]),
             reads=_toks([pT]), writes=_toks([dst]))

    def dn(self, layer):
        with self.c.phase():
            self._dn(layer)

    def tri_inv(self, N, NT, ib, pw, out):
        p = self.p
        idb = self.ident[:, :].unsqueeze(1).to_broadcast([128, 4, 128])

        def msk(dst, src, m, eng="dve"):
            p.op(eng, lambda e: e.tensor_tensor(out=dst[:, :, :], in0=src[:, :, :],
                                                in1=m[:, :].unsqueeze(1).to_broadcast([128, 4, 128]), op=ALU.mult),
                 reads=_toks([src, m]), writes=_toks([dst]))

        def mm4(ps, L, R):
            for e_ in range(4):
                p.op("pe", lambda e, e_=e_: e.matmul(ps[:, e_ * 128:(e_ + 1) * 128], lhsT=L[:, e_, :], rhs=R[:, e_, :],
                                                     start=True, stop=True),
                     reads=_toks([L, R]), writes=_toks([ps]), sig=(e_ == 3))

        def ev_copy(dst, ps, eng):
            if eng == "act":
                p.op("act", lambda e: e.copy(out=dst[:, :, :].rearrange("p a b -> p (a b)"), in_=ps[:, :]),
                     reads=_toks([ps]), writes=_toks([dst]))
            else:
                p.op("dve", lambda e: e.tensor_copy(out=dst[:, :, :].rearrange("p a b -> p (a b)"), in_=ps[:, :]),
                     reads=_toks([ps]), writes=_toks([dst]))

        def ev_comb(dst, base, ps, op):
            p.op("dve", lambda e: e.tensor_tensor(out=dst[:, :, :].rearrange("p a b -> p (a b)"),
                                                  in0=base[:, :, :].rearrange("p a b -> p (a b)"), in1=ps[:, :], op=op),
                 reads=_toks([base, ps]), writes=_toks([dst]))

        A, AT = ib["A"], ib["AT"]
        msk(A[0], N, self.mlev[0])
        msk(AT[0], NT, self.mlev[0], "pool")
        yield
        for lv in range(3):
            ps = pw()
            mm4(ps, AT[lv], A[lv])
            ps2 = pw()
            mm4(ps2, A[lv], AT[lv])
            yield
            ev_copy(A[lv + 1], ps, "act")
            ev_copy(AT[lv + 1], ps2, "act")
            yield
        X, XT = ib["X"], ib["XT"]
        cur = 0
        p.op("dve", lambda e: e.tensor_tensor(out=X[0][:, :, :], in0=idb, in1=A[0][:, :, :], op=ALU.subtract),
             reads=_toks([self.ident, A[0]]), writes=_toks([X[0]]))
        p.op("pool", lambda e: e.tensor_tensor(out=XT[0][:, :, :], in0=idb, in1=AT[0][:, :, :], op=ALU.subtract),
             reads=_toks([self.ident, AT[0]]), writes=_toks([XT[0]]))
        yield
        for lv in range(1, 4):
            nx = 1 - cur
            ps = pw()
            mm4(ps, XT[cur], A[lv])
            ps2 = pw()
            mm4(ps2, A[lv], XT[cur])
            yield
            ev_comb(X[nx], X[cur], ps, ALU.add)
            ev_comb(XT[nx], XT[cur], ps2, ALU.add)
            yield
            cur = nx
        O, OT, Y, W_ = ib["O"], ib["OT"], ib["Y"], ib["W"]
        for li in range(1, 4):
            last = (li == 3)
            msk(O, N, self.mlev[li])
            nx = 1 - cur
            if not last:
                msk(OT, NT, self.mlev[li], "pool")
                yield
                ps = pw()
                mm4(ps, OT, X[cur])
                ps2 = pw()
                mm4(ps2, O, XT[cur])
                yield
                ev_copy(Y, ps, "act")
                ev_copy(W_, ps2, "act")
                yield
                ps = pw()
                mm4(ps, XT[cur], Y)
                ps2 = pw()
                mm4(ps2, X[cur], W_)
                yield
                ev_comb(X[nx], X[cur], ps, ALU.subtract)
                ev_comb(XT[nx], XT[cur], ps2, ALU.subtract)
                yield
            else:
                yield
                ps2 = pw()
                mm4(ps2, O, XT[cur])
                yield
                ev_copy(W_, ps2, "act")
                yield
                ps2 = pw()
                mm4(ps2, X[cur], W_)
                yield
                ev_comb(XT[nx], XT[cur], ps2, ALU.subtract)
                yield
            cur = nx
        out[0] = XT[cur]

    @staticmethod
    def run_gens(gens):
        act = list(gens)
        while act:
            for g_ in list(act):
                try:
                    next(g_)
                except StopIteration:
                    act.remove(g_)

    def _dn(self, layer):
        c, p, nc = self.c, self.p, self.nc
        jx = layer // 3
        NT = self.NT
        TB = 2 if NT % 2 == 0 else 1
        NB = NT // TB
        W = TB * 128
        CWN = 4096 + 32
        a = {}
        a["win"] = [c.sb([128, CWN], BF16, f"win{k}") for k in range(8)]
        a["g"] = c.sb([128, 1, D], F32, "g")
        a["h"] = [c.sb([128, D], F32, "h")] * 2
        a["xn"] = [c.sb([128, D], BF16, "xn")] * 2
        a["xT"] = [c.sb([128, 8, W], BF16, "xT")]
        ob = c.sb([128, 2 * D], BF16, "ob")
        a["junk"] = T(ob.h[:, 0:D], "junk", tok=ob.tok)
        a["ss"] = [c.sb([128, 1], F32, f"ss{i}") for i in range(4)]
        a["rs"] = [c.sb([128, 1], F32, f"rs{i}") for i in range(4)]
        a["pT"] = [c.ps([128, 8, 128], BF16, "pT")]
        pgen = [c.ps([128, 512], F32, f"pgen{i}") for i in range(2)]
        pgate = c.ps([128, 64], F32, "pgate")
        pws = [c.ps([128, 512], F32, f"pw{i}") for i in range(4)]
        pwi = [0]

        def pw():
            pwi[0] += 1
            return (pws + pgen)[pwi[0] % 6]
        cw = self.load_cols([self.dn_conv_w[jx, t_, :] for t_ in range(4)], 32, pgen[0], "cw")
        o = c.sb([128, 2 * D], F32, "o")
        c.stg = [T(o.h[:, 0:1024], "stgA"), T(o.h[:, 1024:2048], "stgB")]
        self._stg_i = 0
        wv_ = self.wload_pieces([T(t_.h[:, 0:4096], "w") for t_ in a["win"]], self.dn_w_in[jx, :, 0:4096])
        for k in range(8):
            self.wload(a["win"][k], a["win"][k][:, 4096:CWN], self.dn_w_in[jx, k * 128:(k + 1) * 128, 6144:6176])
        self.load_g(a["g"], layer, 1)
        g = a["g"]
        dtb = c.sb([128, 16], F32, "dtb")
        aneg = c.sb([128, 16], F32, "aneg")
        ngb = c.sb([128, 128], F32, "ngb")
        self.bcast_row(dtb, dtb[:, :], self.dn_dt_bias[jx, :])
        self.bcast_row(aneg, aneg[:, :], self.dn_a_log[jx, :])
        self.bcast_row(ngb, ngb[:, :], self.dn_norm_g[jx, :])
        p.op("act", lambda e: e.activation(out=aneg[:, :], in_=aneg[:, :], func=AF.Exp), reads=_toks([aneg]), writes=_toks([aneg]))
        p.op("dve", lambda e: e.tensor_scalar(out=aneg[:, :], in0=aneg[:, :], scalar1=-1.0, scalar2=None, op0=ALU.mult),
             reads=_toks([aneg]), writes=_toks([aneg]))
        halo = c.sb([128, 32, 3], F32, "halo")
        p.op("pool", lambda e: e.memset(halo[:, :, :], 0.0), writes=_toks([halo]))
        Sf = [c.sb([128, 4, 128], F32, f"S{h}") for h in range(4)]
        Sb = [c.sb([128, 4, 128], BF16, f"Sb{h}") for h in range(4)]
        for h in range(4):
            p.op("pool", lambda e, h=h: e.memset(Sf[h][:, :, :], 0.0), writes=_toks([Sf[h]]))
            p.op("pool", lambda e, h=h: e.memset(Sb[h][:, :, :], 0.0), writes=_toks([Sb[h]]))
        cb = [c.sb([128, W + 3], F32, f"cb{i}") for i in range(2)]
        sgb = [dict(acc=c.sb([128, W], F32, f"acc{i}"), sg=c.sb([128, W], F32, f"sgc{i}")) for i in range(2)]
        craw = [c.sb([128, W], F32, f"craw{i}") for i in range(2)]
        sq = [c.sb([128, W], F32, f"sq{i}") for i in range(2)]
        rn = [c.sb([128, W], F32, f"rn{i}") for i in range(2)]
        qn = c.sb([128, 8, W], BF16, "qn")
        kn = c.sb([128, 8, W], BF16, "kn")
        vfm = c.sb([128, 8, W], BF16, "vfm")
        vtm = [c.sb([128, 2 * D], BF16, f"vtm{j}") for j in range(TB)]
        ktm = [c.sb([128, D], BF16, f"ktm{j}") for j in range(TB)]
        pbs = c.sb([128, 32], F32, "pbs")
        beta = c.sb([128, 16], F32, "beta")
        gg = c.sb([128, 16], F32, "gg")
        Gc = c.sb([128, 16], F32, "Gc")
        eG = c.sb([128, 16], F32, "eG")
        bg = c.sb([128, 16], F32, "bg")
        elast = c.sb([128, 16], F32, "elast")
        kes = c.sb([128, 16], F32, "kes")
        mk = lambda nm: c.sb([128, 4, 128], BF16, nm)
        mkf = lambda nm: c.sb([128, 4, 128], F32, nm)

        def mkres(r):
            return dict(LH=mkf(f"LH{r}"), Ed=mk(f"Ed{r}"), EdT=mk(f"EdT{r}"),
                        KKm=c.sb([128, 2, 128], BF16, f"KKm{r}"), QKm=c.sb([128, 2, 128], BF16, f"QKm{r}"),
                        N=mk(f"N{r}"), NT=mk(f"NT{r}"), QKT=mk(f"QKT{r}"),
                        ib=dict(A=[mk(f"A{i}_{r}") for i in range(4)], AT=[mk(f"AT{i}_{r}") for i in range(4)],
                                X=[mk(f"X0_{r}"), mk(f"X1_{r}")], XT=[mk(f"XT0_{r}"), mk(f"XT1_{r}")],
                                O=mk(f"O{r}"), OT=mk(f"OT{r}"), Y=mk(f"Y{r}"), W=mk(f"Wt{r}")),
                        kb=mk(f"kb{r}"), ke=mk(f"ke{r}"), vb=mk(f"vb{r}"), wk=mk(f"wk{r}"), vn=mk(f"vn{r}"),
                        ot=mk(f"ot{r}"))
        RES = [mkres(0), mkres(1)]
        ss16 = c.sb([128, 16], F32, "ss16")
        osq = ob
        gi = 0
        hc = 0
        for b in range(NB):
            xT = a["xT"][0]
            for j in range(TB):
                self.pre(b * TB + j, a, g, a["xn"][0])
                self.to_fm(a["xn"][0], xT, j, a)
            def dn_chunk(cc):
                pg = pgen[cc % 2]
                craw_, sq_, rn_ = craw[cc % 2], sq[cc % 2], rn[cc % 2]
                for k in range(8):
                    p.op("pe", lambda e, k=k: e.matmul(
                        pg[:, 0:W], lhsT=a["win"][k][:, cc * 128:(cc + 1) * 128], rhs=xT[:, k, :],
                        start=(k == 0), stop=(k == 7)),
                        reads=_toks([wv_(k, cc * 128), xT]), writes=_toks([pg]), sig=(k == 7))
                yield
                if cc < 16:
                    yield from self.conv_chunk(pg, cb[cc % 2], halo, cw, None, cc, W, craw_[:, :], craw_, sgb[cc % 2], False, b == 0)
                    p.op("pool", lambda e: e.tensor_tensor(out=sq_[:, :], in0=craw_[:, :], in1=craw_[:, :], op=ALU.mult),
                         reads=_toks([craw_]), writes=_toks([sq_]))
                    yield
                    pn = pws[cc % 2]
                    p.op("pe", lambda e: e.matmul(pn[:, 0:W], lhsT=self.onesf[:, :], rhs=sq_[:, :], start=True, stop=True),
                         reads=_toks([self.onesf, sq_]), writes=_toks([pn]))
                    yield
                    p.op("act", lambda e: e.activation(out=rn_[:, :], in_=pn[:, 0:W], func=AF.Ln, bias=self.epsc[:, 0:1]),
                         reads=_toks([pn, self.epsc]), writes=_toks([rn_]))
                    yield
                    p.op("act", lambda e: e.activation(out=rn_[:, :], in_=rn_[:, :], func=AF.Exp, scale=-0.5),
                         reads=_toks([rn_]), writes=_toks([rn_]))
                    yield
                    dstt = qn if cc < 8 else kn
                    scl = 128.0 ** -0.5 if cc < 8 else 1.0
                    p.op("dve", lambda e: e.scalar_tensor_tensor(
                        out=dstt[:, cc % 8, :], in0=craw_[:, :], scalar=scl, in1=rn_[:, :], op0=ALU.mult, op1=ALU.mult),
                        reads=_toks([craw_, rn_]), writes=_toks([dstt]))
                    yield
                else:
                    yield from self.conv_chunk(pg, cb[cc % 2], halo, cw, None, cc, W, vfm[:, cc % 8, :], vfm, sgb[cc % 2], False, b == 0)

            for cc in range(0, 32, 2):
                self.run_gens([dn_chunk(cc), dn_chunk(cc + 1)])
                if cc + 1 == 15:
                    self.fm_to_tm(kn, None, [(ktm[j][:, :], ktm[j]) for j in range(TB)], a)
                if cc + 1 in (23, 31):
                    grp = (cc + 1 - 16) // 8
                    self.fm_to_tm(vfm, None, [(vtm[j][:, grp * D:(grp + 1) * D], vtm[j]) for j in range(TB)], a)
            for j in range(TB):
                i = b * TB + j
                tsl = slice(j * 128, (j + 1) * 128)
                for k in range(8):
                    p.op("pe", lambda e, k=k: e.matmul(pgate[:, 0:32], lhsT=xT[:, k, tsl], rhs=a["win"][k][:, 4096:4128],
                                                       start=(k == 0), stop=(k == 7)),
                         reads=_toks([a["win"][k], xT]), writes=_toks([pgate]), sig=(k == 7))
                p.op("act", lambda e: e.copy(out=beta[:, :], in_=pgate[:, 0:16]), reads=_toks([pgate]), writes=_toks([beta]))
                self.sigmoid_inplace(beta[:, :], beta)
                p.op("dve", lambda e: e.tensor_tensor(out=gg[:, :], in0=pgate[:, 16:32], in1=dtb[:, :], op=ALU.add),
                     reads=_toks([pgate, dtb]), writes=_toks([gg]))
                p.op("act", lambda e: e.activation(out=gg[:, :], in_=gg[:, :], func=AF.Exp), reads=_toks([gg]), writes=_toks([gg]))
                p.op("act", lambda e: e.activation(out=gg[:, :], in_=gg[:, :], func=AF.Ln, bias=self.onec[:, 0:1]),
                     reads=_toks([gg, self.onec]), writes=_toks([gg]))
                p.op("dve", lambda e: e.tensor_tensor(out=gg[:, :], in0=gg[:, :], in1=aneg[:, :], op=ALU.mult),
                     reads=_toks([gg, aneg]), writes=_toks([gg]))
                p.op("pe", lambda e: e.matmul(pgate[:, 32:48], lhsT=self.triLE[:, :], rhs=gg[:, :], start=True, stop=True),
                     reads=_toks([self.triLE, gg]), writes=_toks([pgate]))
                p.op("pe", lambda e: e.matmul(pgate[:, 48:64], lhsT=self.onesf[:, :], rhs=gg[:, :], start=True, stop=True),
                     reads=_toks([self.onesf, gg]), writes=_toks([pgate]))
                p.op("act", lambda e: e.copy(out=Gc[:, :], in_=pgate[:, 32:48]), reads=_toks([pgate]), writes=_toks([Gc]))
                p.op("act", lambda e: e.activation(out=eG[:, :], in_=Gc[:, :], func=AF.Exp), reads=_toks([Gc]), writes=_toks([eG]))
                p.op("act", lambda e: e.activation(out=elast[:, :], in_=pgate[:, 48:64], func=AF.Exp),
                     reads=_toks([pgate]), writes=_toks([elast]))
                p.op("dve", lambda e: e.tensor_tensor(out=kes[:, :], in0=pgate[:, 48:64], in1=Gc[:, :], op=ALU.subtract),
                     reads=_toks([pgate, Gc]), writes=_toks([kes]))
                p.op("act", lambda e: e.activation(out=kes[:, :], in_=kes[:, :], func=AF.Exp), reads=_toks([kes]), writes=_toks([kes]))
                p.op("dve", lambda e: e.tensor_tensor(out=bg[:, :], in0=beta[:, :], in1=eG[:, :], op=ALU.mult),
                     reads=_toks([beta, eG]), writes=_toks([bg]))
                def batch_gen(bt, R, slot):
                    hs = slice(4 * bt, 4 * bt + 4)
                    banks = [pws[2 * slot], pws[2 * slot + 1], pgen[slot]]
                    bi = [0]

                    def pw():
                        bi[0] += 1
                        return banks[bi[0] % 3]
                    LH, Ed, EdT, KKm, QKm, Nn, NnT, QKT = (R[k_] for k_ in ("LH", "Ed", "EdT", "KKm", "QKm", "N", "NT", "QKT"))
                    p.op("pool", lambda e: e.tensor_tensor(
                        out=LH[:, :, :], in0=self.maskGT[:, :].unsqueeze(1).to_broadcast([128, 4, 128]),
                        in1=gg[:, hs].unsqueeze(2).to_broadcast([128, 4, 128]), op=ALU.mult),
                        reads=_toks([self.maskGT, gg]), writes=_toks([LH]))
                    yield
                    pD, pDT, pK = pw(), pw(), pw()
                    for e_ in range(4):
                        p.op("pe", lambda e, e_=e_: e.matmul(pD[:, e_ * 128:(e_ + 1) * 128], lhsT=self.triLE[:, :],
                                                             rhs=LH[:, e_, :], start=True, stop=True),
                             reads=_toks([LH, self.triLE]), writes=_toks([pD]), sig=(e_ == 3))
                    for e_ in range(4):
                        p.op("pe", lambda e, e_=e_: e.matmul(pDT[:, e_ * 128:(e_ + 1) * 128], lhsT=LH[:, e_, :],
                                                             rhs=self.triLE[:, :], start=True, stop=True),
                             reads=_toks([LH, self.triLE]), writes=_toks([pDT]), sig=(e_ == 3))
                    for q_ in range(2):
                        hq = 2 * bt + q_
                        p.op("pe", lambda e, q_=q_, hq=hq: e.matmul(
                            pK[:, q_ * 128:(q_ + 1) * 128], lhsT=kn[:, hq, tsl], rhs=kn[:, hq, tsl], start=True, stop=True),
                            reads=_toks([kn]), writes=_toks([pK]), sig=False)
                        p.op("pe", lambda e, q_=q_, hq=hq: e.matmul(
                            pK[:, 256 + q_ * 128:256 + (q_ + 1) * 128], lhsT=kn[:, hq, tsl], rhs=qn[:, hq, tsl],
                            start=True, stop=True),
                            reads=_toks([kn, qn]), writes=_toks([pK]), sig=(q_ == 1))
                    yield
                    p.op("act", lambda e: e.activation(out=Ed[:, :, :].rearrange("p a b -> p (a b)"), in_=pD[:, :],
                                                       func=AF.Exp), reads=_toks([pD]), writes=_toks([Ed]))
                    p.op("act", lambda e: e.activation(out=EdT[:, :, :].rearrange("p a b -> p (a b)"), in_=pDT[:, :],
                                                       func=AF.Exp), reads=_toks([pDT]), writes=_toks([EdT]))
                    p.op("dve", lambda e: e.tensor_tensor(
                        out=KKm[:, :, :], in0=pK[:, 0:256].rearrange("p (a b) -> p a b", b=128),
                        in1=self.maskGT[:, :].unsqueeze(1).to_broadcast([128, 2, 128]), op=ALU.mult),
                        reads=_toks([pK, self.maskGT]), writes=_toks([KKm]))
                    p.op("dve", lambda e: e.tensor_tensor(
                        out=QKm[:, :, :], in0=pK[:, 256:512].rearrange("p (a b) -> p a b", b=128),
                        in1=self.triLE[:, :].unsqueeze(1).to_broadcast([128, 2, 128]), op=ALU.mult),
                        reads=_toks([pK, self.triLE]), writes=_toks([QKm]))
                    yield
                    for e_ in range(4):
                        h = 4 * bt + e_
                        p.op("dve", lambda e, e_=e_, h=h: e.scalar_tensor_tensor(
                            out=Nn[:, e_, :], in0=Ed[:, e_, :], scalar=beta[:, h:h + 1], in1=KKm[:, e_ // 2, :],
                            op0=ALU.mult, op1=ALU.mult), reads=_toks([Ed, beta, KKm]), writes=_toks([Nn]))
                    p.op("pool", lambda e: e.tensor_tensor(
                        out=QKT[:, :, :].rearrange("p (q r) b -> p q r b", r=2),
                        in0=EdT[:, :, :].rearrange("p (q r) b -> p q r b", r=2),
                        in1=QKm[:, :, :].unsqueeze(2).to_broadcast([128, 2, 2, 128]), op=ALU.mult),
                        reads=_toks([EdT, QKm]), writes=_toks([QKT]))
                    yield
                    pT = a["pT"][0]
                    for e_ in range(4):
                        p.op("pe", lambda e, e_=e_: e.transpose(out=pT[:, e_, :], in_=Nn[:, e_, :], identity=self.ident[:, :]),
                             reads=_toks([Nn, self.ident]), writes=_toks([pT]), sig=(e_ == 3))
                    p.op("act", lambda e: e.copy(out=NnT[:, :, :], in_=pT[:, 0:4, :]), reads=_toks([pT]), writes=_toks([NnT]))
                    yield
                    uo = [None]
                    yield from self.tri_inv(Nn, NnT, R["ib"], pw, uo)
                    U = uo[0]
                    kb, ke, vb4, wk, vn, ot = R["kb"], R["ke"], R["vb"], R["wk"], R["vn"], R["ot"]
                    for e_ in range(4):
                        h = 4 * bt + e_
                        ksl = ktm[j][:, (h // 2) * 128:(h // 2 + 1) * 128]
                        p.op("act", lambda e, e_=e_, ksl=ksl, h=h: e.activation(out=kb[:, e_, :], in_=ksl, func=AF.Copy,
                                                                               scale=bg[:, h:h + 1]),
                             reads=_toks([ktm[j], bg]), writes=_toks([kb]))
                        p.op("act", lambda e, e_=e_, ksl=ksl, h=h: e.activation(out=ke[:, e_, :], in_=ksl, func=AF.Copy,
                                                                               scale=kes[:, h:h + 1]),
                             reads=_toks([ktm[j], kes]), writes=_toks([ke]))
                    p.op("pool", lambda e: e.tensor_tensor(
                        out=vb4[:, :, :], in0=vtm[j][:, 4 * bt * 128:(4 * bt + 4) * 128].rearrange("p (h d) -> p h d", d=128),
                        in1=beta[:, hs].unsqueeze(2).to_broadcast([128, 4, 128]), op=ALU.mult),
                        reads=_toks([vtm[j], beta]), writes=_toks([vb4]))
                    yield
                    ps1 = pw()
                    for e_ in range(4):
                        p.op("pe", lambda e, e_=e_: e.matmul(ps1[:, e_ * 128:(e_ + 1) * 128], lhsT=kb[:, e_, :], rhs=U[:, e_, :],
                                                             start=True, stop=True),
                             reads=_toks([kb, U]), writes=_toks([ps1]), sig=(e_ == 3))
                    yield
                    p.op("act", lambda e: e.activation(out=wk[:, :, :].rearrange("p a b -> p (a b)"), in_=ps1[:, :], func=AF.Copy,
                                                       scale=-1.0), reads=_toks([ps1]), writes=_toks([wk]))
                    yield
                    ps2 = pw()
                    for e_ in range(4):
                        p.op("pe", lambda e, e_=e_: e.matmul(ps2[:, e_ * 128:(e_ + 1) * 128], lhsT=U[:, e_, :], rhs=vb4[:, e_, :],
                                                             start=True, stop=False),
                             reads=_toks([vb4, U]), writes=_toks([ps2]), sig=False)
                        p.op("pe", lambda e, e_=e_: e.matmul(ps2[:, e_ * 128:(e_ + 1) * 128], lhsT=wk[:, e_, :], rhs=Sb[bt][:, e_, :],
                                                             start=False, stop=True),
                             reads=_toks([wk, Sb[bt]]), writes=_toks([ps2]), sig=(e_ == 3))
                    ps3 = pw()
                    for e_ in range(4):
                        hq = (4 * bt + e_) // 2
                        p.op("pe", lambda e, e_=e_, hq=hq: e.matmul(ps3[:, e_ * 128:(e_ + 1) * 128], lhsT=qn[:, hq, tsl],
                                                                    rhs=Sb[bt][:, e_, :], start=True, stop=True),
                             reads=_toks([qn, Sb[bt]]), writes=_toks([ps3]), sig=(e_ == 3))
                    yield
                    p.op("dve", lambda e: e.tensor_copy(out=vn[:, :, :].rearrange("p a b -> p (a b)"), in_=ps2[:, :]),
                         reads=_toks([ps2]), writes=_toks([vn]))
                    p.op("dve", lambda e: e.tensor_tensor(
                        out=ot[:, :, :], in0=ps3[:, :].rearrange("p (a b) -> p a b", b=128),
                        in1=eG[:, hs].unsqueeze(2).to_broadcast([128, 4, 128]), op=ALU.mult),
                        reads=_toks([ps3, eG]), writes=_toks([ot]))
                    yield
                    ps4, ps5 = pw(), pw()
                    for e_ in range(4):
                        p.op("pe", lambda e, e_=e_: e.matmul(ps4[:, e_ * 128:(e_ + 1) * 128], lhsT=QKT[:, e_, :], rhs=vn[:, e_, :],
                                                             start=True, stop=True),
                             reads=_toks([QKT, vn]), writes=_toks([ps4]), sig=(e_ == 3))
                    for e_ in range(4):
                        p.op("pe", lambda e, e_=e_: e.matmul(ps5[:, e_ * 128:(e_ + 1) * 128], lhsT=ke[:, e_, :], rhs=vn[:, e_, :],
                                                             start=True, stop=True),
                             reads=_toks([ke, vn]), writes=_toks([ps5]), sig=(e_ == 3))
                    yield
                    p.op("dve", lambda e: e.tensor_tensor(
                        out=o[:, 4 * bt * 128:(4 * bt + 4) * 128], in0=ot[:, :, :].rearrange("p a b -> p (a b)"), in1=ps4[:, :],
                        op=ALU.add), reads=_toks([ot, ps4]), writes=_toks([o]))
                    p.op("pool", lambda e: e.tensor_tensor(
                        out=Sf[bt][:, :, :], in0=Sf[bt][:, :, :], in1=elast[:, hs].unsqueeze(2).to_broadcast([128, 4, 128]),
                        op=ALU.mult), reads=_toks([Sf[bt], elast]), writes=_toks([Sf[bt]]))
                    yield
                    p.op("dve", lambda e: e.tensor_tensor(
                        out=Sf[bt][:, :, :].rearrange("p a b -> p (a b)"), in0=Sf[bt][:, :, :].rearrange("p a b -> p (a b)"),
                        in1=ps5[:, :], op=ALU.add), reads=_toks([Sf[bt], ps5]), writes=_toks([Sf[bt]]))
                    yield
                    p.op("act", lambda e: e.copy(out=Sb[bt][:, :, :], in_=Sf[bt][:, :, :]),
                         reads=_toks([Sf[bt]]), writes=_toks([Sb[bt]]))
                    yield

                import os as _os
                if _os.environ.get("NOINTER"):
                    for bt_ in range(4):
                        self.run_gens([batch_gen(bt_, RES[bt_ % 2], bt_ % 2)])
                else:
                    self.run_gens([batch_gen(0, RES[0], 0), batch_gen(1, RES[1], 1)])
                    self.run_gens([batch_gen(2, RES[0], 0), batch_gen(3, RES[1], 1)])
                if i == 1:
                    self.dbg("o", o, o[:, :], [128, 2 * D])
                p.op("pool", lambda e: e.tensor_tensor(out=osq[:, :], in0=o[:, :], in1=o[:, :], op=ALU.mult),
                     reads=_toks([o]), writes=_toks([osq]))
                p.op("dve", lambda e: e.tensor_reduce(out=ss16[:, :], in_=osq[:, :].rearrange("p (h d) -> p h d", d=128),
                                                      axis=AX.X, op=ALU.add), reads=_toks([osq]), writes=_toks([ss16]))
                p.op("dve", lambda e: e.tensor_scalar(out=ss16[:, :], in0=ss16[:, :], scalar1=1.0 / 128.0, scalar2=None,
                                                      op0=ALU.mult), reads=_toks([ss16]), writes=_toks([ss16]))
                self.rsqrt_small(ss16[:, :], ss16[:, :], [ss16], [ss16], EPS)
                p.op("dve", lambda e: e.tensor_tensor(
                    out=o[:, :].rearrange("p (h d) -> p h d", d=128), in0=o[:, :].rearrange("p (h d) -> p h d", d=128),
                    in1=ss16[:, :].unsqueeze(2).to_broadcast([128, 16, 128]), op=ALU.mult),
                    reads=_toks([o, ss16]), writes=_toks([o]))
                p.op("pool", lambda e: e.tensor_tensor(
                    out=ob[:, :].rearrange("p (h d) -> p h d", d=128), in0=o[:, :].rearrange("p (h d) -> p h d", d=128),
                    in1=ngb[:, :].unsqueeze(1).to_broadcast([128, 16, 128]), op=ALU.mult),
                    reads=_toks([o, ngb]), writes=_toks([ob]))
                p.op("sp", lambda e, i=i: e.dma_start(out=self.scr[i * 128:(i + 1) * 128, :], in_=ob[:, :]),
                     reads=_toks([ob]), writes=[self.ytok[i]], dma=ob.tok)

    def rw(self, layer):
        with self.c.phase():
            self._rw(layer)

    def _rw(self, layer):
        c, p, nc = self.c, self.p, self.nc
        jx = layer // 3
        NT = self.NT
        TB = 2 if NT % 2 == 0 else 1
        NB = NT // TB
        W = TB * 128
        a = {}
        wr = [c.sb([128, D], BF16, f"wr{k}") for k in range(8)]
        wk = [c.sb([128, D], BF16, f"wk{k}") for k in range(8)]
        wv = [c.sb([128, D], BF16, f"wv{k}") for k in range(8)]
        wl1t = c.sb([128, 8, 288], BF16, "wl1")
        wl1 = [wl1t.view((slice(None), k, slice(None))) for k in range(8)]
        w2t = c.sb([64, D], BF16, "w2t")
        a2t = c.sb([64, D], BF16, "a2t")
        g2a = c.sb([128, D], BF16, "g2a")
        g2b = c.sb([32, D], BF16, "g2b")
        a["g"] = c.sb([128, 1, D], F32, "g")
        a["h"] = [c.sb([128, D], F32, "h")] * 2
        a["xn"] = [c.sb([128, D], BF16, "xn")] * 2
        a["junk"] = c.sb([128, D], BF16, "junk")
        a["ss"] = [c.sb([128, 1], F32, f"ss{i}") for i in range(4)]
        a["rs"] = [c.sb([128, 1], F32, f"rs{i}") for i in range(4)]
        a["pT"] = [c.ps([128, 8, 128], BF16, "pT")]
        pgen = [c.ps([128, 512], F32, f"pgen{i}") for i in range(2)]
        prk = c.ps([128, 16], F32, "prk")
        pws = [c.ps([128, 512], F32, f"pw{i}") for i in range(4)]
        pwi = [0]

        def pw():
            pwi[0] += 1
            return pws[pwi[0] % 4]
        for k in range(8):
            ks = slice(k * 128, (k + 1) * 128)
            self.wload(wr[k], wr[k][:, :], self.rw_w_rkv[jx, 0, ks, :])
            self.wload(wk[k], wk[k][:, :], self.rw_w_rkv[jx, 1, ks, :])
            self.wload(wv[k], wv[k][:, :], self.rw_w_rkv[jx, 2, ks, :])
        for k in range(8):
            ks = slice(k * 128, (k + 1) * 128)
            self.wload(wl1[k], wl1[k][:, 0:64], self.rw_w1[jx, ks, :])
            self.wload(wl1[k], wl1[k][:, 64:128], self.rw_a1[jx, ks, :])
            self.wload(wl1[k], wl1[k][:, 128:288], self.rw_g1[jx, ks, :])
        self.wload(w2t, w2t[:, :], self.rw_w2[jx, :, :])
        self.wload(a2t, a2t[:, :], self.rw_a2[jx, :, :])
        self.wload(g2a, g2a[:, :], self.rw_g2[jx, 0:128, :])
        self.wload(g2b, g2b[:, :], self.rw_g2[jx, 128:160, :])
        self.load_g(a["g"], layer, 1)
        g = a["g"]
        cols = self.load_cols([self.rw_mu[jx, i_, :] for i_ in range(6)] +
                              [self.rw_w0[jx, :], self.rw_a0[jx, :], self.rw_k_k[jx, :], self.rw_k_a[jx, :],
                               self.rw_r_k[jx].rearrange("h d -> (h d)")], 8, pgen[0], "cols")
        ncol = c.sb([128, 8, 2], F32, "ncol")
        p.op("dve", lambda e: e.tensor_scalar(out=ncol[:, :, :], in0=cols[:, :, 6:8], scalar1=-1.0, scalar2=None, op0=ALU.mult),
             reads=_toks([cols]), writes=_toks([ncol]))
        lng = c.sb([128, D], F32, "lng")
        lnb = c.sb([128, D], F32, "lnb")
        self.bcast_row(lng, lng[:, :], self.rw_ln_g[jx, :])
        self.bcast_row(lnb, lnb[:, :], self.rw_ln_b[jx, :])
        ind = c.sb([128, 8, 16], BF16, "ind")
        p.op("pool", lambda e: e.memset(ind[:, :, :], 0.0), writes=_toks([ind]))
        for cc in range(8):
            for hp in range(2):
                p.op("pool", lambda e, cc=cc, hp=hp: e.memset(ind[hp * 64:(hp + 1) * 64, cc, 2 * cc + hp:2 * cc + hp + 1], 1.0),
                     reads=_toks([ind]), writes=_toks([ind]))
        Zc = [c.sb([128, 64], F32, f"Zc{i}") for i in range(8)]
        Zb = [c.sb([128, 64], BF16, f"Zb{i}") for i in range(8)]
        for i_ in range(8):
            p.op("pool", lambda e, i_=i_: e.memset(Zc[i_][:, :], 0.0), writes=_toks([Zc[i_]]))
            p.op("pool", lambda e, i_=i_: e.memset(Zb[i_][:, :], 0.0), writes=_toks([Zb[i_]]))
        xTh = c.sb([128, 8, W + 1], BF16, "xTh")
        p.op("pool", lambda e: e.memset(xTh[:, :, 0:1], 0.0), writes=_toks([xTh]))
        xx = c.sb([128, 8, W], BF16, "xx")
        xm = [c.sb([128, 8, W], BF16, f"xm{i}") for i in range(6)]
        h1 = c.sb([64, W], BF16, "h1")
        h2 = c.sb([64, W], BF16, "h2")
        h3a = c.sb([128, W], BF16, "h3a")
        h3b = c.sb([32, W], BF16, "h3b")
        h3f = c.sb([128, W], F32, "h3f")
        h1f = T(h3f.h[0:64, :], "h1f", tok=h3f.tok)
        fm = lambda nm: c.sb([128, W], F32, nm)
        r_c, k_c, lw, a_c, kkr, sqk, rn, kk, kmod, cl, eW, eWi = [fm(n) for n in
            ("r_c", "k_c", "lw", "a_c", "kkr", "sqk", "rn", "kk", "kmod", "cl", "eW", "eWi")]
        t1, bt_, eWm = sqk, kkr, rn
        rt_ = c.sb([128, 8, W], BF16, "rt")
        kt_ = c.sb([128, 8, W], BF16, "kt")
        at_ = c.sb([128, 8, W], BF16, "at")
        btl = c.sb([128, 8, W], BF16, "btl")
        rkr = c.sb([128, 8, W], BF16, "rkr")
        eWl = c.sb([128, 8, TB], F32, "eWl")
        ktm = [c.sb([128, D], BF16, f"ktm{j}") for j in range(TB)]
        btm = [c.sb([128, D], BF16, f"btm{j}") for j in range(TB)]
        vtm = [c.sb([128, D], BF16, f"vtm{j}") for j in range(TB)]
        gtm = [c.sb([128, D], BF16, f"gtm{j}") for j in range(TB)]
        rks = c.sb([128, 16], F32, "rks")
        mk = lambda nm: c.sb([128, 4, 128], BF16, nm)
        Nn, NnT, LakT, MrbT, MrkT = mk("N"), mk("NT"), mk("LakT"), mk("MrbT"), mk("MrkT")
        ib = dict(A=[mk(f"A{i}") for i in range(4)], AT=[mk(f"AT{i}") for i in range(4)],
                  X=[mk("X0"), mk("X1")], XT=[mk("XT0"), mk("XT1")], O=mk("O"), OT=mk("OT"), Y=mk("Y"), W=mk("Wt"))
        inner = [c.sb([128, 64], BF16, f"inner{i}") for i in range(2)]
        Pm = [c.sb([128, 128], BF16, f"Pm{i}") for i in range(2)]
        ztmp = c.sb([128, 64], F32, "ztmp")
        y = c.sb([128, D], F32, "y")
        yc = a["h"][0]
        s16 = c.sb([128, 16], F32, "s16")
        v16 = c.sb([128, 16], F32, "v16")
        ob = a["xn"][0]
        gi = 0
        ic = 0
        import os as _os
        _stop = _os.environ.get("RW_STOP")
        if _stop == "0":
            return
        for b in range(NB):
            for j in range(TB):
                self.pre(b * TB + j, a, g, a["xn"][0])
                self.to_fm(a["xn"][0], xTh, j, a, off=1)
            xc = xTh[:, :, 1:W + 1]
            p.op("dve", lambda e: e.tensor_tensor(out=xx[:, :, :], in0=xTh[:, :, 0:W], in1=xc, op=ALU.subtract),
                 reads=_toks([xTh]), writes=_toks([xx]))
            for i_ in range(6):
                eng = "dve"
                p.op(eng, lambda e, i_=i_: e.tensor_tensor(
                    out=xm[i_][:, :, :], in0=xx[:, :, :], in1=cols[:, :, i_:i_ + 1].to_broadcast([128, 8, W]), op=ALU.mult),
                    reads=_toks([xx, cols]), writes=_toks([xm[i_]]))
                p.op(eng, lambda e, i_=i_: e.tensor_tensor(out=xm[i_][:, :, :], in0=xm[i_][:, :, :], in1=xc, op=ALU.add),
                     reads=_toks([xm[i_], xTh]), writes=_toks([xm[i_]]))
            p.op("pool", lambda e: e.tensor_copy(out=xTh[:, :, 0:1], in_=xTh[:, :, W:W + 1]),
                 reads=_toks([xTh]), writes=_toks([xTh]))
            xr, xw_, xk, xv, xa, xg = xm
            ph = pw()
            for k in range(8):
                p.op("pe", lambda e, k=k, ph=ph: e.matmul(ph[0:64, 0:W], lhsT=wl1[k][:, 0:64], rhs=xw_[:, k, :],
                                                          start=(k == 0), stop=(k == 7)),
                     reads=_toks([wl1[k], xw_]), writes=_toks([ph]), sig=(k == 7))
            p.op("act", lambda e, ph=ph: e.activation(out=h1f[:, :], in_=ph[0:64, 0:W], func=AF.Exp, scale=-2.0),
                 reads=_toks([ph]), writes=_toks([h1f]))
            p.op("act", lambda e: e.activation(out=h1f[:, :], in_=h1f[:, :], func=AF.Ln, bias=self.onec[0:64, 0:1]),
                 reads=_toks([h1f, self.onec]), writes=_toks([h1f]))
            p.op("act", lambda e: e.activation(out=h1f[:, :], in_=h1f[:, :], func=AF.Exp, scale=-1.0),
                 reads=_toks([h1f]), writes=_toks([h1f]))
            p.op("dve", lambda e: e.tensor_scalar(out=h1[:, :], in0=h1f[:, :], scalar1=2.0, scalar2=-1.0, op0=ALU.mult, op1=ALU.add),
                 reads=_toks([h1f]), writes=_toks([h1]))
            ph = pw()
            for k in range(8):
                p.op("pe", lambda e, k=k, ph=ph: e.matmul(ph[0:64, 0:W], lhsT=wl1[k][:, 64:128], rhs=xa[:, k, :],
                                                          start=(k == 0), stop=(k == 7)),
                     reads=_toks([wl1[k], xa]), writes=_toks([ph]), sig=(k == 7))
            p.op("act", lambda e, ph=ph: e.copy(out=h2[:, :], in_=ph[0:64, 0:W]), reads=_toks([ph]), writes=_toks([h2]))
            for part, (c0_, c1_, rows, dst) in enumerate(((128, 256, 128, h3a), (256, 288, 32, h3b))):
                ph = pw()
                for k in range(8):
                    p.op("pe", lambda e, k=k, ph=ph, c0_=c0_, c1_=c1_, rows=rows: e.matmul(
                        ph[0:rows, 0:W], lhsT=wl1[k][:, c0_:c1_], rhs=xg[:, k, :], start=(k == 0), stop=(k == 7)),
                        reads=_toks([wl1[k], xg]), writes=_toks([ph]), sig=(k == 7))
                p.op("act", lambda e, ph=ph, rows=rows: e.activation(out=h3f[0:rows, :], in_=ph[0:rows, 0:W], func=AF.Exp, scale=-1.0),
                     reads=_toks([ph]), writes=_toks([h3f]))
                p.op("act", lambda e, rows=rows: e.activation(out=h3f[0:rows, :], in_=h3f[0:rows, :], func=AF.Ln,
                                                              bias=self.onec[0:rows, 0:1]),
                     reads=_toks([h3f, self.onec]), writes=_toks([h3f]))
                p.op("act", lambda e, rows=rows, dst=dst: e.activation(out=dst[:, :], in_=h3f[0:rows, :], func=AF.Exp, scale=-1.0),
                     reads=_toks([h3f]), writes=_toks([dst]))
            if _stop == "1":
                return
            for cc in range(8):
                cs = slice(cc * 128, (cc + 1) * 128)
                pr, pk = pgen[0], pgen[1]
                for k in range(8):
                    p.op("pe", lambda e, k=k, cs=cs: e.matmul(pr[:, 0:W], lhsT=wr[k][:, cs], rhs=xr[:, k, :],
                                                              start=(k == 0), stop=(k == 7)),
                         reads=_toks([wr[k], xr]), writes=_toks([pr]), sig=(k == 7))
                for k in range(8):
                    p.op("pe", lambda e, k=k, cs=cs: e.matmul(pk[:, 0:W], lhsT=wk[k][:, cs], rhs=xk[:, k, :],
                                                              start=(k == 0), stop=(k == 7)),
                         reads=_toks([wk[k], xk]), writes=_toks([pk]), sig=(k == 7))
                p.op("act", lambda e: e.copy(out=r_c[:, :], in_=pr[:, 0:W]), reads=_toks([pr]), writes=_toks([r_c]))
                p.op("act", lambda e: e.copy(out=k_c[:, :], in_=pk[:, 0:W]), reads=_toks([pk]), writes=_toks([k_c]))
                pl = pw()
                p.op("pe", lambda e, pl=pl, cs=cs: e.matmul(pl[:, 0:W], lhsT=w2t[0:64, cs], rhs=h1[0:64, :], start=True, stop=True),
                     reads=_toks([w2t, h1]), writes=_toks([pl]))
                p.op("pe", lambda e, pl=pl, cs=cs: e.matmul(pl[:, 256:256 + W], lhsT=a2t[0:64, cs], rhs=h2[0:64, :], start=True, stop=True),
                     reads=_toks([a2t, h2]), writes=_toks([pl]))
                for (dst, off_, ci) in ((lw, 0, 0), (a_c, 256, 1)):
                    p.op("act", lambda e, dst=dst, off_=off_, ci=ci, pl=pl, cc=cc: e.activation(
                        out=dst[:, :], in_=pl[:, off_:off_ + W], func=AF.Exp, scale=-1.0, bias=ncol[:, cc, ci:ci + 1]),
                        reads=_toks([pl, ncol]), writes=_toks([dst]))
                    p.op("act", lambda e, dst=dst: e.activation(out=dst[:, :], in_=dst[:, :], func=AF.Ln, bias=self.onec[:, 0:1]),
                         reads=_toks([dst, self.onec]), writes=_toks([dst]))
                    p.op("act", lambda e, dst=dst: e.activation(out=dst[:, :], in_=dst[:, :], func=AF.Exp, scale=-1.0),
                         reads=_toks([dst]), writes=_toks([dst]))
                p.op("dve", lambda e, cc=cc: e.tensor_scalar(out=kkr[:, :], in0=k_c[:, :], scalar1=cols[:, cc, 8:9], scalar2=None,
                                                             op0=ALU.mult), reads=_toks([k_c, cols]), writes=_toks([kkr]))
                p.op("act", lambda e: e.activation(out=sqk[:, :], in_=kkr[:, :], func=AF.Square),
                     reads=_toks([kkr]), writes=_toks([sqk]))
                pn = pw()
                p.op("pe", lambda e, pn=pn: e.matmul(pn[:, 0:W], lhsT=self.bd64f[:, :], rhs=sqk[:, :], start=True, stop=True),
                     reads=_toks([self.bd64f, sqk]), writes=_toks([pn]))
                p.op("act", lambda e, pn=pn: e.activation(out=rn[:, :], in_=pn[:, 0:W], func=AF.Ln, bias=self.epsc[:, 0:1]),
                     reads=_toks([pn, self.epsc]), writes=_toks([rn]))
                p.op("act", lambda e: e.activation(out=rn[:, :], in_=rn[:, :], func=AF.Exp, scale=-0.5),
                     reads=_toks([rn]), writes=_toks([rn]))
                p.op("dve", lambda e: e.tensor_tensor(out=kk[:, :], in0=kkr[:, :], in1=rn[:, :], op=ALU.mult),
                     reads=_toks([kkr, rn]), writes=_toks([kk]))
                p.op("dve", lambda e, cc=cc: e.tensor_scalar(out=t1[:, :], in0=a_c[:, :], scalar1=-1.0, scalar2=cols[:, cc, 9:10],
                                                             op0=ALU.add, op1=ALU.mult), reads=_toks([a_c, cols]), writes=_toks([t1]))
                p.op("dve", lambda e: e.scalar_tensor_tensor(out=kmod[:, :], in0=t1[:, :], scalar=1.0, in1=k_c[:, :],
                                                             op0=ALU.add, op1=ALU.mult), reads=_toks([t1, k_c]), writes=_toks([kmod]))
                for j in range(TB):
                    sl = slice(j * 128, (j + 1) * 128)
                    p.op("dve", lambda e, sl=sl: e.tensor_tensor_scan(out=cl[:, sl], data0=self.onesf[:, 0:128], data1=lw[:, sl],
                                                                      initial=0.0, op0=ALU.mult, op1=ALU.add),
                         reads=_toks([self.onesf, lw]), writes=_toks([cl]))
                p.op("act", lambda e: e.activation(out=eW[:, :], in_=cl[:, :], func=AF.Exp, scale=-0.6065306597126334),
                     reads=_toks([cl]), writes=_toks([eW]))
                p.op("act", lambda e: e.activation(out=eWi[:, :], in_=cl[:, :], func=AF.Exp, scale=0.6065306597126334),
                     reads=_toks([cl]), writes=_toks([eWi]))
                p.op("dve", lambda e: e.tensor_tensor(out=eWm[:, :], in0=cl[:, :], in1=lw[:, :], op=ALU.subtract),
                     reads=_toks([cl, lw]), writes=_toks([eWm]))
                p.op("act", lambda e: e.activation(out=eWm[:, :], in_=eWm[:, :], func=AF.Exp, scale=-0.6065306597126334),
                     reads=_toks([eWm]), writes=_toks([eWm]))
                p.op("dve", lambda e, cc=cc: e.tensor_tensor(out=rt_[:, cc, :], in0=r_c[:, :], in1=eW[:, :], op=ALU.mult),
                     reads=_toks([r_c, eW]), writes=_toks([rt_]))
                p.op("dve", lambda e, cc=cc: e.tensor_tensor(out=kt_[:, cc, :], in0=kmod[:, :], in1=eWi[:, :], op=ALU.mult),
                     reads=_toks([kmod, eWi]), writes=_toks([kt_]))
                p.op("pool", lambda e: e.tensor_tensor(out=bt_[:, :], in0=kk[:, :], in1=a_c[:, :], op=ALU.mult),
                     reads=_toks([kk, a_c]), writes=_toks([bt_]))
                p.op("dve", lambda e, cc=cc: e.tensor_tensor(out=btl[:, cc, :], in0=bt_[:, :], in1=eWi[:, :], op=ALU.mult),
                     reads=_toks([bt_, eWi]), writes=_toks([btl]))
                p.op("dve", lambda e, cc=cc: e.scalar_tensor_tensor(out=at_[:, cc, :], in0=kk[:, :], scalar=-1.0, in1=eWm[:, :],
                                                                    op0=ALU.mult, op1=ALU.mult),
                     reads=_toks([kk, eWm]), writes=_toks([at_]))
                p.op("dve", lambda e, cc=cc: e.scalar_tensor_tensor(out=rkr[:, cc, :], in0=r_c[:, :], scalar=cols[:, cc, 10:11],
                                                                    in1=kmod[:, :], op0=ALU.mult, op1=ALU.mult),
                     reads=_toks([r_c, cols, kmod]), writes=_toks([rkr]))
                for j in range(TB):
                    p.op("act", lambda e, cc=cc, j=j: e.copy(out=eWl[:, cc, j:j + 1], in_=eW[:, j * 128 + 127:j * 128 + 128]),
                         reads=_toks([eW]), writes=_toks([eWl]))
            if _stop == "2":
                return
            self.fm_to_tm(kt_, None, [(ktm[j][:, :], ktm[j]) for j in range(TB)], a)
            self.fm_to_tm(btl, None, [(btm[j][:, :], btm[j]) for j in range(TB)], a)
            for j in range(TB):
                i = b * TB + j
                tsl = slice(j * 128, (j + 1) * 128)
                for n in range(2):
                    pg = pgen[gi % 2]
                    gi += 1
                    for k in range(8):
                        p.op("pe", lambda e, k=k, n=n, pg=pg: e.matmul(pg[:, :], lhsT=xv[:, k, tsl], rhs=wv[k][:, n * 512:(n + 1) * 512],
                                                                       start=(k == 0), stop=(k == 7)),
                             reads=_toks([wv[k], xv]), writes=_toks([pg]), sig=(k == 7))
                    p.op("act", lambda e, n=n, pg=pg: e.copy(out=vtm[j][:, n * 512:(n + 1) * 512], in_=pg[:, :]),
                         reads=_toks([pg]), writes=_toks([vtm[j]]))
                for n in range(2):
                    pg = pgen[gi % 2]
                    gi += 1
                    p.op("pe", lambda e, n=n, pg=pg: e.matmul(pg[:, :], lhsT=h3a[:, tsl], rhs=g2a[:, n * 512:(n + 1) * 512],
                                                              start=True, stop=False),
                         reads=_toks([h3a, g2a]), writes=_toks([pg]), sig=False)
                    p.op("pe", lambda e, n=n, pg=pg: e.matmul(pg[:, :], lhsT=h3b[0:32, tsl], rhs=g2b[0:32, n * 512:(n + 1) * 512],
                                                              start=False, stop=True),
                         reads=_toks([h3b, g2b]), writes=_toks([pg]))
                    p.op("dve", lambda e, n=n, pg=pg: e.tensor_copy(out=gtm[j][:, n * 512:(n + 1) * 512], in_=pg[:, :]),
                         reads=_toks([pg]), writes=_toks([gtm[j]]))
                for cc in range(8):
                    p.op("pe", lambda e, cc=cc: e.matmul(prk[:, :], lhsT=rkr[:, cc, tsl], rhs=ind[:, cc, :],
                                                         start=(cc == 0), stop=(cc == 7)),
                         reads=_toks([rkr, ind]), writes=_toks([prk]), sig=(cc == 7))
                p.op("act", lambda e: e.copy(out=rks[:, :], in_=prk[:, :]), reads=_toks([prk]), writes=_toks([rks]))
                if _stop == "3":
                    return
                for bt in range(4):
                    xxv = xx[:, :, :].rearrange("p a w -> p (a w)").rearrange("p (t e c) -> p t e c", t=4, e=4)
                    msk_t = {}
                    for ti, src_ in enumerate((at_, btl, kt_, rt_)):
                        for e_ in range(4):
                            h = 4 * bt + e_
                            p.op("act", lambda e, ti=ti, e_=e_, h=h, src_=src_: e.activation(
                                out=xxv[:, ti, e_, :], in_=src_[:, h // 2, tsl], func=AF.Copy, scale=self.hm[:, h % 2:h % 2 + 1]),
                                reads=_toks([src_, self.hm]), writes=_toks([xx]))
                        msk_t[id(src_)] = ti

                    def gram(L, R, dst, mask, neg):
                        ps = pw()
                        ti = msk_t[id(L)]
                        for e_ in range(4):
                            h = 4 * bt + e_
                            cc = h // 2
                            p.op("pe", lambda e, e_=e_, cc=cc, ps=ps, ti=ti: e.matmul(
                                ps[:, e_ * 128:(e_ + 1) * 128], lhsT=xxv[:, ti, e_, :], rhs=R[:, cc, tsl], start=True, stop=True),
                                reads=_toks([xx, R]), writes=_toks([ps]), sig=(e_ == 3))
                        mb = mask[:, :].unsqueeze(1).to_broadcast([128, 4, 128])
                        psv = ps[:, :].rearrange("p (a b) -> p a b", b=128)
                        if neg:
                            p.op("dve", lambda e: e.scalar_tensor_tensor(out=dst[:, :, :], in0=psv, scalar=-1.0, in1=mb,
                                                                         op0=ALU.mult, op1=ALU.mult),
                                 reads=_toks([ps, mask]), writes=_toks([dst]))
                        else:
                            p.op("dve", lambda e: e.tensor_tensor(out=dst[:, :, :], in0=psv, in1=mb, op=ALU.mult),
                                 reads=_toks([ps, mask]), writes=_toks([dst]))
                    gram(at_, btl, Nn, self.maskGT, True)
                    gram(btl, at_, NnT, self.maskLT, True)
                    gram(kt_, at_, LakT, self.maskLT, False)
                    gram(btl, rt_, MrbT, self.triLE, False)
                    gram(kt_, rt_, MrkT, self.triLE, False)
                    uo = [None]
                    for _ in self.tri_inv(Nn, NnT, ib, pw, uo):
                        pass
                    U = uo[0]
                    if _stop == "4":
                        return
                    for q_ in range(2):
                        cc = 2 * bt + q_
                        Pm_ = Pm[ic % 2]
                        ic += 1
                        for hp in range(2):
                            e_ = 2 * q_ + hp
                            h = 2 * cc + hp
                            rs_ = slice(hp * 64, hp * 64 + 64)
                            hsl = slice(h * 64, (h + 1) * 64)
                            in_ = inner[hp]
                            ps1 = pw()
                            p.op("pe", lambda e, ps1=ps1, cc=cc, e_=e_: e.matmul(ps1[:, 0:64], lhsT=xxv[:, 0, e_, :], rhs=Zb[cc][:, :],
                                                                                 start=True, stop=False),
                                 reads=_toks([xx, Zb[cc]]), writes=_toks([ps1]), sig=False)
                            p.op("pe", lambda e, ps1=ps1, e_=e_, hsl=hsl: e.matmul(ps1[:, 0:64], lhsT=LakT[:, e_, :], rhs=vtm[j][:, hsl],
                                                                                   start=False, stop=True),
                                 reads=_toks([LakT, vtm[j]]), writes=_toks([ps1]))
                            p.op("act", lambda e, ps1=ps1, in_=in_: e.copy(out=in_[:, :], in_=ps1[:, 0:64]),
                                 reads=_toks([ps1]), writes=_toks([in_]))
                            p.op("pe", lambda e, ps1=ps1, e_=e_, in_=in_: e.matmul(ps1[:, 64:128], lhsT=U[:, e_, :], rhs=in_[:, :],
                                                                                   start=True, stop=True),
                                 reads=_toks([U, in_]), writes=_toks([ps1]))
                            p.op("dve", lambda e, ps1=ps1, Pm_=Pm_, rs_=rs_: e.tensor_copy(out=Pm_[:, rs_], in_=ps1[:, 64:128]),
                                 reads=_toks([ps1]), writes=_toks([Pm_]))
                            ps2 = pw()
                            p.op("pe", lambda e, ps2=ps2, cc=cc, e_=e_: e.matmul(ps2[:, 0:64], lhsT=xxv[:, 3, e_, :], rhs=Zb[cc][:, :],
                                                                                 start=True, stop=False),
                                 reads=_toks([xx, Zb[cc]]), writes=_toks([ps2]), sig=False)
                            p.op("pe", lambda e, ps2=ps2, e_=e_, Pm_=Pm_, rs_=rs_: e.matmul(ps2[:, 0:64], lhsT=MrbT[:, e_, :], rhs=Pm_[:, rs_],
                                                                                            start=False, stop=False),
                                 reads=_toks([MrbT, Pm_]), writes=_toks([ps2]), sig=False)
                            p.op("pe", lambda e, ps2=ps2, e_=e_, hsl=hsl: e.matmul(ps2[:, 0:64], lhsT=MrkT[:, e_, :], rhs=vtm[j][:, hsl],
                                                                                   start=False, stop=True),
                                 reads=_toks([MrkT, vtm[j]]), writes=_toks([ps2]))
                            p.op("act", lambda e, ps2=ps2, hsl=hsl: e.copy(out=y[:, hsl], in_=ps2[:, 0:64]),
                                 reads=_toks([ps2]), writes=_toks([y]))
                        csl = slice(cc * 128, (cc + 1) * 128)
                        ps3 = pw()
                        p.op("pe", lambda e, ps3=ps3, csl=csl, Pm_=Pm_: e.matmul(ps3[:, 0:128], lhsT=btm[j][:, csl], rhs=Pm_[:, :],
                                                                                 start=True, stop=False),
                             reads=_toks([btm[j], Pm_]), writes=_toks([ps3]), sig=False)
                        p.op("pe", lambda e, ps3=ps3, csl=csl: e.matmul(ps3[:, 0:128], lhsT=ktm[j][:, csl], rhs=vtm[j][:, csl],
                                                                        start=False, stop=True),
                             reads=_toks([ktm[j], vtm[j]]), writes=_toks([ps3]))
                        for hp in range(2):
                            rs_ = slice(hp * 64, hp * 64 + 64)
                            p.op("dve", lambda e, ps3=ps3, cc=cc, rs_=rs_, hp=hp: e.tensor_tensor(
                                out=ztmp[rs_, :], in0=Zc[cc][rs_, :], in1=ps3[rs_, hp * 64:(hp + 1) * 64], op=ALU.add),
                                reads=_toks([Zc[cc], ps3]), writes=_toks([ztmp]))
                            p.op("dve", lambda e, cc=cc, rs_=rs_: e.tensor_scalar(
                                out=Zc[cc][rs_, :], in0=ztmp[rs_, :], scalar1=eWl[rs_, cc, j:j + 1], scalar2=None, op0=ALU.mult),
                                reads=_toks([ztmp, eWl]), writes=_toks([Zc[cc]]))
                        p.op("act", lambda e, cc=cc: e.copy(out=Zb[cc][:, :], in_=Zc[cc][:, :]),
                             reads=_toks([Zc[cc]]), writes=_toks([Zb[cc]]))
                if _stop == "5":
                    return
                y3 = y[:, :].rearrange("p (h d) -> p h d", d=64)
                yc3 = yc[:, :].rearrange("p (h d) -> p h d", d=64)
                p.op("dve", lambda e: e.tensor_reduce(out=s16[:, :], in_=y3, axis=AX.X, op=ALU.add), reads=_toks([y]), writes=_toks([s16]))
                p.op("dve", lambda e: e.tensor_scalar(out=s16[:, :], in0=s16[:, :], scalar1=1.0 / 64.0, scalar2=None, op0=ALU.mult),
                     reads=_toks([s16]), writes=_toks([s16]))
                p.op("dve", lambda e: e.tensor_tensor(out=y3, in0=y3, in1=s16[:, :].unsqueeze(2).to_broadcast([128, 16, 64]),
                                                      op=ALU.subtract), reads=_toks([y, s16]), writes=_toks([y]))
                p.op("pool", lambda e: e.tensor_tensor(out=yc[:, :], in0=y[:, :], in1=y[:, :], op=ALU.mult),
                     reads=_toks([y]), writes=_toks([yc]))
                p.op("dve", lambda e: e.tensor_reduce(out=v16[:, :], in_=yc3, axis=AX.X, op=ALU.add), reads=_toks([yc]), writes=_toks([v16]))
                p.op("dve", lambda e: e.tensor_scalar(out=v16[:, :], in0=v16[:, :], scalar1=1.0 / 64.0, scalar2=None, op0=ALU.mult),
                     reads=_toks([v16]), writes=_toks([v16]))
                self.rsqrt_small(v16[:, :], v16[:, :], [v16], [v16], 64e-5)
                p.op("dve", lambda e: e.tensor_tensor(out=y3, in0=y3, in1=v16[:, :].unsqueeze(2).to_broadcast([128, 16, 64]),
                                                      op=ALU.mult), reads=_toks([y, v16]), writes=_toks([y]))
                p.op("pool", lambda e: e.tensor_tensor(out=y[:, :], in0=y[:, :], in1=lng[:, :], op=ALU.mult),
                     reads=_toks([y, lng]), writes=_toks([y]))
                p.op("pool", lambda e: e.tensor_tensor(out=y[:, :], in0=y[:, :], in1=lnb[:, :], op=ALU.add),
                     reads=_toks([y, lnb]), writes=_toks([y]))
                p.op("dve", lambda e: e.tensor_tensor(out=yc3, in0=vtm[j][:, :].rearrange("p (h d) -> p h d", d=64),
                                                      in1=rks[:, :].unsqueeze(2).to_broadcast([128, 16, 64]), op=ALU.mult),
                     reads=_toks([vtm[j], rks]), writes=_toks([yc]))
                p.op("pool", lambda e: e.tensor_tensor(out=y[:, :], in0=y[:, :], in1=yc[:, :], op=ALU.add),
                     reads=_toks([y, yc]), writes=_toks([y]))
                p.op("dve", lambda e: e.tensor_tensor(out=ob[:, :], in0=y[:, :], in1=gtm[j][:, :], op=ALU.mult),
                     reads=_toks([y, gtm[j]]), writes=_toks([ob]))
                p.op("sp", lambda e, i=i: e.dma_start(out=self.scr[i * 128:(i + 1) * 128, 0:D], in_=ob[:, :]),
                     reads=_toks([ob]), writes=[self.ytok[i]], dma=ob.tok)

    def build(self):
        for (kind, layer, arg) in self.sublayers:
            if kind == "ffn":
                self.ffn(layer, arg)
            elif kind == "xa":
                self.xattn(layer)
            elif kind == "dn":
                self.dn(layer)
                self.mix_out(layer, self.dn_w_out[layer // 3], 2 * D, zgate=(self.dn_w_in[layer // 3], 4096))
            elif kind == "rw":
                self.rw(layer)
                self.mix_out(layer, self.rw_w_out[layer // 3], D)
            elif kind == "ssd":
                self.ssd(layer)
                self.mix_out(layer, self.ssd_w_out[layer // 3], 2 * D)
            else:
                raise ValueError(kind)
        self.p.finish(self.htok + self.dbg_toks)
        return self.nc


FULL = []
for _l in range(4):
    FULL.append(("ffn", _l, 0))
    FULL.append((("dn", "ssd", "rw")[_l % 3], _l, 0))
    FULL.append(("xa", _l, 0))
    FULL.append(("ffn", _l, 1))


def kernel(**inputs):
    x = np.asarray(inputs["x"], dtype=np.float32)
    mem = np.asarray(inputs["mem"], dtype=np.float32)
    B, S, _ = x.shape
    nc = Builder(S // 128, FULL).build()
    in_maps = [make_in_map(inputs, x[b], mem[b]) for b in range(B)]
    res = run_bass_kernel_spmd(nc, in_maps, core_ids=list(range(B)))
    return np.stack([np.asarray(r["out"], dtype=np.float32) for r in res.results], axis=0)
```
